# Optimizing a Trainium2 kernel written in Bass

```python
import math
import jax, jax.numpy as jnp
from jax import lax
import numpy as np

D_MODEL = 1024
BATCH = 2
SEQ = 8192
DEPTH = 2

GRID_W = 64
CTX_LEN = 256
N_HEADS = 8
N_KV_HEADS = 2
HEAD_DIM = 64
Q_GROUP = N_HEADS // N_KV_HEADS
WINDOW = 128
ATTN_BLOCK = 128
ROPE_THETA = 10000.0
D_SSM = D_MODEL // 2
SSM_GROUP = 16
N_SSM_GROUPS = D_SSM // SSM_GROUP
SSM_STATE = 64
N_EXPERT_GROUPS = 4
EXPERTS_PER_GROUP = 8
N_EXPERTS = N_EXPERT_GROUPS * EXPERTS_PER_GROUP
TOP_K = 2
D_EXPERT = D_MODEL // 2
MOE_BLOCK = 128
Q_W = N_HEADS * HEAD_DIM
KV_W = N_KV_HEADS * HEAD_DIM
D_IN = Q_W + 2 * KV_W + D_SSM + 2 * D_MODEL
EPS = 1e-6
NEG_INF = -1e30
F32 = jnp.float32

kernel_name = 'hybrid_s5_swa_hmoe_prefix'


def rms_norm(x, g):
    xf = x.astype(F32)
    y = xf * lax.rsqrt(jnp.mean(xf * xf, axis=-1, keepdims=True) + EPS)
    return (y * g.astype(F32)).astype(x.dtype)


def modulate(h, shift, scale):
    return h * (1 + scale) + shift


def rope_2d(x, rows, cols):
    half = HEAD_DIM // 2
    quarter = half // 2
    freqs = ROPE_THETA ** (-jnp.arange(quarter, dtype=F32) / quarter)

    def rot(xa, pos):
        ang = pos[:, None] * freqs[None, :]
        cos = jnp.cos(ang)[None, :, None, :].astype(x.dtype)
        sin = jnp.sin(ang)[None, :, None, :].astype(x.dtype)
        x1, x2 = xa[..., :quarter], xa[..., quarter:]
        return jnp.concatenate([x1 * cos - x2 * sin, x1 * sin + x2 * cos], axis=-1)

    return jnp.concatenate([rot(x[..., :half], rows), rot(x[..., half:], cols)], axis=-1)


def latent_attention(q, k, v, kc, vc, sink):
    b, s = q.shape[0], q.shape[1]
    nb = s // ATTN_BLOCK
    scale = HEAD_DIM ** -0.5
    qb = q.reshape(b, nb, ATTN_BLOCK, N_KV_HEADS, Q_GROUP, HEAD_DIM)

    def band(t):
        tp = jnp.pad(t, ((0, 0), (ATTN_BLOCK, ATTN_BLOCK), (0, 0), (0, 0)))
        tp = tp.reshape(b, nb + 2, ATTN_BLOCK, N_KV_HEADS, HEAD_DIM)
        return jnp.concatenate([tp[:, :-2], tp[:, 1:-1], tp[:, 2:]], axis=2)

    kb, vb = band(k), band(v)
    s_loc = jnp.einsum('bnqhgd,bnjhd->bnhgqj', qb, kb).astype(F32) * scale
    s_ctx = jnp.einsum('bnqhgd,bchd->bnhgqc', qb, kc).astype(F32) * scale
    qpos = jnp.arange(nb)[:, None] * ATTN_BLOCK + jnp.arange(ATTN_BLOCK)[None, :]
    kpos = (jnp.arange(nb)[:, None] - 1) * ATTN_BLOCK + jnp.arange(3 * ATTN_BLOCK)[None, :]
    kp = kpos[:, None, :]
    valid = (jnp.abs(qpos[:, :, None] - kp) <= WINDOW) & (kp >= 0) & (kp < s)
    s_loc = jnp.where(valid[None, :, None, None], s_loc, NEG_INF)
    sink_l = jnp.broadcast_to(sink.astype(F32).reshape(N_KV_HEADS, Q_GROUP, 1, 1), s_loc.shape[:-1] + (1,))
    p = jax.nn.softmax(jnp.concatenate([s_loc, s_ctx, sink_l], axis=-1), axis=-1).astype(v.dtype)
    n_loc = 3 * ATTN_BLOCK
    n_ctx = kc.shape[1]
    o = (jnp.einsum('bnhgqj,bnjhd->bnqhgd', p[..., :n_loc], vb)
         + jnp.einsum('bnhgqc,bchd->bnqhgd', p[..., n_loc:n_loc + n_ctx], vc))
    return o.reshape(b, s, Q_W)


def context_attention(q, k, v, sink):
    b, n = q.shape[0], q.shape[1]
    qg = q.reshape(b, n, N_KV_HEADS, Q_GROUP, HEAD_DIM)
    sc = jnp.einsum('bqhgd,bjhd->bhgqj', qg, k).astype(F32) * HEAD_DIM ** -0.5
    sink_c = jnp.broadcast_to(sink.astype(F32).reshape(N_KV_HEADS, Q_GROUP, 1, 1), sc.shape[:-1] + (1,))
    p = jax.nn.softmax(jnp.concatenate([sc, sink_c], axis=-1), axis=-1)[..., :-1].astype(v.dtype)
    return jnp.einsum('bhgqj,bjhd->bqhgd', p, v).reshape(b, n, Q_W)


def ssm_discretise(lam_re, lam_im, log_dt, b_re, b_im):
    lr, li = lam_re.astype(F32), lam_im.astype(F32)
    dt = jnp.exp(log_dt.astype(F32))[:, None]
    mag = jnp.exp(lr * dt)
    a_re = mag * jnp.cos(li * dt)
    a_im = mag * jnp.sin(li * dt)
    den = lr * lr + li * li
    nr = a_re - 1.0
    f_re = (nr * lr + a_im * li) / den
    f_im = (a_im * lr - nr * li) / den
    br, bi = b_re.astype(F32), b_im.astype(F32)
    bb_re = f_re[..., None] * br - f_im[..., None] * bi
    bb_im = f_re[..., None] * bi + f_im[..., None] * br
    return a_re, a_im, bb_re, bb_im


def complex_affine_combine(e1, e2):
    a1r, a1i, b1r, b1i = e1
    a2r, a2i, b2r, b2i = e2
    return (a2r * a1r - a2i * a1i, a2r * a1i + a2i * a1r,
            a2r * b1r - a2i * b1i + b2r, a2r * b1i + a2i * b1r + b2i)


def ssm_scan(u, a_re, a_im, bb_re, bb_im, h0, reverse):
    bu_re = jnp.einsum('blgm,gpm->blgp', u, bb_re)
    bu_im = jnp.einsum('blgm,gpm->blgp', u, bb_im)
    if h0 is not None:
        h_re, h_im = h0
        idx = -1 if reverse else 0
        bu_re = bu_re.at[:, idx].add(a_re * h_re - a_im * h_im)
        bu_im = bu_im.at[:, idx].add(a_re * h_im + a_im * h_re)
    ar = jnp.broadcast_to(a_re, bu_re.shape)
    ai = jnp.broadcast_to(a_im, bu_im.shape)
    _, _, x_re, x_im = lax.associative_scan(complex_affine_combine, (ar, ai, bu_re, bu_im), reverse=reverse, axis=1)
    return x_re, x_im


def ssm_readout(x_re, x_im, c_re, c_im):
    return (jnp.einsum('blgp,gmp->blgm', x_re, c_re.astype(F32))
            - jnp.einsum('blgp,gmp->blgm', x_im, c_im.astype(F32)))


def ssm_branch(u, uc, ctx_out, lam_re, lam_im, log_dt, b_re, b_im, c_re, c_im, d_skip, w_glu):
    b, s, _ = u.shape
    n_ctx = uc.shape[1]
    ug = u.astype(F32).reshape(b, s, N_SSM_GROUPS, SSM_GROUP)
    ucg = uc.astype(F32).reshape(b, n_ctx, N_SSM_GROUPS, SSM_GROUP)
    dg = d_skip.astype(F32).reshape(N_SSM_GROUPS, SSM_GROUP)
    y = ug * dg
    yc = ucg * dg if ctx_out else None
    for direction, reverse in ((0, False), (1, True)):
        a_re, a_im, bb_re, bb_im = ssm_discretise(lam_re[direction], lam_im[direction], log_dt[direction],
                                                  b_re[direction], b_im[direction])
        xc_re, xc_im = ssm_scan(ucg, a_re, a_im, bb_re, bb_im, None, reverse)
        end = 0 if reverse else -1
        x_re, x_im = ssm_scan(ug, a_re, a_im, bb_re, bb_im, (xc_re[:, end], xc_im[:, end]), reverse)
        y = y + ssm_readout(x_re, x_im, c_re[direction], c_im[direction])
        if ctx_out:
            yc = yc + ssm_readout(xc_re, xc_im, c_re[direction], c_im[direction])

    def glu(z):
        z = jax.nn.gelu(z.reshape(z.shape[0], z.shape[1], D_SSM)).astype(u.dtype)
        return z * jax.nn.sigmoid(z @ w_glu)

    return glu(y), (glu(yc) if ctx_out else None)


def mixer(h, hc, rows, cols, ctx_out, w_in, sink, lam_re, lam_im, log_dt, b_re, b_im, c_re, c_im,
          d_skip, w_glu, w_br_attn, w_br_ssm, w_out):
    b, s, _ = h.shape
    n_ctx = hc.shape[1]
    o_k = Q_W
    o_v = o_k + KV_W
    o_u = o_v + KV_W
    o_ga = o_u + D_SSM
    o_gs = o_ga + D_MODEL
    p = h @ w_in
    q = rope_2d(p[..., :o_k].reshape(b, s, N_HEADS, HEAD_DIM), rows, cols)
    k = rope_2d(p[..., o_k:o_v].reshape(b, s, N_KV_HEADS, HEAD_DIM), rows, cols)
    v = p[..., o_v:o_u].reshape(b, s, N_KV_HEADS, HEAD_DIM)
    u = p[..., o_u:o_ga]
    ga = p[..., o_ga:o_gs]
    gs = p[..., o_gs:]
    w_ctx = w_in if ctx_out else w_in[:, o_k:o_ga]
    off = 0 if ctx_out else o_k
    pc = hc @ w_ctx
    kc = pc[..., o_k - off:o_v - off].reshape(b, n_ctx, N_KV_HEADS, HEAD_DIM)
    vc = pc[..., o_v - off:o_u - off].reshape(b, n_ctx, N_KV_HEADS, HEAD_DIM)
    uc = pc[..., o_u - off:o_ga - off]
    attn = latent_attention(q, k, v, kc, vc, sink)
    ssm, ssm_c = ssm_branch(u, uc, ctx_out, lam_re, lam_im, log_dt, b_re, b_im, c_re, c_im, d_skip, w_glu)
    y = (jax.nn.sigmoid(ga) * (attn @ w_br_attn) + jax.nn.sigmoid(gs) * (ssm @ w_br_ssm)) @ w_out
    if not ctx_out:
        return y, None
    qc = pc[..., :o_k].reshape(b, n_ctx, N_HEADS, HEAD_DIM)
    attn_c = context_attention(qc, kc, vc, sink)
    yc = (jax.nn.sigmoid(pc[..., o_ga:o_gs]) * (attn_c @ w_br_attn)
          + jax.nn.sigmoid(pc[..., o_gs:]) * (ssm_c @ w_br_ssm)) @ w_out
    return y, yc


def hier_moe(h, w_rg, w_re, w_gate, w_up, w_down):
    t, d = h.shape
    p_grp = jax.nn.softmax((h @ w_rg).astype(F32), axis=-1)
    g_prob, g_idx = lax.top_k(p_grp, 1)
    logits_e = (h @ w_re).astype(F32).reshape(t, N_EXPERT_GROUPS, EXPERTS_PER_GROUP)
    logits_sel = jnp.take_along_axis(logits_e, g_idx[:, :, None], axis=1)[:, 0]
    e_prob, e_idx = lax.top_k(jax.nn.softmax(logits_sel, axis=-1), TOP_K)
    e_prob = e_prob / jnp.sum(e_prob, axis=-1, keepdims=True)
    weight = (g_prob * e_prob).reshape(-1)
    expert = (g_idx * EXPERTS_PER_GROUP + e_idx).reshape(-1)
    token = jnp.repeat(jnp.arange(t), TOP_K)
    n_assign = t * TOP_K
    order = jnp.argsort(expert)
    s_exp, s_tok, s_w = expert[order], token[order], weight[order]
    counts = jnp.bincount(expert, length=N_EXPERTS)
    start = jnp.cumsum(counts) - counts
    pcounts = (counts + MOE_BLOCK - 1) // MOE_BLOCK * MOE_BLOCK
    pend = jnp.cumsum(pcounts)
    pstart = pend - pcounts
    dest = pstart[s_exp] + jnp.arange(n_assign) - start[s_exp]
    n_pad = -(-n_assign // MOE_BLOCK) * MOE_BLOCK + N_EXPERTS * MOE_BLOCK
    n_blk = n_pad // MOE_BLOCK
    buf_tok = jnp.full((n_pad,), t, jnp.int32).at[dest].set(s_tok.astype(jnp.int32))
    buf_w = jnp.zeros((n_pad,), h.dtype).at[dest].set(s_w.astype(h.dtype))
    blk_exp = jnp.minimum(jnp.searchsorted(pend, jnp.arange(n_blk) * MOE_BLOCK, side='right'), N_EXPERTS - 1)
    h_pad = jnp.concatenate([h, jnp.zeros((1, d), h.dtype)], axis=0)
    xb = h_pad[buf_tok].reshape(n_blk, MOE_BLOCK, d)

    def expert_block(args):
        xblk, e = args
        return (jax.nn.silu(xblk @ w_gate[e]) * (xblk @ w_up[e])) @ w_down[e]

    yb = lax.map(expert_block, (xb, blk_exp)).reshape(n_pad, d)
    out = jax.ops.segment_sum(yb * buf_w[:, None], buf_tok, num_segments=t + 1)
    return out[:t]


def setup_inputs(seed: int = 0) -> dict:
    key = jax.random.key(seed)
    ks = iter(jax.random.split(key, 40))
    nrm = lambda shape: jax.random.normal(next(ks), shape, F32)
    L2 = (DEPTH, 2, N_SSM_GROUPS)
    return {
        'x': nrm((BATCH, SEQ, D_MODEL)),
        'c': nrm((BATCH, D_MODEL)),
        'ctx': nrm((BATCH, CTX_LEN, D_MODEL)),
        'c_ctx': nrm((D_MODEL,)),
        'w_mod': nrm((DEPTH, D_MODEL, 6 * D_MODEL)) * (0.5 * D_MODEL ** -0.5),
        'b_mod': nrm((DEPTH, 6 * D_MODEL)) * 0.01,
        'g_norm1': 1.0 + 0.01 * nrm((DEPTH, D_MODEL)),
        'g_norm2': 1.0 + 0.01 * nrm((DEPTH, D_MODEL)),
        'w_in': nrm((DEPTH, D_MODEL, D_IN)) * D_MODEL ** -0.5,
        'attn_sink': 0.1 * nrm((DEPTH, N_HEADS)),
        'ssm_lam_re': -0.5 + 0.01 * nrm(L2 + (SSM_STATE,)),
        'ssm_lam_im': jnp.pi * jnp.arange(SSM_STATE, dtype=F32) + 0.01 * nrm(L2 + (SSM_STATE,)),
        'ssm_log_dt': jax.random.uniform(next(ks), L2, F32, minval=math.log(1e-3), maxval=math.log(1e-1)),
        'ssm_b_re': nrm(L2 + (SSM_STATE, SSM_GROUP)) * (2 * SSM_GROUP) ** -0.5,
        'ssm_b_im': nrm(L2 + (SSM_STATE, SSM_GROUP)) * (2 * SSM_GROUP) ** -0.5,
        'ssm_c_re': nrm(L2 + (SSM_GROUP, SSM_STATE)) * SSM_STATE ** -0.5,
        'ssm_c_im': nrm(L2 + (SSM_GROUP, SSM_STATE)) * SSM_STATE ** -0.5,
        'ssm_d': nrm((DEPTH, D_SSM)),
        'w_glu': nrm((DEPTH, D_SSM, D_SSM)) * D_SSM ** -0.5,
        'w_br_attn': nrm((DEPTH, Q_W, D_MODEL)) * Q_W ** -0.5,
        'w_br_ssm': nrm((DEPTH, D_SSM, D_MODEL)) * D_SSM ** -0.5,
        'w_out': nrm((DEPTH, D_MODEL, D_MODEL)) * D_MODEL ** -0.5,
        'w_router_group': nrm((DEPTH, D_MODEL, N_EXPERT_GROUPS)) * D_MODEL ** -0.5,
        'w_router_expert': nrm((DEPTH, D_MODEL, N_EXPERTS)) * D_MODEL ** -0.5,
        'w_exp_gate': nrm((DEPTH, N_EXPERTS, D_MODEL, D_EXPERT)) * D_MODEL ** -0.5,
        'w_exp_up': nrm((DEPTH, N_EXPERTS, D_MODEL, D_EXPERT)) * D_MODEL ** -0.5,
        'w_exp_down': nrm((DEPTH, N_EXPERTS, D_EXPERT, D_MODEL)) * D_EXPERT ** -0.5,
        'g_final': 1.0 + 0.01 * nrm((D_MODEL,)),
    }


def reference(x, c, ctx, c_ctx, w_mod, b_mod, g_norm1, g_norm2, w_in, attn_sink, ssm_lam_re, ssm_lam_im,
              ssm_log_dt, ssm_b_re, ssm_b_im, ssm_c_re, ssm_c_im, ssm_d, w_glu, w_br_attn, w_br_ssm, w_out,
              w_router_group, w_router_expert, w_exp_gate, w_exp_up, w_exp_down, g_final):
    b, s, d = x.shape
    n_rows = s // GRID_W
    rows = jnp.repeat(jnp.arange(n_rows), GRID_W).astype(F32)
    cols = jnp.tile(jnp.arange(GRID_W), n_rows).astype(F32)
    silu_c = jax.nn.silu(c)
    silu_cc = jax.nn.silu(c_ctx)
    for l in range(DEPTH):
        ctx_out = l < DEPTH - 1
        mod = silu_c @ w_mod[l] + b_mod[l]
        mod_c = silu_cc @ w_mod[l] + b_mod[l]
        sh1, sc1, gt1, sh2, sc2, gt2 = jnp.split(mod[:, None, :], 6, axis=-1)
        csh1, csc1, cgt1, csh2, csc2, cgt2 = jnp.split(mod_c, 6, axis=-1)
        h = modulate(rms_norm(x, g_norm1[l]), sh1, sc1)
        hc = modulate(rms_norm(ctx, g_norm1[l]), csh1, csc1)
        y, yc = mixer(h, hc, rows, cols, ctx_out, w_in[l], attn_sink[l], ssm_lam_re[l], ssm_lam_im[l],
                      ssm_log_dt[l], ssm_b_re[l], ssm_b_im[l], ssm_c_re[l], ssm_c_im[l], ssm_d[l], w_glu[l],
                      w_br_attn[l], w_br_ssm[l], w_out[l])
        x = x + gt1 * y
        h2 = modulate(rms_norm(x, g_norm2[l]), sh2, sc2).reshape(b * s, d)
        if ctx_out:
            ctx = ctx + cgt1 * yc
            hc2 = modulate(rms_norm(ctx, g_norm2[l]), csh2, csc2).reshape(-1, d)
            f = hier_moe(jnp.concatenate([h2, hc2], axis=0), w_router_group[l], w_router_expert[l],
                         w_exp_gate[l], w_exp_up[l], w_exp_down[l])
            x = x + gt2 * f[:b * s].reshape(b, s, d)
            ctx = ctx + cgt2 * f[b * s:].reshape(ctx.shape)
        else:
            f = hier_moe(h2, w_router_group[l], w_router_expert[l], w_exp_gate[l], w_exp_up[l], w_exp_down[l])
            x = x + gt2 * f.reshape(b, s, d)
    return rms_norm(x, g_final)
```

```python
from contextlib import ExitStack
import numpy as np
import concourse.bass as bass
import concourse.mybir as mybir
from concourse.bass_utils import run_bass_kernel_spmd

dt = mybir.dt
ALU = mybir.AluOpType
AF = mybir.ActivationFunctionType
AX = mybir.AxisListType
F32 = dt.float32
BF16 = dt.bfloat16
I32 = dt.int32

NCORES = 8
D = 1024
TOWN = 2048
NT_OWN = 16
NT = 18
TALL = 2304
SEQ = 8192
CTX = 256
STREAM = SEQ + CTX
NEXP = 32
PI = float(np.pi)
GROUPS = [[0, 1, 2, 3], [4, 5, 6, 7]]


class Buf:
    __slots__ = ("name", "last_w", "readers")

    def __init__(self, name=""):
        self.name = name
        self.last_w = None
        self.readers = {}


class Prog:
    ENG = ("pe", "act", "dve", "pool", "sp")

    def __init__(self, nc, stack, ndma=None):
        ndma = ndma or {"sp": 8, "act": 2, "pool": 6}
        self.nc = nc
        self.q = {e: [] for e in self.ENG}
        self.sems = {}
        self.cnt = {e: 0 for e in self.ENG}
        self.waited = {e: {} for e in self.ENG}
        for e in self.ENG:
            self.sems[("c", e)] = stack.enter_context(nc.semaphore("c_" + e))
        self.dma_slots = {}
        self.dma_tot = {}
        self.dma_rr = {}
        for e, n in ndma.items():
            keys = []
            for i in range(n):
                k = ("d", e, i)
                self.sems[k] = stack.enter_context(nc.semaphore("d_%s%d" % (e, i)))
                self.dma_tot[k] = 0
                keys.append(k)
            self.dma_slots[e] = keys
            self.dma_rr[e] = 0

    def _wait(self, eng, k, v):
        if k == ("c", "pe") and eng == "pe":
            return
        if self.waited[eng].get(k, 0) >= v:
            return
        self.waited[eng][k] = v
        self.q[eng].append(("w", k, v))

    def _deps(self, eng, reads, writes):
        deps = {}

        def add(ev):
            if ev is None:
                return
            k, v = ev
            if deps.get(k, 0) < v:
                deps[k] = v

        for b in reads:
            add(b.last_w)
        for b in writes:
            add(b.last_w)
            for k, v in b.readers.items():
                add((k, v))
        for k, v in deps.items():
            self._wait(eng, k, v)

    def _commit(self, ev, reads, writes):
        k, v = ev
        for b in reads:
            if b.readers.get(k, 0) < v:
                b.readers[k] = v
        for b in writes:
            b.last_w = ev
            b.readers = {}

    def op(self, eng, fn, reads=(), writes=()):
        self._deps(eng, reads, writes)
        self.cnt[eng] += 1
        ev = (("c", eng), self.cnt[eng])
        self.q[eng].append(("o", fn, ("c", eng), 1))
        self._commit(ev, reads, writes)
        return ev

    def dma(self, qeng, fn, reads=(), writes=(), inc=16):
        slots = self.dma_slots[qeng]
        k = slots[self.dma_rr[qeng] % len(slots)]
        self.dma_rr[qeng] += 1
        prev = self.dma_tot[k]
        if prev > 0:
            self._wait(qeng, k, prev)
        self._deps(qeng, reads, writes)
        self.dma_tot[k] = prev + inc
        ev = (k, prev + inc)
        self.q[qeng].append(("o", fn, k, inc))
        self._commit(ev, reads, writes)
        return ev

    def wait_all(self, eng, bufs):
        self._deps(eng, bufs, ())

    def barrier(self):
        tot = {("c", e): self.cnt[e] for e in self.ENG}
        tot.update(self.dma_tot)
        for e in self.ENG:
            for k, v in tot.items():
                if v > 0:
                    self._wait(e, k, v)

    def emit(self):
        nc = self.nc
        sems = self.sems

        def replay(e, name):
            for it in self.q[name]:
                if it[0] == "w":
                    e.wait_ge(sems[it[1]], it[2])
                else:
                    ins = it[1](e)
                    ins.then_inc(sems[it[2]], it[3])

        with nc.Block() as block:

            @block.tensor
            def _(e):
                replay(e, "pe")

            @block.vector
            def _(e):
                replay(e, "dve")

            @block.scalar
            def _(e):
                replay(e, "act")

            @block.gpsimd
            def _(e):
                replay(e, "pool")

            @block.sync
            def _(e):
                replay(e, "sp")


class TL:
    def __init__(self, t, nslots=1):
        self.t = t
        self.bs = [Buf() for _ in range(nslots)]

    @property
    def b(self):
        return self.bs[0]


def build_program(nlayers=2, dbg=None):
    dbg = dbg or {}
    NEI = dbg.get("_nexp", NEXP)
    nc = bass.Bass("TRN2", target_bir_lowering=False)
    names_in = []

    def inp(name, shape, d=F32):
        names_in.append(name)
        return TL(nc.dram_tensor(name, list(shape), d, kind="ExternalInput").ap())

    gathers = []

    def ginp(name, shape):
        return inp(name, shape)

    def ginp_unused(name, shape):
        R = int(np.prod(shape[:-1]))
        C = int(shape[-1])
        assert R % NCORES == 0
        names_in.append(name)
        shard = TL(nc.dram_tensor(name, [R // NCORES, C], F32, kind="ExternalInput").ap())
        bounce = TL(nc.dram_tensor(name + "_bnc", [R // NCORES, C], F32).ap())
        full = TL(nc.dram_tensor(name + "_full", list(shape), F32).ap())
        gathers.append((shard, bounce, full))
        return full

    x_own = inp("x_own", [TOWN, D])
    ctx_b = inp("ctx_b", [CTX, D])
    cT = inp("cT", [128, 8, 2])
    pos_d = inp("pos", [128, TALL])
    ropef_d = inp("ropef", [128, 2])
    masks_d = inp("masks", [128, 4, 512])
    selmat_d = inp("selmat", [128, 12, 128])
    ident_d = inp("ident", [128, 128])
    iota_d = inp("iota", [128, 512])
    gfin_d = inp("gfin", [128, D])
    L = []
    for l in range(nlayers):
        w = {}
        w["wmod"] = ginp(f"wmod{l}", [128, 8, 6 * D])
        w["bmodT"] = inp(f"bmodT{l}", [128, 48])
        w["g1T"] = inp(f"g1T{l}", [128, 8])
        w["g2T"] = inp(f"g2T{l}", [128, 8])
        w["w1"] = ginp(f"w1_{l}", [128, 8, 1920])
        w["wus"] = inp(f"wus{l}", [128, 8, 128])
        w["w2g"] = ginp(f"w2g{l}", [8, 128, 8, 256])
        w["wglu"] = inp(f"wglu{l}", [128, 4, 512])
        w["wbra"] = ginp(f"wbra{l}", [128, 4, D])
        w["wbrs"] = ginp(f"wbrs{l}", [128, 4, D])
        w["wout"] = ginp(f"wout{l}", [128, 8, D])
        w["wr"] = inp(f"wr{l}", [128, 8, 36])
        w["weg"] = ginp(f"weg{l}", [NEI, 128, 8, 512])
        w["weu"] = ginp(f"weu{l}", [NEI, 128, 8, 512])
        w["wed"] = ginp(f"wed{l}", [NEI, 128, 4, D])
        w["sink"] = inp(f"sink{l}", [128, 8])
        w["lamre"] = inp(f"lamre{l}", [128, 8])
        w["lamim"] = inp(f"lamim{l}", [128, 8])
        w["ldt"] = inp(f"ldt{l}", [128, 8])
        w["bre"] = inp(f"bre{l}", [128, 8, 16])
        w["bim"] = inp(f"bim{l}", [128, 8, 16])
        w["cre"] = inp(f"cre{l}", [128, 8, 16])
        w["cim"] = inp(f"cim{l}", [128, 8, 16])
        w["dsk"] = inp(f"dsk{l}", [128, 1])
        L.append(w)
    out_d = TL(nc.dram_tensor("out", [TOWN, D], F32, kind="ExternalOutput").ap())
    dbg_out = {}
    for k, shp in dbg.items():
        if k.startswith("_"):
            continue
        dbg_out[k] = TL(nc.dram_tensor("dbg_" + k, list(shp), F32, kind="ExternalOutput").ap())
    AGR = TOWN + 128
    ag1_in = [TL(nc.dram_tensor(f"ag1_in{i}", [r_, 512], BF16).ap()) for i, r_ in enumerate((1024, 1024, 128))]
    ag1_out = [TL(nc.dram_tensor(f"ag1_out{i}", [4 * r_, 512], BF16).ap()) for i, r_ in enumerate((1024, 1024, 128))]
    ag2_in = [TL(nc.dram_tensor(f"ag2_in{i}", [128, c_], BF16).ap()) for i, c_ in enumerate((4096, 4096, CTX))]
    ag2_out = [TL(nc.dram_tensor(f"ag2_out{i}", [512, c_], BF16).ap()) for i, c_ in enumerate((4096, 4096, CTX))]
    cos_d = TL(nc.dram_tensor("cos_d", [128, TALL], F32).ap())
    sin_d = TL(nc.dram_tensor("sin_d", [128, TALL], F32).ap())
    yTf_d = TL(nc.dram_tensor("yTf_d", [128, STREAM], F32).ap())
    attn_d = TL(nc.dram_tensor("attn_d", [128, 4, TALL], BF16).ap())

    with ExitStack() as st:
        P = Prog(nc, st)

        uid = [0]

        def sbuf(stack, name, shape, d=F32, nslots=1):
            uid[0] += 1
            return TL(stack.enter_context(nc.sbuf_tensor("sb%d_%s" % (uid[0], name), list(shape), d)), nslots)

        PS = [TL(st.enter_context(nc.psum_tensor(f"ps{i}", [128, 512], F32))) for i in range(8)]
        ps_rr = [0]

        def bank(exclude=()):
            while True:
                i = ps_rr[0] % 8
                ps_rr[0] += 1
                if i not in exclude:
                    return PS[i]

        def mm(out_tl, out_ap, lhsT_ap, rhs_ap, reads, start=True, stop=True):
            P.op("pe", lambda e: e.matmul(out_ap, lhsT_ap, rhs_ap, start=start, stop=stop), reads, [out_tl.b])

        def tr(out_tl, out_ap, in_ap, ident_ap, reads):
            P.op("pe", lambda e: e.transpose(out_ap, in_ap, ident_ap), reads, [out_tl.b])

        def act(out_ap, in_ap, func, reads, writes, bias=None, scale=None, accum=None):
            kw = {}
            if bias is not None:
                kw["bias"] = bias
            if scale is not None:
                kw["scale"] = scale
            if accum is not None:
                kw["accum_out"] = accum
            P.op("act", lambda e: e.activation(out_ap, in_ap, func, **kw), reads, writes)

        def tt(eng, out_ap, a_ap, b_ap, op, reads, writes):
            P.op(eng, lambda e: e.tensor_tensor(out_ap, a_ap, b_ap, op), reads, writes)

        def ts(eng, out_ap, a_ap, s1, s2, op0, op1, reads, writes):
            if op1 is None:
                P.op(eng, lambda e: e.tensor_scalar(out_ap, a_ap, s1, None, op0), reads, writes)
            else:
                P.op(eng, lambda e: e.tensor_scalar(out_ap, a_ap, s1, s2, op0, op1), reads, writes)

        def stt(eng, out_ap, a_ap, s, b_ap, op0, op1, reads, writes):
            P.op(eng, lambda e: e.scalar_tensor_tensor(out_ap, a_ap, s, b_ap, op0, op1), reads, writes)

        def cp(eng, out_ap, in_ap, reads, writes):
            P.op(eng, lambda e: e.tensor_copy(out_ap, in_ap), reads, writes)

        def dma(q, out_ap, in_ap, reads, writes, **kw):
            P.dma(q, lambda e: e.dma_start(out=out_ap, in_=in_ap, **kw), reads, writes)

        def dump(key, src_tl, src_ap, dst_ap_fn):
            if key in dbg_out:
                dma("sp", dst_ap_fn(dbg_out[key].t), src_ap, [src_tl.b] if isinstance(src_tl, TL) else src_tl,
                    [dbg_out[key].b])

        for shard, bounce, full in gathers:
            nr_ = shard.t.shape[0]
            for r0_ in range(0, nr_, 1024):
                r1_ = min(nr_, r0_ + 1024)
                dma("sp", bounce.t[r0_:r1_, :], shard.t[r0_:r1_, :], [], [bounce.b])
            P.dma("pool", lambda e, bounce=bounce, full=full: e.collective_compute(
                "AllGather", ALU.bypass, replica_groups=[list(range(NCORES))], ins=[bounce.t.opt()], outs=[full.t.opt()]),
                [bounce.b], [full.b], inc=1)

        x_tm = sbuf(st, "x_tm", [128, NT, D], F32, NT)
        ident = sbuf(st, "ident", [128, 128])
        zeros = sbuf(st, "zeros", [128, 512])
        iota = sbuf(st, "iota", [128, 512])
        onesb = sbuf(st, "onesb", [128, 64], BF16)
        onesf = sbuf(st, "onesf", [128, 128])
        siluT = sbuf(st, "siluT", [128, 8, 2])
        modT = sbuf(st, "modT", [128, 48, 2])
        bmodT = sbuf(st, "bmodT", [128, 48])
        gT = sbuf(st, "gT", [128, 16])
        A1 = sbuf(st, "A1", [128, 2, 8])
        A2 = sbuf(st, "A2", [128, 2, 8])
        rstd = sbuf(st, "rstd", [128, NT])
        sstat = sbuf(st, "sstat", [128, NT])
        xs = sbuf(st, "xs", [128, D])
        sm = sbuf(st, "sm", [128, 8])
        ucT = sbuf(st, "ucT", [128, CTX], BF16)
        dg = sbuf(st, "dg", [128, 128])

        def AP_rev(tl, row_elems, col_last, n):
            return bass.AP(tl.t, col_last, [[row_elems, 128], [-1, n]])

        def range_reduce(kf_tl, kf_ap, ki_tl, ki_ap, out_ap, in_ap, shift, reads, writes):
            ts("dve", kf_ap, in_ap, shift, 1.0 / (2 * PI), ALU.add, ALU.mult, reads, [kf_tl.b])
            cp("dve", ki_ap, kf_ap, [kf_tl.b], [ki_tl.b])
            cp("dve", kf_ap, ki_ap, [ki_tl.b], [kf_tl.b])
            stt("dve", kf_ap, kf_ap, -2 * PI, in_ap, ALU.mult, ALU.add, [kf_tl.b] + list(reads), [kf_tl.b])
            ts("dve", out_ap, kf_ap, shift, None, ALU.add, None, [kf_tl.b], writes)
            ts("dve", out_ap, out_ap, 3.14159, -3.14159, ALU.min, ALU.max, writes, writes)

        with ExitStack() as ph:
            posT = sbuf(ph, "posT", [128, TALL])
            ang = sbuf(ph, "ang", [128, TALL])
            ki = sbuf(ph, "ki", [128, TALL], I32)
            kf = sbuf(ph, "kf", [128, TALL])
            tb = sbuf(ph, "tb", [128, TALL])
            ropef = sbuf(ph, "ropef", [128, 2])
            dma("sp", x_tm.t[:, 0:NT_OWN, :], x_own.t.rearrange("(t p) d -> p t d", p=128), [], x_tm.bs[0:NT_OWN])
            dma("sp", x_tm.t[:, NT_OWN:NT, :], ctx_b.t.rearrange("(t p) d -> p t d", p=128), [], x_tm.bs[NT_OWN:NT])
            dma("sp", ident.t[:], ident_d.t[:, :], [], [ident.b])
            dma("sp", posT.t[:], pos_d.t[:, :], [], [posT.b])
            dma("sp", ropef.t[:], ropef_d.t[:, :], [], [ropef.b])
            dma("sp", iota.t[:], iota_d.t[:, :], [], [iota.b])
            dma("sp", siluT.t[:], cT.t[:, :, :], [], [siluT.b])
            P.op("pool", lambda e: e.memset(zeros.t[:], 0.0), [], [zeros.b])
            P.op("pool", lambda e: e.memset(onesb.t[:], 1.0), [], [onesb.b])
            P.op("pool", lambda e: e.memset(onesf.t[:], 1.0), [], [onesf.b])
            act(siluT.t[:], siluT.t[:], AF.Silu, [siluT.b], [siluT.b])
            act(sm.t[:, 0:1], ropef.t[:, 0:1], AF.Exp, [ropef.b], [sm.b], scale=-float(np.log(10000.0)) / 16.0)
            ts("dve", ang.t[:], posT.t[:], sm.t[:, 0:1], None, ALU.mult, None, [posT.b, sm.b], [ang.b])
            range_reduce(kf, kf.t[:], ki, ki.t[:], tb.t[:], ang.t[:], 0.0, [ang.b], [tb.b])
            act(tb.t[:], tb.t[:], AF.Sin, [tb.b], [tb.b])
            ts("dve", tb.t[:], tb.t[:], ropef.t[:, 1:2], None, ALU.mult, None, [tb.b, ropef.b], [tb.b])
            dma("sp", sin_d.t[:, :], tb.t[:], [tb.b], [sin_d.b])
            range_reduce(kf, kf.t[:], ki, ki.t[:], tb.t[:], ang.t[:], PI / 2, [ang.b], [tb.b])
            act(tb.t[:], tb.t[:], AF.Sin, [tb.b], [tb.b])
            dma("sp", cos_d.t[:, :], tb.t[:], [tb.b], [cos_d.b])
            P.barrier()

        def tile_cls(t_):
            return 0 if t_ < NT_OWN else 1

        def compute_rstd(ntiles):
            for t_ in range(ntiles):
                act(xs.t[:], x_tm.t[:, t_, :], AF.Square, [x_tm.bs[t_]], [xs.b, sstat.b], accum=sstat.t[:, t_:t_ + 1])
            ts("dve", rstd.t[:, 0:ntiles], sstat.t[:, 0:ntiles], 1.0 / D, 1e-6, ALU.mult, ALU.add, [sstat.b], [rstd.b])
            act(rstd.t[:, 0:ntiles], rstd.t[:, 0:ntiles], AF.Sqrt, [rstd.b], [rstd.b])
            P.op("dve", lambda e: e.reciprocal(rstd.t[:, 0:ntiles], rstd.t[:, 0:ntiles]), [rstd.b], [rstd.b])

        def norm_tile(t_, Acls, shslot, dst_ap_fn, dst_bufs, f32_dst=None):
            cls = tile_cls(t_)
            act(xs.t[:], x_tm.t[:, t_, :], AF.Identity, [x_tm.bs[t_], rstd.b], [xs.b], scale=rstd.t[:, t_:t_ + 1])
            for half in range(2):
                pb_ = bank()
                for q4 in range(4):
                    fc = half * 4 + q4
                    tr(pb_, pb_.t[:, q4 * 128:(q4 + 1) * 128], xs.t[:, fc * 128:(fc + 1) * 128], ident.t[:], [xs.b, ident.b])
                for q4 in range(4):
                    fc = half * 4 + q4
                    act(dst_ap_fn(fc), pb_.t[:, q4 * 128:(q4 + 1) * 128], AF.Identity, [pb_.b, Acls.b, modT.b], dst_bufs,
                        scale=Acls.t[:, cls, fc:fc + 1], bias=modT.t[:, shslot * 8 + fc, cls:cls + 1])
                    if f32_dst is not None:
                        act(f32_dst.t[:, fc, :], pb_.t[:, q4 * 128:(q4 + 1) * 128], AF.Identity, [pb_.b, Acls.b, modT.b],
                            [f32_dst.b], scale=Acls.t[:, cls, fc:fc + 1], bias=modT.t[:, shslot * 8 + fc, cls:cls + 1])

        def make_gtrow(dst, slot):
            for cls in range(2):
                for fc in range(8):
                    ts("dve", dg.t[:], ident.t[:], modT.t[:, slot * 8 + fc, cls:cls + 1], None, ALU.mult, None,
                       [ident.b, modT.b], [dg.b])
                    pb_ = bank()
                    mm(pb_, pb_.t[:, 0:128], onesf.t[:], dg.t[:], [onesf.b, dg.b])
                    cp("dve", dst.t[:, cls, fc * 128:(fc + 1) * 128], pb_.t[:, 0:128], [pb_.b], [dst.b])

        def blk_of(t_):
            return t_ + 1 if t_ < NT_OWN else 18 + (t_ - NT_OWN)

        DL = dbg.get("_layer", 0)
        stopped = False
        for l in range(nlayers):
            if dbg.get("_stop") == "0":
                break
            W = L[l]
            ctx_out = l < nlayers - 1
            with ExitStack() as ph:
                wm = sbuf(ph, "wm", [128, 2, 8, 512], F32, 2)
                dma("sp", bmodT.t[:], W["bmodT"].t[:, :], [], [bmodT.b])
                dma("sp", gT.t[:, 0:8], W["g1T"].t[:, :], [], [gT.b])
                dma("sp", gT.t[:, 8:16], W["g2T"].t[:, :], [], [gT.b])
                mps = bank()
                for piece in range(12):
                    s = piece % 2
                    dma("sp", wm.t[:, s, :, :], W["wmod"].t[:, :, piece * 512:(piece + 1) * 512], [W["wmod"].b], [wm.bs[s]])
                    for c4 in range(4):
                        cc = piece * 4 + c4
                        for kc in range(8):
                            mm(mps, mps.t[:, cc * 2:cc * 2 + 2], wm.t[:, s, kc, c4 * 128:(c4 + 1) * 128],
                               siluT.t[:, kc, :], [wm.bs[s], siluT.b], kc == 0, kc == 7)
                for cls in range(2):
                    tt("dve", modT.t[:, :, cls], mps.t[:, cls:96:2], bmodT.t[:], ALU.add, [mps.b, bmodT.b], [modT.b])
                for cls in range(2):
                    stt("dve", A1.t[:, cls, :], modT.t[:, 8:16, cls], 1.0, gT.t[:, 0:8], ALU.add, ALU.mult,
                        [modT.b, gT.b], [A1.b])
                    stt("dve", A2.t[:, cls, :], modT.t[:, 32:40, cls], 1.0, gT.t[:, 8:16], ALU.add, ALU.mult,
                        [modT.b, gT.b], [A2.b])
                if l == DL:
                    dump("modT", modT, modT.t[:], lambda o: o[:, :, :])
                P.barrier()

            kvst = ExitStack()
            kT = sbuf(kvst, "kT", [128, 20, 128], BF16, 20)
            vv = sbuf(kvst, "vv", [128, 20, 128], BF16, 20)
            with ExitStack() as ph:
                w1 = sbuf(ph, "w1", [128, 8, 1920], BF16)
                wus = sbuf(ph, "wus", [128, 8, 128], BF16)
                hT = sbuf(ph, "hT", [128, 8, 512], BF16)
                ust = sbuf(ph, "ust", [128, 2, 512], BF16, 2)
                tmpa = sbuf(ph, "tmpa", [128, 512])
                tmpb = sbuf(ph, "tmpb", [128, 512])
                cosT = sbuf(ph, "cosT", [128, TALL])
                sinT = sbuf(ph, "sinT", [128, TALL])
                dma("sp", cosT.t[:], cos_d.t[:, :], [cos_d.b], [cosT.b])
                dma("sp", sinT.t[:], sin_d.t[:, :], [sin_d.b], [sinT.b])
                for kc in range(8):
                    dma("pool", w1.t[:, kc, :], W["w1"].t[:, kc, :], [W["w1"].b], [w1.b])
                dma("pool", wus.t[:], W["wus"].t[:, :, :], [], [wus.b])
                compute_rstd(NT)
                ust_rr = 0
                for ch in range(5):
                    tiles = list(range(ch * 4, min(ch * 4 + 4, NT)))
                    n = len(tiles) * 128
                    c0 = ch * 512
                    for ti, t_ in enumerate(tiles):
                        norm_tile(t_, A1, 0, lambda fc, ti=ti: hT.t[:, fc, ti * 128:(ti + 1) * 128], [hT.b])
                    pk = bank()
                    pkr = bank()
                    for kc in range(8):
                        mm(pk, pk.t[:, 0:n], w1.t[:, kc, 1024:1152], hT.t[:, kc, 0:n], [w1.b, hT.b], kc == 0, kc == 7)
                    for kc in range(8):
                        mm(pkr, pkr.t[:, 0:n], w1.t[:, kc, 1152:1280], hT.t[:, kc, 0:n], [w1.b, hT.b], kc == 0, kc == 7)
                    tt("dve", tmpa.t[:, 0:n], pk.t[:, 0:n], cosT.t[:, c0:c0 + n], ALU.mult, [pk.b, cosT.b], [tmpa.b])
                    tt("dve", tmpb.t[:, 0:n], pkr.t[:, 0:n], sinT.t[:, c0:c0 + n], ALU.mult, [pkr.b, sinT.b], [tmpb.b])
                    for ti, t_ in enumerate(tiles):
                        blk = blk_of(t_)
                        tt("pool", kT.t[:, blk, :], tmpa.t[:, ti * 128:(ti + 1) * 128], tmpb.t[:, ti * 128:(ti + 1) * 128],
                           ALU.add, [tmpa.b, tmpb.b], [kT.bs[blk]])
                    pv = bank()
                    for ti, t_ in enumerate(tiles):
                        for kc in range(8):
                            mm(pv, pv.t[:, ti * 128:(ti + 1) * 128], hT.t[:, kc, ti * 128:(ti + 1) * 128],
                               w1.t[:, kc, 1280:1408], [w1.b, hT.b], kc == 0, kc == 7)
                    for ti, t_ in enumerate(tiles):
                        blk = blk_of(t_)
                        act(vv.t[:, blk, :], pv.t[:, ti * 128:(ti + 1) * 128], AF.Identity, [pv.b], [vv.bs[blk]])
                    if ch < 4:
                        for ti, t_ in enumerate(tiles):
                            pu = bank()
                            for kc in range(8):
                                mm(pu, pu.t[:, :], hT.t[:, kc, ti * 128:(ti + 1) * 128], w1.t[:, kc, 1408:1920],
                                   [w1.b, hT.b], kc == 0, kc == 7)
                            s = ust_rr % 2
                            ust_rr += 1
                            act(ust.t[:, s, :], pu.t[:, :], AF.Identity, [pu.b], [ust.bs[s]])
                            pc_, lr_ = (t_ * 128) // 1024, (t_ * 128) % 1024
                            dma("sp", ag1_in[pc_].t[lr_:lr_ + 128, :], ust.t[:, s, :], [ust.bs[s]], [ag1_in[pc_].b])
                    else:
                        pu = bank()
                        for kc in range(8):
                            mm(pu, pu.t[:, 0:CTX], wus.t[:, kc, :], hT.t[:, kc, 0:CTX], [wus.b, hT.b], kc == 0, kc == 7)
                        act(ucT.t[:], pu.t[:, 0:CTX], AF.Identity, [pu.b], [ucT.b])
                for i4, (tl_, blk) in enumerate(((kT, 1), (kT, 16), (vv, 1), (vv, 16))):
                    dma("sp", ag1_in[2].t[:, i4 * 128:(i4 + 1) * 128], tl_.t[:, blk, :], [tl_.bs[blk]], [ag1_in[2].b])
                for pc_ in range(3):
                    P.dma("pool", lambda e, pc_=pc_: e.collective_compute(
                        "AllGather", ALU.bypass, replica_groups=GROUPS, ins=[ag1_in[pc_].t.opt()], outs=[ag1_out[pc_].t.opt()]),
                        [ag1_in[pc_].b], [ag1_out[pc_].b], inc=1)
                P.barrier()
            if dbg.get("_stop") == "B" and l == DL:
                stopped = True

            if not stopped:
              with ExitStack() as ph:
                uTf = sbuf(ph, "uTf", [128, STREAM], BF16, 17)
                hal = sbuf(ph, "hal", [128, 4, 512], BF16)
                utile = sbuf(ph, "utile", [128, 2, 512], BF16, 2)
                selm = sbuf(ph, "selm", [128, 12, 128], BF16)
                prm = sbuf(ph, "prm", [128, 16, 8])
                bb = sbuf(ph, "bb", [128, 4, 8, 16])
                cc_ = sbuf(ph, "cc", [128, 2, 8, 16])
                Wsc = sbuf(ph, "Wsc", [128, 128])
                lB = sbuf(ph, "lB", [128, 16, 128], BF16)
                lC = sbuf(ph, "lC", [128, 16, 128], BF16)
                dsk = sbuf(ph, "dsk", [128, 1])
                tabc = sbuf(ph, "tabc", [128, 4, 512], F32, 4)
                tabs = sbuf(ph, "tabs", [128, 4, 512], F32, 4)
                rhoT = sbuf(ph, "rhoT", [128, 4, 512], F32, 4)
                kis = sbuf(ph, "kis", [128, 512], I32)
                NW = 2
                wk = [[sbuf(ph, f"wk{s}_{i}", [128, 512]) for i in range(6)] for s in range(NW)]
                xrb = [[sbuf(ph, f"xb{s}_{i}", [128, 512], BF16) for i in range(2)] for s in range(NW)]
                kfs, angs = wk[0][0], wk[0][1]
                ub = sbuf(ph, "ub", [128, 2, 512], BF16, 2)
                init = sbuf(ph, "init", [128, 4, 2], F32, 4)
                ytmp = sbuf(ph, "ytmp", [128, 3, 512], F32, 3)
                yfs = sbuf(ph, "yfs", [128, 2, 512], F32, 2)
                zst = sbuf(ph, "zst", [128, 2, 512], BF16, 2)
                dma("pool", selm.t[:], selmat_d.t[:, :, :], [], [selm.b])
                for i in range(4):
                    dma("sp", hal.t[:, i, :], ag1_out[2].t[i * 128:(i + 1) * 128, :], [ag1_out[2].b], [hal.b])
                ph_ = bank()
                for i4, (selbase, col) in enumerate(((4, 128), (8, 0), (4, 384), (8, 256))):
                    for i in range(4):
                        mm(ph_, ph_.t[:, i4 * 128:(i4 + 1) * 128], selm.t[:, selbase + i, :], hal.t[:, i, col:col + 128],
                           [selm.b, hal.b], i == 0, i == 3)
                for i4, (tl_, blk) in enumerate(((kT, 0), (kT, 17), (vv, 0), (vv, 17))):
                    cp("dve", tl_.t[:, blk, :], ph_.t[:, i4 * 128:(i4 + 1) * 128], [ph_.b], [tl_.bs[blk]])
                cp("dve", uTf.t[:, 0:CTX], ucT.t[:], [ucT.b], [uTf.bs[0]])
                ut_rr = 0
                for i in range(4):
                    for tq in range(4):
                        pu = bank()
                        for t4 in range(4):
                            s = ut_rr % 2
                            ut_rr += 1
                            tr_ = (tq * 4 + t4) * 128
                            pc_, r0 = tr_ // 1024, i * 1024 + tr_ % 1024
                            dma("sp", utile.t[:, s, :], ag1_out[pc_].t[r0:r0 + 128, :], [ag1_out[pc_].b], [utile.bs[s]])
                            for sl in range(4):
                                mm(pu, pu.t[:, t4 * 128:(t4 + 1) * 128], utile.t[:, s, sl * 128:(sl + 1) * 128],
                                   selm.t[:, sl, :], [utile.bs[s], selm.b], sl == 0, sl == 3)
                        nk = i * 4 + tq
                        act(uTf.t[:, CTX + nk * 512:CTX + (nk + 1) * 512], pu.t[:, :], AF.Identity, [pu.b], [uTf.bs[1 + nk]])
                for nm, c_ in (("lamre", 0), ("lamim", 1), ("ldt", 2)):
                    dma("sp", prm.t[:, c_, :], W[nm].t[:, :], [], [prm.b])
                dma("sp", bb.t[:, 0, :, :], W["bre"].t[:, :, :], [], [bb.b])
                dma("sp", bb.t[:, 1, :, :], W["bim"].t[:, :, :], [], [bb.b])
                dma("sp", cc_.t[:, 0, :, :], W["cre"].t[:, :, :], [], [cc_.b])
                dma("sp", cc_.t[:, 1, :, :], W["cim"].t[:, :, :], [], [cc_.b])
                dma("sp", dsk.t[:], W["dsk"].t[:, :], [], [dsk.b])
                pr = lambda c_: prm.t[:, c_, :]
                PB = [prm.b]
                act(pr(3), pr(2), AF.Exp, PB, PB)
                tt("dve", pr(4), pr(0), pr(3), ALU.mult, PB, PB)
                tt("dve", pr(5), pr(1), pr(3), ALU.mult, PB, PB)
                act(pr(6), pr(4), AF.Exp, PB, PB)
                range_reduce(kfs, kfs.t[:, 0:8], kis, kis.t[:, 0:8], angs.t[:, 0:8], pr(5), 0.0, PB, [angs.b])
                act(pr(7), angs.t[:, 0:8], AF.Sin, [angs.b], PB)
                range_reduce(kfs, kfs.t[:, 0:8], kis, kis.t[:, 0:8], angs.t[:, 0:8], pr(5), PI / 2, PB, [angs.b])
                act(pr(8), angs.t[:, 0:8], AF.Sin, [angs.b], PB)
                tt("dve", pr(9), pr(6), pr(8), ALU.mult, PB, PB)
                tt("dve", pr(10), pr(6), pr(7), ALU.mult, PB, PB)
                tt("dve", pr(11), pr(0), pr(0), ALU.mult, PB, PB)
                tt("dve", pr(15), pr(1), pr(1), ALU.mult, PB, PB)
                tt("dve", pr(11), pr(11), pr(15), ALU.add, PB, PB)
                P.op("dve", lambda e: e.reciprocal(pr(11), pr(11)), PB, PB)
                ts("dve", pr(12), pr(9), -1.0, None, ALU.add, None, PB, PB)
                tt("dve", pr(13), pr(12), pr(0), ALU.mult, PB, PB)
                tt("dve", pr(15), pr(10), pr(1), ALU.mult, PB, PB)
                tt("dve", pr(13), pr(13), pr(15), ALU.add, PB, PB)
                tt("dve", pr(13), pr(13), pr(11), ALU.mult, PB, PB)
                tt("dve", pr(14), pr(10), pr(0), ALU.mult, PB, PB)
                tt("dve", pr(15), pr(12), pr(1), ALU.mult, PB, PB)
                tt("dve", pr(14), pr(14), pr(15), ALU.subtract, PB, PB)
                tt("dve", pr(14), pr(14), pr(11), ALU.mult, PB, PB)
                if l == DL:
                    dump("prm", prm, prm.t[:], lambda o: o[:, :, :])
                for r in range(8):
                    fre = prm.t[:, 13, r:r + 1]
                    fim = prm.t[:, 14, r:r + 1]
                    ts("dve", bb.t[:, 2, r, :], bb.t[:, 0, r, :], fre, None, ALU.mult, None, [bb.b, prm.b], [bb.b])
                    ts("dve", bb.t[:, 3, r, :], bb.t[:, 1, r, :], fim, None, ALU.mult, None, [bb.b, prm.b], [bb.b])
                    tt("dve", bb.t[:, 2, r, :], bb.t[:, 2, r, :], bb.t[:, 3, r, :], ALU.subtract, [bb.b], [bb.b])
                    ts("dve", bb.t[:, 3, r, :], bb.t[:, 1, r, :], fre, None, ALU.mult, None, [bb.b, prm.b], [bb.b])
                    stt("dve", bb.t[:, 3, r, :], bb.t[:, 0, r, :], fim, bb.t[:, 3, r, :], ALU.mult, ALU.add,
                        [bb.b, prm.b], [bb.b])
                    gp = r % 4
                    for ri in range(2):
                        P.op("pool", lambda e: e.memset(Wsc.t[:], 0.0), [], [Wsc.b])
                        for g2 in range(2):
                            c0_ = 16 * (2 * gp + g2)
                            cp("dve", Wsc.t[g2 * 64:(g2 + 1) * 64, c0_:c0_ + 16], bb.t[g2 * 64:(g2 + 1) * 64, 2 + ri, r, :],
                               [bb.b], [Wsc.b])
                        pb_ = bank()
                        tr(pb_, pb_.t[:, 0:128], Wsc.t[:], ident.t[:], [Wsc.b, ident.b])
                        act(lB.t[:, r * 2 + ri, :], pb_.t[:, 0:128], AF.Identity, [pb_.b], [lB.b])
                        P.op("pool", lambda e, r=r, ri=ri: e.memset(lC.t[:, r * 2 + ri, :], 0.0), [], [lC.b])
                        for g2 in range(2):
                            c0_ = 16 * (2 * gp + g2)
                            ts("dve", lC.t[g2 * 64:(g2 + 1) * 64, r * 2 + ri, c0_:c0_ + 16],
                               cc_.t[g2 * 64:(g2 + 1) * 64, ri, r, :], (1.0 if ri == 0 else -1.0), None, ALU.mult, None,
                               [cc_.b], [lC.b])
                chunks = [(0, CTX)] + [(CTX + 512 * k, 512) for k in range(16)]
                wk_rr = 0
                for d_ in range(2):
                    for gp in range(4):
                        r = d_ * 4 + gp
                        ts("dve", angs.t[:], iota.t[:], prm.t[:, 5, r:r + 1], None, ALU.mult, None, [iota.b, prm.b], [angs.b])
                        range_reduce(kfs, kfs.t[:], kis, kis.t[:], tabs.t[:, gp, :], angs.t[:], 0.0, [angs.b], [tabs.bs[gp]])
                        act(tabs.t[:, gp, :], tabs.t[:, gp, :], AF.Sin, [tabs.bs[gp]], [tabs.bs[gp]])
                        range_reduce(kfs, kfs.t[:], kis, kis.t[:], tabc.t[:, gp, :], angs.t[:], PI / 2, [angs.b], [tabc.bs[gp]])
                        act(tabc.t[:, gp, :], tabc.t[:, gp, :], AF.Sin, [tabc.bs[gp]], [tabc.bs[gp]])
                        ts("dve", rhoT.t[:, gp, :], zeros.t[:], prm.t[:, 6, r:r + 1], None, ALU.add, None, [zeros.b, prm.b],
                           [rhoT.bs[gp]])
                        P.op("pool", lambda e, gp=gp: e.memset(init.t[:, gp, :], 0.0), [], [init.bs[gp]])
                    for ci, (c0, n) in enumerate(chunks):
                        if d_ == 0:
                            nat_c0, slot = c0, ci
                            u_ap = uTf.t[:, c0:c0 + n]
                            u_reads = [uTf.bs[ci]]
                        else:
                            if ci == 0:
                                nat_c0, slot = 0, 0
                            else:
                                nkk = 16 - ci
                                nat_c0, slot = CTX + 512 * nkk, 1 + nkk
                            s_u = ci % 2
                            cp("dve", ub.t[:, s_u, 0:n], AP_rev(uTf, STREAM, nat_c0 + n - 1, n), [uTf.bs[slot]], [ub.bs[s_u]])
                            u_ap = ub.t[:, s_u, 0:n]
                            u_reads = [ub.bs[s_u]]
                        need = ctx_out or ci > 0
                        yps = bank()
                        excl = (PS.index(yps),)
                        for gp in range(4):
                            r = d_ * 4 + gp
                            ws = wk[wk_rr % NW]
                            xb_ = xrb[wk_rr % NW]
                            wk_rr += 1
                            pre = bank(exclude=excl)
                            pim = bank(exclude=excl)
                            mm(pre, pre.t[:, 0:n], lB.t[:, r * 2, :], u_ap, [lB.b] + u_reads)
                            mm(pim, pim.t[:, 0:n], lB.t[:, r * 2 + 1, :], u_ap, [lB.b] + u_reads)
                            c_ap = tabc.t[:, gp, 0:n]
                            s_ap = tabs.t[:, gp, 0:n]
                            TB = [tabc.bs[gp], tabs.bs[gp]]
                            t1, t2, t3, t4, zr, zi = [w_.t[:, 0:n] for w_ in ws]
                            b1, b2, b3, b4, bzr, bzi = [w_.b for w_ in ws]
                            tt("dve", t1, pre.t[:, 0:n], c_ap, ALU.mult, [pre.b] + TB, [b1])
                            tt("dve", t2, pim.t[:, 0:n], s_ap, ALU.mult, [pim.b] + TB, [b2])
                            tt("pool", t1, t1, t2, ALU.add, [b1, b2], [b1])
                            tt("dve", t3, pim.t[:, 0:n], c_ap, ALU.mult, [pim.b] + TB, [b3])
                            tt("dve", t4, pre.t[:, 0:n], s_ap, ALU.mult, [pre.b] + TB, [b4])
                            tt("pool", t3, t3, t4, ALU.subtract, [b3, b4], [b3])
                            P.op("dve", lambda e, zr=zr, t1=t1, gp=gp, n=n: e.tensor_tensor_scan(
                                zr, rhoT.t[:, gp, 0:n], t1, init.t[:, gp, 0:1], ALU.mult, ALU.add),
                                [rhoT.bs[gp], b1, init.bs[gp]], [bzr])
                            P.op("dve", lambda e, zi=zi, t3=t3, gp=gp, n=n: e.tensor_tensor_scan(
                                zi, rhoT.t[:, gp, 0:n], t3, init.t[:, gp, 1:2], ALU.mult, ALU.add),
                                [rhoT.bs[gp], b3, init.bs[gp]], [bzi])
                            tt("dve", t1, zr, c_ap, ALU.mult, [bzr] + TB, [b1])
                            tt("pool", t2, zi, s_ap, ALU.mult, [bzi] + TB, [b2])
                            tt("dve", t3, zr, s_ap, ALU.mult, [bzr] + TB, [b3])
                            tt("pool", t4, zi, c_ap, ALU.mult, [bzi] + TB, [b4])
                            tt("pool", xb_[0].t[:, 0:n], t1, t2, ALU.subtract, [b1, b2], [xb_[0].b])
                            tt("dve", xb_[1].t[:, 0:n], t3, t4, ALU.add, [b3, b4], [xb_[1].b])
                            tt("dve", init.t[:, gp, 0:1], ws[0].t[:, n - 1:n], ws[1].t[:, n - 1:n], ALU.subtract, [b1, b2],
                               [init.bs[gp]])
                            tt("dve", init.t[:, gp, 1:2], ws[2].t[:, n - 1:n], ws[3].t[:, n - 1:n], ALU.add, [b3, b4],
                               [init.bs[gp]])
                            if need:
                                mm(yps, yps.t[:, 0:n], lC.t[:, r * 2, :], xb_[0].t[:, 0:n], [lC.b, xb_[0].b], gp == 0, False)
                                mm(yps, yps.t[:, 0:n], lC.t[:, r * 2 + 1, :], xb_[1].t[:, 0:n], [lC.b, xb_[1].b], False, gp == 3)
                        if not need:
                            continue
                        if d_ == 0:
                            fs_ = ci % 2
                            act(yfs.t[:, fs_, 0:n], yps.t[:, 0:n], AF.Identity, [yps.b], [yfs.bs[fs_]])
                            dma("sp", yTf_d.t[:, c0:c0 + n], yfs.t[:, fs_, 0:n], [yfs.bs[fs_]], [yTf_d.b])
                        else:
                            ys = ci % 3
                            zs = ci % 2
                            ya = ytmp.t[:, ys, 0:n]
                            YB = [ytmp.bs[ys]]
                            yb2 = ytmp.t[:, (ys + 1) % 3, 0:n]
                            YB2 = [ytmp.bs[(ys + 1) % 3]]
                            fs_ = ci % 2
                            dma("sp", yfs.t[:, fs_, 0:n], yTf_d.t[:, nat_c0:nat_c0 + n], [yTf_d.b], [yfs.bs[fs_]])
                            act(ya, yps.t[:, 0:n], AF.Identity, [yps.b], YB)
                            tt("dve", yb2, AP_rev(ytmp, 3 * 512, ys * 512 + n - 1, n), yfs.t[:, fs_, 0:n], ALU.add,
                               YB + [yfs.bs[fs_]], YB2)
                            stt("dve", yb2, uTf.t[:, nat_c0:nat_c0 + n], dsk.t[:, 0:1], yb2, ALU.mult, ALU.add,
                                [uTf.bs[slot], dsk.b] + YB2, YB2)
                            if l == DL and ci in (0, 16):
                                cc0 = 0 if ci == 0 else 256
                                dump("ssmy", YB2, yb2, lambda o, cc0=cc0, n=n: o[:, cc0:cc0 + n])
                            act(ya, yb2, AF.Square, YB2, YB)
                            ts("dve", ya, ya, 0.044715, 1.0, ALU.mult, ALU.add, YB, YB)
                            tt("pool", ya, ya, yb2, ALU.mult, YB + YB2, YB)
                            act(ya, ya, AF.Sigmoid, YB, YB, scale=1.5957691216057308)
                            tt("pool", zst.t[:, zs, 0:n], ya, yb2, ALU.mult, YB + YB2, [zst.bs[zs]])
                            if ci == 0:
                                pc_, dcol = 2, 0
                            else:
                                pc_, dcol = (nat_c0 - CTX) // 4096, (nat_c0 - CTX) % 4096
                            dma("sp", ag2_in[pc_].t[:, dcol:dcol + n], zst.t[:, zs, 0:n], [zst.bs[zs]], [ag2_in[pc_].b])
                for pc_ in range(3 if ctx_out else 2):
                    P.dma("pool", lambda e, pc_=pc_: e.collective_compute(
                        "AllGather", ALU.bypass, replica_groups=GROUPS, ins=[ag2_in[pc_].t.opt()], outs=[ag2_out[pc_].t.opt()]),
                        [ag2_in[pc_].b], [ag2_out[pc_].b], inc=1)
                P.barrier()
            if dbg.get("_stop") == "C" and l == DL:
                stopped = True

            if not stopped:
              with ExitStack() as ph:
                wq = sbuf(ph, "wq", [128, 8, 1024], BF16)
                hT = sbuf(ph, "hTd", [128, 8, 512], BF16)
                qT = sbuf(ph, "qT", [128, 4, 512], BF16)
                pt = sbuf(ph, "pt", [128, 3, 512], BF16, 3)
                sg = sbuf(ph, "sg", [128, 2, 512], F32, 2)
                ta = sbuf(ph, "ta", [128, 2, 512], F32, 2)
                rec = sbuf(ph, "rec", [128, 512])
                attnT = sbuf(ph, "attnT", [128, 2, 4, 512], BF16, 2)
                cosT = sbuf(ph, "cosT", [128, TALL])
                sinT = sbuf(ph, "sinT", [128, TALL])
                masks = sbuf(ph, "masks", [128, 4, 512], BF16)
                sinkE = sbuf(ph, "sinkE", [128, 8])
                sinkrow = sbuf(ph, "sinkrow", [128, 512])
                dma("sp", cosT.t[:], cos_d.t[:, :], [cos_d.b], [cosT.b])
                dma("sp", sinT.t[:], sin_d.t[:, :], [sin_d.b], [sinT.b])
                for k4 in range(4):
                    dma("pool", masks.t[:, k4, :], masks_d.t[:, k4, :], [], [masks.b])
                for kc in range(8):
                    dma("pool", wq.t[:, kc, :], W["w1"].t[:, kc, 0:1024], [W["w1"].b], [wq.b])
                dma("sp", sinkE.t[:], W["sink"].t[:, :], [], [sinkE.b])
                act(sinkE.t[:], sinkE.t[:], AF.Exp, [sinkE.b], [sinkE.b])
                for c in range(4):
                    ts("dve", sinkrow.t[0:64, c * 128:(c + 1) * 128], zeros.t[0:64, 0:128], sinkE.t[0:64, c:c + 1], None,
                       ALU.add, None, [zeros.b, sinkE.b], [sinkrow.b])
                    ts("dve", sinkrow.t[64:128, c * 128:(c + 1) * 128], zeros.t[64:128, 0:128], sinkE.t[64:128, 4 + c:5 + c],
                       None, ALU.add, None, [zeros.b, sinkE.b], [sinkrow.b])
                nchunks = 5 if ctx_out else 4
                pt_rr = 0
                sg_rr = 0
                for ch in range(nchunks):
                    tiles = list(range(ch * 4, min(ch * 4 + 4, NT)))
                    n = len(tiles) * 128
                    c0 = ch * 512
                    is_ctx = ch == 4
                    for ti, t_ in enumerate(tiles):
                        norm_tile(t_, A1, 0, lambda fc, ti=ti: hT.t[:, fc, ti * 128:(ti + 1) * 128], [hT.b])
                    for c in range(4):
                        pq = bank()
                        pqr = bank()
                        for kc in range(8):
                            mm(pq, pq.t[:, 0:n], wq.t[:, kc, c * 128:(c + 1) * 128], hT.t[:, kc, 0:n], [wq.b, hT.b], kc == 0, kc == 7)
                        for kc in range(8):
                            mm(pqr, pqr.t[:, 0:n], wq.t[:, kc, 512 + c * 128:512 + (c + 1) * 128], hT.t[:, kc, 0:n], [wq.b, hT.b],
                               kc == 0, kc == 7)
                        s_ = sg_rr % 2
                        sg_rr += 1
                        tt("dve", sg.t[:, s_, 0:n], pq.t[:, 0:n], cosT.t[:, c0:c0 + n], ALU.mult, [pq.b, cosT.b], [sg.bs[s_]])
                        tt("dve", ta.t[:, s_, 0:n], pqr.t[:, 0:n], sinT.t[:, c0:c0 + n], ALU.mult, [pqr.b, sinT.b], [ta.bs[s_]])
                        tt("pool", qT.t[:, c, 0:n], sg.t[:, s_, 0:n], ta.t[:, s_, 0:n], ALU.add, [sg.bs[s_], ta.bs[s_]], [qT.b])
                    as_ = ch % 2
                    for qi, t_ in enumerate(tiles):
                        pnum = bank()
                        pden = bank()
                        excl = (PS.index(pnum), PS.index(pden))
                        if is_ctx:
                            kbl = [(18, None), (19, None)]
                        else:
                            kbl = [(t_, 0 if t_ == 0 else 1), (t_ + 1, None), (t_ + 2, 2 if t_ == NT_OWN - 1 else 3),
                                   (18, None), (19, None)]
                        for gk in range(2):
                            pb = 64 * gk

                            def pv_(bi, kb, s_, pb=pb):
                                mm(pnum, pnum.t[pb:pb + 64, :], vv.t[:, kb, pb:pb + 64], pt.t[:, s_, :], [vv.bs[kb], pt.bs[s_]],
                                   bi == 0, bi == len(kbl) - 1)
                                mm(pden, pden.t[pb:pb + 64, :], onesb.t[:, 0:64], pt.t[:, s_, :], [onesb.b, pt.bs[s_]],
                                   bi == 0, bi == len(kbl) - 1)

                            prev_ = None
                            for bi, (kb, mk) in enumerate(kbl):
                                pst = bank(exclude=excl)
                                for c in range(4):
                                    mm(pst, pst.t[:, c * 128:(c + 1) * 128], kT.t[pb:pb + 64, kb, :],
                                       qT.t[pb:pb + 64, c, qi * 128:(qi + 1) * 128], [kT.bs[kb], qT.b])
                                s_ = pt_rr % 3
                                pt_rr += 1
                                act(pt.t[:, s_, :], pst.t[:, :], AF.Exp, [pst.b], [pt.bs[s_]], scale=0.125)
                                if mk is not None:
                                    tt("pool", pt.t[:, s_, :], pt.t[:, s_, :], masks.t[:, mk, :], ALU.mult, [pt.bs[s_], masks.b],
                                       [pt.bs[s_]])
                                if prev_ is not None:
                                    pv_(*prev_)
                                prev_ = (bi, kb, s_)
                            pv_(*prev_)
                        tt("dve", rec.t[:], pden.t[:, :], sinkrow.t[:], ALU.add, [pden.b, sinkrow.b], [rec.b])
                        P.op("dve", lambda e: e.reciprocal(rec.t[:], rec.t[:]), [rec.b], [rec.b])
                        for c in range(4):
                            tt("dve", attnT.t[:, as_, c, qi * 128:(qi + 1) * 128], pnum.t[:, c * 128:(c + 1) * 128],
                               rec.t[:, c * 128:(c + 1) * 128], ALU.mult, [pnum.b, rec.b], [attnT.bs[as_]])
                    dma("sp", attn_d.t[:, :, c0:c0 + n], attnT.t[:, as_, :, 0:n], [attnT.bs[as_]], [attn_d.b])
                P.barrier()
            kvst.close()
            if dbg.get("_stop") == "D1" and l == DL:
                stopped = True

            if not stopped:
              with ExitStack() as ph:
                wglu = sbuf(ph, "wglu", [128, 4, 512], BF16)
                wbra = sbuf(ph, "wbra", [128, 4, D], BF16)
                wbrs = sbuf(ph, "wbrs", [128, 4, D], BF16)
                wout = sbuf(ph, "wout", [128, 8, D], BF16)
                w2s = sbuf(ph, "w2s", [128, 2, 8, 256], BF16, 2)
                hT = sbuf(ph, "hTd2", [128, 8, 512], BF16)
                zstage = sbuf(ph, "zstage", [128, 4, 512], BF16)
                zsb = sbuf(ph, "zsb", [128, 4, 512], BF16)
                ssmT = sbuf(ph, "ssmT", [128, 4, 512], BF16)
                mT = sbuf(ph, "mT", [128, 8, 512], BF16)
                sg = sbuf(ph, "sg2", [128, 2, 512], F32, 2)
                ta = sbuf(ph, "ta2", [128, 2, 512], F32, 2)
                selm = sbuf(ph, "selm2", [128, 4, 128], BF16)
                gt1row = sbuf(ph, "gt1row", [128, 2, D])
                attc = sbuf(ph, "attc", [128, 4, 512], BF16)
                dma("pool", selm.t[:], selmat_d.t[:, 0:4, :], [], [selm.b])
                for kc in range(8):
                    dma("pool", wout.t[:, kc, :], W["wout"].t[:, kc, :], [W["wout"].b], [wout.b])
                for kc in range(4):
                    dma("pool", wglu.t[:, kc, :], W["wglu"].t[:, kc, :], [], [wglu.b])
                    dma("pool", wbra.t[:, kc, :], W["wbra"].t[:, kc, :], [W["wbra"].b], [wbra.b])
                    dma("pool", wbrs.t[:, kc, :], W["wbrs"].t[:, kc, :], [W["wbrs"].b], [wbrs.b])
                make_gtrow(gt1row, 2)
                nchunks = 5 if ctx_out else 4
                sg_rr = 0
                w2_rr = 0
                for ch in range(nchunks):
                    tiles = list(range(ch * 4, min(ch * 4 + 4, NT)))
                    n = len(tiles) * 128
                    c0 = ch * 512
                    is_ctx = ch == 4
                    cls = 1 if is_ctx else 0
                    for ti, t_ in enumerate(tiles):
                        norm_tile(t_, A1, 0, lambda fc, ti=ti: hT.t[:, fc, ti * 128:(ti + 1) * 128], [hT.b])
                    dma("sp", attc.t[:, :, 0:n], attn_d.t[:, :, c0:c0 + n], [attn_d.b], [attc.b])
                    for sl in range(4):
                        if is_ctx:
                            dma("sp", zsb.t[:, sl, 0:n], ag2_out[2].t[sl * 128:(sl + 1) * 128, :], [ag2_out[2].b], [zsb.b])
                        else:
                            for i in range(4):
                                gc_ = i * TOWN + c0
                                dma("sp", zstage.t[:, i, :],
                                    ag2_out[gc_ // 4096].t[sl * 128:(sl + 1) * 128, gc_ % 4096:gc_ % 4096 + 512],
                                    [ag2_out[gc_ // 4096].b], [zstage.b])
                            pz = bank()
                            for i in range(4):
                                mm(pz, pz.t[:, :], selm.t[:, i, :], zstage.t[:, i, :], [selm.b, zstage.b], i == 0, i == 3)
                            act(zsb.t[:, sl, :], pz.t[:, :], AF.Identity, [pz.b], [zsb.b])
                    for fc in range(4):
                        pg = bank()
                        for sl in range(4):
                            mm(pg, pg.t[:, 0:n], wglu.t[:, sl, fc * 128:(fc + 1) * 128], zsb.t[:, sl, 0:n], [wglu.b, zsb.b],
                               sl == 0, sl == 3)
                        s_ = sg_rr % 2
                        sg_rr += 1
                        act(sg.t[:, s_, 0:n], pg.t[:, 0:n], AF.Sigmoid, [pg.b], [sg.bs[s_]])
                        tt("dve", ssmT.t[:, fc, 0:n], zsb.t[:, fc, 0:n], sg.t[:, s_, 0:n], ALU.mult, [zsb.b, sg.bs[s_]], [ssmT.b])
                    if l == DL and ch == 0 and "ssmT" in dbg_out:
                        sdb = sbuf(ph, "sdb", [128, 4, 512])
                        cp("dve", sdb.t[:], ssmT.t[:], [ssmT.b], [sdb.b])
                        dump("ssmT", sdb, sdb.t[:], lambda o: o[:, :, :])
                    for fc in range(8):
                        ws_ = w2_rr % 2
                        w2_rr += 1
                        dma("pool", w2s.t[:, ws_, :, :], W["w2g"].t[fc, :, :, :], [W["w2g"].b], [w2s.bs[ws_]])
                        pA = bank()
                        pS = bank()
                        pga = bank()
                        pgs = bank()
                        fs = slice(fc * 128, (fc + 1) * 128)
                        for c in range(4):
                            mm(pA, pA.t[:, 0:n], wbra.t[:, c, fs], attc.t[:, c, 0:n], [wbra.b, attc.b], c == 0, c == 3)
                        for c in range(4):
                            mm(pS, pS.t[:, 0:n], wbrs.t[:, c, fs], ssmT.t[:, c, 0:n], [wbrs.b, ssmT.b], c == 0, c == 3)
                        for kc in range(8):
                            mm(pga, pga.t[:, 0:n], w2s.t[:, ws_, kc, 0:128], hT.t[:, kc, 0:n], [w2s.bs[ws_], hT.b], kc == 0, kc == 7)
                        for kc in range(8):
                            mm(pgs, pgs.t[:, 0:n], w2s.t[:, ws_, kc, 128:256], hT.t[:, kc, 0:n], [w2s.bs[ws_], hT.b], kc == 0, kc == 7)
                        act(sg.t[:, 0, 0:n], pga.t[:, 0:n], AF.Sigmoid, [pga.b], [sg.bs[0]])
                        act(sg.t[:, 1, 0:n], pgs.t[:, 0:n], AF.Sigmoid, [pgs.b], [sg.bs[1]])
                        tt("dve", ta.t[:, 0, 0:n], pA.t[:, 0:n], sg.t[:, 0, 0:n], ALU.mult, [pA.b, sg.bs[0]], [ta.bs[0]])
                        tt("dve", ta.t[:, 1, 0:n], pS.t[:, 0:n], sg.t[:, 1, 0:n], ALU.mult, [pS.b, sg.bs[1]], [ta.bs[1]])
                        tt("pool", mT.t[:, fc, 0:n], ta.t[:, 0, 0:n], ta.t[:, 1, 0:n], ALU.add, [ta.bs[0], ta.bs[1]], [mT.b])
                    for ti, t_ in enumerate(tiles):
                        for half in range(2):
                            py = bank()
                            hs_ = slice(half * 512, (half + 1) * 512)
                            for kc in range(8):
                                mm(py, py.t[:, :], mT.t[:, kc, ti * 128:(ti + 1) * 128], wout.t[:, kc, hs_], [mT.b, wout.b],
                                   kc == 0, kc == 7)
                            s_ = sg_rr % 2
                            sg_rr += 1
                            tt("dve", sg.t[:, s_, :], py.t[:, :], gt1row.t[:, cls, hs_], ALU.mult, [py.b, gt1row.b], [sg.bs[s_]])
                            tt("pool", x_tm.t[:, t_, hs_], x_tm.t[:, t_, hs_], sg.t[:, s_, :], ALU.add, [sg.bs[s_], x_tm.bs[t_]],
                               [x_tm.bs[t_]])
                P.barrier()
            if l == DL and not stopped:
                dump("xmix", x_tm.bs, x_tm.t[:], lambda o: o[:, :, :])
            if dbg.get("_stop") == "D2" and l == DL:
                stopped = True
            if stopped:
                break

            if not stopped:
              with ExitStack() as ph:
                ntl = NT if ctx_out else NT_OWN
                h2T = sbuf(ph, "h2T", [128, 8, TALL], BF16, NT)
                h2f = sbuf(ph, "h2f", [128, 8, 128])
                wr = sbuf(ph, "wr", [128, 8, 36])
                Wt = sbuf(ph, "Wt", [128, NT, 32], F32, NT)
                lg = sbuf(ph, "lg", [128, 36])
                rs = sbuf(ph, "rs", [128, 16])
                rt = sbuf(ph, "rt", [128, 4, 32])
                weg = sbuf(ph, "weg", [128, 2, 8, 512], BF16, 2)
                weu = sbuf(ph, "weu", [128, 2, 8, 512], BF16, 2)
                wed = sbuf(ph, "wed", [128, 2, 4, D], BF16, 2)
                hid = sbuf(ph, "hid", [128, 2, 4, 512], BF16, 2)
                sgm = sbuf(ph, "sgm", [128, 2, 512], F32, 2)
                ty = sbuf(ph, "ty", [128, 2, 512], F32, 2)
                gt2row = sbuf(ph, "gt2row", [128, 2, D])
                dma("sp", wr.t[:], W["wr"].t[:, :, :], [], [wr.b])
                make_gtrow(gt2row, 5)
                compute_rstd(ntl)
                RB = [rs.b]
                c_ = lambda i: rs.t[:, i:i + 1]
                for t_ in range(ntl):
                    norm_tile(t_, A2, 3, lambda fc, t_=t_: h2T.t[:, fc, t_ * 128:(t_ + 1) * 128], [h2T.bs[t_]], f32_dst=h2f)
                    pl = bank()
                    for kc in range(8):
                        mm(pl, pl.t[:, 0:36], h2f.t[:, kc, :], wr.t[:, kc, :], [h2f.b, wr.b], kc == 0, kc == 7)
                    cp("dve", lg.t[:], pl.t[:, 0:36], [pl.b], [lg.b])
                    P.op("dve", lambda e: e.tensor_reduce(rs.t[:, 0:1], lg.t[:, 0:4], AX.X, ALU.max), [lg.b], RB)
                    ts("dve", c_(1), c_(0), -1.0, None, ALU.mult, None, RB, RB)
                    act(rt.t[:, 0, 0:4], lg.t[:, 0:4], AF.Exp, [lg.b] + RB, [rt.b, rs.b], bias=rs.t[:, 1:2], accum=rs.t[:, 2:3])
                    P.op("dve", lambda e: e.reciprocal(rs.t[:, 3:4], rs.t[:, 2:3]), RB, RB)
                    ts("dve", rt.t[:, 0, 4:8], lg.t[:, 0:4], rs.t[:, 0:1], None, ALU.is_equal, None, [lg.b] + RB, [rt.b])
                    ts("dve", rt.t[:, 0, 4:8], rt.t[:, 0, 4:8], -1.0, 1e30, ALU.add, ALU.mult, [rt.b], [rt.b])
                    for g in range(4):
                        ts("dve", rt.t[:, 1, 8 * g:8 * g + 8], lg.t[:, 4 + 8 * g:12 + 8 * g], rt.t[:, 0, 4 + g:5 + g], None,
                           ALU.add, None, [lg.b, rt.b], [rt.b])
                    P.op("dve", lambda e: e.tensor_reduce(rs.t[:, 4:5], rt.t[:, 1, :], AX.X, ALU.max), [rt.b], RB)
                    ts("dve", rt.t[:, 2, :], rt.t[:, 1, :], rs.t[:, 4:5], None, ALU.is_equal, None, [rt.b] + RB, [rt.b])
                    stt("dve", rt.t[:, 2, :], rt.t[:, 2, :], -1e30, rt.t[:, 1, :], ALU.mult, ALU.add, [rt.b], [rt.b])
                    P.op("dve", lambda e: e.tensor_reduce(rs.t[:, 5:6], rt.t[:, 2, :], AX.X, ALU.max), [rt.b], RB)
                    ts("dve", rt.t[:, 2, :], rt.t[:, 1, :], rs.t[:, 5:6], None, ALU.is_ge, None, [rt.b] + RB, [rt.b])
                    ts("dve", c_(6), c_(4), -1.0, None, ALU.mult, None, RB, RB)
                    act(rt.t[:, 3, :], rt.t[:, 1, :], AF.Exp, [rt.b] + RB, [rt.b], bias=rs.t[:, 6:7])
                    tt("dve", rt.t[:, 3, :], rt.t[:, 3, :], rt.t[:, 2, :], ALU.mult, [rt.b], [rt.b])
                    P.op("dve", lambda e: e.tensor_reduce(rs.t[:, 7:8], rt.t[:, 3, :], AX.X, ALU.add), [rt.b], RB)
                    P.op("dve", lambda e: e.reciprocal(rs.t[:, 7:8], rs.t[:, 7:8]), RB, RB)
                    tt("dve", c_(7), c_(7), c_(3), ALU.mult, RB, RB)
                    ts("dve", Wt.t[:, t_, :], rt.t[:, 3, :], rs.t[:, 7:8], None, ALU.mult, None, [rt.b] + RB, [Wt.bs[t_]])
                if l == DL:
                    dump("Wt", Wt.bs, Wt.t[:], lambda o: o[:, :, :])
                nch = 5 if ctx_out else 4
                ty_rr = 0
                nexp = dbg.get("_nexp", NEXP)
                for e_ in range(nexp):
                    s = e_ % 2
                    for k2 in range(2):
                        dma("pool", weg.t[:, s, 4 * k2:4 * k2 + 4, :], W["weg"].t[e_, :, 4 * k2:4 * k2 + 4, :], [W["weg"].b], [weg.bs[s]])
                        dma("pool", weu.t[:, s, 4 * k2:4 * k2 + 4, :], W["weu"].t[e_, :, 4 * k2:4 * k2 + 4, :], [W["weu"].b], [weu.bs[s]])
                        dma("pool", wed.t[:, s, 2 * k2:2 * k2 + 2, :], W["wed"].t[e_, :, 2 * k2:2 * k2 + 2, :], [W["wed"].b], [wed.bs[s]])
                    for ch in range(nch):
                        tiles = list(range(ch * 4, min(ch * 4 + 4, NT)))
                        n = len(tiles) * 128
                        c0 = ch * 512
                        cls = 1 if ch == 4 else 0
                        hs = ch % 2
                        for hc in range(4):
                            pg = bank()
                            pu = bank()
                            hrd = [h2T.bs[t_] for t_ in tiles]
                            for kc in range(8):
                                mm(pg, pg.t[:, 0:n], weg.t[:, s, kc, hc * 128:(hc + 1) * 128], h2T.t[:, kc, c0:c0 + n],
                                   [weg.bs[s]] + hrd, kc == 0, kc == 7)
                            for kc in range(8):
                                mm(pu, pu.t[:, 0:n], weu.t[:, s, kc, hc * 128:(hc + 1) * 128], h2T.t[:, kc, c0:c0 + n],
                                   [weu.bs[s]] + hrd, kc == 0, kc == 7)
                            ss_ = hc % 2
                            act(sgm.t[:, ss_, 0:n], pg.t[:, 0:n], AF.Silu, [pg.b], [sgm.bs[ss_]])
                            tt("dve", hid.t[:, hs, hc, 0:n], pu.t[:, 0:n], sgm.t[:, ss_, 0:n], ALU.mult, [pu.b, sgm.bs[ss_]],
                               [hid.bs[hs]])
                        for ti, t_ in enumerate(tiles):
                            for half in range(2):
                                py = bank()
                                hs_ = slice(half * 512, (half + 1) * 512)
                                for hc in range(4):
                                    mm(py, py.t[:, :], hid.t[:, hs, hc, ti * 128:(ti + 1) * 128], wed.t[:, s, hc, hs_],
                                       [hid.bs[hs], wed.bs[s]], hc == 0, hc == 3)
                                y_ = ty_rr % 2
                                ty_rr += 1
                                tt("dve", ty.t[:, y_, :], py.t[:, :], gt2row.t[:, cls, hs_], ALU.mult, [py.b, gt2row.b],
                                   [ty.bs[y_]])
                                stt("dve", x_tm.t[:, t_, hs_], ty.t[:, y_, :], Wt.t[:, t_, e_:e_ + 1], x_tm.t[:, t_, hs_],
                                    ALU.mult, ALU.add, [ty.bs[y_], Wt.bs[t_], x_tm.bs[t_]], [x_tm.bs[t_]])
                P.barrier()
            if l == DL and not stopped:
                dump("xout", x_tm.bs, x_tm.t[:], lambda o: o[:, :, :])

        with ExitStack() as ph:
            gfin = sbuf(ph, "gfin", [128, D])
            ob = sbuf(ph, "ob", [128, 2, D], F32, 2)
            dma("sp", gfin.t[:], gfin_d.t[:, :], [], [gfin.b])
            compute_rstd(NT_OWN)
            for t_ in range(NT_OWN):
                s = t_ % 2
                stt("dve", ob.t[:, s, :], x_tm.t[:, t_, :], rstd.t[:, t_:t_ + 1], gfin.t[:], ALU.mult, ALU.mult,
                    [x_tm.bs[t_], rstd.b, gfin.b], [ob.bs[s]])
                dma("sp", out_d.t[t_ * 128:(t_ + 1) * 128, :], ob.t[:, s, :], [ob.bs[s]], [out_d.b])
        P.wait_all("sp", [out_d.b] + [v.b for v in dbg_out.values()])
        P.emit()
    return nc, names_in


def _kc(w):
    K, C = w.shape
    return np.ascontiguousarray(w.reshape(K // 128, 128, C).transpose(1, 0, 2))


def prep_inputs(inputs, nlayers=2, nei=NEXP):
    f = lambda a: np.ascontiguousarray(np.asarray(a, dtype=np.float32))
    I_ = {k: f(v) for k, v in inputs.items()}
    shared = {}
    e = np.arange(64)
    partner = np.where((e % 32) < 16, e + 16, e - 16)
    head_order = [h for c in range(4) for h in (c, c + 4)]
    qcols = np.concatenate([h * 64 + np.arange(64) for h in head_order])
    qrcols = np.concatenate([h * 64 + partner for h in head_order])
    kcols = 512 + np.arange(128)
    krcols = 512 + np.concatenate([hk * 64 + partner for hk in range(2)])
    vcols = 640 + np.arange(128)
    ucols = 768 + np.arange(512)
    w1cols = np.concatenate([qcols, qrcols, kcols, krcols, vcols, ucols])
    brarows = np.concatenate([h * 64 + np.arange(64) for h in head_order])
    shared["ident"] = np.eye(128, dtype=np.float32)
    shared["iota"] = np.ascontiguousarray(np.broadcast_to(np.arange(1, 513, dtype=np.float32), (128, 512)))
    shared["gfin"] = np.ascontiguousarray(np.broadcast_to(I_["g_final"], (128, D)))
    ee = np.arange(128) % 64
    ropef = np.stack([(ee % 16).astype(np.float32), np.where((ee % 32) < 16, -1.0, 1.0).astype(np.float32)], 1)
    shared["ropef"] = np.ascontiguousarray(ropef)
    for l in range(nlayers):
        win = I_["w_in"][l]
        shared[f"wmod{l}"] = _kc(I_["w_mod"][l])
        shared[f"bmodT{l}"] = np.ascontiguousarray(I_["b_mod"][l].reshape(48, 128).T)
        shared[f"g1T{l}"] = np.ascontiguousarray(I_["g_norm1"][l].reshape(8, 128).T)
        shared[f"g2T{l}"] = np.ascontiguousarray(I_["g_norm2"][l].reshape(8, 128).T)
        shared[f"w1_{l}"] = _kc(win[:, w1cols])
        ga = _kc(win[:, 1280:2304]).reshape(128, 8, 8, 128)
        gs = _kc(win[:, 2304:3328]).reshape(128, 8, 8, 128)
        shared[f"w2g{l}"] = np.ascontiguousarray(np.concatenate([ga, gs], axis=3).transpose(2, 0, 1, 3))
        shared[f"wglu{l}"] = _kc(I_["w_glu"][l])
        shared[f"wbra{l}"] = _kc(I_["w_br_attn"][l][brarows, :])
        shared[f"wbrs{l}"] = _kc(I_["w_br_ssm"][l])
        shared[f"wout{l}"] = _kc(I_["w_out"][l])
        shared[f"wr{l}"] = _kc(np.concatenate([I_["w_router_group"][l], I_["w_router_expert"][l]], axis=1))
        shared[f"weg{l}"] = np.ascontiguousarray(I_["w_exp_gate"][l][:nei].reshape(nei, 8, 128, 512).transpose(0, 2, 1, 3))
        shared[f"weu{l}"] = np.ascontiguousarray(I_["w_exp_up"][l][:nei].reshape(nei, 8, 128, 512).transpose(0, 2, 1, 3))
        shared[f"wed{l}"] = np.ascontiguousarray(I_["w_exp_down"][l][:nei].reshape(nei, 4, 128, D).transpose(0, 2, 1, 3))
        shared[f"sink{l}"] = np.ascontiguousarray(np.broadcast_to(I_["attn_sink"][l], (128, 8)))
    kk = np.arange(128)[:, None]
    qq = np.arange(128)[None, :]
    mprev = np.tile((kk >= qq).astype(np.float32), (1, 4))
    mnext = np.tile((kk <= qq).astype(np.float32), (1, 4))
    per_core = []
    for r in range(NCORES):
        b, j = r // 4, r % 4
        m = dict(shared)
        t0 = j * TOWN
        m["x_own"] = np.ascontiguousarray(I_["x"][b, t0:t0 + TOWN])
        m["ctx_b"] = np.ascontiguousarray(I_["ctx"][b])
        cT = np.stack([I_["c"][b].reshape(8, 128).T, I_["c_ctx"].reshape(8, 128).T], axis=2)
        m["cT"] = np.ascontiguousarray(cT)
        tpos = np.arange(t0, t0 + TOWN)
        rows = (tpos // 64).astype(np.float32)
        cols = (tpos % 64).astype(np.float32)
        pos = np.zeros((128, TALL), np.float32)
        is_row = (ee % 64) < 32
        pos[:, :TOWN] = np.where(is_row[:, None], rows[None, :], cols[None, :])
        m["pos"] = pos
        mk = np.zeros((128, 4, 512), np.float32)
        mk[:, 0] = mprev if j > 0 else 0.0
        mk[:, 1] = mprev
        mk[:, 2] = mnext if j < 3 else 0.0
        mk[:, 3] = mnext
        m["masks"] = mk
        sel = np.zeros((128, 12, 128), np.float32)
        eye = np.eye(128, dtype=np.float32)
        sel[:, j] = eye
        if j > 0:
            sel[:, 4 + j - 1] = eye
        if j < 3:
            sel[:, 8 + j + 1] = eye
        m["selmat"] = sel
        for l in range(nlayers):
            win = I_["w_in"][l]
            m[f"wus{l}"] = _kc(win[:, 768 + 128 * j:768 + 128 * (j + 1)])
            g0 = 8 * j

            def rowlay(a):
                a = a.reshape((2, 4, 2, 64) + a.shape[3:])
                perm = (2, 3, 0, 1) + tuple(range(4, a.ndim))
                a = a.transpose(perm)
                return np.ascontiguousarray(a.reshape((128, 8) + a.shape[4:]))

            m[f"lamre{l}"] = rowlay(I_["ssm_lam_re"][l][:, g0:g0 + 8])
            m[f"lamim{l}"] = rowlay(I_["ssm_lam_im"][l][:, g0:g0 + 8])
            ldt = np.broadcast_to(I_["ssm_log_dt"][l][:, g0:g0 + 8, None], (2, 8, 64))
            m[f"ldt{l}"] = rowlay(np.ascontiguousarray(ldt))
            m[f"bre{l}"] = rowlay(I_["ssm_b_re"][l][:, g0:g0 + 8])
            m[f"bim{l}"] = rowlay(I_["ssm_b_im"][l][:, g0:g0 + 8])
            m[f"cre{l}"] = rowlay(np.ascontiguousarray(I_["ssm_c_re"][l][:, g0:g0 + 8].transpose(0, 1, 3, 2)))
            m[f"cim{l}"] = rowlay(np.ascontiguousarray(I_["ssm_c_im"][l][:, g0:g0 + 8].transpose(0, 1, 3, 2)))
            m[f"dsk{l}"] = np.ascontiguousarray(I_["ssm_d"][l][128 * j:128 * (j + 1)].reshape(128, 1))
        for k in list(m.keys()):
            if False:
                a = m[k]
                a2 = a.reshape(-1, a.shape[-1])
                rr = a2.shape[0] // NCORES
                m[k] = np.ascontiguousarray(a2[r * rr:(r + 1) * rr])
        per_core.append(m)
    return per_core


_CACHE = {}


def kernel(**inputs):
    if "nc" not in _CACHE:
        _CACHE["nc"] = build_program(2)
    nc, names = _CACHE["nc"]
    per_core = prep_inputs(inputs, 2)
    in_maps = [{k: m[k] for k in names} for m in per_core]
    res = run_bass_kernel_spmd(nc, in_maps, core_ids=list(range(NCORES)))
    out = np.zeros((2, SEQ, D), np.float32)
    for r in range(NCORES):
        b, j = r // 4, r % 4
        out[b, j * TOWN:(j + 1) * TOWN] = res.results[r]["out"]
    return out
```

```python
from contextlib import ExitStack
import numpy as np
import concourse.bass as bass
import concourse.mybir as mybir
from concourse.bass_utils import run_bass_kernel_spmd

dt = mybir.dt
ALU = mybir.AluOpType
AF = mybir.ActivationFunctionType
AX = mybir.AxisListType
F32 = dt.float32
BF16 = dt.bfloat16
I32 = dt.int32

NCORES = 8
D = 1024
TOWN = 2048
NT_OWN = 16
NT = 18
TALL = 2304
SEQ = 8192
CTX = 256
STREAM = SEQ + CTX
NEXP = 32
PI = float(np.pi)
GROUPS = [[0, 1, 2, 3], [4, 5, 6, 7]]


class Buf:
    __slots__ = ("name", "last_w", "readers")

    def __init__(self, name=""):
        self.name = name
        self.last_w = None
        self.readers = {}


class Prog:
    ENG = ("pe", "act", "dve", "pool", "sp")

    def __init__(self, nc, stack, ndma=None):
        ndma = ndma or {"sp": 8, "act": 2, "pool": 6, "cc": 3}
        self.nc = nc
        self.q = {e: [] for e in self.ENG}
        self.sems = {}
        self.cnt = {e: 0 for e in self.ENG}
        self.waited = {e: {} for e in self.ENG}
        for e in self.ENG:
            self.sems[("c", e)] = stack.enter_context(nc.semaphore("c_" + e))
        self.dma_slots = {}
        self.dma_tot = {}
        self.dma_rr = {}
        for e, n in ndma.items():
            keys = []
            for i in range(n):
                k = ("d", e, i)
                self.sems[k] = stack.enter_context(nc.semaphore("d_%s%d" % (e, i)))
                self.dma_tot[k] = 0
                keys.append(k)
            self.dma_slots[e] = keys
            self.dma_rr[e] = 0

    def _wait(self, eng, k, v):
        if k == ("c", "pe") and eng == "pe":
            return
        if self.waited[eng].get(k, 0) >= v:
            return
        self.waited[eng][k] = v
        self.q[eng].append(("w", k, v))

    def _deps(self, eng, reads, writes):
        deps = {}

        def add(ev):
            if ev is None:
                return
            k, v = ev
            if deps.get(k, 0) < v:
                deps[k] = v

        for b in reads:
            add(b.last_w)
        for b in writes:
            add(b.last_w)
            for k, v in b.readers.items():
                add((k, v))
        for k, v in deps.items():
            self._wait(eng, k, v)

    def _commit(self, ev, reads, writes):
        k, v = ev
        for b in reads:
            if b.readers.get(k, 0) < v:
                b.readers[k] = v
        for b in writes:
            b.last_w = ev
            b.readers = {}

    def op(self, eng, fn, reads=(), writes=()):
        self._deps(eng, reads, writes)
        self.cnt[eng] += 1
        ev = (("c", eng), self.cnt[eng])
        self.q[eng].append(("o", fn, ("c", eng), 1))
        self._commit(ev, reads, writes)
        return ev

    def dma(self, qeng, fn, reads=(), writes=(), inc=16):
        skey = "cc" if inc == 1 else qeng
        slots = self.dma_slots[skey]
        k = slots[self.dma_rr[skey] % len(slots)]
        self.dma_rr[skey] += 1
        prev = self.dma_tot[k]
        if prev > 0:
            self._wait(qeng, k, prev)
        self._deps(qeng, reads, writes)
        self.dma_tot[k] = prev + inc
        ev = (k, prev + inc)
        self.q[qeng].append(("o", fn, k, inc))
        self._commit(ev, reads, writes)
        return ev

    def wait_all(self, eng, bufs):
        self._deps(eng, bufs, ())

    def barrier(self):
        tot = {("c", e): self.cnt[e] for e in self.ENG}
        tot.update(self.dma_tot)
        for e in self.ENG:
            for k, v in tot.items():
                if v > 0:
                    self._wait(e, k, v)

    def emit(self):
        nc = self.nc
        sems = self.sems

        def replay(e, name):
            for it in self.q[name]:
                if it[0] == "w":
                    e.wait_ge(sems[it[1]], it[2])
                else:
                    ins = it[1](e)
                    ins.then_inc(sems[it[2]], it[3])

        with nc.Block() as block:

            @block.tensor
            def _(e):
                replay(e, "pe")

            @block.vector
            def _(e):
                replay(e, "dve")

            @block.scalar
            def _(e):
                replay(e, "act")

            @block.gpsimd
            def _(e):
                replay(e, "pool")

            @block.sync
            def _(e):
                replay(e, "sp")


class TL:
    def __init__(self, t, nslots=1):
        self.t = t
        self.bs = [Buf() for _ in range(nslots)]

    @property
    def b(self):
        return self.bs[0]


def build_program(nlayers=2, dbg=None):
    dbg = dbg or {}
    NEI = dbg.get("_nexp", NEXP)
    nc = bass.Bass("TRN2", target_bir_lowering=False)
    names_in = []

    def inp(name, shape, d=F32):
        names_in.append(name)
        return TL(nc.dram_tensor(name, list(shape), d, kind="ExternalInput").ap())

    gathers = []

    def ginp(name, shape):
        return inp(name, shape)

    def ginp_unused(name, shape):
        R = int(np.prod(shape[:-1]))
        C = int(shape[-1])
        assert R % NCORES == 0
        names_in.append(name)
        shard = TL(nc.dram_tensor(name, [R // NCORES, C], F32, kind="ExternalInput").ap())
        bounce = TL(nc.dram_tensor(name + "_bnc", [R // NCORES, C], F32).ap())
        full = TL(nc.dram_tensor(name + "_full", list(shape), F32).ap())
        gathers.append((shard, bounce, full))
        return full

    x_own = inp("x_own", [TOWN, D])
    ctx_b = inp("ctx_b", [CTX, D])
    cT = inp("cT", [128, 8, 2])
    pos_d = inp("pos", [128, TALL])
    ropef_d = inp("ropef", [128, 2])
    masks_d = inp("masks", [128, 4, 512])
    selmat_d = inp("selmat", [128, 12, 128])
    ident_d = inp("ident", [128, 128])
    iota_d = inp("iota", [128, 512])
    gfin_d = inp("gfin", [128, D])
    L = []
    for l in range(nlayers):
        w = {}
        w["wmod"] = ginp(f"wmod{l}", [128, 8, 6 * D])
        w["bmodT"] = inp(f"bmodT{l}", [128, 48])
        w["g1T"] = inp(f"g1T{l}", [128, 8])
        w["g2T"] = inp(f"g2T{l}", [128, 8])
        w["w1"] = ginp(f"w1_{l}", [128, 8, 1920])
        w["wus"] = inp(f"wus{l}", [128, 8, 128])
        w["w2g"] = ginp(f"w2g{l}", [8, 128, 8, 256])
        w["wglu"] = inp(f"wglu{l}", [128, 4, 512])
        w["wbra"] = ginp(f"wbra{l}", [128, 4, D])
        w["wbrs"] = ginp(f"wbrs{l}", [128, 4, D])
        w["wout"] = ginp(f"wout{l}", [128, 8, D])
        w["wr"] = inp(f"wr{l}", [128, 8, 36])
        w["weg"] = ginp(f"weg{l}", [NEI, 128, 8, 512])
        w["weu"] = ginp(f"weu{l}", [NEI, 128, 8, 512])
        w["wed"] = ginp(f"wed{l}", [NEI, 128, 4, D])
        w["sink"] = inp(f"sink{l}", [128, 8])
        w["lamre"] = inp(f"lamre{l}", [128, 8])
        w["lamim"] = inp(f"lamim{l}", [128, 8])
        w["ldt"] = inp(f"ldt{l}", [128, 8])
        w["bre"] = inp(f"bre{l}", [128, 8, 16])
        w["bim"] = inp(f"bim{l}", [128, 8, 16])
        w["cre"] = inp(f"cre{l}", [128, 8, 16])
        w["cim"] = inp(f"cim{l}", [128, 8, 16])
        w["dsk"] = inp(f"dsk{l}", [128, 1])
        L.append(w)
    out_d = TL(nc.dram_tensor("out", [TOWN, D], F32, kind="ExternalOutput").ap())
    dbg_out = {}
    for k, shp in dbg.items():
        if k.startswith("_"):
            continue
        dbg_out[k] = TL(nc.dram_tensor("dbg_" + k, list(shp), F32, kind="ExternalOutput").ap())
    AGR = TOWN + 128
    ag1_in = [TL(nc.dram_tensor(f"ag1_in{i}", [r_, 512], BF16).ap()) for i, r_ in enumerate((1024, 1024, 128))]
    ag1_out = [TL(nc.dram_tensor(f"ag1_out{i}", [4 * r_, 512], BF16).ap()) for i, r_ in enumerate((1024, 1024, 128))]
    ag2_in = [TL(nc.dram_tensor(f"ag2_in{i}", [128, c_], BF16).ap()) for i, c_ in enumerate((4096, 4096, CTX))]
    ag2_out = [TL(nc.dram_tensor(f"ag2_out{i}", [512, c_], BF16).ap()) for i, c_ in enumerate((4096, 4096, CTX))]
    cos_d = TL(nc.dram_tensor("cos_d", [128, TALL], F32).ap())
    sin_d = TL(nc.dram_tensor("sin_d", [128, TALL], F32).ap())
    yTf_d = TL(nc.dram_tensor("yTf_d", [128, STREAM], F32).ap())
    attn_d = TL(nc.dram_tensor("attn_d", [128, 4, TALL], BF16).ap())

    with ExitStack() as st:
        P = Prog(nc, st)

        uid = [0]

        def sbuf(stack, name, shape, d=F32, nslots=1):
            uid[0] += 1
            return TL(stack.enter_context(nc.sbuf_tensor("sb%d_%s" % (uid[0], name), list(shape), d)), nslots)

        PS = [TL(st.enter_context(nc.psum_tensor(f"ps{i}", [128, 512], F32))) for i in range(8)]
        ps_rr = [0]

        def bank(exclude=()):
            while True:
                i = ps_rr[0] % 8
                ps_rr[0] += 1
                if i not in exclude:
                    return PS[i]

        def mm(out_tl, out_ap, lhsT_ap, rhs_ap, reads, start=True, stop=True):
            P.op("pe", lambda e: e.matmul(out_ap, lhsT_ap, rhs_ap, start=start, stop=stop), reads, [out_tl.b])

        def tr(out_tl, out_ap, in_ap, ident_ap, reads):
            P.op("pe", lambda e: e.transpose(out_ap, in_ap, ident_ap), reads, [out_tl.b])

        def act(out_ap, in_ap, func, reads, writes, bias=None, scale=None, accum=None):
            kw = {}
            if bias is not None:
                kw["bias"] = bias
            if scale is not None:
                kw["scale"] = scale
            if accum is not None:
                kw["accum_out"] = accum
            P.op("act", lambda e: e.activation(out_ap, in_ap, func, **kw), reads, writes)

        def tt(eng, out_ap, a_ap, b_ap, op, reads, writes):
            P.op(eng, lambda e: e.tensor_tensor(out_ap, a_ap, b_ap, op), reads, writes)

        def ts(eng, out_ap, a_ap, s1, s2, op0, op1, reads, writes):
            if op1 is None:
                P.op(eng, lambda e: e.tensor_scalar(out_ap, a_ap, s1, None, op0), reads, writes)
            else:
                P.op(eng, lambda e: e.tensor_scalar(out_ap, a_ap, s1, s2, op0, op1), reads, writes)

        def stt(eng, out_ap, a_ap, s, b_ap, op0, op1, reads, writes):
            P.op(eng, lambda e: e.scalar_tensor_tensor(out_ap, a_ap, s, b_ap, op0, op1), reads, writes)

        def cp(eng, out_ap, in_ap, reads, writes):
            P.op(eng, lambda e: e.tensor_copy(out_ap, in_ap), reads, writes)

        def dma(q, out_ap, in_ap, reads, writes, **kw):
            P.dma(q, lambda e: e.dma_start(out=out_ap, in_=in_ap, **kw), reads, writes)

        def dump(key, src_tl, src_ap, dst_ap_fn):
            if key in dbg_out:
                dma("sp", dst_ap_fn(dbg_out[key].t), src_ap, [src_tl.b] if isinstance(src_tl, TL) else src_tl,
                    [dbg_out[key].b])

        for shard, bounce, full in gathers:
            nr_ = shard.t.shape[0]
            for r0_ in range(0, nr_, 1024):
                r1_ = min(nr_, r0_ + 1024)
                dma("sp", bounce.t[r0_:r1_, :], shard.t[r0_:r1_, :], [], [bounce.b])
            P.dma("pool", lambda e, bounce=bounce, full=full: e.collective_compute(
                "AllGather", ALU.bypass, replica_groups=[list(range(NCORES))], ins=[bounce.t.opt()], outs=[full.t.opt()]),
                [bounce.b], [full.b], inc=1)

        x_tm = sbuf(st, "x_tm", [128, NT, D], F32, NT)
        ident = sbuf(st, "ident", [128, 128])
        zeros = sbuf(st, "zeros", [128, 512])
        iota = sbuf(st, "iota", [128, 512])
        onesb = sbuf(st, "onesb", [128, 64], BF16)
        onesf = sbuf(st, "onesf", [128, 128])
        siluT = sbuf(st, "siluT", [128, 8, 2])
        modT = sbuf(st, "modT", [128, 48, 2])
        bmodT = sbuf(st, "bmodT", [128, 48])
        gT = sbuf(st, "gT", [128, 16])
        A1 = sbuf(st, "A1", [128, 2, 8])
        A2 = sbuf(st, "A2", [128, 2, 8])
        rstd = sbuf(st, "rstd", [128, NT])
        sstat = sbuf(st, "sstat", [128, NT])
        xs = sbuf(st, "xs", [128, D])
        sm = sbuf(st, "sm", [128, 8])
        ucT = sbuf(st, "ucT", [128, CTX], BF16)
        dg = sbuf(st, "dg", [128, 128])

        def AP_rev(tl, row_elems, col_last, n):
            return bass.AP(tl.t, col_last, [[row_elems, 128], [-1, n]])

        def range_reduce(kf_tl, kf_ap, ki_tl, ki_ap, out_ap, in_ap, shift, reads, writes):
            ts("dve", kf_ap, in_ap, shift, 1.0 / (2 * PI), ALU.add, ALU.mult, reads, [kf_tl.b])
            cp("dve", ki_ap, kf_ap, [kf_tl.b], [ki_tl.b])
            cp("dve", kf_ap, ki_ap, [ki_tl.b], [kf_tl.b])
            stt("dve", kf_ap, kf_ap, -2 * PI, in_ap, ALU.mult, ALU.add, [kf_tl.b] + list(reads), [kf_tl.b])
            ts("dve", out_ap, kf_ap, shift, None, ALU.add, None, [kf_tl.b], writes)
            ts("dve", out_ap, out_ap, 3.14159, -3.14159, ALU.min, ALU.max, writes, writes)

        with ExitStack() as ph:
            posT = sbuf(ph, "posT", [128, TALL])
            ang = sbuf(ph, "ang", [128, TALL])
            ki = sbuf(ph, "ki", [128, TALL], I32)
            kf = sbuf(ph, "kf", [128, TALL])
            tb = sbuf(ph, "tb", [128, TALL])
            ropef = sbuf(ph, "ropef", [128, 2])
            dma("sp", x_tm.t[:, 0:NT_OWN, :], x_own.t.rearrange("(t p) d -> p t d", p=128), [], x_tm.bs[0:NT_OWN])
            dma("sp", x_tm.t[:, NT_OWN:NT, :], ctx_b.t.rearrange("(t p) d -> p t d", p=128), [], x_tm.bs[NT_OWN:NT])
            dma("sp", ident.t[:], ident_d.t[:, :], [], [ident.b])
            dma("sp", posT.t[:], pos_d.t[:, :], [], [posT.b])
            dma("sp", ropef.t[:], ropef_d.t[:, :], [], [ropef.b])
            dma("sp", iota.t[:], iota_d.t[:, :], [], [iota.b])
            dma("sp", siluT.t[:], cT.t[:, :, :], [], [siluT.b])
            P.op("pool", lambda e: e.memset(zeros.t[:], 0.0), [], [zeros.b])
            P.op("pool", lambda e: e.memset(onesb.t[:], 1.0), [], [onesb.b])
            P.op("pool", lambda e: e.memset(onesf.t[:], 1.0), [], [onesf.b])
            act(siluT.t[:], siluT.t[:], AF.Silu, [siluT.b], [siluT.b])
            act(sm.t[:, 0:1], ropef.t[:, 0:1], AF.Exp, [ropef.b], [sm.b], scale=-float(np.log(10000.0)) / 16.0)
            ts("dve", ang.t[:], posT.t[:], sm.t[:, 0:1], None, ALU.mult, None, [posT.b, sm.b], [ang.b])
            range_reduce(kf, kf.t[:], ki, ki.t[:], tb.t[:], ang.t[:], 0.0, [ang.b], [tb.b])
            act(tb.t[:], tb.t[:], AF.Sin, [tb.b], [tb.b])
            ts("dve", tb.t[:], tb.t[:], ropef.t[:, 1:2], None, ALU.mult, None, [tb.b, ropef.b], [tb.b])
            dma("sp", sin_d.t[:, :], tb.t[:], [tb.b], [sin_d.b])
            range_reduce(kf, kf.t[:], ki, ki.t[:], tb.t[:], ang.t[:], PI / 2, [ang.b], [tb.b])
            act(tb.t[:], tb.t[:], AF.Sin, [tb.b], [tb.b])
            dma("sp", cos_d.t[:, :], tb.t[:], [tb.b], [cos_d.b])
            P.barrier()

        def tile_cls(t_):
            return 0 if t_ < NT_OWN else 1

        def compute_rstd(ntiles):
            for t_ in range(ntiles):
                act(xs.t[:], x_tm.t[:, t_, :], AF.Square, [x_tm.bs[t_]], [xs.b, sstat.b], accum=sstat.t[:, t_:t_ + 1])
            ts("dve", rstd.t[:, 0:ntiles], sstat.t[:, 0:ntiles], 1.0 / D, 1e-6, ALU.mult, ALU.add, [sstat.b], [rstd.b])
            act(rstd.t[:, 0:ntiles], rstd.t[:, 0:ntiles], AF.Sqrt, [rstd.b], [rstd.b])
            P.op("dve", lambda e: e.reciprocal(rstd.t[:, 0:ntiles], rstd.t[:, 0:ntiles]), [rstd.b], [rstd.b])

        def norm_tile(t_, Acls, shslot, dst_ap_fn, dst_bufs, f32_dst=None):
            cls = tile_cls(t_)
            act(xs.t[:], x_tm.t[:, t_, :], AF.Identity, [x_tm.bs[t_], rstd.b], [xs.b], scale=rstd.t[:, t_:t_ + 1])
            for half in range(2):
                pb_ = bank()
                for q4 in range(4):
                    fc = half * 4 + q4
                    tr(pb_, pb_.t[:, q4 * 128:(q4 + 1) * 128], xs.t[:, fc * 128:(fc + 1) * 128], ident.t[:], [xs.b, ident.b])
                for q4 in range(4):
                    fc = half * 4 + q4
                    act(dst_ap_fn(fc), pb_.t[:, q4 * 128:(q4 + 1) * 128], AF.Identity, [pb_.b, Acls.b, modT.b], dst_bufs,
                        scale=Acls.t[:, cls, fc:fc + 1], bias=modT.t[:, shslot * 8 + fc, cls:cls + 1])
                    if f32_dst is not None:
                        act(f32_dst.t[:, fc, :], pb_.t[:, q4 * 128:(q4 + 1) * 128], AF.Identity, [pb_.b, Acls.b, modT.b],
                            [f32_dst.b], scale=Acls.t[:, cls, fc:fc + 1], bias=modT.t[:, shslot * 8 + fc, cls:cls + 1])

        def make_gtrow(dst, slot):
            for cls in range(2):
                for fc in range(8):
                    ts("dve", dg.t[:], ident.t[:], modT.t[:, slot * 8 + fc, cls:cls + 1], None, ALU.mult, None,
                       [ident.b, modT.b], [dg.b])
                    pb_ = bank()
                    mm(pb_, pb_.t[:, 0:128], onesf.t[:], dg.t[:], [onesf.b, dg.b])
                    cp("dve", dst.t[:, cls, fc * 128:(fc + 1) * 128], pb_.t[:, 0:128], [pb_.b], [dst.b])

        def blk_of(t_):
            return t_ + 1 if t_ < NT_OWN else 18 + (t_ - NT_OWN)

        DL = dbg.get("_layer", 0)
        stopped = False
        for l in range(nlayers):
            if dbg.get("_stop") == "0":
                break
            W = L[l]
            ctx_out = l < nlayers - 1
            with ExitStack() as ph:
                wm = sbuf(ph, "wm", [128, 2, 8, 512], F32, 2)
                dma("sp", bmodT.t[:], W["bmodT"].t[:, :], [], [bmodT.b])
                dma("sp", gT.t[:, 0:8], W["g1T"].t[:, :], [], [gT.b])
                dma("sp", gT.t[:, 8:16], W["g2T"].t[:, :], [], [gT.b])
                mps = bank()
                for piece in range(12):
                    s = piece % 2
                    dma("sp", wm.t[:, s, :, :], W["wmod"].t[:, :, piece * 512:(piece + 1) * 512], [W["wmod"].b], [wm.bs[s]])
                    for c4 in range(4):
                        cc = piece * 4 + c4
                        for kc in range(8):
                            mm(mps, mps.t[:, cc * 2:cc * 2 + 2], wm.t[:, s, kc, c4 * 128:(c4 + 1) * 128],
                               siluT.t[:, kc, :], [wm.bs[s], siluT.b], kc == 0, kc == 7)
                for cls in range(2):
                    tt("dve", modT.t[:, :, cls], mps.t[:, cls:96:2], bmodT.t[:], ALU.add, [mps.b, bmodT.b], [modT.b])
                for cls in range(2):
                    stt("dve", A1.t[:, cls, :], modT.t[:, 8:16, cls], 1.0, gT.t[:, 0:8], ALU.add, ALU.mult,
                        [modT.b, gT.b], [A1.b])
                    stt("dve", A2.t[:, cls, :], modT.t[:, 32:40, cls], 1.0, gT.t[:, 8:16], ALU.add, ALU.mult,
                        [modT.b, gT.b], [A2.b])
                if l == DL:
                    dump("modT", modT, modT.t[:], lambda o: o[:, :, :])
                P.barrier()

            kvst = ExitStack()
            kT = sbuf(kvst, "kT", [128, 20, 128], BF16, 20)
            vv = sbuf(kvst, "vv", [128, 20, 128], BF16, 20)
            with ExitStack() as ph:
                w1 = sbuf(ph, "w1", [128, 8, 1920], BF16)
                wus = sbuf(ph, "wus", [128, 8, 128], BF16)
                hT = sbuf(ph, "hT", [128, 8, 512], BF16)
                ust = sbuf(ph, "ust", [128, 2, 512], BF16, 2)
                tmpa = sbuf(ph, "tmpa", [128, 512])
                tmpb = sbuf(ph, "tmpb", [128, 512])
                cosT = sbuf(ph, "cosT", [128, TALL])
                sinT = sbuf(ph, "sinT", [128, TALL])
                dma("sp", cosT.t[:], cos_d.t[:, :], [cos_d.b], [cosT.b])
                dma("sp", sinT.t[:], sin_d.t[:, :], [sin_d.b], [sinT.b])
                for kc in range(8):
                    dma("pool", w1.t[:, kc, :], W["w1"].t[:, kc, :], [W["w1"].b], [w1.b])
                dma("pool", wus.t[:], W["wus"].t[:, :, :], [], [wus.b])
                compute_rstd(NT)
                ust_rr = 0
                for ch in range(5):
                    tiles = list(range(ch * 4, min(ch * 4 + 4, NT)))
                    n = len(tiles) * 128
                    c0 = ch * 512
                    for ti, t_ in enumerate(tiles):
                        norm_tile(t_, A1, 0, lambda fc, ti=ti: hT.t[:, fc, ti * 128:(ti + 1) * 128], [hT.b])
                    pk = bank()
                    pkr = bank()
                    for kc in range(8):
                        mm(pk, pk.t[:, 0:n], w1.t[:, kc, 1024:1152], hT.t[:, kc, 0:n], [w1.b, hT.b], kc == 0, kc == 7)
                    for kc in range(8):
                        mm(pkr, pkr.t[:, 0:n], w1.t[:, kc, 1152:1280], hT.t[:, kc, 0:n], [w1.b, hT.b], kc == 0, kc == 7)
                    tt("dve", tmpa.t[:, 0:n], pk.t[:, 0:n], cosT.t[:, c0:c0 + n], ALU.mult, [pk.b, cosT.b], [tmpa.b])
                    tt("dve", tmpb.t[:, 0:n], pkr.t[:, 0:n], sinT.t[:, c0:c0 + n], ALU.mult, [pkr.b, sinT.b], [tmpb.b])
                    for ti, t_ in enumerate(tiles):
                        blk = blk_of(t_)
                        tt("pool", kT.t[:, blk, :], tmpa.t[:, ti * 128:(ti + 1) * 128], tmpb.t[:, ti * 128:(ti + 1) * 128],
                           ALU.add, [tmpa.b, tmpb.b], [kT.bs[blk]])
                    pv = bank()
                    for ti, t_ in enumerate(tiles):
                        for kc in range(8):
                            mm(pv, pv.t[:, ti * 128:(ti + 1) * 128], hT.t[:, kc, ti * 128:(ti + 1) * 128],
                               w1.t[:, kc, 1280:1408], [w1.b, hT.b], kc == 0, kc == 7)
                    for ti, t_ in enumerate(tiles):
                        blk = blk_of(t_)
                        act(vv.t[:, blk, :], pv.t[:, ti * 128:(ti + 1) * 128], AF.Identity, [pv.b], [vv.bs[blk]])
                    if ch < 4:
                        for ti, t_ in enumerate(tiles):
                            pu = bank()
                            for kc in range(8):
                                mm(pu, pu.t[:, :], hT.t[:, kc, ti * 128:(ti + 1) * 128], w1.t[:, kc, 1408:1920],
                                   [w1.b, hT.b], kc == 0, kc == 7)
                            s = ust_rr % 2
                            ust_rr += 1
                            act(ust.t[:, s, :], pu.t[:, :], AF.Identity, [pu.b], [ust.bs[s]])
                            pc_, lr_ = (t_ * 128) // 1024, (t_ * 128) % 1024
                            dma("sp", ag1_in[pc_].t[lr_:lr_ + 128, :], ust.t[:, s, :], [ust.bs[s]], [ag1_in[pc_].b])
                    else:
                        pu = bank()
                        for kc in range(8):
                            mm(pu, pu.t[:, 0:CTX], wus.t[:, kc, :], hT.t[:, kc, 0:CTX], [wus.b, hT.b], kc == 0, kc == 7)
                        act(ucT.t[:], pu.t[:, 0:CTX], AF.Identity, [pu.b], [ucT.b])
                for i4, (tl_, blk) in enumerate(((kT, 1), (kT, 16), (vv, 1), (vv, 16))):
                    dma("sp", ag1_in[2].t[:, i4 * 128:(i4 + 1) * 128], tl_.t[:, blk, :], [tl_.bs[blk]], [ag1_in[2].b])
                for pc_ in range(3):
                    P.dma("pool", lambda e, pc_=pc_: e.collective_compute(
                        "AllGather", ALU.bypass, replica_groups=GROUPS, ins=[ag1_in[pc_].t.opt()], outs=[ag1_out[pc_].t.opt()]),
                        [ag1_in[pc_].b], [ag1_out[pc_].b], inc=1)
                P.barrier()
            if dbg.get("_stop") == "B" and l == DL:
                stopped = True

            if not stopped:
              with ExitStack() as ph:
                uTf = sbuf(ph, "uTf", [128, STREAM], BF16, 17)
                hal = sbuf(ph, "hal", [128, 4, 512], BF16)
                utile = sbuf(ph, "utile", [128, 2, 512], BF16, 2)
                selm = sbuf(ph, "selm", [128, 12, 128], BF16)
                prm = sbuf(ph, "prm", [128, 16, 8])
                bb = sbuf(ph, "bb", [128, 4, 8, 16])
                cc_ = sbuf(ph, "cc", [128, 2, 8, 16])
                Wsc = sbuf(ph, "Wsc", [128, 128])
                lB = sbuf(ph, "lB", [128, 16, 128], BF16)
                lC = sbuf(ph, "lC", [128, 16, 128], BF16)
                dsk = sbuf(ph, "dsk", [128, 1])
                tabc = sbuf(ph, "tabc", [128, 4, 512], F32, 4)
                tabs = sbuf(ph, "tabs", [128, 4, 512], F32, 4)
                rhoT = sbuf(ph, "rhoT", [128, 4, 512], F32, 4)
                kis = sbuf(ph, "kis", [128, 512], I32)
                NW = 2
                wk = [[sbuf(ph, f"wk{s}_{i}", [128, 512]) for i in range(6)] for s in range(NW)]
                xrb = [[sbuf(ph, f"xb{s}_{i}", [128, 512], BF16) for i in range(2)] for s in range(NW)]
                kfs, angs = wk[0][0], wk[0][1]
                ub = sbuf(ph, "ub", [128, 2, 512], BF16, 2)
                init = sbuf(ph, "init", [128, 4, 2], F32, 4)
                ytmp = sbuf(ph, "ytmp", [128, 3, 512], F32, 3)
                yfs = sbuf(ph, "yfs", [128, 2, 512], F32, 2)
                zst = sbuf(ph, "zst", [128, 2, 512], BF16, 2)
                dma("pool", selm.t[:], selmat_d.t[:, :, :], [], [selm.b])
                for i in range(4):
                    dma("sp", hal.t[:, i, :], ag1_out[2].t[i * 128:(i + 1) * 128, :], [ag1_out[2].b], [hal.b])
                ph_ = bank()
                for i4, (selbase, col) in enumerate(((4, 128), (8, 0), (4, 384), (8, 256))):
                    for i in range(4):
                        mm(ph_, ph_.t[:, i4 * 128:(i4 + 1) * 128], selm.t[:, selbase + i, :], hal.t[:, i, col:col + 128],
                           [selm.b, hal.b], i == 0, i == 3)
                for i4, (tl_, blk) in enumerate(((kT, 0), (kT, 17), (vv, 0), (vv, 17))):
                    cp("dve", tl_.t[:, blk, :], ph_.t[:, i4 * 128:(i4 + 1) * 128], [ph_.b], [tl_.bs[blk]])
                cp("dve", uTf.t[:, 0:CTX], ucT.t[:], [ucT.b], [uTf.bs[0]])
                ut_rr = 0
                for i in range(4):
                    for tq in range(4):
                        pu = bank()
                        for t4 in range(4):
                            s = ut_rr % 2
                            ut_rr += 1
                            tr_ = (tq * 4 + t4) * 128
                            pc_, r0 = tr_ // 1024, i * 1024 + tr_ % 1024
                            dma("sp", utile.t[:, s, :], ag1_out[pc_].t[r0:r0 + 128, :], [ag1_out[pc_].b], [utile.bs[s]])
                            for sl in range(4):
                                mm(pu, pu.t[:, t4 * 128:(t4 + 1) * 128], utile.t[:, s, sl * 128:(sl + 1) * 128],
                                   selm.t[:, sl, :], [utile.bs[s], selm.b], sl == 0, sl == 3)
                        nk = i * 4 + tq
                        act(uTf.t[:, CTX + nk * 512:CTX + (nk + 1) * 512], pu.t[:, :], AF.Identity, [pu.b], [uTf.bs[1 + nk]])
                for nm, c_ in (("lamre", 0), ("lamim", 1), ("ldt", 2)):
                    dma("sp", prm.t[:, c_, :], W[nm].t[:, :], [], [prm.b])
                dma("sp", bb.t[:, 0, :, :], W["bre"].t[:, :, :], [], [bb.b])
                dma("sp", bb.t[:, 1, :, :], W["bim"].t[:, :, :], [], [bb.b])
                dma("sp", cc_.t[:, 0, :, :], W["cre"].t[:, :, :], [], [cc_.b])
                dma("sp", cc_.t[:, 1, :, :], W["cim"].t[:, :, :], [], [cc_.b])
                dma("sp", dsk.t[:], W["dsk"].t[:, :], [], [dsk.b])
                pr = lambda c_: prm.t[:, c_, :]
                PB = [prm.b]
                act(pr(3), pr(2), AF.Exp, PB, PB)
                tt("dve", pr(4), pr(0), pr(3), ALU.mult, PB, PB)
                tt("dve", pr(5), pr(1), pr(3), ALU.mult, PB, PB)
                act(pr(6), pr(4), AF.Exp, PB, PB)
                range_reduce(kfs, kfs.t[:, 0:8], kis, kis.t[:, 0:8], angs.t[:, 0:8], pr(5), 0.0, PB, [angs.b])
                act(pr(7), angs.t[:, 0:8], AF.Sin, [angs.b], PB)
                range_reduce(kfs, kfs.t[:, 0:8], kis, kis.t[:, 0:8], angs.t[:, 0:8], pr(5), PI / 2, PB, [angs.b])
                act(pr(8), angs.t[:, 0:8], AF.Sin, [angs.b], PB)
                tt("dve", pr(9), pr(6), pr(8), ALU.mult, PB, PB)
                tt("dve", pr(10), pr(6), pr(7), ALU.mult, PB, PB)
                tt("dve", pr(11), pr(0), pr(0), ALU.mult, PB, PB)
                tt("dve", pr(15), pr(1), pr(1), ALU.mult, PB, PB)
                tt("dve", pr(11), pr(11), pr(15), ALU.add, PB, PB)
                P.op("dve", lambda e: e.reciprocal(pr(11), pr(11)), PB, PB)
                ts("dve", pr(12), pr(9), -1.0, None, ALU.add, None, PB, PB)
                tt("dve", pr(13), pr(12), pr(0), ALU.mult, PB, PB)
                tt("dve", pr(15), pr(10), pr(1), ALU.mult, PB, PB)
                tt("dve", pr(13), pr(13), pr(15), ALU.add, PB, PB)
                tt("dve", pr(13), pr(13), pr(11), ALU.mult, PB, PB)
                tt("dve", pr(14), pr(10), pr(0), ALU.mult, PB, PB)
                tt("dve", pr(15), pr(12), pr(1), ALU.mult, PB, PB)
                tt("dve", pr(14), pr(14), pr(15), ALU.subtract, PB, PB)
                tt("dve", pr(14), pr(14), pr(11), ALU.mult, PB, PB)
                if l == DL:
                    dump("prm", prm, prm.t[:], lambda o: o[:, :, :])
                for r in range(8):
                    fre = prm.t[:, 13, r:r + 1]
                    fim = prm.t[:, 14, r:r + 1]
                    ts("dve", bb.t[:, 2, r, :], bb.t[:, 0, r, :], fre, None, ALU.mult, None, [bb.b, prm.b], [bb.b])
                    ts("dve", bb.t[:, 3, r, :], bb.t[:, 1, r, :], fim, None, ALU.mult, None, [bb.b, prm.b], [bb.b])
                    tt("dve", bb.t[:, 2, r, :], bb.t[:, 2, r, :], bb.t[:, 3, r, :], ALU.subtract, [bb.b], [bb.b])
                    ts("dve", bb.t[:, 3, r, :], bb.t[:, 1, r, :], fre, None, ALU.mult, None, [bb.b, prm.b], [bb.b])
                    stt("dve", bb.t[:, 3, r, :], bb.t[:, 0, r, :], fim, bb.t[:, 3, r, :], ALU.mult, ALU.add,
                        [bb.b, prm.b], [bb.b])
                    gp = r % 4
                    for ri in range(2):
                        P.op("pool", lambda e: e.memset(Wsc.t[:], 0.0), [], [Wsc.b])
                        for g2 in range(2):
                            c0_ = 16 * (2 * gp + g2)
                            cp("dve", Wsc.t[g2 * 64:(g2 + 1) * 64, c0_:c0_ + 16], bb.t[g2 * 64:(g2 + 1) * 64, 2 + ri, r, :],
                               [bb.b], [Wsc.b])
                        pb_ = bank()
                        tr(pb_, pb_.t[:, 0:128], Wsc.t[:], ident.t[:], [Wsc.b, ident.b])
                        act(lB.t[:, r * 2 + ri, :], pb_.t[:, 0:128], AF.Identity, [pb_.b], [lB.b])
                        P.op("pool", lambda e, r=r, ri=ri: e.memset(lC.t[:, r * 2 + ri, :], 0.0), [], [lC.b])
                        for g2 in range(2):
                            c0_ = 16 * (2 * gp + g2)
                            ts("dve", lC.t[g2 * 64:(g2 + 1) * 64, r * 2 + ri, c0_:c0_ + 16],
                               cc_.t[g2 * 64:(g2 + 1) * 64, ri, r, :], (1.0 if ri == 0 else -1.0), None, ALU.mult, None,
                               [cc_.b], [lC.b])
                chunks = [(0, CTX)] + [(CTX + 512 * k, 512) for k in range(16)]
                wk_rr = 0
                for d_ in range(2):
                    for gp in range(4):
                        r = d_ * 4 + gp
                        ts("dve", angs.t[:], iota.t[:], prm.t[:, 5, r:r + 1], None, ALU.mult, None, [iota.b, prm.b], [angs.b])
                        range_reduce(kfs, kfs.t[:], kis, kis.t[:], tabs.t[:, gp, :], angs.t[:], 0.0, [angs.b], [tabs.bs[gp]])
                        act(tabs.t[:, gp, :], tabs.t[:, gp, :], AF.Sin, [tabs.bs[gp]], [tabs.bs[gp]])
                        range_reduce(kfs, kfs.t[:], kis, kis.t[:], tabc.t[:, gp, :], angs.t[:], PI / 2, [angs.b], [tabc.bs[gp]])
                        act(tabc.t[:, gp, :], tabc.t[:, gp, :], AF.Sin, [tabc.bs[gp]], [tabc.bs[gp]])
                        ts("dve", rhoT.t[:, gp, :], zeros.t[:], prm.t[:, 6, r:r + 1], None, ALU.add, None, [zeros.b, prm.b],
                           [rhoT.bs[gp]])
                        P.op("pool", lambda e, gp=gp: e.memset(init.t[:, gp, :], 0.0), [], [init.bs[gp]])
                    for ci, (c0, n) in enumerate(chunks):
                        if d_ == 0:
                            nat_c0, slot = c0, ci
                            u_ap = uTf.t[:, c0:c0 + n]
                            u_reads = [uTf.bs[ci]]
                        else:
                            if ci == 0:
                                nat_c0, slot = 0, 0
                            else:
                                nkk = 16 - ci
                                nat_c0, slot = CTX + 512 * nkk, 1 + nkk
                            s_u = ci % 2
                            cp("dve", ub.t[:, s_u, 0:n], AP_rev(uTf, STREAM, nat_c0 + n - 1, n), [uTf.bs[slot]], [ub.bs[s_u]])
                            u_ap = ub.t[:, s_u, 0:n]
                            u_reads = [ub.bs[s_u]]
                        need = ctx_out or ci > 0
                        yps = bank()
                        excl = (PS.index(yps),)
                        for gp in range(4):
                            r = d_ * 4 + gp
                            ws = wk[wk_rr % NW]
                            xb_ = xrb[wk_rr % NW]
                            wk_rr += 1
                            pre = bank(exclude=excl)
                            pim = bank(exclude=excl)
                            mm(pre, pre.t[:, 0:n], lB.t[:, r * 2, :], u_ap, [lB.b] + u_reads)
                            mm(pim, pim.t[:, 0:n], lB.t[:, r * 2 + 1, :], u_ap, [lB.b] + u_reads)
                            c_ap = tabc.t[:, gp, 0:n]
                            s_ap = tabs.t[:, gp, 0:n]
                            TB = [tabc.bs[gp], tabs.bs[gp]]
                            t1, t2, t3, t4, zr, zi = [w_.t[:, 0:n] for w_ in ws]
                            b1, b2, b3, b4, bzr, bzi = [w_.b for w_ in ws]
                            tt("dve", t1, pre.t[:, 0:n], c_ap, ALU.mult, [pre.b] + TB, [b1])
                            tt("dve", t2, pim.t[:, 0:n], s_ap, ALU.mult, [pim.b] + TB, [b2])
                            tt("pool", t1, t1, t2, ALU.add, [b1, b2], [b1])
                            tt("dve", t3, pim.t[:, 0:n], c_ap, ALU.mult, [pim.b] + TB, [b3])
                            tt("dve", t4, pre.t[:, 0:n], s_ap, ALU.mult, [pre.b] + TB, [b4])
                            tt("pool", t3, t3, t4, ALU.subtract, [b3, b4], [b3])
                            P.op("dve", lambda e, zr=zr, t1=t1, gp=gp, n=n: e.tensor_tensor_scan(
                                zr, rhoT.t[:, gp, 0:n], t1, init.t[:, gp, 0:1], ALU.mult, ALU.add),
                                [rhoT.bs[gp], b1, init.bs[gp]], [bzr])
                            P.op("dve", lambda e, zi=zi, t3=t3, gp=gp, n=n: e.tensor_tensor_scan(
                                zi, rhoT.t[:, gp, 0:n], t3, init.t[:, gp, 1:2], ALU.mult, ALU.add),
                                [rhoT.bs[gp], b3, init.bs[gp]], [bzi])
                            tt("dve", t1, zr, c_ap, ALU.mult, [bzr] + TB, [b1])
                            tt("dve", t2, zi, s_ap, ALU.mult, [bzi] + TB, [b2])
                            tt("dve", t3, zr, s_ap, ALU.mult, [bzr] + TB, [b3])
                            tt("dve", t4, zi, c_ap, ALU.mult, [bzi] + TB, [b4])
                            tt("pool", xb_[0].t[:, 0:n], t1, t2, ALU.subtract, [b1, b2], [xb_[0].b])
                            tt("dve", xb_[1].t[:, 0:n], t3, t4, ALU.add, [b3, b4], [xb_[1].b])
                            tt("dve", init.t[:, gp, 0:1], ws[0].t[:, n - 1:n], ws[1].t[:, n - 1:n], ALU.subtract, [b1, b2],
                               [init.bs[gp]])
                            tt("dve", init.t[:, gp, 1:2], ws[2].t[:, n - 1:n], ws[3].t[:, n - 1:n], ALU.add, [b3, b4],
                               [init.bs[gp]])
                            if need:
                                mm(yps, yps.t[:, 0:n], lC.t[:, r * 2, :], xb_[0].t[:, 0:n], [lC.b, xb_[0].b], gp == 0, False)
                                mm(yps, yps.t[:, 0:n], lC.t[:, r * 2 + 1, :], xb_[1].t[:, 0:n], [lC.b, xb_[1].b], False, gp == 3)
                        if not need:
                            continue
                        if d_ == 0:
                            fs_ = ci % 2
                            act(yfs.t[:, fs_, 0:n], yps.t[:, 0:n], AF.Identity, [yps.b], [yfs.bs[fs_]])
                            dma("sp", yTf_d.t[:, c0:c0 + n], yfs.t[:, fs_, 0:n], [yfs.bs[fs_]], [yTf_d.b])
                        else:
                            ys = ci % 3
                            zs = ci % 2
                            ya = ytmp.t[:, ys, 0:n]
                            YB = [ytmp.bs[ys]]
                            yb2 = ytmp.t[:, (ys + 1) % 3, 0:n]
                            YB2 = [ytmp.bs[(ys + 1) % 3]]
                            fs_ = ci % 2
                            dma("sp", yfs.t[:, fs_, 0:n], yTf_d.t[:, nat_c0:nat_c0 + n], [yTf_d.b], [yfs.bs[fs_]])
                            act(ya, yps.t[:, 0:n], AF.Identity, [yps.b], YB)
                            tt("dve", yb2, AP_rev(ytmp, 3 * 512, ys * 512 + n - 1, n), yfs.t[:, fs_, 0:n], ALU.add,
                               YB + [yfs.bs[fs_]], YB2)
                            stt("dve", yb2, uTf.t[:, nat_c0:nat_c0 + n], dsk.t[:, 0:1], yb2, ALU.mult, ALU.add,
                                [uTf.bs[slot], dsk.b] + YB2, YB2)
                            if l == DL and ci in (0, 16):
                                cc0 = 0 if ci == 0 else 256
                                dump("ssmy", YB2, yb2, lambda o, cc0=cc0, n=n: o[:, cc0:cc0 + n])
                            act(ya, yb2, AF.Square, YB2, YB)
                            ts("dve", ya, ya, 0.044715, 1.0, ALU.mult, ALU.add, YB, YB)
                            tt("pool", ya, ya, yb2, ALU.mult, YB + YB2, YB)
                            act(ya, ya, AF.Sigmoid, YB, YB, scale=1.5957691216057308)
                            tt("pool", zst.t[:, zs, 0:n], ya, yb2, ALU.mult, YB + YB2, [zst.bs[zs]])
                            if ci == 0:
                                pc_, dcol = 2, 0
                            else:
                                pc_, dcol = (nat_c0 - CTX) // 4096, (nat_c0 - CTX) % 4096
                            dma("sp", ag2_in[pc_].t[:, dcol:dcol + n], zst.t[:, zs, 0:n], [zst.bs[zs]], [ag2_in[pc_].b])
                for pc_ in range(3 if ctx_out else 2):
                    P.dma("pool", lambda e, pc_=pc_: e.collective_compute(
                        "AllGather", ALU.bypass, replica_groups=GROUPS, ins=[ag2_in[pc_].t.opt()], outs=[ag2_out[pc_].t.opt()]),
                        [ag2_in[pc_].b], [ag2_out[pc_].b], inc=1)
                P.barrier()
            if dbg.get("_stop") == "C" and l == DL:
                stopped = True

            if not stopped:
              with ExitStack() as ph:
                wq = sbuf(ph, "wq", [128, 8, 1024], BF16)
                hT = sbuf(ph, "hTd", [128, 8, 512], BF16)
                qT = sbuf(ph, "qT", [128, 4, 512], BF16)
                pt = sbuf(ph, "pt", [128, 3, 512], BF16, 3)
                sg = sbuf(ph, "sg", [128, 2, 512], F32, 2)
                ta = sbuf(ph, "ta", [128, 2, 512], F32, 2)
                rec = sbuf(ph, "rec", [128, 512])
                attnT = sbuf(ph, "attnT", [128, 2, 4, 512], BF16, 2)
                cosT = sbuf(ph, "cosT", [128, TALL])
                sinT = sbuf(ph, "sinT", [128, TALL])
                masks = sbuf(ph, "masks", [128, 4, 512], BF16)
                sinkE = sbuf(ph, "sinkE", [128, 8])
                sinkrow = sbuf(ph, "sinkrow", [128, 512])
                dma("sp", cosT.t[:], cos_d.t[:, :], [cos_d.b], [cosT.b])
                dma("sp", sinT.t[:], sin_d.t[:, :], [sin_d.b], [sinT.b])
                for k4 in range(4):
                    dma("pool", masks.t[:, k4, :], masks_d.t[:, k4, :], [], [masks.b])
                for kc in range(8):
                    dma("pool", wq.t[:, kc, :], W["w1"].t[:, kc, 0:1024], [W["w1"].b], [wq.b])
                dma("sp", sinkE.t[:], W["sink"].t[:, :], [], [sinkE.b])
                act(sinkE.t[:], sinkE.t[:], AF.Exp, [sinkE.b], [sinkE.b])
                for c in range(4):
                    ts("dve", sinkrow.t[0:64, c * 128:(c + 1) * 128], zeros.t[0:64, 0:128], sinkE.t[0:64, c:c + 1], None,
                       ALU.add, None, [zeros.b, sinkE.b], [sinkrow.b])
                    ts("dve", sinkrow.t[64:128, c * 128:(c + 1) * 128], zeros.t[64:128, 0:128], sinkE.t[64:128, 4 + c:5 + c],
                       None, ALU.add, None, [zeros.b, sinkE.b], [sinkrow.b])
                nchunks = 5 if ctx_out else 4
                pt_rr = 0
                sg_rr = 0
                for ch in range(nchunks):
                    tiles = list(range(ch * 4, min(ch * 4 + 4, NT)))
                    n = len(tiles) * 128
                    c0 = ch * 512
                    is_ctx = ch == 4
                    for ti, t_ in enumerate(tiles):
                        norm_tile(t_, A1, 0, lambda fc, ti=ti: hT.t[:, fc, ti * 128:(ti + 1) * 128], [hT.b])
                    for c in range(4):
                        pq = bank()
                        pqr = bank()
                        for kc in range(8):
                            mm(pq, pq.t[:, 0:n], wq.t[:, kc, c * 128:(c + 1) * 128], hT.t[:, kc, 0:n], [wq.b, hT.b], kc == 0, kc == 7)
                        for kc in range(8):
                            mm(pqr, pqr.t[:, 0:n], wq.t[:, kc, 512 + c * 128:512 + (c + 1) * 128], hT.t[:, kc, 0:n], [wq.b, hT.b],
                               kc == 0, kc == 7)
                        s_ = sg_rr % 2
                        sg_rr += 1
                        tt("dve", sg.t[:, s_, 0:n], pq.t[:, 0:n], cosT.t[:, c0:c0 + n], ALU.mult, [pq.b, cosT.b], [sg.bs[s_]])
                        tt("dve", ta.t[:, s_, 0:n], pqr.t[:, 0:n], sinT.t[:, c0:c0 + n], ALU.mult, [pqr.b, sinT.b], [ta.bs[s_]])
                        tt("pool", qT.t[:, c, 0:n], sg.t[:, s_, 0:n], ta.t[:, s_, 0:n], ALU.add, [sg.bs[s_], ta.bs[s_]], [qT.b])
                    as_ = ch % 2
                    for qi, t_ in enumerate(tiles):
                        pnum = bank()
                        pden = bank()
                        excl = (PS.index(pnum), PS.index(pden))
                        if is_ctx:
                            kbl = [(18, None), (19, None)]
                        else:
                            kbl = [(t_, 0 if t_ == 0 else 1), (t_ + 1, None), (t_ + 2, 2 if t_ == NT_OWN - 1 else 3),
                                   (18, None), (19, None)]
                        for gk in range(2):
                            pb = 64 * gk

                            def pv_(bi, kb, s_, pb=pb):
                                mm(pnum, pnum.t[pb:pb + 64, :], vv.t[:, kb, pb:pb + 64], pt.t[:, s_, :], [vv.bs[kb], pt.bs[s_]],
                                   bi == 0, bi == len(kbl) - 1)
                                mm(pden, pden.t[pb:pb + 64, :], onesb.t[:, 0:64], pt.t[:, s_, :], [onesb.b, pt.bs[s_]],
                                   bi == 0, bi == len(kbl) - 1)

                            prev_ = None
                            for bi, (kb, mk) in enumerate(kbl):
                                pst = bank(exclude=excl)
                                for c in range(4):
                                    mm(pst, pst.t[:, c * 128:(c + 1) * 128], kT.t[pb:pb + 64, kb, :],
                                       qT.t[pb:pb + 64, c, qi * 128:(qi + 1) * 128], [kT.bs[kb], qT.b])
                                s_ = pt_rr % 3
                                pt_rr += 1
                                act(pt.t[:, s_, :], pst.t[:, :], AF.Exp, [pst.b], [pt.bs[s_]], scale=0.125)
                                if mk is not None:
                                    tt("pool", pt.t[:, s_, :], pt.t[:, s_, :], masks.t[:, mk, :], ALU.mult, [pt.bs[s_], masks.b],
                                       [pt.bs[s_]])
                                if prev_ is not None:
                                    pv_(*prev_)
                                prev_ = (bi, kb, s_)
                            pv_(*prev_)
                        tt("dve", rec.t[:], pden.t[:, :], sinkrow.t[:], ALU.add, [pden.b, sinkrow.b], [rec.b])
                        P.op("dve", lambda e: e.reciprocal(rec.t[:], rec.t[:]), [rec.b], [rec.b])
                        for c in range(4):
                            tt("dve", attnT.t[:, as_, c, qi * 128:(qi + 1) * 128], pnum.t[:, c * 128:(c + 1) * 128],
                               rec.t[:, c * 128:(c + 1) * 128], ALU.mult, [pnum.b, rec.b], [attnT.bs[as_]])
                    dma("sp", attn_d.t[:, :, c0:c0 + n], attnT.t[:, as_, :, 0:n], [attnT.bs[as_]], [attn_d.b])
                P.barrier()
            kvst.close()
            if dbg.get("_stop") == "D1" and l == DL:
                stopped = True

            if not stopped:
              with ExitStack() as ph:
                wglu = sbuf(ph, "wglu", [128, 4, 512], BF16)
                wbra = sbuf(ph, "wbra", [128, 4, D], BF16)
                wbrs = sbuf(ph, "wbrs", [128, 4, D], BF16)
                wout = sbuf(ph, "wout", [128, 8, D], BF16)
                w2s = sbuf(ph, "w2s", [128, 2, 8, 256], BF16, 2)
                hT = sbuf(ph, "hTd2", [128, 8, 512], BF16)
                zstage = sbuf(ph, "zstage", [128, 4, 512], BF16)
                zsb = sbuf(ph, "zsb", [128, 4, 512], BF16)
                ssmT = sbuf(ph, "ssmT", [128, 4, 512], BF16)
                mT = sbuf(ph, "mT", [128, 8, 512], BF16)
                sg = sbuf(ph, "sg2", [128, 2, 512], F32, 2)
                ta = sbuf(ph, "ta2", [128, 2, 512], F32, 2)
                selm = sbuf(ph, "selm2", [128, 4, 128], BF16)
                gt1row = sbuf(ph, "gt1row", [128, 2, D])
                attc = sbuf(ph, "attc", [128, 4, 512], BF16)
                dma("pool", selm.t[:], selmat_d.t[:, 0:4, :], [], [selm.b])
                for kc in range(8):
                    dma("pool", wout.t[:, kc, :], W["wout"].t[:, kc, :], [W["wout"].b], [wout.b])
                for kc in range(4):
                    dma("pool", wglu.t[:, kc, :], W["wglu"].t[:, kc, :], [], [wglu.b])
                    dma("pool", wbra.t[:, kc, :], W["wbra"].t[:, kc, :], [W["wbra"].b], [wbra.b])
                    dma("pool", wbrs.t[:, kc, :], W["wbrs"].t[:, kc, :], [W["wbrs"].b], [wbrs.b])
                make_gtrow(gt1row, 2)
                nchunks = 5 if ctx_out else 4
                sg_rr = 0
                w2_rr = 0
                for ch in range(nchunks):
                    tiles = list(range(ch * 4, min(ch * 4 + 4, NT)))
                    n = len(tiles) * 128
                    c0 = ch * 512
                    is_ctx = ch == 4
                    cls = 1 if is_ctx else 0
                    for ti, t_ in enumerate(tiles):
                        norm_tile(t_, A1, 0, lambda fc, ti=ti: hT.t[:, fc, ti * 128:(ti + 1) * 128], [hT.b])
                    dma("sp", attc.t[:, :, 0:n], attn_d.t[:, :, c0:c0 + n], [attn_d.b], [attc.b])
                    for sl in range(4):
                        if is_ctx:
                            dma("sp", zsb.t[:, sl, 0:n], ag2_out[2].t[sl * 128:(sl + 1) * 128, :], [ag2_out[2].b], [zsb.b])
                        else:
                            for i in range(4):
                                gc_ = i * TOWN + c0
                                dma("sp", zstage.t[:, i, :],
                                    ag2_out[gc_ // 4096].t[sl * 128:(sl + 1) * 128, gc_ % 4096:gc_ % 4096 + 512],
                                    [ag2_out[gc_ // 4096].b], [zstage.b])
                            pz = bank()
                            for i in range(4):
                                mm(pz, pz.t[:, :], selm.t[:, i, :], zstage.t[:, i, :], [selm.b, zstage.b], i == 0, i == 3)
                            act(zsb.t[:, sl, :], pz.t[:, :], AF.Identity, [pz.b], [zsb.b])
                    for fc in range(4):
                        pg = bank()
                        for sl in range(4):
                            mm(pg, pg.t[:, 0:n], wglu.t[:, sl, fc * 128:(fc + 1) * 128], zsb.t[:, sl, 0:n], [wglu.b, zsb.b],
                               sl == 0, sl == 3)
                        s_ = sg_rr % 2
                        sg_rr += 1
                        act(sg.t[:, s_, 0:n], pg.t[:, 0:n], AF.Sigmoid, [pg.b], [sg.bs[s_]])
                        tt("dve", ssmT.t[:, fc, 0:n], zsb.t[:, fc, 0:n], sg.t[:, s_, 0:n], ALU.mult, [zsb.b, sg.bs[s_]], [ssmT.b])
                    if l == DL and ch == 0 and "ssmT" in dbg_out:
                        sdb = sbuf(ph, "sdb", [128, 4, 512])
                        cp("dve", sdb.t[:], ssmT.t[:], [ssmT.b], [sdb.b])
                        dump("ssmT", sdb, sdb.t[:], lambda o: o[:, :, :])
                    for fc in range(8):
                        ws_ = w2_rr % 2
                        w2_rr += 1
                        dma("pool", w2s.t[:, ws_, :, :], W["w2g"].t[fc, :, :, :], [W["w2g"].b], [w2s.bs[ws_]])
                        pA = bank()
                        pS = bank()
                        pga = bank()
                        pgs = bank()
                        fs = slice(fc * 128, (fc + 1) * 128)
                        for c in range(4):
                            mm(pA, pA.t[:, 0:n], wbra.t[:, c, fs], attc.t[:, c, 0:n], [wbra.b, attc.b], c == 0, c == 3)
                        for c in range(4):
                            mm(pS, pS.t[:, 0:n], wbrs.t[:, c, fs], ssmT.t[:, c, 0:n], [wbrs.b, ssmT.b], c == 0, c == 3)
                        for kc in range(8):
                            mm(pga, pga.t[:, 0:n], w2s.t[:, ws_, kc, 0:128], hT.t[:, kc, 0:n], [w2s.bs[ws_], hT.b], kc == 0, kc == 7)
                        for kc in range(8):
                            mm(pgs, pgs.t[:, 0:n], w2s.t[:, ws_, kc, 128:256], hT.t[:, kc, 0:n], [w2s.bs[ws_], hT.b], kc == 0, kc == 7)
                        act(sg.t[:, 0, 0:n], pga.t[:, 0:n], AF.Sigmoid, [pga.b], [sg.bs[0]])
                        act(sg.t[:, 1, 0:n], pgs.t[:, 0:n], AF.Sigmoid, [pgs.b], [sg.bs[1]])
                        tt("dve", ta.t[:, 0, 0:n], pA.t[:, 0:n], sg.t[:, 0, 0:n], ALU.mult, [pA.b, sg.bs[0]], [ta.bs[0]])
                        tt("dve", ta.t[:, 1, 0:n], pS.t[:, 0:n], sg.t[:, 1, 0:n], ALU.mult, [pS.b, sg.bs[1]], [ta.bs[1]])
                        tt("pool", mT.t[:, fc, 0:n], ta.t[:, 0, 0:n], ta.t[:, 1, 0:n], ALU.add, [ta.bs[0], ta.bs[1]], [mT.b])
                    for ti, t_ in enumerate(tiles):
                        for half in range(2):
                            py = bank()
                            hs_ = slice(half * 512, (half + 1) * 512)
                            for kc in range(8):
                                mm(py, py.t[:, :], mT.t[:, kc, ti * 128:(ti + 1) * 128], wout.t[:, kc, hs_], [mT.b, wout.b],
                                   kc == 0, kc == 7)
                            s_ = sg_rr % 2
                            sg_rr += 1
                            tt("dve", sg.t[:, s_, :], py.t[:, :], gt1row.t[:, cls, hs_], ALU.mult, [py.b, gt1row.b], [sg.bs[s_]])
                            tt("pool", x_tm.t[:, t_, hs_], x_tm.t[:, t_, hs_], sg.t[:, s_, :], ALU.add, [sg.bs[s_], x_tm.bs[t_]],
                               [x_tm.bs[t_]])
                P.barrier()
            if l == DL and not stopped:
                dump("xmix", x_tm.bs, x_tm.t[:], lambda o: o[:, :, :])
            if dbg.get("_stop") == "D2" and l == DL:
                stopped = True
            if stopped:
                break

            if not stopped:
              with ExitStack() as ph:
                ntl = NT if ctx_out else NT_OWN
                h2T = sbuf(ph, "h2T", [128, 8, TALL], BF16, NT)
                h2f = sbuf(ph, "h2f", [128, 8, 128])
                wr = sbuf(ph, "wr", [128, 8, 36])
                Wt = sbuf(ph, "Wt", [128, NT, 32], F32, NT)
                lg = sbuf(ph, "lg", [128, 36])
                rs = sbuf(ph, "rs", [128, 16])
                rt = sbuf(ph, "rt", [128, 4, 32])
                weg = sbuf(ph, "weg", [128, 2, 8, 512], BF16, 2)
                weu = sbuf(ph, "weu", [128, 2, 8, 512], BF16, 2)
                wed = sbuf(ph, "wed", [128, 2, 4, D], BF16, 2)
                hid = sbuf(ph, "hid", [128, 2, 4, 512], BF16, 2)
                sgm = sbuf(ph, "sgm", [128, 2, 512], F32, 2)
                ty = sbuf(ph, "ty", [128, 2, 512], F32, 2)
                gt2row = sbuf(ph, "gt2row", [128, 2, D])
                dma("sp", wr.t[:], W["wr"].t[:, :, :], [], [wr.b])
                make_gtrow(gt2row, 5)
                compute_rstd(ntl)
                RB = [rs.b]
                c_ = lambda i: rs.t[:, i:i + 1]
                for t_ in range(ntl):
                    norm_tile(t_, A2, 3, lambda fc, t_=t_: h2T.t[:, fc, t_ * 128:(t_ + 1) * 128], [h2T.bs[t_]], f32_dst=h2f)
                    pl = bank()
                    for kc in range(8):
                        mm(pl, pl.t[:, 0:36], h2f.t[:, kc, :], wr.t[:, kc, :], [h2f.b, wr.b], kc == 0, kc == 7)
                    cp("dve", lg.t[:], pl.t[:, 0:36], [pl.b], [lg.b])
                    P.op("dve", lambda e: e.tensor_reduce(rs.t[:, 0:1], lg.t[:, 0:4], AX.X, ALU.max), [lg.b], RB)
                    ts("dve", c_(1), c_(0), -1.0, None, ALU.mult, None, RB, RB)
                    act(rt.t[:, 0, 0:4], lg.t[:, 0:4], AF.Exp, [lg.b] + RB, [rt.b, rs.b], bias=rs.t[:, 1:2], accum=rs.t[:, 2:3])
                    P.op("dve", lambda e: e.reciprocal(rs.t[:, 3:4], rs.t[:, 2:3]), RB, RB)
                    ts("dve", rt.t[:, 0, 4:8], lg.t[:, 0:4], rs.t[:, 0:1], None, ALU.is_equal, None, [lg.b] + RB, [rt.b])
                    ts("dve", rt.t[:, 0, 4:8], rt.t[:, 0, 4:8], -1.0, 1e30, ALU.add, ALU.mult, [rt.b], [rt.b])
                    for g in range(4):
                        ts("dve", rt.t[:, 1, 8 * g:8 * g + 8], lg.t[:, 4 + 8 * g:12 + 8 * g], rt.t[:, 0, 4 + g:5 + g], None,
                           ALU.add, None, [lg.b, rt.b], [rt.b])
                    P.op("dve", lambda e: e.tensor_reduce(rs.t[:, 4:5], rt.t[:, 1, :], AX.X, ALU.max), [rt.b], RB)
                    ts("dve", rt.t[:, 2, :], rt.t[:, 1, :], rs.t[:, 4:5], None, ALU.is_equal, None, [rt.b] + RB, [rt.b])
                    stt("dve", rt.t[:, 2, :], rt.t[:, 2, :], -1e30, rt.t[:, 1, :], ALU.mult, ALU.add, [rt.b], [rt.b])
                    P.op("dve", lambda e: e.tensor_reduce(rs.t[:, 5:6], rt.t[:, 2, :], AX.X, ALU.max), [rt.b], RB)
                    ts("dve", rt.t[:, 2, :], rt.t[:, 1, :], rs.t[:, 5:6], None, ALU.is_ge, None, [rt.b] + RB, [rt.b])
                    ts("dve", c_(6), c_(4), -1.0, None, ALU.mult, None, RB, RB)
                    act(rt.t[:, 3, :], rt.t[:, 1, :], AF.Exp, [rt.b] + RB, [rt.b], bias=rs.t[:, 6:7])
                    tt("dve", rt.t[:, 3, :], rt.t[:, 3, :], rt.t[:, 2, :], ALU.mult, [rt.b], [rt.b])
                    P.op("dve", lambda e: e.tensor_reduce(rs.t[:, 7:8], rt.t[:, 3, :], AX.X, ALU.add), [rt.b], RB)
                    P.op("dve", lambda e: e.reciprocal(rs.t[:, 7:8], rs.t[:, 7:8]), RB, RB)
                    tt("dve", c_(7), c_(7), c_(3), ALU.mult, RB, RB)
                    ts("dve", Wt.t[:, t_, :], rt.t[:, 3, :], rs.t[:, 7:8], None, ALU.mult, None, [rt.b] + RB, [Wt.bs[t_]])
                if l == DL:
                    dump("Wt", Wt.bs, Wt.t[:], lambda o: o[:, :, :])
                nch = 5 if ctx_out else 4
                ty_rr = 0
                nexp = dbg.get("_nexp", NEXP)
                for e_ in range(nexp):
                    s = e_ % 2
                    for k2 in range(2):
                        dma("pool", weg.t[:, s, 4 * k2:4 * k2 + 4, :], W["weg"].t[e_, :, 4 * k2:4 * k2 + 4, :], [W["weg"].b], [weg.bs[s]])
                        dma("pool", weu.t[:, s, 4 * k2:4 * k2 + 4, :], W["weu"].t[e_, :, 4 * k2:4 * k2 + 4, :], [W["weu"].b], [weu.bs[s]])
                        dma("pool", wed.t[:, s, 2 * k2:2 * k2 + 2, :], W["wed"].t[e_, :, 2 * k2:2 * k2 + 2, :], [W["wed"].b], [wed.bs[s]])
                    for ch in range(nch):
                        tiles = list(range(ch * 4, min(ch * 4 + 4, NT)))
                        n = len(tiles) * 128
                        c0 = ch * 512
                        cls = 1 if ch == 4 else 0
                        hs = ch % 2
                        for hc in range(4):
                            pg = bank()
                            pu = bank()
                            hrd = [h2T.bs[t_] for t_ in tiles]
                            for kc in range(8):
                                mm(pg, pg.t[:, 0:n], weg.t[:, s, kc, hc * 128:(hc + 1) * 128], h2T.t[:, kc, c0:c0 + n],
                                   [weg.bs[s]] + hrd, kc == 0, kc == 7)
                            for kc in range(8):
                                mm(pu, pu.t[:, 0:n], weu.t[:, s, kc, hc * 128:(hc + 1) * 128], h2T.t[:, kc, c0:c0 + n],
                                   [weu.bs[s]] + hrd, kc == 0, kc == 7)
                            ss_ = hc % 2
                            act(sgm.t[:, ss_, 0:n], pg.t[:, 0:n], AF.Silu, [pg.b], [sgm.bs[ss_]])
                            tt("dve", hid.t[:, hs, hc, 0:n], pu.t[:, 0:n], sgm.t[:, ss_, 0:n], ALU.mult, [pu.b, sgm.bs[ss_]],
                               [hid.bs[hs]])
                        for ti, t_ in enumerate(tiles):
                            for half in range(2):
                                py = bank()
                                hs_ = slice(half * 512, (half + 1) * 512)
                                for hc in range(4):
                                    mm(py, py.t[:, :], hid.t[:, hs, hc, ti * 128:(ti + 1) * 128], wed.t[:, s, hc, hs_],
                                       [hid.bs[hs], wed.bs[s]], hc == 0, hc == 3)
                                y_ = ty_rr % 2
                                ty_rr += 1
                                tt("dve", ty.t[:, y_, :], py.t[:, :], gt2row.t[:, cls, hs_], ALU.mult, [py.b, gt2row.b],
                                   [ty.bs[y_]])
                                stt("dve", x_tm.t[:, t_, hs_], ty.t[:, y_, :], Wt.t[:, t_, e_:e_ + 1], x_tm.t[:, t_, hs_],
                                    ALU.mult, ALU.add, [ty.bs[y_], Wt.bs[t_], x_tm.bs[t_]], [x_tm.bs[t_]])
                P.barrier()
            if l == DL and not stopped:
                dump("xout", x_tm.bs, x_tm.t[:], lambda o: o[:, :, :])

        with ExitStack() as ph:
            gfin = sbuf(ph, "gfin", [128, D])
            ob = sbuf(ph, "ob", [128, 2, D], F32, 2)
            dma("sp", gfin.t[:], gfin_d.t[:, :], [], [gfin.b])
            compute_rstd(NT_OWN)
            for t_ in range(NT_OWN):
                s = t_ % 2
                stt("dve", ob.t[:, s, :], x_tm.t[:, t_, :], rstd.t[:, t_:t_ + 1], gfin.t[:], ALU.mult, ALU.mult,
                    [x_tm.bs[t_], rstd.b, gfin.b], [ob.bs[s]])
                dma("sp", out_d.t[t_ * 128:(t_ + 1) * 128, :], ob.t[:, s, :], [ob.bs[s]], [out_d.b])
        P.wait_all("sp", [out_d.b] + [v.b for v in dbg_out.values()])
        P.emit()
    return nc, names_in


def _kc(w):
    K, C = w.shape
    return np.ascontiguousarray(w.reshape(K // 128, 128, C).transpose(1, 0, 2))


def prep_inputs(inputs, nlayers=2, nei=NEXP):
    f = lambda a: np.ascontiguousarray(np.asarray(a, dtype=np.float32))
    I_ = {k: f(v) for k, v in inputs.items()}
    shared = {}
    e = np.arange(64)
    partner = np.where((e % 32) < 16, e + 16, e - 16)
    head_order = [h for c in range(4) for h in (c, c + 4)]
    qcols = np.concatenate([h * 64 + np.arange(64) for h in head_order])
    qrcols = np.concatenate([h * 64 + partner for h in head_order])
    kcols = 512 + np.arange(128)
    krcols = 512 + np.concatenate([hk * 64 + partner for hk in range(2)])
    vcols = 640 + np.arange(128)
    ucols = 768 + np.arange(512)
    w1cols = np.concatenate([qcols, qrcols, kcols, krcols, vcols, ucols])
    brarows = np.concatenate([h * 64 + np.arange(64) for h in head_order])
    shared["ident"] = np.eye(128, dtype=np.float32)
    shared["iota"] = np.ascontiguousarray(np.broadcast_to(np.arange(1, 513, dtype=np.float32), (128, 512)))
    shared["gfin"] = np.ascontiguousarray(np.broadcast_to(I_["g_final"], (128, D)))
    ee = np.arange(128) % 64
    ropef = np.stack([(ee % 16).astype(np.float32), np.where((ee % 32) < 16, -1.0, 1.0).astype(np.float32)], 1)
    shared["ropef"] = np.ascontiguousarray(ropef)
    for l in range(nlayers):
        win = I_["w_in"][l]
        shared[f"wmod{l}"] = _kc(I_["w_mod"][l])
        shared[f"bmodT{l}"] = np.ascontiguousarray(I_["b_mod"][l].reshape(48, 128).T)
        shared[f"g1T{l}"] = np.ascontiguousarray(I_["g_norm1"][l].reshape(8, 128).T)
        shared[f"g2T{l}"] = np.ascontiguousarray(I_["g_norm2"][l].reshape(8, 128).T)
        shared[f"w1_{l}"] = _kc(win[:, w1cols])
        ga = _kc(win[:, 1280:2304]).reshape(128, 8, 8, 128)
        gs = _kc(win[:, 2304:3328]).reshape(128, 8, 8, 128)
        shared[f"w2g{l}"] = np.ascontiguousarray(np.concatenate([ga, gs], axis=3).transpose(2, 0, 1, 3))
        shared[f"wglu{l}"] = _kc(I_["w_glu"][l])
        shared[f"wbra{l}"] = _kc(I_["w_br_attn"][l][brarows, :])
        shared[f"wbrs{l}"] = _kc(I_["w_br_ssm"][l])
        shared[f"wout{l}"] = _kc(I_["w_out"][l])
        shared[f"wr{l}"] = _kc(np.concatenate([I_["w_router_group"][l], I_["w_router_expert"][l]], axis=1))
        shared[f"weg{l}"] = np.ascontiguousarray(I_["w_exp_gate"][l][:nei].reshape(nei, 8, 128, 512).transpose(0, 2, 1, 3))
        shared[f"weu{l}"] = np.ascontiguousarray(I_["w_exp_up"][l][:nei].reshape(nei, 8, 128, 512).transpose(0, 2, 1, 3))
        shared[f"wed{l}"] = np.ascontiguousarray(I_["w_exp_down"][l][:nei].reshape(nei, 4, 128, D).transpose(0, 2, 1, 3))
        shared[f"sink{l}"] = np.ascontiguousarray(np.broadcast_to(I_["attn_sink"][l], (128, 8)))
    kk = np.arange(128)[:, None]
    qq = np.arange(128)[None, :]
    mprev = np.tile((kk >= qq).astype(np.float32), (1, 4))
    mnext = np.tile((kk <= qq).astype(np.float32), (1, 4))
    per_core = []
    for r in range(NCORES):
        b, j = r // 4, r % 4
        m = dict(shared)
        t0 = j * TOWN
        m["x_own"] = np.ascontiguousarray(I_["x"][b, t0:t0 + TOWN])
        m["ctx_b"] = np.ascontiguousarray(I_["ctx"][b])
        cT = np.stack([I_["c"][b].reshape(8, 128).T, I_["c_ctx"].reshape(8, 128).T], axis=2)
        m["cT"] = np.ascontiguousarray(cT)
        tpos = np.arange(t0, t0 + TOWN)
        rows = (tpos // 64).astype(np.float32)
        cols = (tpos % 64).astype(np.float32)
        pos = np.zeros((128, TALL), np.float32)
        is_row = (ee % 64) < 32
        pos[:, :TOWN] = np.where(is_row[:, None], rows[None, :], cols[None, :])
        m["pos"] = pos
        mk = np.zeros((128, 4, 512), np.float32)
        mk[:, 0] = mprev if j > 0 else 0.0
        mk[:, 1] = mprev
        mk[:, 2] = mnext if j < 3 else 0.0
        mk[:, 3] = mnext
        m["masks"] = mk
        sel = np.zeros((128, 12, 128), np.float32)
        eye = np.eye(128, dtype=np.float32)
        sel[:, j] = eye
        if j > 0:
            sel[:, 4 + j - 1] = eye
        if j < 3:
            sel[:, 8 + j + 1] = eye
        m["selmat"] = sel
        for l in range(nlayers):
            win = I_["w_in"][l]
            m[f"wus{l}"] = _kc(win[:, 768 + 128 * j:768 + 128 * (j + 1)])
            g0 = 8 * j

            def rowlay(a):
                a = a.reshape((2, 4, 2, 64) + a.shape[3:])
                perm = (2, 3, 0, 1) + tuple(range(4, a.ndim))
                a = a.transpose(perm)
                return np.ascontiguousarray(a.reshape((128, 8) + a.shape[4:]))

            m[f"lamre{l}"] = rowlay(I_["ssm_lam_re"][l][:, g0:g0 + 8])
            m[f"lamim{l}"] = rowlay(I_["ssm_lam_im"][l][:, g0:g0 + 8])
            ldt = np.broadcast_to(I_["ssm_log_dt"][l][:, g0:g0 + 8, None], (2, 8, 64))
            m[f"ldt{l}"] = rowlay(np.ascontiguousarray(ldt))
            m[f"bre{l}"] = rowlay(I_["ssm_b_re"][l][:, g0:g0 + 8])
            m[f"bim{l}"] = rowlay(I_["ssm_b_im"][l][:, g0:g0 + 8])
            m[f"cre{l}"] = rowlay(np.ascontiguousarray(I_["ssm_c_re"][l][:, g0:g0 + 8].transpose(0, 1, 3, 2)))
            m[f"cim{l}"] = rowlay(np.ascontiguousarray(I_["ssm_c_im"][l][:, g0:g0 + 8].transpose(0, 1, 3, 2)))
            m[f"dsk{l}"] = np.ascontiguousarray(I_["ssm_d"][l][128 * j:128 * (j + 1)].reshape(128, 1))
        for k in list(m.keys()):
            if False:
                a = m[k]
                a2 = a.reshape(-1, a.shape[-1])
                rr = a2.shape[0] // NCORES
                m[k] = np.ascontiguousarray(a2[r * rr:(r + 1) * rr])
        per_core.append(m)
    return per_core


_CACHE = {}


def kernel(**inputs):
    if "nc" not in _CACHE:
        _CACHE["nc"] = build_program(2)
    nc, names = _CACHE["nc"]
    per_core = prep_inputs(inputs, 2)
    in_maps = [{k: m[k] for k in names} for m in per_core]
    res = run_bass_kernel_spmd(nc, in_maps, core_ids=list(range(NCORES)))
    out = np.zeros((2, SEQ, D), np.float32)
    for r in range(NCORES):
        b, j = r // 4, r % 4
        out[b, j * TOWN:(j + 1) * TOWN] = res.results[r]["out"]
    return out
```

```python
from contextlib import ExitStack
import numpy as np
import concourse.bass as bass
import concourse.mybir as mybir
from concourse.bass_utils import run_bass_kernel_spmd

dt = mybir.dt
ALU = mybir.AluOpType
AF = mybir.ActivationFunctionType
AX = mybir.AxisListType
F32 = dt.float32
BF16 = dt.bfloat16
I32 = dt.int32

NCORES = 8
D = 1024
TOWN = 2048
NT_OWN = 16
NT = 18
TALL = 2304
SEQ = 8192
CTX = 256
STREAM = SEQ + CTX
NEXP = 32
PI = float(np.pi)
GROUPS = [[0, 1, 2, 3], [4, 5, 6, 7]]


class Buf:
    __slots__ = ("name", "last_w", "readers")

    def __init__(self, name=""):
        self.name = name
        self.last_w = None
        self.readers = {}


class Prog:
    ENG = ("pe", "act", "dve", "pool", "sp")

    def __init__(self, nc, stack, ndma=None):
        ndma = ndma or {"sp": 8, "act": 2, "pool": 6, "cc": 3}
        self.nc = nc
        self.q = {e: [] for e in self.ENG}
        self.sems = {}
        self.cnt = {e: 0 for e in self.ENG}
        self.waited = {e: {} for e in self.ENG}
        for e in self.ENG:
            self.sems[("c", e)] = stack.enter_context(nc.semaphore("c_" + e))
        self.dma_slots = {}
        self.dma_tot = {}
        self.dma_rr = {}
        for e, n in ndma.items():
            keys = []
            for i in range(n):
                k = ("d", e, i)
                self.sems[k] = stack.enter_context(nc.semaphore("d_%s%d" % (e, i)))
                self.dma_tot[k] = 0
                keys.append(k)
            self.dma_slots[e] = keys
            self.dma_rr[e] = 0

    def _wait(self, eng, k, v):
        if k == ("c", "pe") and eng == "pe":
            return
        if self.waited[eng].get(k, 0) >= v:
            return
        self.waited[eng][k] = v
        self.q[eng].append(("w", k, v))

    def _deps(self, eng, reads, writes):
        deps = {}

        def add(ev):
            if ev is None:
                return
            k, v = ev
            if deps.get(k, 0) < v:
                deps[k] = v

        for b in reads:
            add(b.last_w)
        for b in writes:
            add(b.last_w)
            for k, v in b.readers.items():
                add((k, v))
        for k, v in deps.items():
            self._wait(eng, k, v)

    def _commit(self, ev, reads, writes):
        k, v = ev
        for b in reads:
            if b.readers.get(k, 0) < v:
                b.readers[k] = v
        for b in writes:
            b.last_w = ev
            b.readers = {}

    def op(self, eng, fn, reads=(), writes=()):
        self._deps(eng, reads, writes)
        self.cnt[eng] += 1
        ev = (("c", eng), self.cnt[eng])
        self.q[eng].append(("o", fn, ("c", eng), 1))
        self._commit(ev, reads, writes)
        return ev

    def dma(self, qeng, fn, reads=(), writes=(), inc=16):
        skey = "cc" if inc == 1 else qeng
        slots = self.dma_slots[skey]
        k = slots[self.dma_rr[skey] % len(slots)]
        self.dma_rr[skey] += 1
        prev = self.dma_tot[k]
        if prev > 0:
            self._wait(qeng, k, prev)
        self._deps(qeng, reads, writes)
        self.dma_tot[k] = prev + inc
        ev = (k, prev + inc)
        self.q[qeng].append(("o", fn, k, inc))
        self._commit(ev, reads, writes)
        return ev

    def wait_all(self, eng, bufs):
        self._deps(eng, bufs, ())

    def barrier(self):
        tot = {("c", e): self.cnt[e] for e in self.ENG}
        tot.update(self.dma_tot)
        for e in self.ENG:
            for k, v in tot.items():
                if v > 0:
                    self._wait(e, k, v)

    def emit(self):
        nc = self.nc
        sems = self.sems

        def replay(e, name):
            for it in self.q[name]:
                if it[0] == "w":
                    e.wait_ge(sems[it[1]], it[2])
                else:
                    ins = it[1](e)
                    ins.then_inc(sems[it[2]], it[3])

        with nc.Block() as block:

            @block.tensor
            def _(e):
                replay(e, "pe")

            @block.vector
            def _(e):
                replay(e, "dve")

            @block.scalar
            def _(e):
                replay(e, "act")

            @block.gpsimd
            def _(e):
                replay(e, "pool")

            @block.sync
            def _(e):
                replay(e, "sp")


class TL:
    def __init__(self, t, nslots=1):
        self.t = t
        self.bs = [Buf() for _ in range(nslots)]

    @property
    def b(self):
        return self.bs[0]


def build_program(nlayers=2, dbg=None):
    dbg = dbg or {}
    NEI = dbg.get("_nexp", NEXP)
    nc = bass.Bass("TRN2", target_bir_lowering=False)
    names_in = []

    def inp(name, shape, d=F32):
        names_in.append(name)
        return TL(nc.dram_tensor(name, list(shape), d, kind="ExternalInput").ap())

    gathers = []

    def ginp(name, shape):
        return inp(name, shape)

    def ginp_unused(name, shape):
        R = int(np.prod(shape[:-1]))
        C = int(shape[-1])
        assert R % NCORES == 0
        names_in.append(name)
        shard = TL(nc.dram_tensor(name, [R // NCORES, C], F32, kind="ExternalInput").ap())
        bounce = TL(nc.dram_tensor(name + "_bnc", [R // NCORES, C], F32).ap())
        full = TL(nc.dram_tensor(name + "_full", list(shape), F32).ap())
        gathers.append((shard, bounce, full))
        return full

    x_own = inp("x_own", [TOWN, D])
    ctx_b = inp("ctx_b", [CTX, D])
    cT = inp("cT", [128, 8, 2])
    pos_d = inp("pos", [128, TALL])
    ropef_d = inp("ropef", [128, 2])
    masks_d = inp("masks", [128, 4, 512])
    selmat_d = inp("selmat", [128, 12, 128])
    ident_d = inp("ident", [128, 128])
    iota_d = inp("iota", [128, 512])
    gfin_d = inp("gfin", [128, D])
    L = []
    for l in range(nlayers):
        w = {}
        w["wmod"] = ginp(f"wmod{l}", [128, 8, 6 * D])
        w["bmodT"] = inp(f"bmodT{l}", [128, 48])
        w["g1T"] = inp(f"g1T{l}", [128, 8])
        w["g2T"] = inp(f"g2T{l}", [128, 8])
        w["w1"] = ginp(f"w1_{l}", [128, 8, 1920])
        w["wus"] = inp(f"wus{l}", [128, 8, 128])
        w["w2g"] = ginp(f"w2g{l}", [8, 128, 8, 256])
        w["wglu"] = inp(f"wglu{l}", [128, 4, 512])
        w["wbra"] = ginp(f"wbra{l}", [128, 4, D])
        w["wbrs"] = ginp(f"wbrs{l}", [128, 4, D])
        w["wout"] = ginp(f"wout{l}", [128, 8, D])
        w["wr"] = inp(f"wr{l}", [128, 8, 36])
        w["weg"] = ginp(f"weg{l}", [NEI, 128, 8, 512])
        w["weu"] = ginp(f"weu{l}", [NEI, 128, 8, 512])
        w["wed"] = ginp(f"wed{l}", [NEI, 128, 4, D])
        w["sink"] = inp(f"sink{l}", [128, 8])
        w["lamre"] = inp(f"lamre{l}", [128, 8])
        w["lamim"] = inp(f"lamim{l}", [128, 8])
        w["ldt"] = inp(f"ldt{l}", [128, 8])
        w["bre"] = inp(f"bre{l}", [128, 8, 16])
        w["bim"] = inp(f"bim{l}", [128, 8, 16])
        w["cre"] = inp(f"cre{l}", [128, 8, 16])
        w["cim"] = inp(f"cim{l}", [128, 8, 16])
        w["dsk"] = inp(f"dsk{l}", [128, 1])
        L.append(w)
    out_d = TL(nc.dram_tensor("out", [TOWN, D], F32, kind="ExternalOutput").ap())
    dbg_out = {}
    for k, shp in dbg.items():
        if k.startswith("_"):
            continue
        dbg_out[k] = TL(nc.dram_tensor("dbg_" + k, list(shp), F32, kind="ExternalOutput").ap())
    AGR = TOWN + 128
    ag1_in = [TL(nc.dram_tensor(f"ag1_in{i}", [r_, 512], BF16).ap()) for i, r_ in enumerate((1024, 1024, 128))]
    ag1_out = [TL(nc.dram_tensor(f"ag1_out{i}", [4 * r_, 512], BF16).ap()) for i, r_ in enumerate((1024, 1024, 128))]
    ag2_in = [TL(nc.dram_tensor(f"ag2_in{i}", [128, c_], BF16).ap()) for i, c_ in enumerate((4096, 4096, CTX))]
    ag2_out = [TL(nc.dram_tensor(f"ag2_out{i}", [512, c_], BF16).ap()) for i, c_ in enumerate((4096, 4096, CTX))]
    cos_d = TL(nc.dram_tensor("cos_d", [128, TALL], F32).ap())
    sin_d = TL(nc.dram_tensor("sin_d", [128, TALL], F32).ap())
    yTf_d = TL(nc.dram_tensor("yTf_d", [128, STREAM], F32).ap())
    attn_d = TL(nc.dram_tensor("attn_d", [128, 4, TALL], BF16).ap())

    with ExitStack() as st:
        P = Prog(nc, st)

        uid = [0]

        def sbuf(stack, name, shape, d=F32, nslots=1):
            uid[0] += 1
            return TL(stack.enter_context(nc.sbuf_tensor("sb%d_%s" % (uid[0], name), list(shape), d)), nslots)

        PS = [TL(st.enter_context(nc.psum_tensor(f"ps{i}", [128, 512], F32))) for i in range(8)]
        ps_rr = [0]

        def bank(exclude=()):
            while True:
                i = ps_rr[0] % 8
                ps_rr[0] += 1
                if i not in exclude:
                    return PS[i]

        def mm(out_tl, out_ap, lhsT_ap, rhs_ap, reads, start=True, stop=True):
            P.op("pe", lambda e: e.matmul(out_ap, lhsT_ap, rhs_ap, start=start, stop=stop), reads, [out_tl.b])

        def tr(out_tl, out_ap, in_ap, ident_ap, reads):
            P.op("pe", lambda e: e.transpose(out_ap, in_ap, ident_ap), reads, [out_tl.b])

        def act(out_ap, in_ap, func, reads, writes, bias=None, scale=None, accum=None):
            kw = {}
            if bias is not None:
                kw["bias"] = bias
            if scale is not None:
                kw["scale"] = scale
            if accum is not None:
                kw["accum_out"] = accum
            P.op("act", lambda e: e.activation(out_ap, in_ap, func, **kw), reads, writes)

        def tt(eng, out_ap, a_ap, b_ap, op, reads, writes):
            P.op(eng, lambda e: e.tensor_tensor(out_ap, a_ap, b_ap, op), reads, writes)

        def ts(eng, out_ap, a_ap, s1, s2, op0, op1, reads, writes):
            if op1 is None:
                P.op(eng, lambda e: e.tensor_scalar(out_ap, a_ap, s1, None, op0), reads, writes)
            else:
                P.op(eng, lambda e: e.tensor_scalar(out_ap, a_ap, s1, s2, op0, op1), reads, writes)

        def stt(eng, out_ap, a_ap, s, b_ap, op0, op1, reads, writes):
            P.op(eng, lambda e: e.scalar_tensor_tensor(out_ap, a_ap, s, b_ap, op0, op1), reads, writes)

        def cp(eng, out_ap, in_ap, reads, writes):
            P.op(eng, lambda e: e.tensor_copy(out_ap, in_ap), reads, writes)

        def dma(q, out_ap, in_ap, reads, writes, **kw):
            P.dma(q, lambda e: e.dma_start(out=out_ap, in_=in_ap, **kw), reads, writes)

        def dump(key, src_tl, src_ap, dst_ap_fn):
            if key in dbg_out:
                dma("sp", dst_ap_fn(dbg_out[key].t), src_ap, [src_tl.b] if isinstance(src_tl, TL) else src_tl,
                    [dbg_out[key].b])

        for shard, bounce, full in gathers:
            nr_ = shard.t.shape[0]
            for r0_ in range(0, nr_, 1024):
                r1_ = min(nr_, r0_ + 1024)
                dma("sp", bounce.t[r0_:r1_, :], shard.t[r0_:r1_, :], [], [bounce.b])
            P.dma("pool", lambda e, bounce=bounce, full=full: e.collective_compute(
                "AllGather", ALU.bypass, replica_groups=[list(range(NCORES))], ins=[bounce.t.opt()], outs=[full.t.opt()]),
                [bounce.b], [full.b], inc=1)

        WBF = []
        for l in range(nlayers):
            wb = {}
            for nm in ("w1", "w2g", "wout", "wbra", "wbrs", "wglu"):
                src = L[l][nm]
                dst = TL(nc.dram_tensor(f"{nm}bf{l}", list(src.t.shape), BF16).ap())
                for i0 in range(src.t.shape[0] if nm == "w2g" else src.t.shape[1]):
                    if nm == "w2g":
                        dma("pool", dst.t[i0, :, :, :], src.t[i0, :, :, :], [src.b], [dst.b])
                    else:
                        dma("pool", dst.t[:, i0, :], src.t[:, i0, :], [src.b], [dst.b])
                wb[nm] = dst
            WBF.append(wb)

        x_tm = sbuf(st, "x_tm", [128, NT, D], F32, NT)
        ident = sbuf(st, "ident", [128, 128])
        zeros = sbuf(st, "zeros", [128, 512])
        iota = sbuf(st, "iota", [128, 512])
        onesb = sbuf(st, "onesb", [128, 64], BF16)
        onesf = sbuf(st, "onesf", [128, 128])
        siluT = sbuf(st, "siluT", [128, 8, 2])
        modT = sbuf(st, "modT", [128, 48, 2])
        bmodT = sbuf(st, "bmodT", [128, 48])
        gT = sbuf(st, "gT", [128, 16])
        A1 = sbuf(st, "A1", [128, 2, 8])
        A2 = sbuf(st, "A2", [128, 2, 8])
        rstd = sbuf(st, "rstd", [128, NT])
        sstat = sbuf(st, "sstat", [128, NT])
        xs = sbuf(st, "xs", [128, D])
        sm = sbuf(st, "sm", [128, 8])
        ucT = sbuf(st, "ucT", [128, CTX], BF16)
        dg = sbuf(st, "dg", [128, 128])

        def AP_rev(tl, row_elems, col_last, n):
            return bass.AP(tl.t, col_last, [[row_elems, 128], [-1, n]])

        def range_reduce(kf_tl, kf_ap, ki_tl, ki_ap, out_ap, in_ap, shift, reads, writes):
            ts("dve", kf_ap, in_ap, shift, 1.0 / (2 * PI), ALU.add, ALU.mult, reads, [kf_tl.b])
            cp("dve", ki_ap, kf_ap, [kf_tl.b], [ki_tl.b])
            cp("dve", kf_ap, ki_ap, [ki_tl.b], [kf_tl.b])
            stt("dve", kf_ap, kf_ap, -2 * PI, in_ap, ALU.mult, ALU.add, [kf_tl.b] + list(reads), [kf_tl.b])
            ts("dve", out_ap, kf_ap, shift, None, ALU.add, None, [kf_tl.b], writes)
            ts("dve", out_ap, out_ap, 3.14159, -3.14159, ALU.min, ALU.max, writes, writes)

        with ExitStack() as ph:
            posT = sbuf(ph, "posT", [128, TALL])
            ang = sbuf(ph, "ang", [128, TALL])
            ki = sbuf(ph, "ki", [128, TALL], I32)
            kf = sbuf(ph, "kf", [128, TALL])
            tb = sbuf(ph, "tb", [128, TALL])
            ropef = sbuf(ph, "ropef", [128, 2])
            dma("sp", x_tm.t[:, 0:NT_OWN, :], x_own.t.rearrange("(t p) d -> p t d", p=128), [], x_tm.bs[0:NT_OWN])
            dma("sp", x_tm.t[:, NT_OWN:NT, :], ctx_b.t.rearrange("(t p) d -> p t d", p=128), [], x_tm.bs[NT_OWN:NT])
            dma("sp", ident.t[:], ident_d.t[:, :], [], [ident.b])
            dma("sp", posT.t[:], pos_d.t[:, :], [], [posT.b])
            dma("sp", ropef.t[:], ropef_d.t[:, :], [], [ropef.b])
            dma("sp", iota.t[:], iota_d.t[:, :], [], [iota.b])
            dma("sp", siluT.t[:], cT.t[:, :, :], [], [siluT.b])
            P.op("pool", lambda e: e.memset(zeros.t[:], 0.0), [], [zeros.b])
            P.op("pool", lambda e: e.memset(onesb.t[:], 1.0), [], [onesb.b])
            P.op("pool", lambda e: e.memset(onesf.t[:], 1.0), [], [onesf.b])
            act(siluT.t[:], siluT.t[:], AF.Silu, [siluT.b], [siluT.b])
            act(sm.t[:, 0:1], ropef.t[:, 0:1], AF.Exp, [ropef.b], [sm.b], scale=-float(np.log(10000.0)) / 16.0)
            ts("dve", ang.t[:], posT.t[:], sm.t[:, 0:1], None, ALU.mult, None, [posT.b, sm.b], [ang.b])
            range_reduce(kf, kf.t[:], ki, ki.t[:], tb.t[:], ang.t[:], 0.0, [ang.b], [tb.b])
            act(tb.t[:], tb.t[:], AF.Sin, [tb.b], [tb.b])
            ts("dve", tb.t[:], tb.t[:], ropef.t[:, 1:2], None, ALU.mult, None, [tb.b, ropef.b], [tb.b])
            dma("sp", sin_d.t[:, :], tb.t[:], [tb.b], [sin_d.b])
            range_reduce(kf, kf.t[:], ki, ki.t[:], tb.t[:], ang.t[:], PI / 2, [ang.b], [tb.b])
            act(tb.t[:], tb.t[:], AF.Sin, [tb.b], [tb.b])
            dma("sp", cos_d.t[:, :], tb.t[:], [tb.b], [cos_d.b])
            P.barrier()

        def tile_cls(t_):
            return 0 if t_ < NT_OWN else 1

        def compute_rstd(ntiles):
            for t_ in range(ntiles):
                act(xs.t[:], x_tm.t[:, t_, :], AF.Square, [x_tm.bs[t_]], [xs.b, sstat.b], accum=sstat.t[:, t_:t_ + 1])
            ts("dve", rstd.t[:, 0:ntiles], sstat.t[:, 0:ntiles], 1.0 / D, 1e-6, ALU.mult, ALU.add, [sstat.b], [rstd.b])
            act(rstd.t[:, 0:ntiles], rstd.t[:, 0:ntiles], AF.Sqrt, [rstd.b], [rstd.b])
            P.op("dve", lambda e: e.reciprocal(rstd.t[:, 0:ntiles], rstd.t[:, 0:ntiles]), [rstd.b], [rstd.b])

        def norm_tile(t_, Acls, shslot, dst_ap_fn, dst_bufs, f32_dst=None):
            cls = tile_cls(t_)
            act(xs.t[:], x_tm.t[:, t_, :], AF.Identity, [x_tm.bs[t_], rstd.b], [xs.b], scale=rstd.t[:, t_:t_ + 1])
            for half in range(2):
                pb_ = bank()
                for q4 in range(4):
                    fc = half * 4 + q4
                    tr(pb_, pb_.t[:, q4 * 128:(q4 + 1) * 128], xs.t[:, fc * 128:(fc + 1) * 128], ident.t[:], [xs.b, ident.b])
                for q4 in range(4):
                    fc = half * 4 + q4
                    act(dst_ap_fn(fc), pb_.t[:, q4 * 128:(q4 + 1) * 128], AF.Identity, [pb_.b, Acls.b, modT.b], dst_bufs,
                        scale=Acls.t[:, cls, fc:fc + 1], bias=modT.t[:, shslot * 8 + fc, cls:cls + 1])
                    if f32_dst is not None:
                        act(f32_dst.t[:, fc, :], pb_.t[:, q4 * 128:(q4 + 1) * 128], AF.Identity, [pb_.b, Acls.b, modT.b],
                            [f32_dst.b], scale=Acls.t[:, cls, fc:fc + 1], bias=modT.t[:, shslot * 8 + fc, cls:cls + 1])

        def make_gtrow(dst, slot):
            for cls in range(2):
                for fc in range(8):
                    ts("dve", dg.t[:], ident.t[:], modT.t[:, slot * 8 + fc, cls:cls + 1], None, ALU.mult, None,
                       [ident.b, modT.b], [dg.b])
                    pb_ = bank()
                    mm(pb_, pb_.t[:, 0:128], onesf.t[:], dg.t[:], [onesf.b, dg.b])
                    cp("dve", dst.t[:, cls, fc * 128:(fc + 1) * 128], pb_.t[:, 0:128], [pb_.b], [dst.b])

        def blk_of(t_):
            return t_ + 1 if t_ < NT_OWN else 18 + (t_ - NT_OWN)

        DL = dbg.get("_layer", 0)
        stopped = False
        for l in range(nlayers):
            if dbg.get("_stop") == "0":
                break
            W = L[l]
            WB = WBF[l]
            ctx_out = l < nlayers - 1
            with ExitStack() as ph:
                wm = sbuf(ph, "wm", [128, 2, 8, 512], F32, 2)
                dma("sp", bmodT.t[:], W["bmodT"].t[:, :], [], [bmodT.b])
                dma("sp", gT.t[:, 0:8], W["g1T"].t[:, :], [], [gT.b])
                dma("sp", gT.t[:, 8:16], W["g2T"].t[:, :], [], [gT.b])
                mps = bank()
                for piece in range(12):
                    s = piece % 2
                    dma("sp", wm.t[:, s, :, :], W["wmod"].t[:, :, piece * 512:(piece + 1) * 512], [W["wmod"].b], [wm.bs[s]])
                    for c4 in range(4):
                        cc = piece * 4 + c4
                        for kc in range(8):
                            mm(mps, mps.t[:, cc * 2:cc * 2 + 2], wm.t[:, s, kc, c4 * 128:(c4 + 1) * 128],
                               siluT.t[:, kc, :], [wm.bs[s], siluT.b], kc == 0, kc == 7)
                for cls in range(2):
                    tt("dve", modT.t[:, :, cls], mps.t[:, cls:96:2], bmodT.t[:], ALU.add, [mps.b, bmodT.b], [modT.b])
                for cls in range(2):
                    stt("dve", A1.t[:, cls, :], modT.t[:, 8:16, cls], 1.0, gT.t[:, 0:8], ALU.add, ALU.mult,
                        [modT.b, gT.b], [A1.b])
                    stt("dve", A2.t[:, cls, :], modT.t[:, 32:40, cls], 1.0, gT.t[:, 8:16], ALU.add, ALU.mult,
                        [modT.b, gT.b], [A2.b])
                if l == DL:
                    dump("modT", modT, modT.t[:], lambda o: o[:, :, :])
                P.barrier()

            kvst = ExitStack()
            kT = sbuf(kvst, "kT", [128, 20, 128], BF16, 20)
            vv = sbuf(kvst, "vv", [128, 20, 128], BF16, 20)
            with ExitStack() as ph:
                w1 = sbuf(ph, "w1", [128, 8, 1920], BF16)
                wus = sbuf(ph, "wus", [128, 8, 128], BF16)
                hT = sbuf(ph, "hT", [128, 8, 512], BF16)
                ust = sbuf(ph, "ust", [128, 2, 512], BF16, 2)
                tmpa = sbuf(ph, "tmpa", [128, 512])
                tmpb = sbuf(ph, "tmpb", [128, 512])
                cosT = sbuf(ph, "cosT", [128, TALL])
                sinT = sbuf(ph, "sinT", [128, TALL])
                dma("sp", cosT.t[:], cos_d.t[:, :], [cos_d.b], [cosT.b])
                dma("sp", sinT.t[:], sin_d.t[:, :], [sin_d.b], [sinT.b])
                dma("sp", w1.t[:, :, :], WB["w1"].t[:, :, :], [WB["w1"].b], [w1.b])
                dma("pool", wus.t[:], W["wus"].t[:, :, :], [], [wus.b])
                compute_rstd(NT)
                ust_rr = 0
                for ch in range(5):
                    tiles = list(range(ch * 4, min(ch * 4 + 4, NT)))
                    n = len(tiles) * 128
                    c0 = ch * 512
                    for ti, t_ in enumerate(tiles):
                        norm_tile(t_, A1, 0, lambda fc, ti=ti: hT.t[:, fc, ti * 128:(ti + 1) * 128], [hT.b])
                    pk = bank()
                    pkr = bank()
                    for kc in range(8):
                        mm(pk, pk.t[:, 0:n], w1.t[:, kc, 1024:1152], hT.t[:, kc, 0:n], [w1.b, hT.b], kc == 0, kc == 7)
                    for kc in range(8):
                        mm(pkr, pkr.t[:, 0:n], w1.t[:, kc, 1152:1280], hT.t[:, kc, 0:n], [w1.b, hT.b], kc == 0, kc == 7)
                    tt("dve", tmpa.t[:, 0:n], pk.t[:, 0:n], cosT.t[:, c0:c0 + n], ALU.mult, [pk.b, cosT.b], [tmpa.b])
                    tt("dve", tmpb.t[:, 0:n], pkr.t[:, 0:n], sinT.t[:, c0:c0 + n], ALU.mult, [pkr.b, sinT.b], [tmpb.b])
                    for ti, t_ in enumerate(tiles):
                        blk = blk_of(t_)
                        tt("pool", kT.t[:, blk, :], tmpa.t[:, ti * 128:(ti + 1) * 128], tmpb.t[:, ti * 128:(ti + 1) * 128],
                           ALU.add, [tmpa.b, tmpb.b], [kT.bs[blk]])
                    pv = bank()
                    for ti, t_ in enumerate(tiles):
                        for kc in range(8):
                            mm(pv, pv.t[:, ti * 128:(ti + 1) * 128], hT.t[:, kc, ti * 128:(ti + 1) * 128],
                               w1.t[:, kc, 1280:1408], [w1.b, hT.b], kc == 0, kc == 7)
                    for ti, t_ in enumerate(tiles):
                        blk = blk_of(t_)
                        act(vv.t[:, blk, :], pv.t[:, ti * 128:(ti + 1) * 128], AF.Identity, [pv.b], [vv.bs[blk]])
                    if ch < 4:
                        for ti, t_ in enumerate(tiles):
                            pu = bank()
                            for kc in range(8):
                                mm(pu, pu.t[:, :], hT.t[:, kc, ti * 128:(ti + 1) * 128], w1.t[:, kc, 1408:1920],
                                   [w1.b, hT.b], kc == 0, kc == 7)
                            s = ust_rr % 2
                            ust_rr += 1
                            act(ust.t[:, s, :], pu.t[:, :], AF.Identity, [pu.b], [ust.bs[s]])
                            pc_, lr_ = (t_ * 128) // 1024, (t_ * 128) % 1024
                            dma("sp", ag1_in[pc_].t[lr_:lr_ + 128, :], ust.t[:, s, :], [ust.bs[s]], [ag1_in[pc_].b])
                    else:
                        pu = bank()
                        for kc in range(8):
                            mm(pu, pu.t[:, 0:CTX], wus.t[:, kc, :], hT.t[:, kc, 0:CTX], [wus.b, hT.b], kc == 0, kc == 7)
                        act(ucT.t[:], pu.t[:, 0:CTX], AF.Identity, [pu.b], [ucT.b])
                for i4, (tl_, blk) in enumerate(((kT, 1), (kT, 16), (vv, 1), (vv, 16))):
                    dma("sp", ag1_in[2].t[:, i4 * 128:(i4 + 1) * 128], tl_.t[:, blk, :], [tl_.bs[blk]], [ag1_in[2].b])
                for pc_ in range(3):
                    P.dma("pool", lambda e, pc_=pc_: e.collective_compute(
                        "AllGather", ALU.bypass, replica_groups=GROUPS, ins=[ag1_in[pc_].t.opt()], outs=[ag1_out[pc_].t.opt()]),
                        [ag1_in[pc_].b], [ag1_out[pc_].b], inc=1)
                P.barrier()
            if dbg.get("_stop") == "B" and l == DL:
                stopped = True

            if not stopped:
              with ExitStack() as ph:
                uTf = sbuf(ph, "uTf", [128, STREAM], BF16, 17)
                hal = sbuf(ph, "hal", [128, 4, 512], BF16)
                utile = sbuf(ph, "utile", [128, 2, 512], BF16, 2)
                selm = sbuf(ph, "selm", [128, 12, 128], BF16)
                prm = sbuf(ph, "prm", [128, 16, 8])
                bb = sbuf(ph, "bb", [128, 4, 8, 16])
                cc_ = sbuf(ph, "cc", [128, 2, 8, 16])
                Wsc = sbuf(ph, "Wsc", [128, 128])
                lB = sbuf(ph, "lB", [128, 16, 128], BF16)
                lC = sbuf(ph, "lC", [128, 16, 128], BF16)
                dsk = sbuf(ph, "dsk", [128, 1])
                tabc = sbuf(ph, "tabc", [128, 4, 512], F32, 4)
                tabs = sbuf(ph, "tabs", [128, 4, 512], F32, 4)
                rhoT = sbuf(ph, "rhoT", [128, 4, 512], F32, 4)
                kis = sbuf(ph, "kis", [128, 512], I32)
                NW = 2
                wk = [[sbuf(ph, f"wk{s}_{i}", [128, 512]) for i in range(6)] for s in range(NW)]
                xrb = [[sbuf(ph, f"xb{s}_{i}", [128, 512], BF16) for i in range(2)] for s in range(NW)]
                kfs, angs = wk[0][0], wk[0][1]
                ub = sbuf(ph, "ub", [128, 2, 512], BF16, 2)
                init = sbuf(ph, "init", [128, 4, 2], F32, 4)
                ytmp = sbuf(ph, "ytmp", [128, 3, 512], F32, 3)
                yfs = sbuf(ph, "yfs", [128, 2, 512], F32, 2)
                zst = sbuf(ph, "zst", [128, 2, 512], BF16, 2)
                dma("pool", selm.t[:], selmat_d.t[:, :, :], [], [selm.b])
                for i in range(4):
                    dma("sp", hal.t[:, i, :], ag1_out[2].t[i * 128:(i + 1) * 128, :], [ag1_out[2].b], [hal.b])
                ph_ = bank()
                for i4, (selbase, col) in enumerate(((4, 128), (8, 0), (4, 384), (8, 256))):
                    for i in range(4):
                        mm(ph_, ph_.t[:, i4 * 128:(i4 + 1) * 128], selm.t[:, selbase + i, :], hal.t[:, i, col:col + 128],
                           [selm.b, hal.b], i == 0, i == 3)
                for i4, (tl_, blk) in enumerate(((kT, 0), (kT, 17), (vv, 0), (vv, 17))):
                    cp("dve", tl_.t[:, blk, :], ph_.t[:, i4 * 128:(i4 + 1) * 128], [ph_.b], [tl_.bs[blk]])
                cp("dve", uTf.t[:, 0:CTX], ucT.t[:], [ucT.b], [uTf.bs[0]])
                ut_rr = 0
                for i in range(4):
                    for tq in range(4):
                        pu = bank()
                        for t4 in range(4):
                            s = ut_rr % 2
                            ut_rr += 1
                            tr_ = (tq * 4 + t4) * 128
                            pc_, r0 = tr_ // 1024, i * 1024 + tr_ % 1024
                            dma("sp", utile.t[:, s, :], ag1_out[pc_].t[r0:r0 + 128, :], [ag1_out[pc_].b], [utile.bs[s]])
                            for sl in range(4):
                                mm(pu, pu.t[:, t4 * 128:(t4 + 1) * 128], utile.t[:, s, sl * 128:(sl + 1) * 128],
                                   selm.t[:, sl, :], [utile.bs[s], selm.b], sl == 0, sl == 3)
                        nk = i * 4 + tq
                        act(uTf.t[:, CTX + nk * 512:CTX + (nk + 1) * 512], pu.t[:, :], AF.Identity, [pu.b], [uTf.bs[1 + nk]])
                for nm, c_ in (("lamre", 0), ("lamim", 1), ("ldt", 2)):
                    dma("sp", prm.t[:, c_, :], W[nm].t[:, :], [], [prm.b])
                dma("sp", bb.t[:, 0, :, :], W["bre"].t[:, :, :], [], [bb.b])
                dma("sp", bb.t[:, 1, :, :], W["bim"].t[:, :, :], [], [bb.b])
                dma("sp", cc_.t[:, 0, :, :], W["cre"].t[:, :, :], [], [cc_.b])
                dma("sp", cc_.t[:, 1, :, :], W["cim"].t[:, :, :], [], [cc_.b])
                dma("sp", dsk.t[:], W["dsk"].t[:, :], [], [dsk.b])
                pr = lambda c_: prm.t[:, c_, :]
                PB = [prm.b]
                act(pr(3), pr(2), AF.Exp, PB, PB)
                tt("dve", pr(4), pr(0), pr(3), ALU.mult, PB, PB)
                tt("dve", pr(5), pr(1), pr(3), ALU.mult, PB, PB)
                act(pr(6), pr(4), AF.Exp, PB, PB)
                range_reduce(kfs, kfs.t[:, 0:8], kis, kis.t[:, 0:8], angs.t[:, 0:8], pr(5), 0.0, PB, [angs.b])
                act(pr(7), angs.t[:, 0:8], AF.Sin, [angs.b], PB)
                range_reduce(kfs, kfs.t[:, 0:8], kis, kis.t[:, 0:8], angs.t[:, 0:8], pr(5), PI / 2, PB, [angs.b])
                act(pr(8), angs.t[:, 0:8], AF.Sin, [angs.b], PB)
                tt("dve", pr(9), pr(6), pr(8), ALU.mult, PB, PB)
                tt("dve", pr(10), pr(6), pr(7), ALU.mult, PB, PB)
                tt("dve", pr(11), pr(0), pr(0), ALU.mult, PB, PB)
                tt("dve", pr(15), pr(1), pr(1), ALU.mult, PB, PB)
                tt("dve", pr(11), pr(11), pr(15), ALU.add, PB, PB)
                P.op("dve", lambda e: e.reciprocal(pr(11), pr(11)), PB, PB)
                ts("dve", pr(12), pr(9), -1.0, None, ALU.add, None, PB, PB)
                tt("dve", pr(13), pr(12), pr(0), ALU.mult, PB, PB)
                tt("dve", pr(15), pr(10), pr(1), ALU.mult, PB, PB)
                tt("dve", pr(13), pr(13), pr(15), ALU.add, PB, PB)
                tt("dve", pr(13), pr(13), pr(11), ALU.mult, PB, PB)
                tt("dve", pr(14), pr(10), pr(0), ALU.mult, PB, PB)
                tt("dve", pr(15), pr(12), pr(1), ALU.mult, PB, PB)
                tt("dve", pr(14), pr(14), pr(15), ALU.subtract, PB, PB)
                tt("dve", pr(14), pr(14), pr(11), ALU.mult, PB, PB)
                if l == DL:
                    dump("prm", prm, prm.t[:], lambda o: o[:, :, :])
                for r in range(8):
                    fre = prm.t[:, 13, r:r + 1]
                    fim = prm.t[:, 14, r:r + 1]
                    ts("dve", bb.t[:, 2, r, :], bb.t[:, 0, r, :], fre, None, ALU.mult, None, [bb.b, prm.b], [bb.b])
                    ts("dve", bb.t[:, 3, r, :], bb.t[:, 1, r, :], fim, None, ALU.mult, None, [bb.b, prm.b], [bb.b])
                    tt("dve", bb.t[:, 2, r, :], bb.t[:, 2, r, :], bb.t[:, 3, r, :], ALU.subtract, [bb.b], [bb.b])
                    ts("dve", bb.t[:, 3, r, :], bb.t[:, 1, r, :], fre, None, ALU.mult, None, [bb.b, prm.b], [bb.b])
                    stt("dve", bb.t[:, 3, r, :], bb.t[:, 0, r, :], fim, bb.t[:, 3, r, :], ALU.mult, ALU.add,
                        [bb.b, prm.b], [bb.b])
                    gp = r % 4
                    for ri in range(2):
                        P.op("pool", lambda e: e.memset(Wsc.t[:], 0.0), [], [Wsc.b])
                        for g2 in range(2):
                            c0_ = 16 * (2 * gp + g2)
                            cp("dve", Wsc.t[g2 * 64:(g2 + 1) * 64, c0_:c0_ + 16], bb.t[g2 * 64:(g2 + 1) * 64, 2 + ri, r, :],
                               [bb.b], [Wsc.b])
                        pb_ = bank()
                        tr(pb_, pb_.t[:, 0:128], Wsc.t[:], ident.t[:], [Wsc.b, ident.b])
                        act(lB.t[:, r * 2 + ri, :], pb_.t[:, 0:128], AF.Identity, [pb_.b], [lB.b])
                        P.op("pool", lambda e, r=r, ri=ri: e.memset(lC.t[:, r * 2 + ri, :], 0.0), [], [lC.b])
                        for g2 in range(2):
                            c0_ = 16 * (2 * gp + g2)
                            ts("dve", lC.t[g2 * 64:(g2 + 1) * 64, r * 2 + ri, c0_:c0_ + 16],
                               cc_.t[g2 * 64:(g2 + 1) * 64, ri, r, :], (1.0 if ri == 0 else -1.0), None, ALU.mult, None,
                               [cc_.b], [lC.b])
                chunks = [(0, CTX)] + [(CTX + 512 * k, 512) for k in range(16)]
                wk_rr = 0
                for d_ in range(2):
                    for gp in range(4):
                        r = d_ * 4 + gp
                        ts("dve", angs.t[:], iota.t[:], prm.t[:, 5, r:r + 1], None, ALU.mult, None, [iota.b, prm.b], [angs.b])
                        range_reduce(kfs, kfs.t[:], kis, kis.t[:], tabs.t[:, gp, :], angs.t[:], 0.0, [angs.b], [tabs.bs[gp]])
                        act(tabs.t[:, gp, :], tabs.t[:, gp, :], AF.Sin, [tabs.bs[gp]], [tabs.bs[gp]])
                        range_reduce(kfs, kfs.t[:], kis, kis.t[:], tabc.t[:, gp, :], angs.t[:], PI / 2, [angs.b], [tabc.bs[gp]])
                        act(tabc.t[:, gp, :], tabc.t[:, gp, :], AF.Sin, [tabc.bs[gp]], [tabc.bs[gp]])
                        ts("dve", rhoT.t[:, gp, :], zeros.t[:], prm.t[:, 6, r:r + 1], None, ALU.add, None, [zeros.b, prm.b],
                           [rhoT.bs[gp]])
                        P.op("pool", lambda e, gp=gp: e.memset(init.t[:, gp, :], 0.0), [], [init.bs[gp]])
                    for ci, (c0, n) in enumerate(chunks):
                        if d_ == 0:
                            nat_c0, slot = c0, ci
                            u_ap = uTf.t[:, c0:c0 + n]
                            u_reads = [uTf.bs[ci]]
                        else:
                            if ci == 0:
                                nat_c0, slot = 0, 0
                            else:
                                nkk = 16 - ci
                                nat_c0, slot = CTX + 512 * nkk, 1 + nkk
                            s_u = ci % 2
                            cp("dve", ub.t[:, s_u, 0:n], AP_rev(uTf, STREAM, nat_c0 + n - 1, n), [uTf.bs[slot]], [ub.bs[s_u]])
                            u_ap = ub.t[:, s_u, 0:n]
                            u_reads = [ub.bs[s_u]]
                        need = ctx_out or ci > 0
                        yps = bank()
                        excl = (PS.index(yps),)
                        for gp in range(4):
                            r = d_ * 4 + gp
                            ws = wk[wk_rr % NW]
                            xb_ = xrb[wk_rr % NW]
                            wk_rr += 1
                            pre = bank(exclude=excl)
                            pim = bank(exclude=excl)
                            mm(pre, pre.t[:, 0:n], lB.t[:, r * 2, :], u_ap, [lB.b] + u_reads)
                            mm(pim, pim.t[:, 0:n], lB.t[:, r * 2 + 1, :], u_ap, [lB.b] + u_reads)
                            c_ap = tabc.t[:, gp, 0:n]
                            s_ap = tabs.t[:, gp, 0:n]
                            TB = [tabc.bs[gp], tabs.bs[gp]]
                            t1, t2, t3, t4, zr, zi = [w_.t[:, 0:n] for w_ in ws]
                            b1, b2, b3, b4, bzr, bzi = [w_.b for w_ in ws]
                            tt("dve", t1, pre.t[:, 0:n], c_ap, ALU.mult, [pre.b] + TB, [b1])
                            tt("dve", t2, pim.t[:, 0:n], s_ap, ALU.mult, [pim.b] + TB, [b2])
                            tt("pool", t1, t1, t2, ALU.add, [b1, b2], [b1])
                            tt("dve", t3, pim.t[:, 0:n], c_ap, ALU.mult, [pim.b] + TB, [b3])
                            tt("dve", t4, pre.t[:, 0:n], s_ap, ALU.mult, [pre.b] + TB, [b4])
                            tt("pool", t3, t3, t4, ALU.subtract, [b3, b4], [b3])
                            P.op("dve", lambda e, zr=zr, t1=t1, gp=gp, n=n: e.tensor_tensor_scan(
                                zr, rhoT.t[:, gp, 0:n], t1, init.t[:, gp, 0:1], ALU.mult, ALU.add),
                                [rhoT.bs[gp], b1, init.bs[gp]], [bzr])
                            P.op("dve", lambda e, zi=zi, t3=t3, gp=gp, n=n: e.tensor_tensor_scan(
                                zi, rhoT.t[:, gp, 0:n], t3, init.t[:, gp, 1:2], ALU.mult, ALU.add),
                                [rhoT.bs[gp], b3, init.bs[gp]], [bzi])
                            tt("dve", t1, zr, c_ap, ALU.mult, [bzr] + TB, [b1])
                            tt("dve", t2, zi, s_ap, ALU.mult, [bzi] + TB, [b2])
                            tt("dve", t3, zr, s_ap, ALU.mult, [bzr] + TB, [b3])
                            tt("dve", t4, zi, c_ap, ALU.mult, [bzi] + TB, [b4])
                            tt("pool", xb_[0].t[:, 0:n], t1, t2, ALU.subtract, [b1, b2], [xb_[0].b])
                            tt("dve", xb_[1].t[:, 0:n], t3, t4, ALU.add, [b3, b4], [xb_[1].b])
                            tt("dve", init.t[:, gp, 0:1], ws[0].t[:, n - 1:n], ws[1].t[:, n - 1:n], ALU.subtract, [b1, b2],
                               [init.bs[gp]])
                            tt("dve", init.t[:, gp, 1:2], ws[2].t[:, n - 1:n], ws[3].t[:, n - 1:n], ALU.add, [b3, b4],
                               [init.bs[gp]])
                            if need:
                                mm(yps, yps.t[:, 0:n], lC.t[:, r * 2, :], xb_[0].t[:, 0:n], [lC.b, xb_[0].b], gp == 0, False)
                                mm(yps, yps.t[:, 0:n], lC.t[:, r * 2 + 1, :], xb_[1].t[:, 0:n], [lC.b, xb_[1].b], False, gp == 3)
                        if not need:
                            continue
                        if d_ == 0:
                            fs_ = ci % 2
                            act(yfs.t[:, fs_, 0:n], yps.t[:, 0:n], AF.Identity, [yps.b], [yfs.bs[fs_]])
                            dma("sp", yTf_d.t[:, c0:c0 + n], yfs.t[:, fs_, 0:n], [yfs.bs[fs_]], [yTf_d.b])
                        else:
                            ys = ci % 3
                            zs = ci % 2
                            ya = ytmp.t[:, ys, 0:n]
                            YB = [ytmp.bs[ys]]
                            yb2 = ytmp.t[:, (ys + 1) % 3, 0:n]
                            YB2 = [ytmp.bs[(ys + 1) % 3]]
                            fs_ = ci % 2
                            dma("sp", yfs.t[:, fs_, 0:n], yTf_d.t[:, nat_c0:nat_c0 + n], [yTf_d.b], [yfs.bs[fs_]])
                            act(ya, yps.t[:, 0:n], AF.Identity, [yps.b], YB)
                            tt("dve", yb2, AP_rev(ytmp, 3 * 512, ys * 512 + n - 1, n), yfs.t[:, fs_, 0:n], ALU.add,
                               YB + [yfs.bs[fs_]], YB2)
                            stt("dve", yb2, uTf.t[:, nat_c0:nat_c0 + n], dsk.t[:, 0:1], yb2, ALU.mult, ALU.add,
                                [uTf.bs[slot], dsk.b] + YB2, YB2)
                            if l == DL and ci in (0, 16):
                                cc0 = 0 if ci == 0 else 256
                                dump("ssmy", YB2, yb2, lambda o, cc0=cc0, n=n: o[:, cc0:cc0 + n])
                            act(ya, yb2, AF.Square, YB2, YB)
                            ts("dve", ya, ya, 0.044715, 1.0, ALU.mult, ALU.add, YB, YB)
                            tt("pool", ya, ya, yb2, ALU.mult, YB + YB2, YB)
                            act(ya, ya, AF.Sigmoid, YB, YB, scale=1.5957691216057308)
                            tt("pool", zst.t[:, zs, 0:n], ya, yb2, ALU.mult, YB + YB2, [zst.bs[zs]])
                            if ci == 0:
                                pc_, dcol = 2, 0
                            else:
                                pc_, dcol = (nat_c0 - CTX) // 4096, (nat_c0 - CTX) % 4096
                            dma("sp", ag2_in[pc_].t[:, dcol:dcol + n], zst.t[:, zs, 0:n], [zst.bs[zs]], [ag2_in[pc_].b])
                for pc_ in range(3 if ctx_out else 2):
                    P.dma("pool", lambda e, pc_=pc_: e.collective_compute(
                        "AllGather", ALU.bypass, replica_groups=GROUPS, ins=[ag2_in[pc_].t.opt()], outs=[ag2_out[pc_].t.opt()]),
                        [ag2_in[pc_].b], [ag2_out[pc_].b], inc=1)
                P.barrier()
            if dbg.get("_stop") == "C" and l == DL:
                stopped = True

            if not stopped:
              with ExitStack() as ph:
                wq = sbuf(ph, "wq", [128, 8, 1024], BF16)
                hT = sbuf(ph, "hTd", [128, 8, 512], BF16)
                qT = sbuf(ph, "qT", [128, 4, 512], BF16)
                pt = sbuf(ph, "pt", [128, 3, 512], BF16, 3)
                sg = sbuf(ph, "sg", [128, 2, 512], F32, 2)
                ta = sbuf(ph, "ta", [128, 2, 512], F32, 2)
                rec = sbuf(ph, "rec", [128, 512])
                attnT = sbuf(ph, "attnT", [128, 2, 4, 512], BF16, 2)
                cosT = sbuf(ph, "cosT", [128, TALL])
                sinT = sbuf(ph, "sinT", [128, TALL])
                masks = sbuf(ph, "masks", [128, 4, 512], BF16)
                sinkE = sbuf(ph, "sinkE", [128, 8])
                sinkrow = sbuf(ph, "sinkrow", [128, 512])
                dma("sp", cosT.t[:], cos_d.t[:, :], [cos_d.b], [cosT.b])
                dma("sp", sinT.t[:], sin_d.t[:, :], [sin_d.b], [sinT.b])
                for k4 in range(4):
                    dma("pool", masks.t[:, k4, :], masks_d.t[:, k4, :], [], [masks.b])
                dma("sp", wq.t[:, :, :], WB["w1"].t[:, :, 0:1024], [WB["w1"].b], [wq.b])
                dma("sp", sinkE.t[:], W["sink"].t[:, :], [], [sinkE.b])
                act(sinkE.t[:], sinkE.t[:], AF.Exp, [sinkE.b], [sinkE.b])
                for c in range(4):
                    ts("dve", sinkrow.t[0:64, c * 128:(c + 1) * 128], zeros.t[0:64, 0:128], sinkE.t[0:64, c:c + 1], None,
                       ALU.add, None, [zeros.b, sinkE.b], [sinkrow.b])
                    ts("dve", sinkrow.t[64:128, c * 128:(c + 1) * 128], zeros.t[64:128, 0:128], sinkE.t[64:128, 4 + c:5 + c],
                       None, ALU.add, None, [zeros.b, sinkE.b], [sinkrow.b])
                nchunks = 5 if ctx_out else 4
                pt_rr = 0
                sg_rr = 0
                for ch in range(nchunks):
                    tiles = list(range(ch * 4, min(ch * 4 + 4, NT)))
                    n = len(tiles) * 128
                    c0 = ch * 512
                    is_ctx = ch == 4
                    for ti, t_ in enumerate(tiles):
                        norm_tile(t_, A1, 0, lambda fc, ti=ti: hT.t[:, fc, ti * 128:(ti + 1) * 128], [hT.b])
                    for c in range(4):
                        pq = bank()
                        pqr = bank()
                        for kc in range(8):
                            mm(pq, pq.t[:, 0:n], wq.t[:, kc, c * 128:(c + 1) * 128], hT.t[:, kc, 0:n], [wq.b, hT.b], kc == 0, kc == 7)
                        for kc in range(8):
                            mm(pqr, pqr.t[:, 0:n], wq.t[:, kc, 512 + c * 128:512 + (c + 1) * 128], hT.t[:, kc, 0:n], [wq.b, hT.b],
                               kc == 0, kc == 7)
                        s_ = sg_rr % 2
                        sg_rr += 1
                        tt("dve", sg.t[:, s_, 0:n], pq.t[:, 0:n], cosT.t[:, c0:c0 + n], ALU.mult, [pq.b, cosT.b], [sg.bs[s_]])
                        tt("dve", ta.t[:, s_, 0:n], pqr.t[:, 0:n], sinT.t[:, c0:c0 + n], ALU.mult, [pqr.b, sinT.b], [ta.bs[s_]])
                        tt("pool", qT.t[:, c, 0:n], sg.t[:, s_, 0:n], ta.t[:, s_, 0:n], ALU.add, [sg.bs[s_], ta.bs[s_]], [qT.b])
                    as_ = ch % 2
                    for qi, t_ in enumerate(tiles):
                        pnum = bank()
                        pden = bank()
                        excl = (PS.index(pnum), PS.index(pden))
                        if is_ctx:
                            kbl = [(18, None), (19, None)]
                        else:
                            kbl = [(t_, 0 if t_ == 0 else 1), (t_ + 1, None), (t_ + 2, 2 if t_ == NT_OWN - 1 else 3),
                                   (18, None), (19, None)]
                        for gk in range(2):
                            pb = 64 * gk

                            def pv_(bi, kb, s_, pb=pb):
                                mm(pnum, pnum.t[pb:pb + 64, :], vv.t[:, kb, pb:pb + 64], pt.t[:, s_, :], [vv.bs[kb], pt.bs[s_]],
                                   bi == 0, bi == len(kbl) - 1)
                                mm(pden, pden.t[pb:pb + 64, :], onesb.t[:, 0:64], pt.t[:, s_, :], [onesb.b, pt.bs[s_]],
                                   bi == 0, bi == len(kbl) - 1)

                            prev_ = None
                            for bi, (kb, mk) in enumerate(kbl):
                                pst = bank(exclude=excl)
                                for c in range(4):
                                    mm(pst, pst.t[:, c * 128:(c + 1) * 128], kT.t[pb:pb + 64, kb, :],
                                       qT.t[pb:pb + 64, c, qi * 128:(qi + 1) * 128], [kT.bs[kb], qT.b])
                                s_ = pt_rr % 3
                                pt_rr += 1
                                act(pt.t[:, s_, :], pst.t[:, :], AF.Exp, [pst.b], [pt.bs[s_]], scale=0.125)
                                if mk is not None:
                                    tt("pool", pt.t[:, s_, :], pt.t[:, s_, :], masks.t[:, mk, :], ALU.mult, [pt.bs[s_], masks.b],
                                       [pt.bs[s_]])
                                if prev_ is not None:
                                    pv_(*prev_)
                                prev_ = (bi, kb, s_)
                            pv_(*prev_)
                        tt("dve", rec.t[:], pden.t[:, :], sinkrow.t[:], ALU.add, [pden.b, sinkrow.b], [rec.b])
                        P.op("dve", lambda e: e.reciprocal(rec.t[:], rec.t[:]), [rec.b], [rec.b])
                        for c in range(4):
                            tt("dve", attnT.t[:, as_, c, qi * 128:(qi + 1) * 128], pnum.t[:, c * 128:(c + 1) * 128],
                               rec.t[:, c * 128:(c + 1) * 128], ALU.mult, [pnum.b, rec.b], [attnT.bs[as_]])
                    dma("sp", attn_d.t[:, :, c0:c0 + n], attnT.t[:, as_, :, 0:n], [attnT.bs[as_]], [attn_d.b])
                P.barrier()
            kvst.close()
            if dbg.get("_stop") == "D1" and l == DL:
                stopped = True

            if not stopped:
              with ExitStack() as ph:
                wglu = sbuf(ph, "wglu", [128, 4, 512], BF16)
                wbra = sbuf(ph, "wbra", [128, 4, D], BF16)
                wbrs = sbuf(ph, "wbrs", [128, 4, D], BF16)
                wout = sbuf(ph, "wout", [128, 8, D], BF16)
                w2s = sbuf(ph, "w2s", [128, 2, 8, 256], BF16, 2)
                hT = sbuf(ph, "hTd2", [128, 8, 512], BF16)
                zstage = sbuf(ph, "zstage", [128, 4, 512], BF16)
                zsb = sbuf(ph, "zsb", [128, 4, 512], BF16)
                ssmT = sbuf(ph, "ssmT", [128, 4, 512], BF16)
                mT = sbuf(ph, "mT", [128, 8, 512], BF16)
                sg = sbuf(ph, "sg2", [128, 2, 512], F32, 2)
                ta = sbuf(ph, "ta2", [128, 2, 512], F32, 2)
                selm = sbuf(ph, "selm2", [128, 4, 128], BF16)
                gt1row = sbuf(ph, "gt1row", [128, 2, D])
                attc = sbuf(ph, "attc", [128, 4, 512], BF16)
                dma("pool", selm.t[:], selmat_d.t[:, 0:4, :], [], [selm.b])
                dma("sp", wout.t[:, :, :], WB["wout"].t[:, :, :], [WB["wout"].b], [wout.b])
                dma("sp", wglu.t[:, :, :], WB["wglu"].t[:, :, :], [WB["wglu"].b], [wglu.b])
                dma("sp", wbra.t[:, :, :], WB["wbra"].t[:, :, :], [WB["wbra"].b], [wbra.b])
                dma("sp", wbrs.t[:, :, :], WB["wbrs"].t[:, :, :], [WB["wbrs"].b], [wbrs.b])
                make_gtrow(gt1row, 2)
                nchunks = 5 if ctx_out else 4
                sg_rr = 0
                w2_rr = 0
                for ch in range(nchunks):
                    tiles = list(range(ch * 4, min(ch * 4 + 4, NT)))
                    n = len(tiles) * 128
                    c0 = ch * 512
                    is_ctx = ch == 4
                    cls = 1 if is_ctx else 0
                    for ti, t_ in enumerate(tiles):
                        norm_tile(t_, A1, 0, lambda fc, ti=ti: hT.t[:, fc, ti * 128:(ti + 1) * 128], [hT.b])
                    dma("sp", attc.t[:, :, 0:n], attn_d.t[:, :, c0:c0 + n], [attn_d.b], [attc.b])
                    for sl in range(4):
                        if is_ctx:
                            dma("sp", zsb.t[:, sl, 0:n], ag2_out[2].t[sl * 128:(sl + 1) * 128, :], [ag2_out[2].b], [zsb.b])
                        else:
                            for i in range(4):
                                gc_ = i * TOWN + c0
                                dma("sp", zstage.t[:, i, :],
                                    ag2_out[gc_ // 4096].t[sl * 128:(sl + 1) * 128, gc_ % 4096:gc_ % 4096 + 512],
                                    [ag2_out[gc_ // 4096].b], [zstage.b])
                            pz = bank()
                            for i in range(4):
                                mm(pz, pz.t[:, :], selm.t[:, i, :], zstage.t[:, i, :], [selm.b, zstage.b], i == 0, i == 3)
                            act(zsb.t[:, sl, :], pz.t[:, :], AF.Identity, [pz.b], [zsb.b])
                    for fc in range(4):
                        pg = bank()
                        for sl in range(4):
                            mm(pg, pg.t[:, 0:n], wglu.t[:, sl, fc * 128:(fc + 1) * 128], zsb.t[:, sl, 0:n], [wglu.b, zsb.b],
                               sl == 0, sl == 3)
                        s_ = sg_rr % 2
                        sg_rr += 1
                        act(sg.t[:, s_, 0:n], pg.t[:, 0:n], AF.Sigmoid, [pg.b], [sg.bs[s_]])
                        tt("dve", ssmT.t[:, fc, 0:n], zsb.t[:, fc, 0:n], sg.t[:, s_, 0:n], ALU.mult, [zsb.b, sg.bs[s_]], [ssmT.b])
                    if l == DL and ch == 0 and "ssmT" in dbg_out:
                        sdb = sbuf(ph, "sdb", [128, 4, 512])
                        cp("dve", sdb.t[:], ssmT.t[:], [ssmT.b], [sdb.b])
                        dump("ssmT", sdb, sdb.t[:], lambda o: o[:, :, :])
                    for fc in range(8):
                        ws_ = w2_rr % 2
                        w2_rr += 1
                        dma("sp", w2s.t[:, ws_, :, :], WB["w2g"].t[fc, :, :, :], [WB["w2g"].b], [w2s.bs[ws_]])
                        pA = bank()
                        pS = bank()
                        pga = bank()
                        pgs = bank()
                        fs = slice(fc * 128, (fc + 1) * 128)
                        for c in range(4):
                            mm(pA, pA.t[:, 0:n], wbra.t[:, c, fs], attc.t[:, c, 0:n], [wbra.b, attc.b], c == 0, c == 3)
                        for c in range(4):
                            mm(pS, pS.t[:, 0:n], wbrs.t[:, c, fs], ssmT.t[:, c, 0:n], [wbrs.b, ssmT.b], c == 0, c == 3)
                        for kc in range(8):
                            mm(pga, pga.t[:, 0:n], w2s.t[:, ws_, kc, 0:128], hT.t[:, kc, 0:n], [w2s.bs[ws_], hT.b], kc == 0, kc == 7)
                        for kc in range(8):
                            mm(pgs, pgs.t[:, 0:n], w2s.t[:, ws_, kc, 128:256], hT.t[:, kc, 0:n], [w2s.bs[ws_], hT.b], kc == 0, kc == 7)
                        act(sg.t[:, 0, 0:n], pga.t[:, 0:n], AF.Sigmoid, [pga.b], [sg.bs[0]])
                        act(sg.t[:, 1, 0:n], pgs.t[:, 0:n], AF.Sigmoid, [pgs.b], [sg.bs[1]])
                        tt("dve", ta.t[:, 0, 0:n], pA.t[:, 0:n], sg.t[:, 0, 0:n], ALU.mult, [pA.b, sg.bs[0]], [ta.bs[0]])
                        tt("dve", ta.t[:, 1, 0:n], pS.t[:, 0:n], sg.t[:, 1, 0:n], ALU.mult, [pS.b, sg.bs[1]], [ta.bs[1]])
                        tt("pool", mT.t[:, fc, 0:n], ta.t[:, 0, 0:n], ta.t[:, 1, 0:n], ALU.add, [ta.bs[0], ta.bs[1]], [mT.b])
                    for ti, t_ in enumerate(tiles):
                        for half in range(2):
                            py = bank()
                            hs_ = slice(half * 512, (half + 1) * 512)
                            for kc in range(8):
                                mm(py, py.t[:, :], mT.t[:, kc, ti * 128:(ti + 1) * 128], wout.t[:, kc, hs_], [mT.b, wout.b],
                                   kc == 0, kc == 7)
                            s_ = sg_rr % 2
                            sg_rr += 1
                            tt("dve", sg.t[:, s_, :], py.t[:, :], gt1row.t[:, cls, hs_], ALU.mult, [py.b, gt1row.b], [sg.bs[s_]])
                            tt("pool", x_tm.t[:, t_, hs_], x_tm.t[:, t_, hs_], sg.t[:, s_, :], ALU.add, [sg.bs[s_], x_tm.bs[t_]],
                               [x_tm.bs[t_]])
                P.barrier()
            if l == DL and not stopped:
                dump("xmix", x_tm.bs, x_tm.t[:], lambda o: o[:, :, :])
            if dbg.get("_stop") == "D2" and l == DL:
                stopped = True
            if stopped:
                break

            if not stopped:
              with ExitStack() as ph:
                ntl = NT if ctx_out else NT_OWN
                h2T = sbuf(ph, "h2T", [128, 8, TALL], BF16, NT)
                h2f = sbuf(ph, "h2f", [128, 8, 128])
                wr = sbuf(ph, "wr", [128, 8, 36])
                Wt = sbuf(ph, "Wt", [128, NT, 32], F32, NT)
                lg = sbuf(ph, "lg", [128, 36])
                rs = sbuf(ph, "rs", [128, 16])
                rt = sbuf(ph, "rt", [128, 4, 32])
                weg = sbuf(ph, "weg", [128, 2, 8, 512], BF16, 2)
                weu = sbuf(ph, "weu", [128, 2, 8, 512], BF16, 2)
                wed = sbuf(ph, "wed", [128, 2, 4, D], BF16, 2)
                hid = sbuf(ph, "hid", [128, 2, 4, 512], BF16, 2)
                sgm = sbuf(ph, "sgm", [128, 2, 512], F32, 2)
                ty = sbuf(ph, "ty", [128, 2, 512], F32, 2)
                gt2row = sbuf(ph, "gt2row", [128, 2, D])
                dma("sp", wr.t[:], W["wr"].t[:, :, :], [], [wr.b])
                make_gtrow(gt2row, 5)
                compute_rstd(ntl)
                RB = [rs.b]
                c_ = lambda i: rs.t[:, i:i + 1]
                for t_ in range(ntl):
                    norm_tile(t_, A2, 3, lambda fc, t_=t_: h2T.t[:, fc, t_ * 128:(t_ + 1) * 128], [h2T.bs[t_]], f32_dst=h2f)
                    pl = bank()
                    for kc in range(8):
                        mm(pl, pl.t[:, 0:36], h2f.t[:, kc, :], wr.t[:, kc, :], [h2f.b, wr.b], kc == 0, kc == 7)
                    cp("dve", lg.t[:], pl.t[:, 0:36], [pl.b], [lg.b])
                    P.op("dve", lambda e: e.tensor_reduce(rs.t[:, 0:1], lg.t[:, 0:4], AX.X, ALU.max), [lg.b], RB)
                    ts("dve", c_(1), c_(0), -1.0, None, ALU.mult, None, RB, RB)
                    act(rt.t[:, 0, 0:4], lg.t[:, 0:4], AF.Exp, [lg.b] + RB, [rt.b, rs.b], bias=rs.t[:, 1:2], accum=rs.t[:, 2:3])
                    P.op("dve", lambda e: e.reciprocal(rs.t[:, 3:4], rs.t[:, 2:3]), RB, RB)
                    ts("dve", rt.t[:, 0, 4:8], lg.t[:, 0:4], rs.t[:, 0:1], None, ALU.is_equal, None, [lg.b] + RB, [rt.b])
                    ts("dve", rt.t[:, 0, 4:8], rt.t[:, 0, 4:8], -1.0, 1e30, ALU.add, ALU.mult, [rt.b], [rt.b])
                    for g in range(4):
                        ts("dve", rt.t[:, 1, 8 * g:8 * g + 8], lg.t[:, 4 + 8 * g:12 + 8 * g], rt.t[:, 0, 4 + g:5 + g], None,
                           ALU.add, None, [lg.b, rt.b], [rt.b])
                    P.op("dve", lambda e: e.tensor_reduce(rs.t[:, 4:5], rt.t[:, 1, :], AX.X, ALU.max), [rt.b], RB)
                    ts("dve", rt.t[:, 2, :], rt.t[:, 1, :], rs.t[:, 4:5], None, ALU.is_equal, None, [rt.b] + RB, [rt.b])
                    stt("dve", rt.t[:, 2, :], rt.t[:, 2, :], -1e30, rt.t[:, 1, :], ALU.mult, ALU.add, [rt.b], [rt.b])
                    P.op("dve", lambda e: e.tensor_reduce(rs.t[:, 5:6], rt.t[:, 2, :], AX.X, ALU.max), [rt.b], RB)
                    ts("dve", rt.t[:, 2, :], rt.t[:, 1, :], rs.t[:, 5:6], None, ALU.is_ge, None, [rt.b] + RB, [rt.b])
                    ts("dve", c_(6), c_(4), -1.0, None, ALU.mult, None, RB, RB)
                    act(rt.t[:, 3, :], rt.t[:, 1, :], AF.Exp, [rt.b] + RB, [rt.b], bias=rs.t[:, 6:7])
                    tt("dve", rt.t[:, 3, :], rt.t[:, 3, :], rt.t[:, 2, :], ALU.mult, [rt.b], [rt.b])
                    P.op("dve", lambda e: e.tensor_reduce(rs.t[:, 7:8], rt.t[:, 3, :], AX.X, ALU.add), [rt.b], RB)
                    P.op("dve", lambda e: e.reciprocal(rs.t[:, 7:8], rs.t[:, 7:8]), RB, RB)
                    tt("dve", c_(7), c_(7), c_(3), ALU.mult, RB, RB)
                    ts("dve", Wt.t[:, t_, :], rt.t[:, 3, :], rs.t[:, 7:8], None, ALU.mult, None, [rt.b] + RB, [Wt.bs[t_]])
                if l == DL:
                    dump("Wt", Wt.bs, Wt.t[:], lambda o: o[:, :, :])
                nch = 5 if ctx_out else 4
                ty_rr = 0
                nexp = dbg.get("_nexp", NEXP)
                for e_ in range(nexp):
                    s = e_ % 2
                    for k2 in range(2):
                        dma("pool", weg.t[:, s, 4 * k2:4 * k2 + 4, :], W["weg"].t[e_, :, 4 * k2:4 * k2 + 4, :], [W["weg"].b], [weg.bs[s]])
                        dma("pool", weu.t[:, s, 4 * k2:4 * k2 + 4, :], W["weu"].t[e_, :, 4 * k2:4 * k2 + 4, :], [W["weu"].b], [weu.bs[s]])
                        dma("pool", wed.t[:, s, 2 * k2:2 * k2 + 2, :], W["wed"].t[e_, :, 2 * k2:2 * k2 + 2, :], [W["wed"].b], [wed.bs[s]])
                    for ch in range(nch):
                        tiles = list(range(ch * 4, min(ch * 4 + 4, NT)))
                        n = len(tiles) * 128
                        c0 = ch * 512
                        cls = 1 if ch == 4 else 0
                        hs = ch % 2
                        for hc in range(4):
                            pg = bank()
                            pu = bank()
                            hrd = [h2T.bs[t_] for t_ in tiles]
                            for kc in range(8):
                                mm(pg, pg.t[:, 0:n], weg.t[:, s, kc, hc * 128:(hc + 1) * 128], h2T.t[:, kc, c0:c0 + n],
                                   [weg.bs[s]] + hrd, kc == 0, kc == 7)
                            for kc in range(8):
                                mm(pu, pu.t[:, 0:n], weu.t[:, s, kc, hc * 128:(hc + 1) * 128], h2T.t[:, kc, c0:c0 + n],
                                   [weu.bs[s]] + hrd, kc == 0, kc == 7)
                            ss_ = hc % 2
                            act(sgm.t[:, ss_, 0:n], pg.t[:, 0:n], AF.Silu, [pg.b], [sgm.bs[ss_]])
                            tt("dve", hid.t[:, hs, hc, 0:n], pu.t[:, 0:n], sgm.t[:, ss_, 0:n], ALU.mult, [pu.b, sgm.bs[ss_]],
                               [hid.bs[hs]])
                        for ti, t_ in enumerate(tiles):
                            for half in range(2):
                                py = bank()
                                hs_ = slice(half * 512, (half + 1) * 512)
                                for hc in range(4):
                                    mm(py, py.t[:, :], hid.t[:, hs, hc, ti * 128:(ti + 1) * 128], wed.t[:, s, hc, hs_],
                                       [hid.bs[hs], wed.bs[s]], hc == 0, hc == 3)
                                y_ = ty_rr % 2
                                ty_rr += 1
                                tt("dve", ty.t[:, y_, :], py.t[:, :], gt2row.t[:, cls, hs_], ALU.mult, [py.b, gt2row.b],
                                   [ty.bs[y_]])
                                stt("dve", x_tm.t[:, t_, hs_], ty.t[:, y_, :], Wt.t[:, t_, e_:e_ + 1], x_tm.t[:, t_, hs_],
                                    ALU.mult, ALU.add, [ty.bs[y_], Wt.bs[t_], x_tm.bs[t_]], [x_tm.bs[t_]])
                P.barrier()
            if l == DL and not stopped:
                dump("xout", x_tm.bs, x_tm.t[:], lambda o: o[:, :, :])

        with ExitStack() as ph:
            gfin = sbuf(ph, "gfin", [128, D])
            ob = sbuf(ph, "ob", [128, 2, D], F32, 2)
            dma("sp", gfin.t[:], gfin_d.t[:, :], [], [gfin.b])
            compute_rstd(NT_OWN)
            for t_ in range(NT_OWN):
                s = t_ % 2
                stt("dve", ob.t[:, s, :], x_tm.t[:, t_, :], rstd.t[:, t_:t_ + 1], gfin.t[:], ALU.mult, ALU.mult,
                    [x_tm.bs[t_], rstd.b, gfin.b], [ob.bs[s]])
                dma("sp", out_d.t[t_ * 128:(t_ + 1) * 128, :], ob.t[:, s, :], [ob.bs[s]], [out_d.b])
        P.wait_all("sp", [out_d.b] + [v.b for v in dbg_out.values()])
        P.emit()
    return nc, names_in


def _kc(w):
    K, C = w.shape
    return np.ascontiguousarray(w.reshape(K // 128, 128, C).transpose(1, 0, 2))


def prep_inputs(inputs, nlayers=2, nei=NEXP):
    f = lambda a: np.ascontiguousarray(np.asarray(a, dtype=np.float32))
    I_ = {k: f(v) for k, v in inputs.items()}
    shared = {}
    e = np.arange(64)
    partner = np.where((e % 32) < 16, e + 16, e - 16)
    head_order = [h for c in range(4) for h in (c, c + 4)]
    qcols = np.concatenate([h * 64 + np.arange(64) for h in head_order])
    qrcols = np.concatenate([h * 64 + partner for h in head_order])
    kcols = 512 + np.arange(128)
    krcols = 512 + np.concatenate([hk * 64 + partner for hk in range(2)])
    vcols = 640 + np.arange(128)
    ucols = 768 + np.arange(512)
    w1cols = np.concatenate([qcols, qrcols, kcols, krcols, vcols, ucols])
    brarows = np.concatenate([h * 64 + np.arange(64) for h in head_order])
    shared["ident"] = np.eye(128, dtype=np.float32)
    shared["iota"] = np.ascontiguousarray(np.broadcast_to(np.arange(1, 513, dtype=np.float32), (128, 512)))
    shared["gfin"] = np.ascontiguousarray(np.broadcast_to(I_["g_final"], (128, D)))
    ee = np.arange(128) % 64
    ropef = np.stack([(ee % 16).astype(np.float32), np.where((ee % 32) < 16, -1.0, 1.0).astype(np.float32)], 1)
    shared["ropef"] = np.ascontiguousarray(ropef)
    for l in range(nlayers):
        win = I_["w_in"][l]
        shared[f"wmod{l}"] = _kc(I_["w_mod"][l])
        shared[f"bmodT{l}"] = np.ascontiguousarray(I_["b_mod"][l].reshape(48, 128).T)
        shared[f"g1T{l}"] = np.ascontiguousarray(I_["g_norm1"][l].reshape(8, 128).T)
        shared[f"g2T{l}"] = np.ascontiguousarray(I_["g_norm2"][l].reshape(8, 128).T)
        shared[f"w1_{l}"] = _kc(win[:, w1cols])
        ga = _kc(win[:, 1280:2304]).reshape(128, 8, 8, 128)
        gs = _kc(win[:, 2304:3328]).reshape(128, 8, 8, 128)
        shared[f"w2g{l}"] = np.ascontiguousarray(np.concatenate([ga, gs], axis=3).transpose(2, 0, 1, 3))
        shared[f"wglu{l}"] = _kc(I_["w_glu"][l])
        shared[f"wbra{l}"] = _kc(I_["w_br_attn"][l][brarows, :])
        shared[f"wbrs{l}"] = _kc(I_["w_br_ssm"][l])
        shared[f"wout{l}"] = _kc(I_["w_out"][l])
        shared[f"wr{l}"] = _kc(np.concatenate([I_["w_router_group"][l], I_["w_router_expert"][l]], axis=1))
        shared[f"weg{l}"] = np.ascontiguousarray(I_["w_exp_gate"][l][:nei].reshape(nei, 8, 128, 512).transpose(0, 2, 1, 3))
        shared[f"weu{l}"] = np.ascontiguousarray(I_["w_exp_up"][l][:nei].reshape(nei, 8, 128, 512).transpose(0, 2, 1, 3))
        shared[f"wed{l}"] = np.ascontiguousarray(I_["w_exp_down"][l][:nei].reshape(nei, 4, 128, D).transpose(0, 2, 1, 3))
        shared[f"sink{l}"] = np.ascontiguousarray(np.broadcast_to(I_["attn_sink"][l], (128, 8)))
    kk = np.arange(128)[:, None]
    qq = np.arange(128)[None, :]
    mprev = np.tile((kk >= qq).astype(np.float32), (1, 4))
    mnext = np.tile((kk <= qq).astype(np.float32), (1, 4))
    per_core = []
    for r in range(NCORES):
        b, j = r // 4, r % 4
        m = dict(shared)
        t0 = j * TOWN
        m["x_own"] = np.ascontiguousarray(I_["x"][b, t0:t0 + TOWN])
        m["ctx_b"] = np.ascontiguousarray(I_["ctx"][b])
        cT = np.stack([I_["c"][b].reshape(8, 128).T, I_["c_ctx"].reshape(8, 128).T], axis=2)
        m["cT"] = np.ascontiguousarray(cT)
        tpos = np.arange(t0, t0 + TOWN)
        rows = (tpos // 64).astype(np.float32)
        cols = (tpos % 64).astype(np.float32)
        pos = np.zeros((128, TALL), np.float32)
        is_row = (ee % 64) < 32
        pos[:, :TOWN] = np.where(is_row[:, None], rows[None, :], cols[None, :])
        m["pos"] = pos
        mk = np.zeros((128, 4, 512), np.float32)
        mk[:, 0] = mprev if j > 0 else 0.0
        mk[:, 1] = mprev
        mk[:, 2] = mnext if j < 3 else 0.0
        mk[:, 3] = mnext
        m["masks"] = mk
        sel = np.zeros((128, 12, 128), np.float32)
        eye = np.eye(128, dtype=np.float32)
        sel[:, j] = eye
        if j > 0:
            sel[:, 4 + j - 1] = eye
        if j < 3:
            sel[:, 8 + j + 1] = eye
        m["selmat"] = sel
        for l in range(nlayers):
            win = I_["w_in"][l]
            m[f"wus{l}"] = _kc(win[:, 768 + 128 * j:768 + 128 * (j + 1)])
            g0 = 8 * j

            def rowlay(a):
                a = a.reshape((2, 4, 2, 64) + a.shape[3:])
                perm = (2, 3, 0, 1) + tuple(range(4, a.ndim))
                a = a.transpose(perm)
                return np.ascontiguousarray(a.reshape((128, 8) + a.shape[4:]))

            m[f"lamre{l}"] = rowlay(I_["ssm_lam_re"][l][:, g0:g0 + 8])
            m[f"lamim{l}"] = rowlay(I_["ssm_lam_im"][l][:, g0:g0 + 8])
            ldt = np.broadcast_to(I_["ssm_log_dt"][l][:, g0:g0 + 8, None], (2, 8, 64))
            m[f"ldt{l}"] = rowlay(np.ascontiguousarray(ldt))
            m[f"bre{l}"] = rowlay(I_["ssm_b_re"][l][:, g0:g0 + 8])
            m[f"bim{l}"] = rowlay(I_["ssm_b_im"][l][:, g0:g0 + 8])
            m[f"cre{l}"] = rowlay(np.ascontiguousarray(I_["ssm_c_re"][l][:, g0:g0 + 8].transpose(0, 1, 3, 2)))
            m[f"cim{l}"] = rowlay(np.ascontiguousarray(I_["ssm_c_im"][l][:, g0:g0 + 8].transpose(0, 1, 3, 2)))
            m[f"dsk{l}"] = np.ascontiguousarray(I_["ssm_d"][l][128 * j:128 * (j + 1)].reshape(128, 1))
        for k in list(m.keys()):
            if False:
                a = m[k]
                a2 = a.reshape(-1, a.shape[-1])
                rr = a2.shape[0] // NCORES
                m[k] = np.ascontiguousarray(a2[r * rr:(r + 1) * rr])
        per_core.append(m)
    return per_core


_CACHE = {}


def kernel(**inputs):
    if "nc" not in _CACHE:
        _CACHE["nc"] = build_program(2)
    nc, names = _CACHE["nc"]
    per_core = prep_inputs(inputs, 2)
    in_maps = [{k: m[k] for k in names} for m in per_core]
    res = run_bass_kernel_spmd(nc, in_maps, core_ids=list(range(NCORES)))
    out = np.zeros((2, SEQ, D), np.float32)
    for r in range(NCORES):
        b, j = r // 4, r % 4
        out[b, j * TOWN:(j + 1) * TOWN] = res.results[r]["out"]
    return out
```

```python
from contextlib import ExitStack
import numpy as np
import concourse.bass as bass
import concourse.mybir as mybir
from concourse.bass_utils import run_bass_kernel_spmd

dt = mybir.dt
ALU = mybir.AluOpType
AF = mybir.ActivationFunctionType
AX = mybir.AxisListType
F32 = dt.float32
BF16 = dt.bfloat16
I32 = dt.int32

NCORES = 8
D = 1024
TOWN = 2048
NT_OWN = 16
NT = 18
TALL = 2304
SEQ = 8192
CTX = 256
STREAM = SEQ + CTX
NEXP = 32
PI = float(np.pi)
GROUPS = [[0, 1, 2, 3], [4, 5, 6, 7]]


class Buf:
    __slots__ = ("name", "last_w", "readers")

    def __init__(self, name=""):
        self.name = name
        self.last_w = None
        self.readers = {}


class Prog:
    ENG = ("pe", "act", "dve", "pool", "sp")

    def __init__(self, nc, stack, ndma=None):
        ndma = ndma or {"sp": 8, "act": 2, "pool": 6, "cc": 3}
        self.nc = nc
        self.q = {e: [] for e in self.ENG}
        self.sems = {}
        self.cnt = {e: 0 for e in self.ENG}
        self.waited = {e: {} for e in self.ENG}
        for e in self.ENG:
            self.sems[("c", e)] = stack.enter_context(nc.semaphore("c_" + e))
        self.dma_slots = {}
        self.dma_tot = {}
        self.dma_rr = {}
        for e, n in ndma.items():
            keys = []
            for i in range(n):
                k = ("d", e, i)
                self.sems[k] = stack.enter_context(nc.semaphore("d_%s%d" % (e, i)))
                self.dma_tot[k] = 0
                keys.append(k)
            self.dma_slots[e] = keys
            self.dma_rr[e] = 0

    def _wait(self, eng, k, v):
        if k == ("c", "pe") and eng == "pe":
            return
        if self.waited[eng].get(k, 0) >= v:
            return
        self.waited[eng][k] = v
        self.q[eng].append(("w", k, v))

    def _deps(self, eng, reads, writes):
        deps = {}

        def add(ev):
            if ev is None:
                return
            k, v = ev
            if deps.get(k, 0) < v:
                deps[k] = v

        for b in reads:
            add(b.last_w)
        for b in writes:
            add(b.last_w)
            for k, v in b.readers.items():
                add((k, v))
        for k, v in deps.items():
            self._wait(eng, k, v)

    def _commit(self, ev, reads, writes):
        k, v = ev
        for b in reads:
            if b.readers.get(k, 0) < v:
                b.readers[k] = v
        for b in writes:
            b.last_w = ev
            b.readers = {}

    def op(self, eng, fn, reads=(), writes=()):
        self._deps(eng, reads, writes)
        self.cnt[eng] += 1
        ev = (("c", eng), self.cnt[eng])
        self.q[eng].append(("o", fn, ("c", eng), 1))
        self._commit(ev, reads, writes)
        return ev

    def dma(self, qeng, fn, reads=(), writes=(), inc=16):
        skey = "cc" if inc == 1 else qeng
        slots = self.dma_slots[skey]
        k = slots[self.dma_rr[skey] % len(slots)]
        self.dma_rr[skey] += 1
        prev = self.dma_tot[k]
        if prev > 0:
            self._wait(qeng, k, prev)
        self._deps(qeng, reads, writes)
        self.dma_tot[k] = prev + inc
        ev = (k, prev + inc)
        self.q[qeng].append(("o", fn, k, inc))
        self._commit(ev, reads, writes)
        return ev

    def wait_all(self, eng, bufs):
        self._deps(eng, bufs, ())

    def barrier(self):
        tot = {("c", e): self.cnt[e] for e in self.ENG}
        tot.update(self.dma_tot)
        for e in self.ENG:
            for k, v in tot.items():
                if v > 0:
                    self._wait(e, k, v)

    def emit(self):
        nc = self.nc
        sems = self.sems

        def replay(e, name):
            for it in self.q[name]:
                if it[0] == "w":
                    e.wait_ge(sems[it[1]], it[2])
                else:
                    ins = it[1](e)
                    ins.then_inc(sems[it[2]], it[3])

        with nc.Block() as block:

            @block.tensor
            def _(e):
                replay(e, "pe")

            @block.vector
            def _(e):
                replay(e, "dve")

            @block.scalar
            def _(e):
                replay(e, "act")

            @block.gpsimd
            def _(e):
                replay(e, "pool")

            @block.sync
            def _(e):
                replay(e, "sp")


class TL:
    def __init__(self, t, nslots=1):
        self.t = t
        self.bs = [Buf() for _ in range(nslots)]

    @property
    def b(self):
        return self.bs[0]


def build_program(nlayers=2, dbg=None):
    dbg = dbg or {}
    NEI = dbg.get("_nexp", NEXP)
    nc = bass.Bass("TRN2", target_bir_lowering=False)
    names_in = []

    def inp(name, shape, d=F32):
        names_in.append(name)
        return TL(nc.dram_tensor(name, list(shape), d, kind="ExternalInput").ap())

    gathers = []

    def ginp(name, shape):
        return inp(name, shape)

    def ginp_unused(name, shape):
        R = int(np.prod(shape[:-1]))
        C = int(shape[-1])
        assert R % NCORES == 0
        names_in.append(name)
        shard = TL(nc.dram_tensor(name, [R // NCORES, C], F32, kind="ExternalInput").ap())
        bounce = TL(nc.dram_tensor(name + "_bnc", [R // NCORES, C], F32).ap())
        full = TL(nc.dram_tensor(name + "_full", list(shape), F32).ap())
        gathers.append((shard, bounce, full))
        return full

    x_own = inp("x_own", [TOWN, D])
    ctx_b = inp("ctx_b", [CTX, D])
    cT = inp("cT", [128, 8, 2])
    pos_d = inp("pos", [128, TALL])
    ropef_d = inp("ropef", [128, 2])
    masks_d = inp("masks", [128, 4, 512])
    selmat_d = inp("selmat", [128, 12, 128])
    ident_d = inp("ident", [128, 128])
    iota_d = inp("iota", [128, 512])
    gfin_d = inp("gfin", [128, D])
    L = []
    for l in range(nlayers):
        w = {}
        w["wmod"] = ginp(f"wmod{l}", [128, 8, 6 * D])
        w["bmodT"] = inp(f"bmodT{l}", [128, 48])
        w["g1T"] = inp(f"g1T{l}", [128, 8])
        w["g2T"] = inp(f"g2T{l}", [128, 8])
        w["w1"] = ginp(f"w1_{l}", [128, 8, 1920])
        w["wus"] = inp(f"wus{l}", [128, 8, 128])
        w["w2g"] = ginp(f"w2g{l}", [8, 128, 8, 256])
        w["wglu"] = inp(f"wglu{l}", [128, 4, 512])
        w["wbra"] = ginp(f"wbra{l}", [128, 4, D])
        w["wbrs"] = ginp(f"wbrs{l}", [128, 4, D])
        w["wout"] = ginp(f"wout{l}", [128, 8, D])
        w["wr"] = inp(f"wr{l}", [128, 8, 36])
        w["weg"] = ginp(f"weg{l}", [NEI, 128, 8, 512])
        w["weu"] = ginp(f"weu{l}", [NEI, 128, 8, 512])
        w["wed"] = ginp(f"wed{l}", [NEI, 128, 4, D])
        w["sink"] = inp(f"sink{l}", [128, 8])
        w["lamre"] = inp(f"lamre{l}", [128, 8])
        w["lamim"] = inp(f"lamim{l}", [128, 8])
        w["ldt"] = inp(f"ldt{l}", [128, 8])
        w["bre"] = inp(f"bre{l}", [128, 8, 16])
        w["bim"] = inp(f"bim{l}", [128, 8, 16])
        w["cre"] = inp(f"cre{l}", [128, 8, 16])
        w["cim"] = inp(f"cim{l}", [128, 8, 16])
        w["dsk"] = inp(f"dsk{l}", [128, 1])
        L.append(w)
    out_d = TL(nc.dram_tensor("out", [TOWN, D], F32, kind="ExternalOutput").ap())
    dbg_out = {}
    for k, shp in dbg.items():
        if k.startswith("_"):
            continue
        dbg_out[k] = TL(nc.dram_tensor("dbg_" + k, list(shp), F32, kind="ExternalOutput").ap())
    AGR = TOWN + 128
    ag1_in = [TL(nc.dram_tensor(f"ag1_in{i}", [r_, 512], BF16).ap()) for i, r_ in enumerate((1024, 1024, 128))]
    ag1_out = [TL(nc.dram_tensor(f"ag1_out{i}", [4 * r_, 512], BF16).ap()) for i, r_ in enumerate((1024, 1024, 128))]
    ag2_in = [TL(nc.dram_tensor(f"ag2_in{i}", [128, c_], BF16).ap()) for i, c_ in enumerate((4096, 4096, CTX))]
    ag2_out = [TL(nc.dram_tensor(f"ag2_out{i}", [512, c_], BF16).ap()) for i, c_ in enumerate((4096, 4096, CTX))]
    cos_d = TL(nc.dram_tensor("cos_d", [128, TALL], F32).ap())
    sin_d = TL(nc.dram_tensor("sin_d", [128, TALL], F32).ap())
    yTf_d = TL(nc.dram_tensor("yTf_d", [128, STREAM], F32).ap())
    attn_d = TL(nc.dram_tensor("attn_d", [128, 4, TALL], BF16).ap())

    with ExitStack() as st:
        P = Prog(nc, st)

        uid = [0]

        def sbuf(stack, name, shape, d=F32, nslots=1):
            uid[0] += 1
            return TL(stack.enter_context(nc.sbuf_tensor("sb%d_%s" % (uid[0], name), list(shape), d)), nslots)

        PS = [TL(st.enter_context(nc.psum_tensor(f"ps{i}", [128, 512], F32))) for i in range(8)]
        ps_rr = [0]

        def bank(exclude=()):
            while True:
                i = ps_rr[0] % 8
                ps_rr[0] += 1
                if i not in exclude:
                    return PS[i]

        def mm(out_tl, out_ap, lhsT_ap, rhs_ap, reads, start=True, stop=True):
            P.op("pe", lambda e: e.matmul(out_ap, lhsT_ap, rhs_ap, start=start, stop=stop), reads, [out_tl.b])

        def tr(out_tl, out_ap, in_ap, ident_ap, reads):
            P.op("pe", lambda e: e.transpose(out_ap, in_ap, ident_ap), reads, [out_tl.b])

        def act(out_ap, in_ap, func, reads, writes, bias=None, scale=None, accum=None):
            kw = {}
            if bias is not None:
                kw["bias"] = bias
            if scale is not None:
                kw["scale"] = scale
            if accum is not None:
                kw["accum_out"] = accum
            P.op("act", lambda e: e.activation(out_ap, in_ap, func, **kw), reads, writes)

        def tt(eng, out_ap, a_ap, b_ap, op, reads, writes):
            P.op(eng, lambda e: e.tensor_tensor(out_ap, a_ap, b_ap, op), reads, writes)

        def ts(eng, out_ap, a_ap, s1, s2, op0, op1, reads, writes):
            if op1 is None:
                P.op(eng, lambda e: e.tensor_scalar(out_ap, a_ap, s1, None, op0), reads, writes)
            else:
                P.op(eng, lambda e: e.tensor_scalar(out_ap, a_ap, s1, s2, op0, op1), reads, writes)

        def stt(eng, out_ap, a_ap, s, b_ap, op0, op1, reads, writes):
            P.op(eng, lambda e: e.scalar_tensor_tensor(out_ap, a_ap, s, b_ap, op0, op1), reads, writes)

        def cp(eng, out_ap, in_ap, reads, writes):
            P.op(eng, lambda e: e.tensor_copy(out_ap, in_ap), reads, writes)

        def dma(q, out_ap, in_ap, reads, writes, **kw):
            P.dma(q, lambda e: e.dma_start(out=out_ap, in_=in_ap, **kw), reads, writes)

        def dump(key, src_tl, src_ap, dst_ap_fn):
            if key in dbg_out:
                dma("sp", dst_ap_fn(dbg_out[key].t), src_ap, [src_tl.b] if isinstance(src_tl, TL) else src_tl,
                    [dbg_out[key].b])

        for shard, bounce, full in gathers:
            nr_ = shard.t.shape[0]
            for r0_ in range(0, nr_, 1024):
                r1_ = min(nr_, r0_ + 1024)
                dma("sp", bounce.t[r0_:r1_, :], shard.t[r0_:r1_, :], [], [bounce.b])
            P.dma("pool", lambda e, bounce=bounce, full=full: e.collective_compute(
                "AllGather", ALU.bypass, replica_groups=[list(range(NCORES))], ins=[bounce.t.opt()], outs=[full.t.opt()]),
                [bounce.b], [full.b], inc=1)

        WBF = []
        for l in range(nlayers):
            wb = {}
            for nm in ("w1", "w2g", "wout", "wbra", "wbrs", "wglu"):
                src = L[l][nm]
                dst = TL(nc.dram_tensor(f"{nm}bf{l}", list(src.t.shape), BF16).ap())
                for i0 in range(src.t.shape[0] if nm == "w2g" else src.t.shape[1]):
                    if nm == "w2g":
                        dma("pool", dst.t[i0, :, :, :], src.t[i0, :, :, :], [src.b], [dst.b])
                    else:
                        dma("pool", dst.t[:, i0, :], src.t[:, i0, :], [src.b], [dst.b])
                wb[nm] = dst
            WBF.append(wb)

        x_tm = sbuf(st, "x_tm", [128, NT, D], F32, NT)
        ident = sbuf(st, "ident", [128, 128])
        zeros = sbuf(st, "zeros", [128, 512])
        iota = sbuf(st, "iota", [128, 512])
        onesb = sbuf(st, "onesb", [128, 64], BF16)
        onesf = sbuf(st, "onesf", [128, 128])
        siluT = sbuf(st, "siluT", [128, 8, 2])
        modT = sbuf(st, "modT", [128, 48, 2])
        bmodT = sbuf(st, "bmodT", [128, 48])
        gT = sbuf(st, "gT", [128, 16])
        A1 = sbuf(st, "A1", [128, 2, 8])
        A2 = sbuf(st, "A2", [128, 2, 8])
        rstd = sbuf(st, "rstd", [128, NT])
        sstat = sbuf(st, "sstat", [128, NT])
        xs = sbuf(st, "xs", [128, D])
        sm = sbuf(st, "sm", [128, 8])
        ucT = sbuf(st, "ucT", [128, CTX], BF16)
        dg = sbuf(st, "dg", [128, 128])

        def AP_rev(tl, row_elems, col_last, n):
            return bass.AP(tl.t, col_last, [[row_elems, 128], [-1, n]])

        def range_reduce(kf_tl, kf_ap, ki_tl, ki_ap, out_ap, in_ap, shift, reads, writes):
            ts("dve", kf_ap, in_ap, shift, 1.0 / (2 * PI), ALU.add, ALU.mult, reads, [kf_tl.b])
            cp("dve", ki_ap, kf_ap, [kf_tl.b], [ki_tl.b])
            cp("dve", kf_ap, ki_ap, [ki_tl.b], [kf_tl.b])
            stt("dve", kf_ap, kf_ap, -2 * PI, in_ap, ALU.mult, ALU.add, [kf_tl.b] + list(reads), [kf_tl.b])
            ts("dve", out_ap, kf_ap, shift, None, ALU.add, None, [kf_tl.b], writes)
            ts("dve", out_ap, out_ap, 3.14159, -3.14159, ALU.min, ALU.max, writes, writes)

        with ExitStack() as ph:
            posT = sbuf(ph, "posT", [128, TALL])
            ang = sbuf(ph, "ang", [128, TALL])
            ki = sbuf(ph, "ki", [128, TALL], I32)
            kf = sbuf(ph, "kf", [128, TALL])
            tb = sbuf(ph, "tb", [128, TALL])
            ropef = sbuf(ph, "ropef", [128, 2])
            dma("sp", x_tm.t[:, 0:NT_OWN, :], x_own.t.rearrange("(t p) d -> p t d", p=128), [], x_tm.bs[0:NT_OWN])
            dma("sp", x_tm.t[:, NT_OWN:NT, :], ctx_b.t.rearrange("(t p) d -> p t d", p=128), [], x_tm.bs[NT_OWN:NT])
            dma("sp", ident.t[:], ident_d.t[:, :], [], [ident.b])
            dma("sp", posT.t[:], pos_d.t[:, :], [], [posT.b])
            dma("sp", ropef.t[:], ropef_d.t[:, :], [], [ropef.b])
            dma("sp", iota.t[:], iota_d.t[:, :], [], [iota.b])
            dma("sp", siluT.t[:], cT.t[:, :, :], [], [siluT.b])
            P.op("pool", lambda e: e.memset(zeros.t[:], 0.0), [], [zeros.b])
            P.op("pool", lambda e: e.memset(onesb.t[:], 1.0), [], [onesb.b])
            P.op("pool", lambda e: e.memset(onesf.t[:], 1.0), [], [onesf.b])
            act(siluT.t[:], siluT.t[:], AF.Silu, [siluT.b], [siluT.b])
            act(sm.t[:, 0:1], ropef.t[:, 0:1], AF.Exp, [ropef.b], [sm.b], scale=-float(np.log(10000.0)) / 16.0)
            ts("dve", ang.t[:], posT.t[:], sm.t[:, 0:1], None, ALU.mult, None, [posT.b, sm.b], [ang.b])
            range_reduce(kf, kf.t[:], ki, ki.t[:], tb.t[:], ang.t[:], 0.0, [ang.b], [tb.b])
            act(tb.t[:], tb.t[:], AF.Sin, [tb.b], [tb.b])
            ts("dve", tb.t[:], tb.t[:], ropef.t[:, 1:2], None, ALU.mult, None, [tb.b, ropef.b], [tb.b])
            dma("sp", sin_d.t[:, :], tb.t[:], [tb.b], [sin_d.b])
            range_reduce(kf, kf.t[:], ki, ki.t[:], tb.t[:], ang.t[:], PI / 2, [ang.b], [tb.b])
            act(tb.t[:], tb.t[:], AF.Sin, [tb.b], [tb.b])
            dma("sp", cos_d.t[:, :], tb.t[:], [tb.b], [cos_d.b])
            P.barrier()

        def tile_cls(t_):
            return 0 if t_ < NT_OWN else 1

        def compute_rstd(ntiles):
            for t_ in range(ntiles):
                act(xs.t[:], x_tm.t[:, t_, :], AF.Square, [x_tm.bs[t_]], [xs.b, sstat.b], accum=sstat.t[:, t_:t_ + 1])
            ts("dve", rstd.t[:, 0:ntiles], sstat.t[:, 0:ntiles], 1.0 / D, 1e-6, ALU.mult, ALU.add, [sstat.b], [rstd.b])
            act(rstd.t[:, 0:ntiles], rstd.t[:, 0:ntiles], AF.Sqrt, [rstd.b], [rstd.b])
            P.op("dve", lambda e: e.reciprocal(rstd.t[:, 0:ntiles], rstd.t[:, 0:ntiles]), [rstd.b], [rstd.b])

        def norm_tile(t_, Acls, shslot, dst_ap_fn, dst_bufs, f32_dst=None):
            cls = tile_cls(t_)
            act(xs.t[:], x_tm.t[:, t_, :], AF.Identity, [x_tm.bs[t_], rstd.b], [xs.b], scale=rstd.t[:, t_:t_ + 1])
            for half in range(2):
                pb_ = bank()
                for q4 in range(4):
                    fc = half * 4 + q4
                    tr(pb_, pb_.t[:, q4 * 128:(q4 + 1) * 128], xs.t[:, fc * 128:(fc + 1) * 128], ident.t[:], [xs.b, ident.b])
                for q4 in range(4):
                    fc = half * 4 + q4
                    act(dst_ap_fn(fc), pb_.t[:, q4 * 128:(q4 + 1) * 128], AF.Identity, [pb_.b, Acls.b, modT.b], dst_bufs,
                        scale=Acls.t[:, cls, fc:fc + 1], bias=modT.t[:, shslot * 8 + fc, cls:cls + 1])
                    if f32_dst is not None:
                        act(f32_dst.t[:, fc, :], pb_.t[:, q4 * 128:(q4 + 1) * 128], AF.Identity, [pb_.b, Acls.b, modT.b],
                            [f32_dst.b], scale=Acls.t[:, cls, fc:fc + 1], bias=modT.t[:, shslot * 8 + fc, cls:cls + 1])

        def make_gtrow(dst, slot):
            for cls in range(2):
                for fc in range(8):
                    ts("dve", dg.t[:], ident.t[:], modT.t[:, slot * 8 + fc, cls:cls + 1], None, ALU.mult, None,
                       [ident.b, modT.b], [dg.b])
                    pb_ = bank()
                    mm(pb_, pb_.t[:, 0:128], onesf.t[:], dg.t[:], [onesf.b, dg.b])
                    cp("dve", dst.t[:, cls, fc * 128:(fc + 1) * 128], pb_.t[:, 0:128], [pb_.b], [dst.b])

        def blk_of(t_):
            return t_ + 1 if t_ < NT_OWN else 18 + (t_ - NT_OWN)

        DL = dbg.get("_layer", 0)
        stopped = False
        for l in range(nlayers):
            if dbg.get("_stop") == "0":
                break
            W = L[l]
            WB = WBF[l]
            ctx_out = l < nlayers - 1
            with ExitStack() as ph:
                wm = sbuf(ph, "wm", [128, 2, 8, 512], F32, 2)
                dma("sp", bmodT.t[:], W["bmodT"].t[:, :], [], [bmodT.b])
                dma("sp", gT.t[:, 0:8], W["g1T"].t[:, :], [], [gT.b])
                dma("sp", gT.t[:, 8:16], W["g2T"].t[:, :], [], [gT.b])
                mps = bank()
                for piece in range(12):
                    s = piece % 2
                    dma("sp", wm.t[:, s, :, :], W["wmod"].t[:, :, piece * 512:(piece + 1) * 512], [W["wmod"].b], [wm.bs[s]])
                    for c4 in range(4):
                        cc = piece * 4 + c4
                        for kc in range(8):
                            mm(mps, mps.t[:, cc * 2:cc * 2 + 2], wm.t[:, s, kc, c4 * 128:(c4 + 1) * 128],
                               siluT.t[:, kc, :], [wm.bs[s], siluT.b], kc == 0, kc == 7)
                for cls in range(2):
                    tt("dve", modT.t[:, :, cls], mps.t[:, cls:96:2], bmodT.t[:], ALU.add, [mps.b, bmodT.b], [modT.b])
                for cls in range(2):
                    stt("dve", A1.t[:, cls, :], modT.t[:, 8:16, cls], 1.0, gT.t[:, 0:8], ALU.add, ALU.mult,
                        [modT.b, gT.b], [A1.b])
                    stt("dve", A2.t[:, cls, :], modT.t[:, 32:40, cls], 1.0, gT.t[:, 8:16], ALU.add, ALU.mult,
                        [modT.b, gT.b], [A2.b])
                if l == DL:
                    dump("modT", modT, modT.t[:], lambda o: o[:, :, :])
                P.barrier()

            kvst = ExitStack()
            kT = sbuf(kvst, "kT", [128, 20, 128], BF16, 20)
            vv = sbuf(kvst, "vv", [128, 20, 128], BF16, 20)
            with ExitStack() as ph:
                w1 = sbuf(ph, "w1", [128, 8, 1920], BF16)
                wus = sbuf(ph, "wus", [128, 8, 128], BF16)
                hT = sbuf(ph, "hT", [128, 8, 512], BF16)
                ust = sbuf(ph, "ust", [128, 2, 512], BF16, 2)
                tmpa = sbuf(ph, "tmpa", [128, 512])
                tmpb = sbuf(ph, "tmpb", [128, 512])
                cosT = sbuf(ph, "cosT", [128, TALL])
                sinT = sbuf(ph, "sinT", [128, TALL])
                dma("sp", cosT.t[:], cos_d.t[:, :], [cos_d.b], [cosT.b])
                dma("sp", sinT.t[:], sin_d.t[:, :], [sin_d.b], [sinT.b])
                dma("sp", w1.t[:, :, :], WB["w1"].t[:, :, :], [WB["w1"].b], [w1.b])
                dma("pool", wus.t[:], W["wus"].t[:, :, :], [], [wus.b])
                compute_rstd(NT)
                ust_rr = 0
                for ch in range(5):
                    tiles = list(range(ch * 4, min(ch * 4 + 4, NT)))
                    n = len(tiles) * 128
                    c0 = ch * 512
                    for ti, t_ in enumerate(tiles):
                        norm_tile(t_, A1, 0, lambda fc, ti=ti: hT.t[:, fc, ti * 128:(ti + 1) * 128], [hT.b])
                    pk = bank()
                    pkr = bank()
                    for kc in range(8):
                        mm(pk, pk.t[:, 0:n], w1.t[:, kc, 1024:1152], hT.t[:, kc, 0:n], [w1.b, hT.b], kc == 0, kc == 7)
                    for kc in range(8):
                        mm(pkr, pkr.t[:, 0:n], w1.t[:, kc, 1152:1280], hT.t[:, kc, 0:n], [w1.b, hT.b], kc == 0, kc == 7)
                    tt("dve", tmpa.t[:, 0:n], pk.t[:, 0:n], cosT.t[:, c0:c0 + n], ALU.mult, [pk.b, cosT.b], [tmpa.b])
                    tt("dve", tmpb.t[:, 0:n], pkr.t[:, 0:n], sinT.t[:, c0:c0 + n], ALU.mult, [pkr.b, sinT.b], [tmpb.b])
                    for ti, t_ in enumerate(tiles):
                        blk = blk_of(t_)
                        tt("pool", kT.t[:, blk, :], tmpa.t[:, ti * 128:(ti + 1) * 128], tmpb.t[:, ti * 128:(ti + 1) * 128],
                           ALU.add, [tmpa.b, tmpb.b], [kT.bs[blk]])
                    pv = bank()
                    for ti, t_ in enumerate(tiles):
                        for kc in range(8):
                            mm(pv, pv.t[:, ti * 128:(ti + 1) * 128], hT.t[:, kc, ti * 128:(ti + 1) * 128],
                               w1.t[:, kc, 1280:1408], [w1.b, hT.b], kc == 0, kc == 7)
                    for ti, t_ in enumerate(tiles):
                        blk = blk_of(t_)
                        act(vv.t[:, blk, :], pv.t[:, ti * 128:(ti + 1) * 128], AF.Identity, [pv.b], [vv.bs[blk]])
                    if ch < 4:
                        for ti, t_ in enumerate(tiles):
                            pu = bank()
                            for kc in range(8):
                                mm(pu, pu.t[:, :], hT.t[:, kc, ti * 128:(ti + 1) * 128], w1.t[:, kc, 1408:1920],
                                   [w1.b, hT.b], kc == 0, kc == 7)
                            s = ust_rr % 2
                            ust_rr += 1
                            act(ust.t[:, s, :], pu.t[:, :], AF.Identity, [pu.b], [ust.bs[s]])
                            pc_, lr_ = (t_ * 128) // 1024, (t_ * 128) % 1024
                            dma("sp", ag1_in[pc_].t[lr_:lr_ + 128, :], ust.t[:, s, :], [ust.bs[s]], [ag1_in[pc_].b])
                    else:
                        pu = bank()
                        for kc in range(8):
                            mm(pu, pu.t[:, 0:CTX], wus.t[:, kc, :], hT.t[:, kc, 0:CTX], [wus.b, hT.b], kc == 0, kc == 7)
                        act(ucT.t[:], pu.t[:, 0:CTX], AF.Identity, [pu.b], [ucT.b])
                for i4, (tl_, blk) in enumerate(((kT, 1), (kT, 16), (vv, 1), (vv, 16))):
                    dma("sp", ag1_in[2].t[:, i4 * 128:(i4 + 1) * 128], tl_.t[:, blk, :], [tl_.bs[blk]], [ag1_in[2].b])
                for pc_ in range(3):
                    P.dma("pool", lambda e, pc_=pc_: e.collective_compute(
                        "AllGather", ALU.bypass, replica_groups=GROUPS, ins=[ag1_in[pc_].t.opt()], outs=[ag1_out[pc_].t.opt()]),
                        [ag1_in[pc_].b], [ag1_out[pc_].b], inc=1)
                P.barrier()
            if dbg.get("_stop") == "B" and l == DL:
                stopped = True

            if not stopped:
              with ExitStack() as ph:
                uTf = sbuf(ph, "uTf", [128, STREAM], BF16, 17)
                hal = sbuf(ph, "hal", [128, 4, 512], BF16)
                utile = sbuf(ph, "utile", [128, 2, 512], BF16, 2)
                selm = sbuf(ph, "selm", [128, 12, 128], BF16)
                prm = sbuf(ph, "prm", [128, 16, 8])
                bb = sbuf(ph, "bb", [128, 4, 8, 16])
                cc_ = sbuf(ph, "cc", [128, 2, 8, 16])
                Wsc = sbuf(ph, "Wsc", [128, 128])
                lB = sbuf(ph, "lB", [128, 16, 128], BF16)
                lC = sbuf(ph, "lC", [128, 16, 128], BF16)
                dsk = sbuf(ph, "dsk", [128, 1])
                tabc = sbuf(ph, "tabc", [128, 4, 512], F32, 4)
                tabs = sbuf(ph, "tabs", [128, 4, 512], F32, 4)
                rhoT = sbuf(ph, "rhoT", [128, 4, 512], F32, 4)
                kis = sbuf(ph, "kis", [128, 512], I32)
                NW = 2
                wk = [[sbuf(ph, f"wk{s}_{i}", [128, 512]) for i in range(6)] for s in range(NW)]
                xrb = [[sbuf(ph, f"xb{s}_{i}", [128, 512], BF16) for i in range(2)] for s in range(NW)]
                kfs, angs = wk[0][0], wk[0][1]
                ub = sbuf(ph, "ub", [128, 2, 512], BF16, 2)
                init = sbuf(ph, "init", [128, 4, 2], F32, 4)
                ytmp = sbuf(ph, "ytmp", [128, 3, 512], F32, 3)
                yfs = sbuf(ph, "yfs", [128, 2, 512], F32, 2)
                zst = sbuf(ph, "zst", [128, 2, 512], BF16, 2)
                dma("pool", selm.t[:], selmat_d.t[:, :, :], [], [selm.b])
                for i in range(4):
                    dma("sp", hal.t[:, i, :], ag1_out[2].t[i * 128:(i + 1) * 128, :], [ag1_out[2].b], [hal.b])
                ph_ = bank()
                for i4, (selbase, col) in enumerate(((4, 128), (8, 0), (4, 384), (8, 256))):
                    for i in range(4):
                        mm(ph_, ph_.t[:, i4 * 128:(i4 + 1) * 128], selm.t[:, selbase + i, :], hal.t[:, i, col:col + 128],
                           [selm.b, hal.b], i == 0, i == 3)
                for i4, (tl_, blk) in enumerate(((kT, 0), (kT, 17), (vv, 0), (vv, 17))):
                    cp("dve", tl_.t[:, blk, :], ph_.t[:, i4 * 128:(i4 + 1) * 128], [ph_.b], [tl_.bs[blk]])
                cp("dve", uTf.t[:, 0:CTX], ucT.t[:], [ucT.b], [uTf.bs[0]])
                ut_rr = 0
                for i in range(4):
                    for tq in range(4):
                        pu = bank()
                        for t4 in range(4):
                            s = ut_rr % 2
                            ut_rr += 1
                            tr_ = (tq * 4 + t4) * 128
                            pc_, r0 = tr_ // 1024, i * 1024 + tr_ % 1024
                            dma("sp", utile.t[:, s, :], ag1_out[pc_].t[r0:r0 + 128, :], [ag1_out[pc_].b], [utile.bs[s]])
                            for sl in range(4):
                                mm(pu, pu.t[:, t4 * 128:(t4 + 1) * 128], utile.t[:, s, sl * 128:(sl + 1) * 128],
                                   selm.t[:, sl, :], [utile.bs[s], selm.b], sl == 0, sl == 3)
                        nk = i * 4 + tq
                        act(uTf.t[:, CTX + nk * 512:CTX + (nk + 1) * 512], pu.t[:, :], AF.Identity, [pu.b], [uTf.bs[1 + nk]])
                for nm, c_ in (("lamre", 0), ("lamim", 1), ("ldt", 2)):
                    dma("sp", prm.t[:, c_, :], W[nm].t[:, :], [], [prm.b])
                dma("sp", bb.t[:, 0, :, :], W["bre"].t[:, :, :], [], [bb.b])
                dma("sp", bb.t[:, 1, :, :], W["bim"].t[:, :, :], [], [bb.b])
                dma("sp", cc_.t[:, 0, :, :], W["cre"].t[:, :, :], [], [cc_.b])
                dma("sp", cc_.t[:, 1, :, :], W["cim"].t[:, :, :], [], [cc_.b])
                dma("sp", dsk.t[:], W["dsk"].t[:, :], [], [dsk.b])
                pr = lambda c_: prm.t[:, c_, :]
                PB = [prm.b]
                act(pr(3), pr(2), AF.Exp, PB, PB)
                tt("dve", pr(4), pr(0), pr(3), ALU.mult, PB, PB)
                tt("dve", pr(5), pr(1), pr(3), ALU.mult, PB, PB)
                act(pr(6), pr(4), AF.Exp, PB, PB)
                range_reduce(kfs, kfs.t[:, 0:8], kis, kis.t[:, 0:8], angs.t[:, 0:8], pr(5), 0.0, PB, [angs.b])
                act(pr(7), angs.t[:, 0:8], AF.Sin, [angs.b], PB)
                range_reduce(kfs, kfs.t[:, 0:8], kis, kis.t[:, 0:8], angs.t[:, 0:8], pr(5), PI / 2, PB, [angs.b])
                act(pr(8), angs.t[:, 0:8], AF.Sin, [angs.b], PB)
                tt("dve", pr(9), pr(6), pr(8), ALU.mult, PB, PB)
                tt("dve", pr(10), pr(6), pr(7), ALU.mult, PB, PB)
                tt("dve", pr(11), pr(0), pr(0), ALU.mult, PB, PB)
                tt("dve", pr(15), pr(1), pr(1), ALU.mult, PB, PB)
                tt("dve", pr(11), pr(11), pr(15), ALU.add, PB, PB)
                P.op("dve", lambda e: e.reciprocal(pr(11), pr(11)), PB, PB)
                ts("dve", pr(12), pr(9), -1.0, None, ALU.add, None, PB, PB)
                tt("dve", pr(13), pr(12), pr(0), ALU.mult, PB, PB)
                tt("dve", pr(15), pr(10), pr(1), ALU.mult, PB, PB)
                tt("dve", pr(13), pr(13), pr(15), ALU.add, PB, PB)
                tt("dve", pr(13), pr(13), pr(11), ALU.mult, PB, PB)
                tt("dve", pr(14), pr(10), pr(0), ALU.mult, PB, PB)
                tt("dve", pr(15), pr(12), pr(1), ALU.mult, PB, PB)
                tt("dve", pr(14), pr(14), pr(15), ALU.subtract, PB, PB)
                tt("dve", pr(14), pr(14), pr(11), ALU.mult, PB, PB)
                if l == DL:
                    dump("prm", prm, prm.t[:], lambda o: o[:, :, :])
                for r in range(8):
                    fre = prm.t[:, 13, r:r + 1]
                    fim = prm.t[:, 14, r:r + 1]
                    ts("dve", bb.t[:, 2, r, :], bb.t[:, 0, r, :], fre, None, ALU.mult, None, [bb.b, prm.b], [bb.b])
                    ts("dve", bb.t[:, 3, r, :], bb.t[:, 1, r, :], fim, None, ALU.mult, None, [bb.b, prm.b], [bb.b])
                    tt("dve", bb.t[:, 2, r, :], bb.t[:, 2, r, :], bb.t[:, 3, r, :], ALU.subtract, [bb.b], [bb.b])
                    ts("dve", bb.t[:, 3, r, :], bb.t[:, 1, r, :], fre, None, ALU.mult, None, [bb.b, prm.b], [bb.b])
                    stt("dve", bb.t[:, 3, r, :], bb.t[:, 0, r, :], fim, bb.t[:, 3, r, :], ALU.mult, ALU.add,
                        [bb.b, prm.b], [bb.b])
                    gp = r % 4
                    for ri in range(2):
                        P.op("pool", lambda e: e.memset(Wsc.t[:], 0.0), [], [Wsc.b])
                        for g2 in range(2):
                            c0_ = 16 * (2 * gp + g2)
                            cp("dve", Wsc.t[g2 * 64:(g2 + 1) * 64, c0_:c0_ + 16], bb.t[g2 * 64:(g2 + 1) * 64, 2 + ri, r, :],
                               [bb.b], [Wsc.b])
                        pb_ = bank()
                        tr(pb_, pb_.t[:, 0:128], Wsc.t[:], ident.t[:], [Wsc.b, ident.b])
                        act(lB.t[:, r * 2 + ri, :], pb_.t[:, 0:128], AF.Identity, [pb_.b], [lB.b])
                        P.op("pool", lambda e, r=r, ri=ri: e.memset(lC.t[:, r * 2 + ri, :], 0.0), [], [lC.b])
                        for g2 in range(2):
                            c0_ = 16 * (2 * gp + g2)
                            ts("dve", lC.t[g2 * 64:(g2 + 1) * 64, r * 2 + ri, c0_:c0_ + 16],
                               cc_.t[g2 * 64:(g2 + 1) * 64, ri, r, :], (1.0 if ri == 0 else -1.0), None, ALU.mult, None,
                               [cc_.b], [lC.b])
                chunks = [(0, CTX)] + [(CTX + 512 * k, 512) for k in range(16)]
                wk_rr = 0
                for d_ in range(2):
                    for gp in range(4):
                        r = d_ * 4 + gp
                        ts("dve", angs.t[:], iota.t[:], prm.t[:, 5, r:r + 1], None, ALU.mult, None, [iota.b, prm.b], [angs.b])
                        range_reduce(kfs, kfs.t[:], kis, kis.t[:], tabs.t[:, gp, :], angs.t[:], 0.0, [angs.b], [tabs.bs[gp]])
                        act(tabs.t[:, gp, :], tabs.t[:, gp, :], AF.Sin, [tabs.bs[gp]], [tabs.bs[gp]])
                        range_reduce(kfs, kfs.t[:], kis, kis.t[:], tabc.t[:, gp, :], angs.t[:], PI / 2, [angs.b], [tabc.bs[gp]])
                        act(tabc.t[:, gp, :], tabc.t[:, gp, :], AF.Sin, [tabc.bs[gp]], [tabc.bs[gp]])
                        ts("dve", rhoT.t[:, gp, :], zeros.t[:], prm.t[:, 6, r:r + 1], None, ALU.add, None, [zeros.b, prm.b],
                           [rhoT.bs[gp]])
                        P.op("pool", lambda e, gp=gp: e.memset(init.t[:, gp, :], 0.0), [], [init.bs[gp]])
                    for ci, (c0, n) in enumerate(chunks):
                        if d_ == 0:
                            nat_c0, slot = c0, ci
                            u_ap = uTf.t[:, c0:c0 + n]
                            u_reads = [uTf.bs[ci]]
                        else:
                            if ci == 0:
                                nat_c0, slot = 0, 0
                            else:
                                nkk = 16 - ci
                                nat_c0, slot = CTX + 512 * nkk, 1 + nkk
                            s_u = ci % 2
                            cp("dve", ub.t[:, s_u, 0:n], AP_rev(uTf, STREAM, nat_c0 + n - 1, n), [uTf.bs[slot]], [ub.bs[s_u]])
                            u_ap = ub.t[:, s_u, 0:n]
                            u_reads = [ub.bs[s_u]]
                        need = ctx_out or ci > 0
                        yps = bank()
                        excl = (PS.index(yps),)
                        for gp in range(4):
                            r = d_ * 4 + gp
                            ws = wk[wk_rr % NW]
                            xb_ = xrb[wk_rr % NW]
                            wk_rr += 1
                            pre = bank(exclude=excl)
                            pim = bank(exclude=excl)
                            mm(pre, pre.t[:, 0:n], lB.t[:, r * 2, :], u_ap, [lB.b] + u_reads)
                            mm(pim, pim.t[:, 0:n], lB.t[:, r * 2 + 1, :], u_ap, [lB.b] + u_reads)
                            c_ap = tabc.t[:, gp, 0:n]
                            s_ap = tabs.t[:, gp, 0:n]
                            TB = [tabc.bs[gp], tabs.bs[gp]]
                            t1, t2, t3, t4, zr, zi = [w_.t[:, 0:n] for w_ in ws]
                            b1, b2, b3, b4, bzr, bzi = [w_.b for w_ in ws]
                            tt("dve", t1, pre.t[:, 0:n], c_ap, ALU.mult, [pre.b] + TB, [b1])
                            tt("dve", t2, pim.t[:, 0:n], s_ap, ALU.mult, [pim.b] + TB, [b2])
                            tt("pool", t1, t1, t2, ALU.add, [b1, b2], [b1])
                            tt("dve", t3, pim.t[:, 0:n], c_ap, ALU.mult, [pim.b] + TB, [b3])
                            tt("dve", t4, pre.t[:, 0:n], s_ap, ALU.mult, [pre.b] + TB, [b4])
                            tt("pool", t3, t3, t4, ALU.subtract, [b3, b4], [b3])
                            P.op("dve", lambda e, zr=zr, t1=t1, gp=gp, n=n: e.tensor_tensor_scan(
                                zr, rhoT.t[:, gp, 0:n], t1, init.t[:, gp, 0:1], ALU.mult, ALU.add),
                                [rhoT.bs[gp], b1, init.bs[gp]], [bzr])
                            P.op("dve", lambda e, zi=zi, t3=t3, gp=gp, n=n: e.tensor_tensor_scan(
                                zi, rhoT.t[:, gp, 0:n], t3, init.t[:, gp, 1:2], ALU.mult, ALU.add),
                                [rhoT.bs[gp], b3, init.bs[gp]], [bzi])
                            tt("dve", t1, zr, c_ap, ALU.mult, [bzr] + TB, [b1])
                            tt("dve", t2, zi, s_ap, ALU.mult, [bzi] + TB, [b2])
                            tt("dve", t3, zr, s_ap, ALU.mult, [bzr] + TB, [b3])
                            tt("dve", t4, zi, c_ap, ALU.mult, [bzi] + TB, [b4])
                            tt("pool", xb_[0].t[:, 0:n], t1, t2, ALU.subtract, [b1, b2], [xb_[0].b])
                            tt("dve", xb_[1].t[:, 0:n], t3, t4, ALU.add, [b3, b4], [xb_[1].b])
                            tt("dve", init.t[:, gp, 0:1], ws[0].t[:, n - 1:n], ws[1].t[:, n - 1:n], ALU.subtract, [b1, b2],
                               [init.bs[gp]])
                            tt("dve", init.t[:, gp, 1:2], ws[2].t[:, n - 1:n], ws[3].t[:, n - 1:n], ALU.add, [b3, b4],
                               [init.bs[gp]])
                            if need:
                                mm(yps, yps.t[:, 0:n], lC.t[:, r * 2, :], xb_[0].t[:, 0:n], [lC.b, xb_[0].b], gp == 0, False)
                                mm(yps, yps.t[:, 0:n], lC.t[:, r * 2 + 1, :], xb_[1].t[:, 0:n], [lC.b, xb_[1].b], False, gp == 3)
                        if not need:
                            continue
                        if d_ == 0:
                            fs_ = ci % 2
                            act(yfs.t[:, fs_, 0:n], yps.t[:, 0:n], AF.Identity, [yps.b], [yfs.bs[fs_]])
                            dma("sp", yTf_d.t[:, c0:c0 + n], yfs.t[:, fs_, 0:n], [yfs.bs[fs_]], [yTf_d.b])
                        else:
                            ys = ci % 3
                            zs = ci % 2
                            ya = ytmp.t[:, ys, 0:n]
                            YB = [ytmp.bs[ys]]
                            yb2 = ytmp.t[:, (ys + 1) % 3, 0:n]
                            YB2 = [ytmp.bs[(ys + 1) % 3]]
                            fs_ = ci % 2
                            dma("sp", yfs.t[:, fs_, 0:n], yTf_d.t[:, nat_c0:nat_c0 + n], [yTf_d.b], [yfs.bs[fs_]])
                            act(ya, yps.t[:, 0:n], AF.Identity, [yps.b], YB)
                            tt("dve", yb2, AP_rev(ytmp, 3 * 512, ys * 512 + n - 1, n), yfs.t[:, fs_, 0:n], ALU.add,
                               YB + [yfs.bs[fs_]], YB2)
                            stt("dve", yb2, uTf.t[:, nat_c0:nat_c0 + n], dsk.t[:, 0:1], yb2, ALU.mult, ALU.add,
                                [uTf.bs[slot], dsk.b] + YB2, YB2)
                            if l == DL and ci in (0, 16):
                                cc0 = 0 if ci == 0 else 256
                                dump("ssmy", YB2, yb2, lambda o, cc0=cc0, n=n: o[:, cc0:cc0 + n])
                            act(ya, yb2, AF.Square, YB2, YB)
                            ts("dve", ya, ya, 0.044715, 1.0, ALU.mult, ALU.add, YB, YB)
                            tt("pool", ya, ya, yb2, ALU.mult, YB + YB2, YB)
                            act(ya, ya, AF.Sigmoid, YB, YB, scale=1.5957691216057308)
                            tt("pool", zst.t[:, zs, 0:n], ya, yb2, ALU.mult, YB + YB2, [zst.bs[zs]])
                            if ci == 0:
                                pc_, dcol = 2, 0
                            else:
                                pc_, dcol = (nat_c0 - CTX) // 4096, (nat_c0 - CTX) % 4096
                            dma("sp", ag2_in[pc_].t[:, dcol:dcol + n], zst.t[:, zs, 0:n], [zst.bs[zs]], [ag2_in[pc_].b])
                for pc_ in range(3 if ctx_out else 2):
                    P.dma("pool", lambda e, pc_=pc_: e.collective_compute(
                        "AllGather", ALU.bypass, replica_groups=GROUPS, ins=[ag2_in[pc_].t.opt()], outs=[ag2_out[pc_].t.opt()]),
                        [ag2_in[pc_].b], [ag2_out[pc_].b], inc=1)
                P.barrier()
            if dbg.get("_stop") == "C" and l == DL:
                stopped = True

            if not stopped:
              with ExitStack() as ph:
                wq = sbuf(ph, "wq", [128, 8, 1024], BF16)
                hT = sbuf(ph, "hTd", [128, 8, 512], BF16)
                qT = sbuf(ph, "qT", [128, 4, 512], BF16)
                pt = sbuf(ph, "pt", [128, 3, 512], BF16, 3)
                sg = sbuf(ph, "sg", [128, 2, 512], F32, 2)
                ta = sbuf(ph, "ta", [128, 2, 512], F32, 2)
                rec = sbuf(ph, "rec", [128, 512])
                attnT = sbuf(ph, "attnT", [128, 2, 4, 512], BF16, 2)
                cosT = sbuf(ph, "cosT", [128, TALL])
                sinT = sbuf(ph, "sinT", [128, TALL])
                masks = sbuf(ph, "masks", [128, 4, 512], BF16)
                sinkE = sbuf(ph, "sinkE", [128, 8])
                sinkrow = sbuf(ph, "sinkrow", [128, 512])
                dma("sp", cosT.t[:], cos_d.t[:, :], [cos_d.b], [cosT.b])
                dma("sp", sinT.t[:], sin_d.t[:, :], [sin_d.b], [sinT.b])
                for k4 in range(4):
                    dma("pool", masks.t[:, k4, :], masks_d.t[:, k4, :], [], [masks.b])
                dma("sp", wq.t[:, :, :], WB["w1"].t[:, :, 0:1024], [WB["w1"].b], [wq.b])
                dma("sp", sinkE.t[:], W["sink"].t[:, :], [], [sinkE.b])
                act(sinkE.t[:], sinkE.t[:], AF.Exp, [sinkE.b], [sinkE.b])
                for c in range(4):
                    ts("dve", sinkrow.t[0:64, c * 128:(c + 1) * 128], zeros.t[0:64, 0:128], sinkE.t[0:64, c:c + 1], None,
                       ALU.add, None, [zeros.b, sinkE.b], [sinkrow.b])
                    ts("dve", sinkrow.t[64:128, c * 128:(c + 1) * 128], zeros.t[64:128, 0:128], sinkE.t[64:128, 4 + c:5 + c],
                       None, ALU.add, None, [zeros.b, sinkE.b], [sinkrow.b])
                nchunks = 5 if ctx_out else 4
                pt_rr = 0
                sg_rr = 0
                for ch in range(nchunks):
                    tiles = list(range(ch * 4, min(ch * 4 + 4, NT)))
                    n = len(tiles) * 128
                    c0 = ch * 512
                    is_ctx = ch == 4
                    for ti, t_ in enumerate(tiles):
                        norm_tile(t_, A1, 0, lambda fc, ti=ti: hT.t[:, fc, ti * 128:(ti + 1) * 128], [hT.b])
                    for c in range(4):
                        pq = bank()
                        pqr = bank()
                        for kc in range(8):
                            mm(pq, pq.t[:, 0:n], wq.t[:, kc, c * 128:(c + 1) * 128], hT.t[:, kc, 0:n], [wq.b, hT.b], kc == 0, kc == 7)
                        for kc in range(8):
                            mm(pqr, pqr.t[:, 0:n], wq.t[:, kc, 512 + c * 128:512 + (c + 1) * 128], hT.t[:, kc, 0:n], [wq.b, hT.b],
                               kc == 0, kc == 7)
                        s_ = sg_rr % 2
                        sg_rr += 1
                        tt("dve", sg.t[:, s_, 0:n], pq.t[:, 0:n], cosT.t[:, c0:c0 + n], ALU.mult, [pq.b, cosT.b], [sg.bs[s_]])
                        tt("dve", ta.t[:, s_, 0:n], pqr.t[:, 0:n], sinT.t[:, c0:c0 + n], ALU.mult, [pqr.b, sinT.b], [ta.bs[s_]])
                        tt("dve", qT.t[:, c, 0:n], sg.t[:, s_, 0:n], ta.t[:, s_, 0:n], ALU.add, [sg.bs[s_], ta.bs[s_]], [qT.b])
                    as_ = ch % 2
                    for qi, t_ in enumerate(tiles):
                        pnum = bank()
                        pden = bank()
                        excl = (PS.index(pnum), PS.index(pden))
                        if is_ctx:
                            kbl = [(18, None), (19, None)]
                        else:
                            kbl = [(t_, 0 if t_ == 0 else 1), (t_ + 1, None), (t_ + 2, 2 if t_ == NT_OWN - 1 else 3),
                                   (18, None), (19, None)]
                        for gk in range(2):
                            pb = 64 * gk

                            def pv_(bi, kb, s_, pb=pb):
                                mm(pnum, pnum.t[pb:pb + 64, :], vv.t[:, kb, pb:pb + 64], pt.t[:, s_, :], [vv.bs[kb], pt.bs[s_]],
                                   bi == 0, bi == len(kbl) - 1)
                                mm(pden, pden.t[pb:pb + 64, :], onesb.t[:, 0:64], pt.t[:, s_, :], [onesb.b, pt.bs[s_]],
                                   bi == 0, bi == len(kbl) - 1)

                            prev_ = None
                            for bi, (kb, mk) in enumerate(kbl):
                                pst = bank(exclude=excl)
                                for c in range(4):
                                    mm(pst, pst.t[:, c * 128:(c + 1) * 128], kT.t[pb:pb + 64, kb, :],
                                       qT.t[pb:pb + 64, c, qi * 128:(qi + 1) * 128], [kT.bs[kb], qT.b])
                                s_ = pt_rr % 3
                                pt_rr += 1
                                act(pt.t[:, s_, :], pst.t[:, :], AF.Exp, [pst.b], [pt.bs[s_]], scale=0.125)
                                if mk is not None:
                                    tt("dve", pt.t[:, s_, :], pt.t[:, s_, :], masks.t[:, mk, :], ALU.mult, [pt.bs[s_], masks.b],
                                       [pt.bs[s_]])
                                if prev_ is not None:
                                    pv_(*prev_)
                                prev_ = (bi, kb, s_)
                            pv_(*prev_)
                        tt("dve", rec.t[:], pden.t[:, :], sinkrow.t[:], ALU.add, [pden.b, sinkrow.b], [rec.b])
                        P.op("dve", lambda e: e.reciprocal(rec.t[:], rec.t[:]), [rec.b], [rec.b])
                        for c in range(4):
                            tt("dve", attnT.t[:, as_, c, qi * 128:(qi + 1) * 128], pnum.t[:, c * 128:(c + 1) * 128],
                               rec.t[:, c * 128:(c + 1) * 128], ALU.mult, [pnum.b, rec.b], [attnT.bs[as_]])
                    dma("sp", attn_d.t[:, :, c0:c0 + n], attnT.t[:, as_, :, 0:n], [attnT.bs[as_]], [attn_d.b])
                P.barrier()
            kvst.close()
            if dbg.get("_stop") == "D1" and l == DL:
                stopped = True

            if not stopped:
              with ExitStack() as ph:
                wglu = sbuf(ph, "wglu", [128, 4, 512], BF16)
                wbra = sbuf(ph, "wbra", [128, 4, D], BF16)
                wbrs = sbuf(ph, "wbrs", [128, 4, D], BF16)
                wout = sbuf(ph, "wout", [128, 8, D], BF16)
                w2s = sbuf(ph, "w2s", [128, 2, 8, 256], BF16, 2)
                hT = sbuf(ph, "hTd2", [128, 8, 512], BF16)
                zstage = sbuf(ph, "zstage", [128, 4, 512], BF16)
                zsb = sbuf(ph, "zsb", [128, 4, 512], BF16)
                ssmT = sbuf(ph, "ssmT", [128, 4, 512], BF16)
                mT = sbuf(ph, "mT", [128, 8, 512], BF16)
                sg = sbuf(ph, "sg2", [128, 2, 512], F32, 2)
                ta = sbuf(ph, "ta2", [128, 2, 512], F32, 2)
                selm = sbuf(ph, "selm2", [128, 4, 128], BF16)
                gt1row = sbuf(ph, "gt1row", [128, 2, D])
                attc = sbuf(ph, "attc", [128, 4, 512], BF16)
                dma("pool", selm.t[:], selmat_d.t[:, 0:4, :], [], [selm.b])
                dma("sp", wout.t[:, :, :], WB["wout"].t[:, :, :], [WB["wout"].b], [wout.b])
                dma("sp", wglu.t[:, :, :], WB["wglu"].t[:, :, :], [WB["wglu"].b], [wglu.b])
                dma("sp", wbra.t[:, :, :], WB["wbra"].t[:, :, :], [WB["wbra"].b], [wbra.b])
                dma("sp", wbrs.t[:, :, :], WB["wbrs"].t[:, :, :], [WB["wbrs"].b], [wbrs.b])
                make_gtrow(gt1row, 2)
                nchunks = 5 if ctx_out else 4
                sg_rr = 0
                w2_rr = 0
                for ch in range(nchunks):
                    tiles = list(range(ch * 4, min(ch * 4 + 4, NT)))
                    n = len(tiles) * 128
                    c0 = ch * 512
                    is_ctx = ch == 4
                    cls = 1 if is_ctx else 0
                    for ti, t_ in enumerate(tiles):
                        norm_tile(t_, A1, 0, lambda fc, ti=ti: hT.t[:, fc, ti * 128:(ti + 1) * 128], [hT.b])
                    dma("sp", attc.t[:, :, 0:n], attn_d.t[:, :, c0:c0 + n], [attn_d.b], [attc.b])
                    for sl in range(4):
                        if is_ctx:
                            dma("sp", zsb.t[:, sl, 0:n], ag2_out[2].t[sl * 128:(sl + 1) * 128, :], [ag2_out[2].b], [zsb.b])
                        else:
                            for i in range(4):
                                gc_ = i * TOWN + c0
                                dma("sp", zstage.t[:, i, :],
                                    ag2_out[gc_ // 4096].t[sl * 128:(sl + 1) * 128, gc_ % 4096:gc_ % 4096 + 512],
                                    [ag2_out[gc_ // 4096].b], [zstage.b])
                            pz = bank()
                            for i in range(4):
                                mm(pz, pz.t[:, :], selm.t[:, i, :], zstage.t[:, i, :], [selm.b, zstage.b], i == 0, i == 3)
                            act(zsb.t[:, sl, :], pz.t[:, :], AF.Identity, [pz.b], [zsb.b])
                    for fc in range(4):
                        pg = bank()
                        for sl in range(4):
                            mm(pg, pg.t[:, 0:n], wglu.t[:, sl, fc * 128:(fc + 1) * 128], zsb.t[:, sl, 0:n], [wglu.b, zsb.b],
                               sl == 0, sl == 3)
                        s_ = sg_rr % 2
                        sg_rr += 1
                        act(sg.t[:, s_, 0:n], pg.t[:, 0:n], AF.Sigmoid, [pg.b], [sg.bs[s_]])
                        tt("dve", ssmT.t[:, fc, 0:n], zsb.t[:, fc, 0:n], sg.t[:, s_, 0:n], ALU.mult, [zsb.b, sg.bs[s_]], [ssmT.b])
                    if l == DL and ch == 0 and "ssmT" in dbg_out:
                        sdb = sbuf(ph, "sdb", [128, 4, 512])
                        cp("dve", sdb.t[:], ssmT.t[:], [ssmT.b], [sdb.b])
                        dump("ssmT", sdb, sdb.t[:], lambda o: o[:, :, :])
                    for fc in range(8):
                        ws_ = w2_rr % 2
                        w2_rr += 1
                        dma("sp", w2s.t[:, ws_, :, :], WB["w2g"].t[fc, :, :, :], [WB["w2g"].b], [w2s.bs[ws_]])
                        pA = bank()
                        pS = bank()
                        pga = bank()
                        pgs = bank()
                        fs = slice(fc * 128, (fc + 1) * 128)
                        for c in range(4):
                            mm(pA, pA.t[:, 0:n], wbra.t[:, c, fs], attc.t[:, c, 0:n], [wbra.b, attc.b], c == 0, c == 3)
                        for c in range(4):
                            mm(pS, pS.t[:, 0:n], wbrs.t[:, c, fs], ssmT.t[:, c, 0:n], [wbrs.b, ssmT.b], c == 0, c == 3)
                        for kc in range(8):
                            mm(pga, pga.t[:, 0:n], w2s.t[:, ws_, kc, 0:128], hT.t[:, kc, 0:n], [w2s.bs[ws_], hT.b], kc == 0, kc == 7)
                        for kc in range(8):
                            mm(pgs, pgs.t[:, 0:n], w2s.t[:, ws_, kc, 128:256], hT.t[:, kc, 0:n], [w2s.bs[ws_], hT.b], kc == 0, kc == 7)
                        act(sg.t[:, 0, 0:n], pga.t[:, 0:n], AF.Sigmoid, [pga.b], [sg.bs[0]])
                        act(sg.t[:, 1, 0:n], pgs.t[:, 0:n], AF.Sigmoid, [pgs.b], [sg.bs[1]])
                        tt("dve", ta.t[:, 0, 0:n], pA.t[:, 0:n], sg.t[:, 0, 0:n], ALU.mult, [pA.b, sg.bs[0]], [ta.bs[0]])
                        tt("dve", ta.t[:, 1, 0:n], pS.t[:, 0:n], sg.t[:, 1, 0:n], ALU.mult, [pS.b, sg.bs[1]], [ta.bs[1]])
                        tt("pool", mT.t[:, fc, 0:n], ta.t[:, 0, 0:n], ta.t[:, 1, 0:n], ALU.add, [ta.bs[0], ta.bs[1]], [mT.b])
                    for ti, t_ in enumerate(tiles):
                        for half in range(2):
                            py = bank()
                            hs_ = slice(half * 512, (half + 1) * 512)
                            for kc in range(8):
                                mm(py, py.t[:, :], mT.t[:, kc, ti * 128:(ti + 1) * 128], wout.t[:, kc, hs_], [mT.b, wout.b],
                                   kc == 0, kc == 7)
                            s_ = sg_rr % 2
                            sg_rr += 1
                            tt("dve", sg.t[:, s_, :], py.t[:, :], gt1row.t[:, cls, hs_], ALU.mult, [py.b, gt1row.b], [sg.bs[s_]])
                            tt("pool", x_tm.t[:, t_, hs_], x_tm.t[:, t_, hs_], sg.t[:, s_, :], ALU.add, [sg.bs[s_], x_tm.bs[t_]],
                               [x_tm.bs[t_]])
                P.barrier()
            if l == DL and not stopped:
                dump("xmix", x_tm.bs, x_tm.t[:], lambda o: o[:, :, :])
            if dbg.get("_stop") == "D2" and l == DL:
                stopped = True
            if stopped:
                break

            if not stopped:
              with ExitStack() as ph:
                ntl = NT if ctx_out else NT_OWN
                h2T = sbuf(ph, "h2T", [128, 8, TALL], BF16, NT)
                h2f = sbuf(ph, "h2f", [128, 8, 128])
                wr = sbuf(ph, "wr", [128, 8, 36])
                Wt = sbuf(ph, "Wt", [128, NT, 32], F32, NT)
                lg = sbuf(ph, "lg", [128, 36])
                rs = sbuf(ph, "rs", [128, 16])
                rt = sbuf(ph, "rt", [128, 4, 32])
                weg = sbuf(ph, "weg", [128, 2, 8, 512], BF16, 2)
                weu = sbuf(ph, "weu", [128, 2, 8, 512], BF16, 2)
                wed = sbuf(ph, "wed", [128, 2, 4, D], BF16, 2)
                hid = sbuf(ph, "hid", [128, 2, 4, 512], BF16, 2)
                sgm = sbuf(ph, "sgm", [128, 2, 512], F32, 2)
                ty = sbuf(ph, "ty", [128, 2, 512], F32, 2)
                gt2row = sbuf(ph, "gt2row", [128, 2, D])
                dma("sp", wr.t[:], W["wr"].t[:, :, :], [], [wr.b])
                make_gtrow(gt2row, 5)
                compute_rstd(ntl)
                RB = [rs.b]
                c_ = lambda i: rs.t[:, i:i + 1]
                def route_tile(t_):
                    norm_tile(t_, A2, 3, lambda fc, t_=t_: h2T.t[:, fc, t_ * 128:(t_ + 1) * 128], [h2T.bs[t_]], f32_dst=h2f)
                    pl = bank()
                    for kc in range(8):
                        mm(pl, pl.t[:, 0:36], h2f.t[:, kc, :], wr.t[:, kc, :], [h2f.b, wr.b], kc == 0, kc == 7)
                    cp("dve", lg.t[:], pl.t[:, 0:36], [pl.b], [lg.b])
                    P.op("dve", lambda e: e.tensor_reduce(rs.t[:, 0:1], lg.t[:, 0:4], AX.X, ALU.max), [lg.b], RB)
                    ts("dve", c_(1), c_(0), -1.0, None, ALU.mult, None, RB, RB)
                    act(rt.t[:, 0, 0:4], lg.t[:, 0:4], AF.Exp, [lg.b] + RB, [rt.b, rs.b], bias=rs.t[:, 1:2], accum=rs.t[:, 2:3])
                    P.op("dve", lambda e: e.reciprocal(rs.t[:, 3:4], rs.t[:, 2:3]), RB, RB)
                    ts("dve", rt.t[:, 0, 4:8], lg.t[:, 0:4], rs.t[:, 0:1], None, ALU.is_equal, None, [lg.b] + RB, [rt.b])
                    ts("dve", rt.t[:, 0, 4:8], rt.t[:, 0, 4:8], -1.0, 1e30, ALU.add, ALU.mult, [rt.b], [rt.b])
                    for g in range(4):
                        ts("dve", rt.t[:, 1, 8 * g:8 * g + 8], lg.t[:, 4 + 8 * g:12 + 8 * g], rt.t[:, 0, 4 + g:5 + g], None,
                           ALU.add, None, [lg.b, rt.b], [rt.b])
                    P.op("dve", lambda e: e.tensor_reduce(rs.t[:, 4:5], rt.t[:, 1, :], AX.X, ALU.max), [rt.b], RB)
                    ts("dve", rt.t[:, 2, :], rt.t[:, 1, :], rs.t[:, 4:5], None, ALU.is_equal, None, [rt.b] + RB, [rt.b])
                    stt("dve", rt.t[:, 2, :], rt.t[:, 2, :], -1e30, rt.t[:, 1, :], ALU.mult, ALU.add, [rt.b], [rt.b])
                    P.op("dve", lambda e: e.tensor_reduce(rs.t[:, 5:6], rt.t[:, 2, :], AX.X, ALU.max), [rt.b], RB)
                    ts("dve", rt.t[:, 2, :], rt.t[:, 1, :], rs.t[:, 5:6], None, ALU.is_ge, None, [rt.b] + RB, [rt.b])
                    ts("dve", c_(6), c_(4), -1.0, None, ALU.mult, None, RB, RB)
                    act(rt.t[:, 3, :], rt.t[:, 1, :], AF.Exp, [rt.b] + RB, [rt.b], bias=rs.t[:, 6:7])
                    tt("dve", rt.t[:, 3, :], rt.t[:, 3, :], rt.t[:, 2, :], ALU.mult, [rt.b], [rt.b])
                    P.op("dve", lambda e: e.tensor_reduce(rs.t[:, 7:8], rt.t[:, 3, :], AX.X, ALU.add), [rt.b], RB)
                    P.op("dve", lambda e: e.reciprocal(rs.t[:, 7:8], rs.t[:, 7:8]), RB, RB)
                    tt("dve", c_(7), c_(7), c_(3), ALU.mult, RB, RB)
                    ts("dve", Wt.t[:, t_, :], rt.t[:, 3, :], rs.t[:, 7:8], None, ALU.mult, None, [rt.b] + RB, [Wt.bs[t_]])

                nch = 5 if ctx_out else 4
                ty_rr = 0
                nexp = dbg.get("_nexp", NEXP)
                for e_ in range(nexp):
                    s = e_ % 2
                    for k2 in range(2):
                        dma("pool", weg.t[:, s, 4 * k2:4 * k2 + 4, :], W["weg"].t[e_, :, 4 * k2:4 * k2 + 4, :], [W["weg"].b], [weg.bs[s]])
                        dma("pool", weu.t[:, s, 4 * k2:4 * k2 + 4, :], W["weu"].t[e_, :, 4 * k2:4 * k2 + 4, :], [W["weu"].b], [weu.bs[s]])
                        dma("pool", wed.t[:, s, 2 * k2:2 * k2 + 2, :], W["wed"].t[e_, :, 2 * k2:2 * k2 + 2, :], [W["wed"].b], [wed.bs[s]])
                    for ch in range(nch):
                        tiles = list(range(ch * 4, min(ch * 4 + 4, NT)))
                        n = len(tiles) * 128
                        c0 = ch * 512
                        cls = 1 if ch == 4 else 0
                        hs = ch % 2
                        if e_ == 0:
                            for t_ in tiles:
                                route_tile(t_)
                        for hc in range(4):
                            pg = bank()
                            pu = bank()
                            hrd = [h2T.bs[t_] for t_ in tiles]
                            for kc in range(8):
                                mm(pg, pg.t[:, 0:n], weg.t[:, s, kc, hc * 128:(hc + 1) * 128], h2T.t[:, kc, c0:c0 + n],
                                   [weg.bs[s]] + hrd, kc == 0, kc == 7)
                            for kc in range(8):
                                mm(pu, pu.t[:, 0:n], weu.t[:, s, kc, hc * 128:(hc + 1) * 128], h2T.t[:, kc, c0:c0 + n],
                                   [weu.bs[s]] + hrd, kc == 0, kc == 7)
                            ss_ = hc % 2
                            act(sgm.t[:, ss_, 0:n], pg.t[:, 0:n], AF.Silu, [pg.b], [sgm.bs[ss_]])
                            tt("dve", hid.t[:, hs, hc, 0:n], pu.t[:, 0:n], sgm.t[:, ss_, 0:n], ALU.mult, [pu.b, sgm.bs[ss_]],
                               [hid.bs[hs]])
                        for ti, t_ in enumerate(tiles):
                            for half in range(2):
                                py = bank()
                                hs_ = slice(half * 512, (half + 1) * 512)
                                for hc in range(4):
                                    mm(py, py.t[:, :], hid.t[:, hs, hc, ti * 128:(ti + 1) * 128], wed.t[:, s, hc, hs_],
                                       [hid.bs[hs], wed.bs[s]], hc == 0, hc == 3)
                                y_ = ty_rr % 2
                                ty_rr += 1
                                tt("dve", ty.t[:, y_, :], py.t[:, :], gt2row.t[:, cls, hs_], ALU.mult, [py.b, gt2row.b],
                                   [ty.bs[y_]])
                                stt("dve", x_tm.t[:, t_, hs_], ty.t[:, y_, :], Wt.t[:, t_, e_:e_ + 1], x_tm.t[:, t_, hs_],
                                    ALU.mult, ALU.add, [ty.bs[y_], Wt.bs[t_], x_tm.bs[t_]], [x_tm.bs[t_]])
                P.barrier()
            if l == DL and not stopped:
                dump("xout", x_tm.bs, x_tm.t[:], lambda o: o[:, :, :])

        with ExitStack() as ph:
            gfin = sbuf(ph, "gfin", [128, D])
            ob = sbuf(ph, "ob", [128, 2, D], F32, 2)
            dma("sp", gfin.t[:], gfin_d.t[:, :], [], [gfin.b])
            compute_rstd(NT_OWN)
            for t_ in range(NT_OWN):
                s = t_ % 2
                stt("dve", ob.t[:, s, :], x_tm.t[:, t_, :], rstd.t[:, t_:t_ + 1], gfin.t[:], ALU.mult, ALU.mult,
                    [x_tm.bs[t_], rstd.b, gfin.b], [ob.bs[s]])
                dma("sp", out_d.t[t_ * 128:(t_ + 1) * 128, :], ob.t[:, s, :], [ob.bs[s]], [out_d.b])
        P.wait_all("sp", [out_d.b] + [v.b for v in dbg_out.values()])
        P.emit()
    return nc, names_in


def _kc(w):
    K, C = w.shape
    return np.ascontiguousarray(w.reshape(K // 128, 128, C).transpose(1, 0, 2))


def prep_inputs(inputs, nlayers=2, nei=NEXP):
    f = lambda a: np.ascontiguousarray(np.asarray(a, dtype=np.float32))
    I_ = {k: f(v) for k, v in inputs.items()}
    shared = {}
    e = np.arange(64)
    partner = np.where((e % 32) < 16, e + 16, e - 16)
    head_order = [h for c in range(4) for h in (c, c + 4)]
    qcols = np.concatenate([h * 64 + np.arange(64) for h in head_order])
    qrcols = np.concatenate([h * 64 + partner for h in head_order])
    kcols = 512 + np.arange(128)
    krcols = 512 + np.concatenate([hk * 64 + partner for hk in range(2)])
    vcols = 640 + np.arange(128)
    ucols = 768 + np.arange(512)
    w1cols = np.concatenate([qcols, qrcols, kcols, krcols, vcols, ucols])
    brarows = np.concatenate([h * 64 + np.arange(64) for h in head_order])
    shared["ident"] = np.eye(128, dtype=np.float32)
    shared["iota"] = np.ascontiguousarray(np.broadcast_to(np.arange(1, 513, dtype=np.float32), (128, 512)))
    shared["gfin"] = np.ascontiguousarray(np.broadcast_to(I_["g_final"], (128, D)))
    ee = np.arange(128) % 64
    ropef = np.stack([(ee % 16).astype(np.float32), np.where((ee % 32) < 16, -1.0, 1.0).astype(np.float32)], 1)
    shared["ropef"] = np.ascontiguousarray(ropef)
    for l in range(nlayers):
        win = I_["w_in"][l]
        shared[f"wmod{l}"] = _kc(I_["w_mod"][l])
        shared[f"bmodT{l}"] = np.ascontiguousarray(I_["b_mod"][l].reshape(48, 128).T)
        shared[f"g1T{l}"] = np.ascontiguousarray(I_["g_norm1"][l].reshape(8, 128).T)
        shared[f"g2T{l}"] = np.ascontiguousarray(I_["g_norm2"][l].reshape(8, 128).T)
        shared[f"w1_{l}"] = _kc(win[:, w1cols])
        ga = _kc(win[:, 1280:2304]).reshape(128, 8, 8, 128)
        gs = _kc(win[:, 2304:3328]).reshape(128, 8, 8, 128)
        shared[f"w2g{l}"] = np.ascontiguousarray(np.concatenate([ga, gs], axis=3).transpose(2, 0, 1, 3))
        shared[f"wglu{l}"] = _kc(I_["w_glu"][l])
        shared[f"wbra{l}"] = _kc(I_["w_br_attn"][l][brarows, :])
        shared[f"wbrs{l}"] = _kc(I_["w_br_ssm"][l])
        shared[f"wout{l}"] = _kc(I_["w_out"][l])
        shared[f"wr{l}"] = _kc(np.concatenate([I_["w_router_group"][l], I_["w_router_expert"][l]], axis=1))
        shared[f"weg{l}"] = np.ascontiguousarray(I_["w_exp_gate"][l][:nei].reshape(nei, 8, 128, 512).transpose(0, 2, 1, 3))
        shared[f"weu{l}"] = np.ascontiguousarray(I_["w_exp_up"][l][:nei].reshape(nei, 8, 128, 512).transpose(0, 2, 1, 3))
        shared[f"wed{l}"] = np.ascontiguousarray(I_["w_exp_down"][l][:nei].reshape(nei, 4, 128, D).transpose(0, 2, 1, 3))
        shared[f"sink{l}"] = np.ascontiguousarray(np.broadcast_to(I_["attn_sink"][l], (128, 8)))
    kk = np.arange(128)[:, None]
    qq = np.arange(128)[None, :]
    mprev = np.tile((kk >= qq).astype(np.float32), (1, 4))
    mnext = np.tile((kk <= qq).astype(np.float32), (1, 4))
    per_core = []
    for r in range(NCORES):
        b, j = r // 4, r % 4
        m = dict(shared)
        t0 = j * TOWN
        m["x_own"] = np.ascontiguousarray(I_["x"][b, t0:t0 + TOWN])
        m["ctx_b"] = np.ascontiguousarray(I_["ctx"][b])
        cT = np.stack([I_["c"][b].reshape(8, 128).T, I_["c_ctx"].reshape(8, 128).T], axis=2)
        m["cT"] = np.ascontiguousarray(cT)
        tpos = np.arange(t0, t0 + TOWN)
        rows = (tpos // 64).astype(np.float32)
        cols = (tpos % 64).astype(np.float32)
        pos = np.zeros((128, TALL), np.float32)
        is_row = (ee % 64) < 32
        pos[:, :TOWN] = np.where(is_row[:, None], rows[None, :], cols[None, :])
        m["pos"] = pos
        mk = np.zeros((128, 4, 512), np.float32)
        mk[:, 0] = mprev if j > 0 else 0.0
        mk[:, 1] = mprev
        mk[:, 2] = mnext if j < 3 else 0.0
        mk[:, 3] = mnext
        m["masks"] = mk
        sel = np.zeros((128, 12, 128), np.float32)
        eye = np.eye(128, dtype=np.float32)
        sel[:, j] = eye
        if j > 0:
            sel[:, 4 + j - 1] = eye
        if j < 3:
            sel[:, 8 + j + 1] = eye
        m["selmat"] = sel
        for l in range(nlayers):
            win = I_["w_in"][l]
            m[f"wus{l}"] = _kc(win[:, 768 + 128 * j:768 + 128 * (j + 1)])
            g0 = 8 * j

            def rowlay(a):
                a = a.reshape((2, 4, 2, 64) + a.shape[3:])
                perm = (2, 3, 0, 1) + tuple(range(4, a.ndim))
                a = a.transpose(perm)
                return np.ascontiguousarray(a.reshape((128, 8) + a.shape[4:]))

            m[f"lamre{l}"] = rowlay(I_["ssm_lam_re"][l][:, g0:g0 + 8])
            m[f"lamim{l}"] = rowlay(I_["ssm_lam_im"][l][:, g0:g0 + 8])
            ldt = np.broadcast_to(I_["ssm_log_dt"][l][:, g0:g0 + 8, None], (2, 8, 64))
            m[f"ldt{l}"] = rowlay(np.ascontiguousarray(ldt))
            m[f"bre{l}"] = rowlay(I_["ssm_b_re"][l][:, g0:g0 + 8])
            m[f"bim{l}"] = rowlay(I_["ssm_b_im"][l][:, g0:g0 + 8])
            m[f"cre{l}"] = rowlay(np.ascontiguousarray(I_["ssm_c_re"][l][:, g0:g0 + 8].transpose(0, 1, 3, 2)))
            m[f"cim{l}"] = rowlay(np.ascontiguousarray(I_["ssm_c_im"][l][:, g0:g0 + 8].transpose(0, 1, 3, 2)))
            m[f"dsk{l}"] = np.ascontiguousarray(I_["ssm_d"][l][128 * j:128 * (j + 1)].reshape(128, 1))
        for k in list(m.keys()):
            if False:
                a = m[k]
                a2 = a.reshape(-1, a.shape[-1])
                rr = a2.shape[0] // NCORES
                m[k] = np.ascontiguousarray(a2[r * rr:(r + 1) * rr])
        per_core.append(m)
    return per_core


_CACHE = {}


def kernel(**inputs):
    if "nc" not in _CACHE:
        _CACHE["nc"] = build_program(2)
    nc, names = _CACHE["nc"]
    per_core = prep_inputs(inputs, 2)
    in_maps = [{k: m[k] for k in names} for m in per_core]
    res = run_bass_kernel_spmd(nc, in_maps, core_ids=list(range(NCORES)))
    out = np.zeros((2, SEQ, D), np.float32)
    for r in range(NCORES):
        b, j = r // 4, r % 4
        out[b, j * TOWN:(j + 1) * TOWN] = res.results[r]["out"]
    return out
```

```python
from contextlib import ExitStack
import numpy as np
import concourse.bass as bass
import concourse.mybir as mybir
from concourse.bass_utils import run_bass_kernel_spmd

dt = mybir.dt
ALU = mybir.AluOpType
AF = mybir.ActivationFunctionType
AX = mybir.AxisListType
F32 = dt.float32
BF16 = dt.bfloat16
I32 = dt.int32

NCORES = 8
D = 1024
TOWN = 2048
NT_OWN = 16
NT = 18
TALL = 2304
SEQ = 8192
CTX = 256
STREAM = SEQ + CTX
NEXP = 32
PI = float(np.pi)
GROUPS = [[0, 1, 2, 3], [4, 5, 6, 7]]


class Buf:
    __slots__ = ("name", "last_w", "readers")

    def __init__(self, name=""):
        self.name = name
        self.last_w = None
        self.readers = {}


class Prog:
    ENG = ("pe", "act", "dve", "pool", "sp")

    def __init__(self, nc, stack, ndma=None):
        ndma = ndma or {"sp": 8, "act": 2, "pool": 6, "cc": 3}
        self.nc = nc
        self.q = {e: [] for e in self.ENG}
        self.sems = {}
        self.cnt = {e: 0 for e in self.ENG}
        self.waited = {e: {} for e in self.ENG}
        for e in self.ENG:
            self.sems[("c", e)] = stack.enter_context(nc.semaphore("c_" + e))
        self.dma_slots = {}
        self.dma_tot = {}
        self.dma_rr = {}
        for e, n in ndma.items():
            keys = []
            for i in range(n):
                k = ("d", e, i)
                self.sems[k] = stack.enter_context(nc.semaphore("d_%s%d" % (e, i)))
                self.dma_tot[k] = 0
                keys.append(k)
            self.dma_slots[e] = keys
            self.dma_rr[e] = 0

    def _wait(self, eng, k, v):
        if k == ("c", "pe") and eng == "pe":
            return
        if self.waited[eng].get(k, 0) >= v:
            return
        self.waited[eng][k] = v
        self.q[eng].append(("w", k, v))

    def _deps(self, eng, reads, writes):
        deps = {}

        def add(ev):
            if ev is None:
                return
            k, v = ev
            if deps.get(k, 0) < v:
                deps[k] = v

        for b in reads:
            add(b.last_w)
        for b in writes:
            add(b.last_w)
            for k, v in b.readers.items():
                add((k, v))
        for k, v in deps.items():
            self._wait(eng, k, v)

    def _commit(self, ev, reads, writes):
        k, v = ev
        for b in reads:
            if b.readers.get(k, 0) < v:
                b.readers[k] = v
        for b in writes:
            b.last_w = ev
            b.readers = {}

    def op(self, eng, fn, reads=(), writes=()):
        self._deps(eng, reads, writes)
        self.cnt[eng] += 1
        ev = (("c", eng), self.cnt[eng])
        self.q[eng].append(("o", fn, ("c", eng), 1))
        self._commit(ev, reads, writes)
        return ev

    def dma(self, qeng, fn, reads=(), writes=(), inc=16):
        skey = "cc" if inc == 1 else qeng
        slots = self.dma_slots[skey]
        k = slots[self.dma_rr[skey] % len(slots)]
        self.dma_rr[skey] += 1
        prev = self.dma_tot[k]
        if prev > 0:
            self._wait(qeng, k, prev)
        self._deps(qeng, reads, writes)
        self.dma_tot[k] = prev + inc
        ev = (k, prev + inc)
        self.q[qeng].append(("o", fn, k, inc))
        self._commit(ev, reads, writes)
        return ev

    def wait_all(self, eng, bufs):
        self._deps(eng, bufs, ())

    def barrier(self):
        tot = {("c", e): self.cnt[e] for e in self.ENG}
        tot.update(self.dma_tot)
        for e in self.ENG:
            for k, v in tot.items():
                if v > 0:
                    self._wait(e, k, v)

    def emit(self):
        nc = self.nc
        sems = self.sems

        def replay(e, name):
            for it in self.q[name]:
                if it[0] == "w":
                    e.wait_ge(sems[it[1]], it[2])
                else:
                    ins = it[1](e)
                    ins.then_inc(sems[it[2]], it[3])

        with nc.Block() as block:

            @block.tensor
            def _(e):
                replay(e, "pe")

            @block.vector
            def _(e):
                replay(e, "dve")

            @block.scalar
            def _(e):
                replay(e, "act")

            @block.gpsimd
            def _(e):
                replay(e, "pool")

            @block.sync
            def _(e):
                replay(e, "sp")


class TL:
    def __init__(self, t, nslots=1):
        self.t = t
        self.bs = [Buf() for _ in range(nslots)]

    @property
    def b(self):
        return self.bs[0]


def build_program(nlayers=2, dbg=None):
    dbg = dbg or {}
    NEI = dbg.get("_nexp", NEXP)
    nc = bass.Bass("TRN2", target_bir_lowering=False)
    names_in = []

    def inp(name, shape, d=F32):
        names_in.append(name)
        return TL(nc.dram_tensor(name, list(shape), d, kind="ExternalInput").ap())

    gathers = []

    def ginp(name, shape):
        return inp(name, shape)

    def ginp_unused(name, shape):
        R = int(np.prod(shape[:-1]))
        C = int(shape[-1])
        assert R % NCORES == 0
        names_in.append(name)
        shard = TL(nc.dram_tensor(name, [R // NCORES, C], F32, kind="ExternalInput").ap())
        bounce = TL(nc.dram_tensor(name + "_bnc", [R // NCORES, C], F32).ap())
        full = TL(nc.dram_tensor(name + "_full", list(shape), F32).ap())
        gathers.append((shard, bounce, full))
        return full

    x_own = inp("x_own", [TOWN, D])
    ctx_b = inp("ctx_b", [CTX, D])
    cT = inp("cT", [128, 8, 2])
    pos_d = inp("pos", [128, TALL])
    ropef_d = inp("ropef", [128, 2])
    masks_d = inp("masks", [128, 4, 512])
    selmat_d = inp("selmat", [128, 12, 128])
    ident_d = inp("ident", [128, 128])
    iota_d = inp("iota", [128, 512])
    gfin_d = inp("gfin", [128, D])
    L = []
    for l in range(nlayers):
        w = {}
        w["wmod"] = ginp(f"wmod{l}", [128, 8, 6 * D])
        w["bmodT"] = inp(f"bmodT{l}", [128, 48])
        w["g1T"] = inp(f"g1T{l}", [128, 8])
        w["g2T"] = inp(f"g2T{l}", [128, 8])
        w["w1"] = ginp(f"w1_{l}", [128, 8, 1920])
        w["wus"] = inp(f"wus{l}", [128, 8, 128])
        w["w2g"] = ginp(f"w2g{l}", [8, 128, 8, 256])
        w["wglu"] = inp(f"wglu{l}", [128, 4, 512])
        w["wbra"] = ginp(f"wbra{l}", [128, 4, D])
        w["wbrs"] = ginp(f"wbrs{l}", [128, 4, D])
        w["wout"] = ginp(f"wout{l}", [128, 8, D])
        w["wr"] = inp(f"wr{l}", [128, 8, 36])
        w["weg"] = ginp(f"weg{l}", [NEI, 128, 8, 512])
        w["weu"] = ginp(f"weu{l}", [NEI, 128, 8, 512])
        w["wed"] = ginp(f"wed{l}", [NEI, 128, 4, D])
        w["sink"] = inp(f"sink{l}", [128, 8])
        w["lamre"] = inp(f"lamre{l}", [128, 8])
        w["lamim"] = inp(f"lamim{l}", [128, 8])
        w["ldt"] = inp(f"ldt{l}", [128, 8])
        w["bre"] = inp(f"bre{l}", [128, 8, 16])
        w["bim"] = inp(f"bim{l}", [128, 8, 16])
        w["cre"] = inp(f"cre{l}", [128, 8, 16])
        w["cim"] = inp(f"cim{l}", [128, 8, 16])
        w["dsk"] = inp(f"dsk{l}", [128, 1])
        L.append(w)
    out_d = TL(nc.dram_tensor("out", [TOWN, D], F32, kind="ExternalOutput").ap())
    dbg_out = {}
    for k, shp in dbg.items():
        if k.startswith("_"):
            continue
        dbg_out[k] = TL(nc.dram_tensor("dbg_" + k, list(shp), F32, kind="ExternalOutput").ap())
    AGR = TOWN + 128
    ag1_in = [TL(nc.dram_tensor(f"ag1_in{i}", [r_, 512], BF16).ap()) for i, r_ in enumerate((1024, 1024, 128))]
    ag1_out = [TL(nc.dram_tensor(f"ag1_out{i}", [4 * r_, 512], BF16).ap()) for i, r_ in enumerate((1024, 1024, 128))]
    ag2_in = [TL(nc.dram_tensor(f"ag2_in{i}", [128, c_], BF16).ap()) for i, c_ in enumerate((4096, 4096, CTX))]
    ag2_out = [TL(nc.dram_tensor(f"ag2_out{i}", [512, c_], BF16).ap()) for i, c_ in enumerate((4096, 4096, CTX))]
    cos_d = TL(nc.dram_tensor("cos_d", [128, TALL], F32).ap())
    sin_d = TL(nc.dram_tensor("sin_d", [128, TALL], F32).ap())
    yTf_d = TL(nc.dram_tensor("yTf_d", [128, STREAM], F32).ap())
    attn_d = TL(nc.dram_tensor("attn_d", [128, 4, TALL], BF16).ap())

    with ExitStack() as st:
        P = Prog(nc, st)

        uid = [0]

        def sbuf(stack, name, shape, d=F32, nslots=1):
            uid[0] += 1
            return TL(stack.enter_context(nc.sbuf_tensor("sb%d_%s" % (uid[0], name), list(shape), d)), nslots)

        PS = [TL(st.enter_context(nc.psum_tensor(f"ps{i}", [128, 512], F32))) for i in range(8)]
        ps_rr = [0]

        def bank(exclude=()):
            while True:
                i = ps_rr[0] % 8
                ps_rr[0] += 1
                if i not in exclude:
                    return PS[i]

        def mm(out_tl, out_ap, lhsT_ap, rhs_ap, reads, start=True, stop=True):
            P.op("pe", lambda e: e.matmul(out_ap, lhsT_ap, rhs_ap, start=start, stop=stop), reads, [out_tl.b])

        def tr(out_tl, out_ap, in_ap, ident_ap, reads):
            P.op("pe", lambda e: e.transpose(out_ap, in_ap, ident_ap), reads, [out_tl.b])

        def act(out_ap, in_ap, func, reads, writes, bias=None, scale=None, accum=None):
            kw = {}
            if bias is not None:
                kw["bias"] = bias
            if scale is not None:
                kw["scale"] = scale
            if accum is not None:
                kw["accum_out"] = accum
            P.op("act", lambda e: e.activation(out_ap, in_ap, func, **kw), reads, writes)

        def tt(eng, out_ap, a_ap, b_ap, op, reads, writes):
            P.op(eng, lambda e: e.tensor_tensor(out_ap, a_ap, b_ap, op), reads, writes)

        def ts(eng, out_ap, a_ap, s1, s2, op0, op1, reads, writes):
            if op1 is None:
                P.op(eng, lambda e: e.tensor_scalar(out_ap, a_ap, s1, None, op0), reads, writes)
            else:
                P.op(eng, lambda e: e.tensor_scalar(out_ap, a_ap, s1, s2, op0, op1), reads, writes)

        def stt(eng, out_ap, a_ap, s, b_ap, op0, op1, reads, writes):
            P.op(eng, lambda e: e.scalar_tensor_tensor(out_ap, a_ap, s, b_ap, op0, op1), reads, writes)

        def cp(eng, out_ap, in_ap, reads, writes):
            P.op(eng, lambda e: e.tensor_copy(out_ap, in_ap), reads, writes)

        def dma(q, out_ap, in_ap, reads, writes, **kw):
            P.dma(q, lambda e: e.dma_start(out=out_ap, in_=in_ap, **kw), reads, writes)

        def dump(key, src_tl, src_ap, dst_ap_fn):
            if key in dbg_out:
                dma("sp", dst_ap_fn(dbg_out[key].t), src_ap, [src_tl.b] if isinstance(src_tl, TL) else src_tl,
                    [dbg_out[key].b])

        for shard, bounce, full in gathers:
            nr_ = shard.t.shape[0]
            for r0_ in range(0, nr_, 1024):
                r1_ = min(nr_, r0_ + 1024)
                dma("sp", bounce.t[r0_:r1_, :], shard.t[r0_:r1_, :], [], [bounce.b])
            P.dma("pool", lambda e, bounce=bounce, full=full: e.collective_compute(
                "AllGather", ALU.bypass, replica_groups=[list(range(NCORES))], ins=[bounce.t.opt()], outs=[full.t.opt()]),
                [bounce.b], [full.b], inc=1)

        WBF = []
        for l in range(nlayers):
            wb = {}
            for nm in ("w1", "w2g", "wout", "wbra", "wbrs", "wglu"):
                src = L[l][nm]
                dst = TL(nc.dram_tensor(f"{nm}bf{l}", list(src.t.shape), BF16).ap())
                for i0 in range(src.t.shape[0] if nm == "w2g" else src.t.shape[1]):
                    if nm == "w2g":
                        dma("pool", dst.t[i0, :, :, :], src.t[i0, :, :, :], [src.b], [dst.b])
                    else:
                        dma("pool", dst.t[:, i0, :], src.t[:, i0, :], [src.b], [dst.b])
                wb[nm] = dst
            WBF.append(wb)

        x_tm = sbuf(st, "x_tm", [128, NT, D], F32, NT)
        ident = sbuf(st, "ident", [128, 128])
        zeros = sbuf(st, "zeros", [128, 512])
        iota = sbuf(st, "iota", [128, 512])
        onesb = sbuf(st, "onesb", [128, 64], BF16)
        onesf = sbuf(st, "onesf", [128, 128])
        siluT = sbuf(st, "siluT", [128, 8, 2])
        modT = sbuf(st, "modT", [128, 48, 2])
        bmodT = sbuf(st, "bmodT", [128, 48])
        gT = sbuf(st, "gT", [128, 16])
        A1 = sbuf(st, "A1", [128, 2, 8])
        A2 = sbuf(st, "A2", [128, 2, 8])
        rstd = sbuf(st, "rstd", [128, NT])
        sstat = sbuf(st, "sstat", [128, NT])
        xs = sbuf(st, "xs", [128, D])
        sm = sbuf(st, "sm", [128, 8])
        ucT = sbuf(st, "ucT", [128, CTX], BF16)
        dg = sbuf(st, "dg", [128, 128])

        def AP_rev(tl, row_elems, col_last, n):
            return bass.AP(tl.t, col_last, [[row_elems, 128], [-1, n]])

        def range_reduce(kf_tl, kf_ap, ki_tl, ki_ap, out_ap, in_ap, shift, reads, writes):
            ts("dve", kf_ap, in_ap, shift, 1.0 / (2 * PI), ALU.add, ALU.mult, reads, [kf_tl.b])
            cp("dve", ki_ap, kf_ap, [kf_tl.b], [ki_tl.b])
            cp("dve", kf_ap, ki_ap, [ki_tl.b], [kf_tl.b])
            stt("dve", kf_ap, kf_ap, -2 * PI, in_ap, ALU.mult, ALU.add, [kf_tl.b] + list(reads), [kf_tl.b])
            ts("dve", out_ap, kf_ap, shift, None, ALU.add, None, [kf_tl.b], writes)
            ts("dve", out_ap, out_ap, 3.14159, -3.14159, ALU.min, ALU.max, writes, writes)

        with ExitStack() as ph:
            posT = sbuf(ph, "posT", [128, TALL])
            ang = sbuf(ph, "ang", [128, TALL])
            ki = sbuf(ph, "ki", [128, TALL], I32)
            kf = sbuf(ph, "kf", [128, TALL])
            tb = sbuf(ph, "tb", [128, TALL])
            ropef = sbuf(ph, "ropef", [128, 2])
            dma("sp", x_tm.t[:, 0:NT_OWN, :], x_own.t.rearrange("(t p) d -> p t d", p=128), [], x_tm.bs[0:NT_OWN])
            dma("sp", x_tm.t[:, NT_OWN:NT, :], ctx_b.t.rearrange("(t p) d -> p t d", p=128), [], x_tm.bs[NT_OWN:NT])
            dma("sp", ident.t[:], ident_d.t[:, :], [], [ident.b])
            dma("sp", posT.t[:], pos_d.t[:, :], [], [posT.b])
            dma("sp", ropef.t[:], ropef_d.t[:, :], [], [ropef.b])
            dma("sp", iota.t[:], iota_d.t[:, :], [], [iota.b])
            dma("sp", siluT.t[:], cT.t[:, :, :], [], [siluT.b])
            P.op("pool", lambda e: e.memset(zeros.t[:], 0.0), [], [zeros.b])
            P.op("pool", lambda e: e.memset(onesb.t[:], 1.0), [], [onesb.b])
            P.op("pool", lambda e: e.memset(onesf.t[:], 1.0), [], [onesf.b])
            act(siluT.t[:], siluT.t[:], AF.Silu, [siluT.b], [siluT.b])
            act(sm.t[:, 0:1], ropef.t[:, 0:1], AF.Exp, [ropef.b], [sm.b], scale=-float(np.log(10000.0)) / 16.0)
            ts("dve", ang.t[:], posT.t[:], sm.t[:, 0:1], None, ALU.mult, None, [posT.b, sm.b], [ang.b])
            range_reduce(kf, kf.t[:], ki, ki.t[:], tb.t[:], ang.t[:], 0.0, [ang.b], [tb.b])
            act(tb.t[:], tb.t[:], AF.Sin, [tb.b], [tb.b])
            ts("dve", tb.t[:], tb.t[:], ropef.t[:, 1:2], None, ALU.mult, None, [tb.b, ropef.b], [tb.b])
            dma("sp", sin_d.t[:, :], tb.t[:], [tb.b], [sin_d.b])
            range_reduce(kf, kf.t[:], ki, ki.t[:], tb.t[:], ang.t[:], PI / 2, [ang.b], [tb.b])
            act(tb.t[:], tb.t[:], AF.Sin, [tb.b], [tb.b])
            dma("sp", cos_d.t[:, :], tb.t[:], [tb.b], [cos_d.b])
            P.barrier()

        def tile_cls(t_):
            return 0 if t_ < NT_OWN else 1

        def compute_rstd(ntiles):
            for t_ in range(ntiles):
                act(xs.t[:], x_tm.t[:, t_, :], AF.Square, [x_tm.bs[t_]], [xs.b, sstat.b], accum=sstat.t[:, t_:t_ + 1])
            ts("dve", rstd.t[:, 0:ntiles], sstat.t[:, 0:ntiles], 1.0 / D, 1e-6, ALU.mult, ALU.add, [sstat.b], [rstd.b])
            act(rstd.t[:, 0:ntiles], rstd.t[:, 0:ntiles], AF.Sqrt, [rstd.b], [rstd.b])
            P.op("dve", lambda e: e.reciprocal(rstd.t[:, 0:ntiles], rstd.t[:, 0:ntiles]), [rstd.b], [rstd.b])

        def norm_tile(t_, Acls, shslot, dst_ap_fn, dst_bufs, f32_dst=None):
            cls = tile_cls(t_)
            act(xs.t[:], x_tm.t[:, t_, :], AF.Identity, [x_tm.bs[t_], rstd.b], [xs.b], scale=rstd.t[:, t_:t_ + 1])
            for half in range(2):
                pb_ = bank()
                for q4 in range(4):
                    fc = half * 4 + q4
                    tr(pb_, pb_.t[:, q4 * 128:(q4 + 1) * 128], xs.t[:, fc * 128:(fc + 1) * 128], ident.t[:], [xs.b, ident.b])
                for q4 in range(4):
                    fc = half * 4 + q4
                    act(dst_ap_fn(fc), pb_.t[:, q4 * 128:(q4 + 1) * 128], AF.Identity, [pb_.b, Acls.b, modT.b], dst_bufs,
                        scale=Acls.t[:, cls, fc:fc + 1], bias=modT.t[:, shslot * 8 + fc, cls:cls + 1])
                    if f32_dst is not None:
                        act(f32_dst.t[:, fc, :], pb_.t[:, q4 * 128:(q4 + 1) * 128], AF.Identity, [pb_.b, Acls.b, modT.b],
                            [f32_dst.b], scale=Acls.t[:, cls, fc:fc + 1], bias=modT.t[:, shslot * 8 + fc, cls:cls + 1])

        def make_gtrow(dst, slot):
            for cls in range(2):
                for fc in range(8):
                    ts("dve", dg.t[:], ident.t[:], modT.t[:, slot * 8 + fc, cls:cls + 1], None, ALU.mult, None,
                       [ident.b, modT.b], [dg.b])
                    pb_ = bank()
                    mm(pb_, pb_.t[:, 0:128], onesf.t[:], dg.t[:], [onesf.b, dg.b])
                    cp("dve", dst.t[:, cls, fc * 128:(fc + 1) * 128], pb_.t[:, 0:128], [pb_.b], [dst.b])

        def blk_of(t_):
            return t_ + 1 if t_ < NT_OWN else 18 + (t_ - NT_OWN)

        DL = dbg.get("_layer", 0)
        stopped = False
        for l in range(nlayers):
            if dbg.get("_stop") == "0":
                break
            W = L[l]
            WB = WBF[l]
            ctx_out = l < nlayers - 1
            with ExitStack() as ph:
                wm = sbuf(ph, "wm", [128, 2, 8, 512], F32, 2)
                dma("sp", bmodT.t[:], W["bmodT"].t[:, :], [], [bmodT.b])
                dma("sp", gT.t[:, 0:8], W["g1T"].t[:, :], [], [gT.b])
                dma("sp", gT.t[:, 8:16], W["g2T"].t[:, :], [], [gT.b])
                mps = bank()
                for piece in range(12):
                    s = piece % 2
                    dma("sp", wm.t[:, s, :, :], W["wmod"].t[:, :, piece * 512:(piece + 1) * 512], [W["wmod"].b], [wm.bs[s]])
                    for c4 in range(4):
                        cc = piece * 4 + c4
                        for kc in range(8):
                            mm(mps, mps.t[:, cc * 2:cc * 2 + 2], wm.t[:, s, kc, c4 * 128:(c4 + 1) * 128],
                               siluT.t[:, kc, :], [wm.bs[s], siluT.b], kc == 0, kc == 7)
                for cls in range(2):
                    tt("dve", modT.t[:, :, cls], mps.t[:, cls:96:2], bmodT.t[:], ALU.add, [mps.b, bmodT.b], [modT.b])
                for cls in range(2):
                    stt("dve", A1.t[:, cls, :], modT.t[:, 8:16, cls], 1.0, gT.t[:, 0:8], ALU.add, ALU.mult,
                        [modT.b, gT.b], [A1.b])
                    stt("dve", A2.t[:, cls, :], modT.t[:, 32:40, cls], 1.0, gT.t[:, 8:16], ALU.add, ALU.mult,
                        [modT.b, gT.b], [A2.b])
                if l == DL:
                    dump("modT", modT, modT.t[:], lambda o: o[:, :, :])
                P.barrier()

            kvst = ExitStack()
            kT = sbuf(kvst, "kT", [128, 20, 128], BF16, 20)
            vv = sbuf(kvst, "vv", [128, 20, 128], BF16, 20)
            with ExitStack() as ph:
                w1 = sbuf(ph, "w1", [128, 8, 1920], BF16)
                wus = sbuf(ph, "wus", [128, 8, 128], BF16)
                hT = sbuf(ph, "hT", [128, 8, 512], BF16)
                ust = sbuf(ph, "ust", [128, 2, 512], BF16, 2)
                tmpa = sbuf(ph, "tmpa", [128, 512])
                tmpb = sbuf(ph, "tmpb", [128, 512])
                cosT = sbuf(ph, "cosT", [128, TALL])
                sinT = sbuf(ph, "sinT", [128, TALL])
                dma("sp", cosT.t[:], cos_d.t[:, :], [cos_d.b], [cosT.b])
                dma("sp", sinT.t[:], sin_d.t[:, :], [sin_d.b], [sinT.b])
                dma("sp", w1.t[:, :, :], WB["w1"].t[:, :, :], [WB["w1"].b], [w1.b])
                dma("pool", wus.t[:], W["wus"].t[:, :, :], [], [wus.b])
                compute_rstd(NT)
                ust_rr = 0
                for ch in range(5):
                    tiles = list(range(ch * 4, min(ch * 4 + 4, NT)))
                    n = len(tiles) * 128
                    c0 = ch * 512
                    for ti, t_ in enumerate(tiles):
                        norm_tile(t_, A1, 0, lambda fc, ti=ti: hT.t[:, fc, ti * 128:(ti + 1) * 128], [hT.b])
                    pk = bank()
                    pkr = bank()
                    for kc in range(8):
                        mm(pk, pk.t[:, 0:n], w1.t[:, kc, 1024:1152], hT.t[:, kc, 0:n], [w1.b, hT.b], kc == 0, kc == 7)
                    for kc in range(8):
                        mm(pkr, pkr.t[:, 0:n], w1.t[:, kc, 1152:1280], hT.t[:, kc, 0:n], [w1.b, hT.b], kc == 0, kc == 7)
                    tt("dve", tmpa.t[:, 0:n], pk.t[:, 0:n], cosT.t[:, c0:c0 + n], ALU.mult, [pk.b, cosT.b], [tmpa.b])
                    tt("dve", tmpb.t[:, 0:n], pkr.t[:, 0:n], sinT.t[:, c0:c0 + n], ALU.mult, [pkr.b, sinT.b], [tmpb.b])
                    for ti, t_ in enumerate(tiles):
                        blk = blk_of(t_)
                        tt("dve", kT.t[:, blk, :], tmpa.t[:, ti * 128:(ti + 1) * 128], tmpb.t[:, ti * 128:(ti + 1) * 128],
                           ALU.add, [tmpa.b, tmpb.b], [kT.bs[blk]])
                    pv = bank()
                    for ti, t_ in enumerate(tiles):
                        for kc in range(8):
                            mm(pv, pv.t[:, ti * 128:(ti + 1) * 128], hT.t[:, kc, ti * 128:(ti + 1) * 128],
                               w1.t[:, kc, 1280:1408], [w1.b, hT.b], kc == 0, kc == 7)
                    for ti, t_ in enumerate(tiles):
                        blk = blk_of(t_)
                        act(vv.t[:, blk, :], pv.t[:, ti * 128:(ti + 1) * 128], AF.Identity, [pv.b], [vv.bs[blk]])
                    if ch < 4:
                        for ti, t_ in enumerate(tiles):
                            pu = bank()
                            for kc in range(8):
                                mm(pu, pu.t[:, :], hT.t[:, kc, ti * 128:(ti + 1) * 128], w1.t[:, kc, 1408:1920],
                                   [w1.b, hT.b], kc == 0, kc == 7)
                            s = ust_rr % 2
                            ust_rr += 1
                            act(ust.t[:, s, :], pu.t[:, :], AF.Identity, [pu.b], [ust.bs[s]])
                            pc_, lr_ = (t_ * 128) // 1024, (t_ * 128) % 1024
                            dma("sp", ag1_in[pc_].t[lr_:lr_ + 128, :], ust.t[:, s, :], [ust.bs[s]], [ag1_in[pc_].b])
                    else:
                        pu = bank()
                        for kc in range(8):
                            mm(pu, pu.t[:, 0:CTX], wus.t[:, kc, :], hT.t[:, kc, 0:CTX], [wus.b, hT.b], kc == 0, kc == 7)
                        act(ucT.t[:], pu.t[:, 0:CTX], AF.Identity, [pu.b], [ucT.b])
                for i4, (tl_, blk) in enumerate(((kT, 1), (kT, 16), (vv, 1), (vv, 16))):
                    dma("sp", ag1_in[2].t[:, i4 * 128:(i4 + 1) * 128], tl_.t[:, blk, :], [tl_.bs[blk]], [ag1_in[2].b])
                for pc_ in range(3):
                    P.dma("pool", lambda e, pc_=pc_: e.collective_compute(
                        "AllGather", ALU.bypass, replica_groups=GROUPS, ins=[ag1_in[pc_].t.opt()], outs=[ag1_out[pc_].t.opt()]),
                        [ag1_in[pc_].b], [ag1_out[pc_].b], inc=1)
                P.barrier()
            if dbg.get("_stop") == "B" and l == DL:
                stopped = True

            if not stopped:
              with ExitStack() as ph:
                uTf = sbuf(ph, "uTf", [128, STREAM], BF16, 17)
                hal = sbuf(ph, "hal", [128, 4, 512], BF16)
                utile = sbuf(ph, "utile", [128, 2, 512], BF16, 2)
                selm = sbuf(ph, "selm", [128, 12, 128], BF16)
                prm = sbuf(ph, "prm", [128, 16, 8])
                bb = sbuf(ph, "bb", [128, 4, 8, 16])
                cc_ = sbuf(ph, "cc", [128, 2, 8, 16])
                Wsc = sbuf(ph, "Wsc", [128, 128])
                lB = sbuf(ph, "lB", [128, 16, 128], BF16)
                lC = sbuf(ph, "lC", [128, 16, 128], BF16)
                dsk = sbuf(ph, "dsk", [128, 1])
                tabc = sbuf(ph, "tabc", [128, 4, 512], F32, 4)
                tabs = sbuf(ph, "tabs", [128, 4, 512], F32, 4)
                rhoT = sbuf(ph, "rhoT", [128, 4, 512], F32, 4)
                kis = sbuf(ph, "kis", [128, 512], I32)
                NW = 2
                wk = [[sbuf(ph, f"wk{s}_{i}", [128, 512]) for i in range(6)] for s in range(NW)]
                xrb = [[sbuf(ph, f"xb{s}_{i}", [128, 512], BF16) for i in range(2)] for s in range(NW)]
                kfs, angs = wk[0][0], wk[0][1]
                ub = sbuf(ph, "ub", [128, 2, 512], BF16, 2)
                init = sbuf(ph, "init", [128, 4, 2], F32, 4)
                ytmp = sbuf(ph, "ytmp", [128, 3, 512], F32, 3)
                yfs = sbuf(ph, "yfs", [128, 2, 512], F32, 2)
                zst = sbuf(ph, "zst", [128, 2, 512], BF16, 2)
                dma("pool", selm.t[:], selmat_d.t[:, :, :], [], [selm.b])
                for i in range(4):
                    dma("sp", hal.t[:, i, :], ag1_out[2].t[i * 128:(i + 1) * 128, :], [ag1_out[2].b], [hal.b])
                ph_ = bank()
                for i4, (selbase, col) in enumerate(((4, 128), (8, 0), (4, 384), (8, 256))):
                    for i in range(4):
                        mm(ph_, ph_.t[:, i4 * 128:(i4 + 1) * 128], selm.t[:, selbase + i, :], hal.t[:, i, col:col + 128],
                           [selm.b, hal.b], i == 0, i == 3)
                for i4, (tl_, blk) in enumerate(((kT, 0), (kT, 17), (vv, 0), (vv, 17))):
                    cp("dve", tl_.t[:, blk, :], ph_.t[:, i4 * 128:(i4 + 1) * 128], [ph_.b], [tl_.bs[blk]])
                cp("dve", uTf.t[:, 0:CTX], ucT.t[:], [ucT.b], [uTf.bs[0]])
                ut_rr = 0
                for i in range(4):
                    for tq in range(4):
                        pu = bank()
                        for t4 in range(4):
                            s = ut_rr % 2
                            ut_rr += 1
                            tr_ = (tq * 4 + t4) * 128
                            pc_, r0 = tr_ // 1024, i * 1024 + tr_ % 1024
                            dma("sp", utile.t[:, s, :], ag1_out[pc_].t[r0:r0 + 128, :], [ag1_out[pc_].b], [utile.bs[s]])
                            for sl in range(4):
                                mm(pu, pu.t[:, t4 * 128:(t4 + 1) * 128], utile.t[:, s, sl * 128:(sl + 1) * 128],
                                   selm.t[:, sl, :], [utile.bs[s], selm.b], sl == 0, sl == 3)
                        nk = i * 4 + tq
                        act(uTf.t[:, CTX + nk * 512:CTX + (nk + 1) * 512], pu.t[:, :], AF.Identity, [pu.b], [uTf.bs[1 + nk]])
                for nm, c_ in (("lamre", 0), ("lamim", 1), ("ldt", 2)):
                    dma("sp", prm.t[:, c_, :], W[nm].t[:, :], [], [prm.b])
                dma("sp", bb.t[:, 0, :, :], W["bre"].t[:, :, :], [], [bb.b])
                dma("sp", bb.t[:, 1, :, :], W["bim"].t[:, :, :], [], [bb.b])
                dma("sp", cc_.t[:, 0, :, :], W["cre"].t[:, :, :], [], [cc_.b])
                dma("sp", cc_.t[:, 1, :, :], W["cim"].t[:, :, :], [], [cc_.b])
                dma("sp", dsk.t[:], W["dsk"].t[:, :], [], [dsk.b])
                pr = lambda c_: prm.t[:, c_, :]
                PB = [prm.b]
                act(pr(3), pr(2), AF.Exp, PB, PB)
                tt("dve", pr(4), pr(0), pr(3), ALU.mult, PB, PB)
                tt("dve", pr(5), pr(1), pr(3), ALU.mult, PB, PB)
                act(pr(6), pr(4), AF.Exp, PB, PB)
                range_reduce(kfs, kfs.t[:, 0:8], kis, kis.t[:, 0:8], angs.t[:, 0:8], pr(5), 0.0, PB, [angs.b])
                act(pr(7), angs.t[:, 0:8], AF.Sin, [angs.b], PB)
                range_reduce(kfs, kfs.t[:, 0:8], kis, kis.t[:, 0:8], angs.t[:, 0:8], pr(5), PI / 2, PB, [angs.b])
                act(pr(8), angs.t[:, 0:8], AF.Sin, [angs.b], PB)
                tt("dve", pr(9), pr(6), pr(8), ALU.mult, PB, PB)
                tt("dve", pr(10), pr(6), pr(7), ALU.mult, PB, PB)
                tt("dve", pr(11), pr(0), pr(0), ALU.mult, PB, PB)
                tt("dve", pr(15), pr(1), pr(1), ALU.mult, PB, PB)
                tt("dve", pr(11), pr(11), pr(15), ALU.add, PB, PB)
                P.op("dve", lambda e: e.reciprocal(pr(11), pr(11)), PB, PB)
                ts("dve", pr(12), pr(9), -1.0, None, ALU.add, None, PB, PB)
                tt("dve", pr(13), pr(12), pr(0), ALU.mult, PB, PB)
                tt("dve", pr(15), pr(10), pr(1), ALU.mult, PB, PB)
                tt("dve", pr(13), pr(13), pr(15), ALU.add, PB, PB)
                tt("dve", pr(13), pr(13), pr(11), ALU.mult, PB, PB)
                tt("dve", pr(14), pr(10), pr(0), ALU.mult, PB, PB)
                tt("dve", pr(15), pr(12), pr(1), ALU.mult, PB, PB)
                tt("dve", pr(14), pr(14), pr(15), ALU.subtract, PB, PB)
                tt("dve", pr(14), pr(14), pr(11), ALU.mult, PB, PB)
                if l == DL:
                    dump("prm", prm, prm.t[:], lambda o: o[:, :, :])
                for r in range(8):
                    fre = prm.t[:, 13, r:r + 1]
                    fim = prm.t[:, 14, r:r + 1]
                    ts("dve", bb.t[:, 2, r, :], bb.t[:, 0, r, :], fre, None, ALU.mult, None, [bb.b, prm.b], [bb.b])
                    ts("dve", bb.t[:, 3, r, :], bb.t[:, 1, r, :], fim, None, ALU.mult, None, [bb.b, prm.b], [bb.b])
                    tt("dve", bb.t[:, 2, r, :], bb.t[:, 2, r, :], bb.t[:, 3, r, :], ALU.subtract, [bb.b], [bb.b])
                    ts("dve", bb.t[:, 3, r, :], bb.t[:, 1, r, :], fre, None, ALU.mult, None, [bb.b, prm.b], [bb.b])
                    stt("dve", bb.t[:, 3, r, :], bb.t[:, 0, r, :], fim, bb.t[:, 3, r, :], ALU.mult, ALU.add,
                        [bb.b, prm.b], [bb.b])
                    gp = r % 4
                    for ri in range(2):
                        P.op("pool", lambda e: e.memset(Wsc.t[:], 0.0), [], [Wsc.b])
                        for g2 in range(2):
                            c0_ = 16 * (2 * gp + g2)
                            cp("dve", Wsc.t[g2 * 64:(g2 + 1) * 64, c0_:c0_ + 16], bb.t[g2 * 64:(g2 + 1) * 64, 2 + ri, r, :],
                               [bb.b], [Wsc.b])
                        pb_ = bank()
                        tr(pb_, pb_.t[:, 0:128], Wsc.t[:], ident.t[:], [Wsc.b, ident.b])
                        act(lB.t[:, r * 2 + ri, :], pb_.t[:, 0:128], AF.Identity, [pb_.b], [lB.b])
                        P.op("pool", lambda e, r=r, ri=ri: e.memset(lC.t[:, r * 2 + ri, :], 0.0), [], [lC.b])
                        for g2 in range(2):
                            c0_ = 16 * (2 * gp + g2)
                            ts("dve", lC.t[g2 * 64:(g2 + 1) * 64, r * 2 + ri, c0_:c0_ + 16],
                               cc_.t[g2 * 64:(g2 + 1) * 64, ri, r, :], (1.0 if ri == 0 else -1.0), None, ALU.mult, None,
                               [cc_.b], [lC.b])
                chunks = [(0, CTX)] + [(CTX + 512 * k, 512) for k in range(16)]
                wk_rr = 0
                for d_ in range(2):
                    for gp in range(4):
                        r = d_ * 4 + gp
                        ts("dve", angs.t[:], iota.t[:], prm.t[:, 5, r:r + 1], None, ALU.mult, None, [iota.b, prm.b], [angs.b])
                        range_reduce(kfs, kfs.t[:], kis, kis.t[:], tabs.t[:, gp, :], angs.t[:], 0.0, [angs.b], [tabs.bs[gp]])
                        act(tabs.t[:, gp, :], tabs.t[:, gp, :], AF.Sin, [tabs.bs[gp]], [tabs.bs[gp]])
                        range_reduce(kfs, kfs.t[:], kis, kis.t[:], tabc.t[:, gp, :], angs.t[:], PI / 2, [angs.b], [tabc.bs[gp]])
                        act(tabc.t[:, gp, :], tabc.t[:, gp, :], AF.Sin, [tabc.bs[gp]], [tabc.bs[gp]])
                        ts("dve", rhoT.t[:, gp, :], zeros.t[:], prm.t[:, 6, r:r + 1], None, ALU.add, None, [zeros.b, prm.b],
                           [rhoT.bs[gp]])
                        P.op("pool", lambda e, gp=gp: e.memset(init.t[:, gp, :], 0.0), [], [init.bs[gp]])
                    for ci, (c0, n) in enumerate(chunks):
                        if d_ == 0:
                            nat_c0, slot = c0, ci
                            u_ap = uTf.t[:, c0:c0 + n]
                            u_reads = [uTf.bs[ci]]
                        else:
                            if ci == 0:
                                nat_c0, slot = 0, 0
                            else:
                                nkk = 16 - ci
                                nat_c0, slot = CTX + 512 * nkk, 1 + nkk
                            s_u = ci % 2
                            cp("dve", ub.t[:, s_u, 0:n], AP_rev(uTf, STREAM, nat_c0 + n - 1, n), [uTf.bs[slot]], [ub.bs[s_u]])
                            u_ap = ub.t[:, s_u, 0:n]
                            u_reads = [ub.bs[s_u]]
                        need = ctx_out or ci > 0
                        yps = bank()
                        excl = (PS.index(yps),)
                        for gp in range(4):
                            r = d_ * 4 + gp
                            ws = wk[wk_rr % NW]
                            xb_ = xrb[wk_rr % NW]
                            wk_rr += 1
                            pre = bank(exclude=excl)
                            pim = bank(exclude=excl)
                            mm(pre, pre.t[:, 0:n], lB.t[:, r * 2, :], u_ap, [lB.b] + u_reads)
                            mm(pim, pim.t[:, 0:n], lB.t[:, r * 2 + 1, :], u_ap, [lB.b] + u_reads)
                            c_ap = tabc.t[:, gp, 0:n]
                            s_ap = tabs.t[:, gp, 0:n]
                            TB = [tabc.bs[gp], tabs.bs[gp]]
                            t1, t2, t3, t4, zr, zi = [w_.t[:, 0:n] for w_ in ws]
                            b1, b2, b3, b4, bzr, bzi = [w_.b for w_ in ws]
                            tt("dve", t1, pre.t[:, 0:n], c_ap, ALU.mult, [pre.b] + TB, [b1])
                            tt("dve", t2, pim.t[:, 0:n], s_ap, ALU.mult, [pim.b] + TB, [b2])
                            tt("pool", t1, t1, t2, ALU.add, [b1, b2], [b1])
                            tt("dve", t3, pim.t[:, 0:n], c_ap, ALU.mult, [pim.b] + TB, [b3])
                            tt("dve", t4, pre.t[:, 0:n], s_ap, ALU.mult, [pre.b] + TB, [b4])
                            tt("pool", t3, t3, t4, ALU.subtract, [b3, b4], [b3])
                            P.op("dve", lambda e, zr=zr, t1=t1, gp=gp, n=n: e.tensor_tensor_scan(
                                zr, rhoT.t[:, gp, 0:n], t1, init.t[:, gp, 0:1], ALU.mult, ALU.add),
                                [rhoT.bs[gp], b1, init.bs[gp]], [bzr])
                            P.op("dve", lambda e, zi=zi, t3=t3, gp=gp, n=n: e.tensor_tensor_scan(
                                zi, rhoT.t[:, gp, 0:n], t3, init.t[:, gp, 1:2], ALU.mult, ALU.add),
                                [rhoT.bs[gp], b3, init.bs[gp]], [bzi])
                            tt("dve", t1, zr, c_ap, ALU.mult, [bzr] + TB, [b1])
                            tt("dve", t2, zi, s_ap, ALU.mult, [bzi] + TB, [b2])
                            tt("dve", t3, zr, s_ap, ALU.mult, [bzr] + TB, [b3])
                            tt("dve", t4, zi, c_ap, ALU.mult, [bzi] + TB, [b4])
                            tt("pool", xb_[0].t[:, 0:n], t1, t2, ALU.subtract, [b1, b2], [xb_[0].b])
                            tt("dve", xb_[1].t[:, 0:n], t3, t4, ALU.add, [b3, b4], [xb_[1].b])
                            tt("dve", init.t[:, gp, 0:1], ws[0].t[:, n - 1:n], ws[1].t[:, n - 1:n], ALU.subtract, [b1, b2],
                               [init.bs[gp]])
                            tt("dve", init.t[:, gp, 1:2], ws[2].t[:, n - 1:n], ws[3].t[:, n - 1:n], ALU.add, [b3, b4],
                               [init.bs[gp]])
                            if need:
                                mm(yps, yps.t[:, 0:n], lC.t[:, r * 2, :], xb_[0].t[:, 0:n], [lC.b, xb_[0].b], gp == 0, False)
                                mm(yps, yps.t[:, 0:n], lC.t[:, r * 2 + 1, :], xb_[1].t[:, 0:n], [lC.b, xb_[1].b], False, gp == 3)
                        if not need:
                            continue
                        if d_ == 0:
                            fs_ = ci % 2
                            act(yfs.t[:, fs_, 0:n], yps.t[:, 0:n], AF.Identity, [yps.b], [yfs.bs[fs_]])
                            dma("sp", yTf_d.t[:, c0:c0 + n], yfs.t[:, fs_, 0:n], [yfs.bs[fs_]], [yTf_d.b])
                        else:
                            ys = ci % 3
                            zs = ci % 2
                            ya = ytmp.t[:, ys, 0:n]
                            YB = [ytmp.bs[ys]]
                            yb2 = ytmp.t[:, (ys + 1) % 3, 0:n]
                            YB2 = [ytmp.bs[(ys + 1) % 3]]
                            fs_ = ci % 2
                            dma("sp", yfs.t[:, fs_, 0:n], yTf_d.t[:, nat_c0:nat_c0 + n], [yTf_d.b], [yfs.bs[fs_]])
                            act(ya, yps.t[:, 0:n], AF.Identity, [yps.b], YB)
                            tt("dve", yb2, AP_rev(ytmp, 3 * 512, ys * 512 + n - 1, n), yfs.t[:, fs_, 0:n], ALU.add,
                               YB + [yfs.bs[fs_]], YB2)
                            stt("dve", yb2, uTf.t[:, nat_c0:nat_c0 + n], dsk.t[:, 0:1], yb2, ALU.mult, ALU.add,
                                [uTf.bs[slot], dsk.b] + YB2, YB2)
                            if l == DL and ci in (0, 16):
                                cc0 = 0 if ci == 0 else 256
                                dump("ssmy", YB2, yb2, lambda o, cc0=cc0, n=n: o[:, cc0:cc0 + n])
                            act(ya, yb2, AF.Square, YB2, YB)
                            ts("dve", ya, ya, 0.044715, 1.0, ALU.mult, ALU.add, YB, YB)
                            tt("pool", ya, ya, yb2, ALU.mult, YB + YB2, YB)
                            act(ya, ya, AF.Sigmoid, YB, YB, scale=1.5957691216057308)
                            tt("pool", zst.t[:, zs, 0:n], ya, yb2, ALU.mult, YB + YB2, [zst.bs[zs]])
                            if ci == 0:
                                pc_, dcol = 2, 0
                            else:
                                pc_, dcol = (nat_c0 - CTX) // 4096, (nat_c0 - CTX) % 4096
                            dma("sp", ag2_in[pc_].t[:, dcol:dcol + n], zst.t[:, zs, 0:n], [zst.bs[zs]], [ag2_in[pc_].b])
                for pc_ in range(3 if ctx_out else 2):
                    P.dma("pool", lambda e, pc_=pc_: e.collective_compute(
                        "AllGather", ALU.bypass, replica_groups=GROUPS, ins=[ag2_in[pc_].t.opt()], outs=[ag2_out[pc_].t.opt()]),
                        [ag2_in[pc_].b], [ag2_out[pc_].b], inc=1)
                P.barrier()
            if dbg.get("_stop") == "C" and l == DL:
                stopped = True

            if not stopped:
              with ExitStack() as ph:
                wq = sbuf(ph, "wq", [128, 8, 1024], BF16)
                hT = sbuf(ph, "hTd", [128, 8, 512], BF16)
                qT = sbuf(ph, "qT", [128, 4, 512], BF16)
                pt = sbuf(ph, "pt", [128, 3, 512], BF16, 3)
                sg = sbuf(ph, "sg", [128, 2, 512], F32, 2)
                ta = sbuf(ph, "ta", [128, 2, 512], F32, 2)
                rec = sbuf(ph, "rec", [128, 512])
                attnT = sbuf(ph, "attnT", [128, 2, 4, 512], BF16, 2)
                cosT = sbuf(ph, "cosT", [128, TALL])
                sinT = sbuf(ph, "sinT", [128, TALL])
                masks = sbuf(ph, "masks", [128, 4, 512], BF16)
                sinkE = sbuf(ph, "sinkE", [128, 8])
                sinkrow = sbuf(ph, "sinkrow", [128, 512])
                dma("sp", cosT.t[:], cos_d.t[:, :], [cos_d.b], [cosT.b])
                dma("sp", sinT.t[:], sin_d.t[:, :], [sin_d.b], [sinT.b])
                for k4 in range(4):
                    dma("pool", masks.t[:, k4, :], masks_d.t[:, k4, :], [], [masks.b])
                dma("sp", wq.t[:, :, :], WB["w1"].t[:, :, 0:1024], [WB["w1"].b], [wq.b])
                dma("sp", sinkE.t[:], W["sink"].t[:, :], [], [sinkE.b])
                act(sinkE.t[:], sinkE.t[:], AF.Exp, [sinkE.b], [sinkE.b])
                for c in range(4):
                    ts("dve", sinkrow.t[0:64, c * 128:(c + 1) * 128], zeros.t[0:64, 0:128], sinkE.t[0:64, c:c + 1], None,
                       ALU.add, None, [zeros.b, sinkE.b], [sinkrow.b])
                    ts("dve", sinkrow.t[64:128, c * 128:(c + 1) * 128], zeros.t[64:128, 0:128], sinkE.t[64:128, 4 + c:5 + c],
                       None, ALU.add, None, [zeros.b, sinkE.b], [sinkrow.b])
                nchunks = 5 if ctx_out else 4
                pt_rr = 0
                sg_rr = 0
                for ch in range(nchunks):
                    tiles = list(range(ch * 4, min(ch * 4 + 4, NT)))
                    n = len(tiles) * 128
                    c0 = ch * 512
                    is_ctx = ch == 4
                    for ti, t_ in enumerate(tiles):
                        norm_tile(t_, A1, 0, lambda fc, ti=ti: hT.t[:, fc, ti * 128:(ti + 1) * 128], [hT.b])
                    for c in range(4):
                        pq = bank()
                        pqr = bank()
                        for kc in range(8):
                            mm(pq, pq.t[:, 0:n], wq.t[:, kc, c * 128:(c + 1) * 128], hT.t[:, kc, 0:n], [wq.b, hT.b], kc == 0, kc == 7)
                        for kc in range(8):
                            mm(pqr, pqr.t[:, 0:n], wq.t[:, kc, 512 + c * 128:512 + (c + 1) * 128], hT.t[:, kc, 0:n], [wq.b, hT.b],
                               kc == 0, kc == 7)
                        s_ = sg_rr % 2
                        sg_rr += 1
                        tt("dve", sg.t[:, s_, 0:n], pq.t[:, 0:n], cosT.t[:, c0:c0 + n], ALU.mult, [pq.b, cosT.b], [sg.bs[s_]])
                        tt("dve", ta.t[:, s_, 0:n], pqr.t[:, 0:n], sinT.t[:, c0:c0 + n], ALU.mult, [pqr.b, sinT.b], [ta.bs[s_]])
                        tt("dve", qT.t[:, c, 0:n], sg.t[:, s_, 0:n], ta.t[:, s_, 0:n], ALU.add, [sg.bs[s_], ta.bs[s_]], [qT.b])
                    as_ = ch % 2
                    for qi, t_ in enumerate(tiles):
                        pnum = bank()
                        pden = bank()
                        excl = (PS.index(pnum), PS.index(pden))
                        if is_ctx:
                            kbl = [(18, None), (19, None)]
                        else:
                            kbl = [(t_, 0 if t_ == 0 else 1), (t_ + 1, None), (t_ + 2, 2 if t_ == NT_OWN - 1 else 3),
                                   (18, None), (19, None)]
                        for gk in range(2):
                            pb = 64 * gk

                            def pv_(bi, kb, s_, pb=pb):
                                mm(pnum, pnum.t[pb:pb + 64, :], vv.t[:, kb, pb:pb + 64], pt.t[:, s_, :], [vv.bs[kb], pt.bs[s_]],
                                   bi == 0, bi == len(kbl) - 1)
                                mm(pden, pden.t[pb:pb + 64, :], onesb.t[:, 0:64], pt.t[:, s_, :], [onesb.b, pt.bs[s_]],
                                   bi == 0, bi == len(kbl) - 1)

                            prev_ = None
                            for bi, (kb, mk) in enumerate(kbl):
                                pst = bank(exclude=excl)
                                for c in range(4):
                                    mm(pst, pst.t[:, c * 128:(c + 1) * 128], kT.t[pb:pb + 64, kb, :],
                                       qT.t[pb:pb + 64, c, qi * 128:(qi + 1) * 128], [kT.bs[kb], qT.b])
                                s_ = pt_rr % 3
                                pt_rr += 1
                                act(pt.t[:, s_, :], pst.t[:, :], AF.Exp, [pst.b], [pt.bs[s_]], scale=0.125)
                                if mk is not None:
                                    tt("dve", pt.t[:, s_, :], pt.t[:, s_, :], masks.t[:, mk, :], ALU.mult, [pt.bs[s_], masks.b],
                                       [pt.bs[s_]])
                                if prev_ is not None:
                                    pv_(*prev_)
                                prev_ = (bi, kb, s_)
                            pv_(*prev_)
                        tt("dve", rec.t[:], pden.t[:, :], sinkrow.t[:], ALU.add, [pden.b, sinkrow.b], [rec.b])
                        P.op("dve", lambda e: e.reciprocal(rec.t[:], rec.t[:]), [rec.b], [rec.b])
                        for c in range(4):
                            tt("dve", attnT.t[:, as_, c, qi * 128:(qi + 1) * 128], pnum.t[:, c * 128:(c + 1) * 128],
                               rec.t[:, c * 128:(c + 1) * 128], ALU.mult, [pnum.b, rec.b], [attnT.bs[as_]])
                    dma("sp", attn_d.t[:, :, c0:c0 + n], attnT.t[:, as_, :, 0:n], [attnT.bs[as_]], [attn_d.b])
                P.barrier()
            kvst.close()
            if dbg.get("_stop") == "D1" and l == DL:
                stopped = True

            if not stopped:
              with ExitStack() as ph:
                wglu = sbuf(ph, "wglu", [128, 4, 512], BF16)
                wbra = sbuf(ph, "wbra", [128, 4, D], BF16)
                wbrs = sbuf(ph, "wbrs", [128, 4, D], BF16)
                wout = sbuf(ph, "wout", [128, 8, D], BF16)
                w2s = sbuf(ph, "w2s", [128, 2, 8, 256], BF16, 2)
                hT = sbuf(ph, "hTd2", [128, 8, 512], BF16)
                zstage = sbuf(ph, "zstage", [128, 4, 512], BF16)
                zsb = sbuf(ph, "zsb", [128, 4, 512], BF16)
                ssmT = sbuf(ph, "ssmT", [128, 4, 512], BF16)
                mT = sbuf(ph, "mT", [128, 8, 512], BF16)
                sg = sbuf(ph, "sg2", [128, 2, 512], F32, 2)
                ta = sbuf(ph, "ta2", [128, 2, 512], F32, 2)
                selm = sbuf(ph, "selm2", [128, 4, 128], BF16)
                gt1row = sbuf(ph, "gt1row", [128, 2, D])
                attc = sbuf(ph, "attc", [128, 4, 512], BF16)
                dma("pool", selm.t[:], selmat_d.t[:, 0:4, :], [], [selm.b])
                dma("sp", wout.t[:, :, :], WB["wout"].t[:, :, :], [WB["wout"].b], [wout.b])
                dma("sp", wglu.t[:, :, :], WB["wglu"].t[:, :, :], [WB["wglu"].b], [wglu.b])
                dma("sp", wbra.t[:, :, :], WB["wbra"].t[:, :, :], [WB["wbra"].b], [wbra.b])
                dma("sp", wbrs.t[:, :, :], WB["wbrs"].t[:, :, :], [WB["wbrs"].b], [wbrs.b])
                make_gtrow(gt1row, 2)
                nchunks = 5 if ctx_out else 4
                sg_rr = 0
                w2_rr = 0
                for ch in range(nchunks):
                    tiles = list(range(ch * 4, min(ch * 4 + 4, NT)))
                    n = len(tiles) * 128
                    c0 = ch * 512
                    is_ctx = ch == 4
                    cls = 1 if is_ctx else 0
                    for ti, t_ in enumerate(tiles):
                        norm_tile(t_, A1, 0, lambda fc, ti=ti: hT.t[:, fc, ti * 128:(ti + 1) * 128], [hT.b])
                    dma("sp", attc.t[:, :, 0:n], attn_d.t[:, :, c0:c0 + n], [attn_d.b], [attc.b])
                    for sl in range(4):
                        if is_ctx:
                            dma("sp", zsb.t[:, sl, 0:n], ag2_out[2].t[sl * 128:(sl + 1) * 128, :], [ag2_out[2].b], [zsb.b])
                        else:
                            for i in range(4):
                                gc_ = i * TOWN + c0
                                dma("sp", zstage.t[:, i, :],
                                    ag2_out[gc_ // 4096].t[sl * 128:(sl + 1) * 128, gc_ % 4096:gc_ % 4096 + 512],
                                    [ag2_out[gc_ // 4096].b], [zstage.b])
                            pz = bank()
                            for i in range(4):
                                mm(pz, pz.t[:, :], selm.t[:, i, :], zstage.t[:, i, :], [selm.b, zstage.b], i == 0, i == 3)
                            act(zsb.t[:, sl, :], pz.t[:, :], AF.Identity, [pz.b], [zsb.b])
                    for fc in range(4):
                        pg = bank()
                        for sl in range(4):
                            mm(pg, pg.t[:, 0:n], wglu.t[:, sl, fc * 128:(fc + 1) * 128], zsb.t[:, sl, 0:n], [wglu.b, zsb.b],
                               sl == 0, sl == 3)
                        s_ = sg_rr % 2
                        sg_rr += 1
                        act(sg.t[:, s_, 0:n], pg.t[:, 0:n], AF.Sigmoid, [pg.b], [sg.bs[s_]])
                        tt("dve", ssmT.t[:, fc, 0:n], zsb.t[:, fc, 0:n], sg.t[:, s_, 0:n], ALU.mult, [zsb.b, sg.bs[s_]], [ssmT.b])
                    if l == DL and ch == 0 and "ssmT" in dbg_out:
                        sdb = sbuf(ph, "sdb", [128, 4, 512])
                        cp("dve", sdb.t[:], ssmT.t[:], [ssmT.b], [sdb.b])
                        dump("ssmT", sdb, sdb.t[:], lambda o: o[:, :, :])
                    for fc in range(8):
                        ws_ = w2_rr % 2
                        w2_rr += 1
                        dma("sp", w2s.t[:, ws_, :, :], WB["w2g"].t[fc, :, :, :], [WB["w2g"].b], [w2s.bs[ws_]])
                        pA = bank()
                        pS = bank()
                        pga = bank()
                        pgs = bank()
                        fs = slice(fc * 128, (fc + 1) * 128)
                        for c in range(4):
                            mm(pA, pA.t[:, 0:n], wbra.t[:, c, fs], attc.t[:, c, 0:n], [wbra.b, attc.b], c == 0, c == 3)
                        for c in range(4):
                            mm(pS, pS.t[:, 0:n], wbrs.t[:, c, fs], ssmT.t[:, c, 0:n], [wbrs.b, ssmT.b], c == 0, c == 3)
                        for kc in range(8):
                            mm(pga, pga.t[:, 0:n], w2s.t[:, ws_, kc, 0:128], hT.t[:, kc, 0:n], [w2s.bs[ws_], hT.b], kc == 0, kc == 7)
                        for kc in range(8):
                            mm(pgs, pgs.t[:, 0:n], w2s.t[:, ws_, kc, 128:256], hT.t[:, kc, 0:n], [w2s.bs[ws_], hT.b], kc == 0, kc == 7)
                        act(sg.t[:, 0, 0:n], pga.t[:, 0:n], AF.Sigmoid, [pga.b], [sg.bs[0]])
                        act(sg.t[:, 1, 0:n], pgs.t[:, 0:n], AF.Sigmoid, [pgs.b], [sg.bs[1]])
                        tt("dve", ta.t[:, 0, 0:n], pA.t[:, 0:n], sg.t[:, 0, 0:n], ALU.mult, [pA.b, sg.bs[0]], [ta.bs[0]])
                        tt("dve", ta.t[:, 1, 0:n], pS.t[:, 0:n], sg.t[:, 1, 0:n], ALU.mult, [pS.b, sg.bs[1]], [ta.bs[1]])
                        tt("dve", mT.t[:, fc, 0:n], ta.t[:, 0, 0:n], ta.t[:, 1, 0:n], ALU.add, [ta.bs[0], ta.bs[1]], [mT.b])
                    for ti, t_ in enumerate(tiles):
                        for half in range(2):
                            py = bank()
                            hs_ = slice(half * 512, (half + 1) * 512)
                            for kc in range(8):
                                mm(py, py.t[:, :], mT.t[:, kc, ti * 128:(ti + 1) * 128], wout.t[:, kc, hs_], [mT.b, wout.b],
                                   kc == 0, kc == 7)
                            s_ = sg_rr % 2
                            sg_rr += 1
                            tt("dve", sg.t[:, s_, :], py.t[:, :], gt1row.t[:, cls, hs_], ALU.mult, [py.b, gt1row.b], [sg.bs[s_]])
                            tt("dve", x_tm.t[:, t_, hs_], x_tm.t[:, t_, hs_], sg.t[:, s_, :], ALU.add, [sg.bs[s_], x_tm.bs[t_]],
                               [x_tm.bs[t_]])
                P.barrier()
            if l == DL and not stopped:
                dump("xmix", x_tm.bs, x_tm.t[:], lambda o: o[:, :, :])
            if dbg.get("_stop") == "D2" and l == DL:
                stopped = True
            if stopped:
                break

            if not stopped:
              with ExitStack() as ph:
                ntl = NT if ctx_out else NT_OWN
                h2T = sbuf(ph, "h2T", [128, 8, TALL], BF16, NT)
                h2f = sbuf(ph, "h2f", [128, 8, 128])
                wr = sbuf(ph, "wr", [128, 8, 36])
                Wt = sbuf(ph, "Wt", [128, NT, 32], F32, NT)
                lg = sbuf(ph, "lg", [128, 36])
                rs = sbuf(ph, "rs", [128, 16])
                rt = sbuf(ph, "rt", [128, 4, 32])
                weg = sbuf(ph, "weg", [128, 2, 8, 512], BF16, 2)
                weu = sbuf(ph, "weu", [128, 2, 8, 512], BF16, 2)
                wed = sbuf(ph, "wed", [128, 2, 4, D], BF16, 2)
                hid = sbuf(ph, "hid", [128, 2, 4, 512], BF16, 2)
                sgm = sbuf(ph, "sgm", [128, 2, 512], F32, 2)
                ty = sbuf(ph, "ty", [128, 2, 512], F32, 2)
                gt2row = sbuf(ph, "gt2row", [128, 2, D])
                dma("sp", wr.t[:], W["wr"].t[:, :, :], [], [wr.b])
                make_gtrow(gt2row, 5)
                compute_rstd(ntl)
                RB = [rs.b]
                c_ = lambda i: rs.t[:, i:i + 1]
                def route_tile(t_):
                    norm_tile(t_, A2, 3, lambda fc, t_=t_: h2T.t[:, fc, t_ * 128:(t_ + 1) * 128], [h2T.bs[t_]], f32_dst=h2f)
                    pl = bank()
                    for kc in range(8):
                        mm(pl, pl.t[:, 0:36], h2f.t[:, kc, :], wr.t[:, kc, :], [h2f.b, wr.b], kc == 0, kc == 7)
                    cp("dve", lg.t[:], pl.t[:, 0:36], [pl.b], [lg.b])
                    P.op("dve", lambda e: e.tensor_reduce(rs.t[:, 0:1], lg.t[:, 0:4], AX.X, ALU.max), [lg.b], RB)
                    ts("dve", c_(1), c_(0), -1.0, None, ALU.mult, None, RB, RB)
                    act(rt.t[:, 0, 0:4], lg.t[:, 0:4], AF.Exp, [lg.b] + RB, [rt.b, rs.b], bias=rs.t[:, 1:2], accum=rs.t[:, 2:3])
                    P.op("dve", lambda e: e.reciprocal(rs.t[:, 3:4], rs.t[:, 2:3]), RB, RB)
                    ts("dve", rt.t[:, 0, 4:8], lg.t[:, 0:4], rs.t[:, 0:1], None, ALU.is_equal, None, [lg.b] + RB, [rt.b])
                    ts("dve", rt.t[:, 0, 4:8], rt.t[:, 0, 4:8], -1.0, 1e30, ALU.add, ALU.mult, [rt.b], [rt.b])
                    for g in range(4):
                        ts("dve", rt.t[:, 1, 8 * g:8 * g + 8], lg.t[:, 4 + 8 * g:12 + 8 * g], rt.t[:, 0, 4 + g:5 + g], None,
                           ALU.add, None, [lg.b, rt.b], [rt.b])
                    P.op("dve", lambda e: e.tensor_reduce(rs.t[:, 4:5], rt.t[:, 1, :], AX.X, ALU.max), [rt.b], RB)
                    ts("dve", rt.t[:, 2, :], rt.t[:, 1, :], rs.t[:, 4:5], None, ALU.is_equal, None, [rt.b] + RB, [rt.b])
                    stt("dve", rt.t[:, 2, :], rt.t[:, 2, :], -1e30, rt.t[:, 1, :], ALU.mult, ALU.add, [rt.b], [rt.b])
                    P.op("dve", lambda e: e.tensor_reduce(rs.t[:, 5:6], rt.t[:, 2, :], AX.X, ALU.max), [rt.b], RB)
                    ts("dve", rt.t[:, 2, :], rt.t[:, 1, :], rs.t[:, 5:6], None, ALU.is_ge, None, [rt.b] + RB, [rt.b])
                    ts("dve", c_(6), c_(4), -1.0, None, ALU.mult, None, RB, RB)
                    act(rt.t[:, 3, :], rt.t[:, 1, :], AF.Exp, [rt.b] + RB, [rt.b], bias=rs.t[:, 6:7])
                    tt("dve", rt.t[:, 3, :], rt.t[:, 3, :], rt.t[:, 2, :], ALU.mult, [rt.b], [rt.b])
                    P.op("dve", lambda e: e.tensor_reduce(rs.t[:, 7:8], rt.t[:, 3, :], AX.X, ALU.add), [rt.b], RB)
                    P.op("dve", lambda e: e.reciprocal(rs.t[:, 7:8], rs.t[:, 7:8]), RB, RB)
                    tt("dve", c_(7), c_(7), c_(3), ALU.mult, RB, RB)
                    ts("dve", Wt.t[:, t_, :], rt.t[:, 3, :], rs.t[:, 7:8], None, ALU.mult, None, [rt.b] + RB, [Wt.bs[t_]])

                nch = 5 if ctx_out else 4
                ty_rr = 0
                nexp = dbg.get("_nexp", NEXP)
                for e_ in range(nexp):
                    s = e_ % 2
                    for k2 in range(2):
                        dma("pool", weg.t[:, s, 4 * k2:4 * k2 + 4, :], W["weg"].t[e_, :, 4 * k2:4 * k2 + 4, :], [W["weg"].b], [weg.bs[s]])
                        dma("pool", weu.t[:, s, 4 * k2:4 * k2 + 4, :], W["weu"].t[e_, :, 4 * k2:4 * k2 + 4, :], [W["weu"].b], [weu.bs[s]])
                        dma("pool", wed.t[:, s, 2 * k2:2 * k2 + 2, :], W["wed"].t[e_, :, 2 * k2:2 * k2 + 2, :], [W["wed"].b], [wed.bs[s]])
                    for ch in range(nch):
                        tiles = list(range(ch * 4, min(ch * 4 + 4, NT)))
                        n = len(tiles) * 128
                        c0 = ch * 512
                        cls = 1 if ch == 4 else 0
                        hs = ch % 2
                        if e_ == 0:
                            for t_ in tiles:
                                route_tile(t_)
                        for hc in range(4):
                            pg = bank()
                            pu = bank()
                            hrd = [h2T.bs[t_] for t_ in tiles]
                            for kc in range(8):
                                mm(pg, pg.t[:, 0:n], weg.t[:, s, kc, hc * 128:(hc + 1) * 128], h2T.t[:, kc, c0:c0 + n],
                                   [weg.bs[s]] + hrd, kc == 0, kc == 7)
                            for kc in range(8):
                                mm(pu, pu.t[:, 0:n], weu.t[:, s, kc, hc * 128:(hc + 1) * 128], h2T.t[:, kc, c0:c0 + n],
                                   [weu.bs[s]] + hrd, kc == 0, kc == 7)
                            ss_ = hc % 2
                            act(sgm.t[:, ss_, 0:n], pg.t[:, 0:n], AF.Silu, [pg.b], [sgm.bs[ss_]])
                            tt("dve", hid.t[:, hs, hc, 0:n], pu.t[:, 0:n], sgm.t[:, ss_, 0:n], ALU.mult, [pu.b, sgm.bs[ss_]],
                               [hid.bs[hs]])
                        for ti, t_ in enumerate(tiles):
                            for half in range(2):
                                py = bank()
                                hs_ = slice(half * 512, (half + 1) * 512)
                                for hc in range(4):
                                    mm(py, py.t[:, :], hid.t[:, hs, hc, ti * 128:(ti + 1) * 128], wed.t[:, s, hc, hs_],
                                       [hid.bs[hs], wed.bs[s]], hc == 0, hc == 3)
                                y_ = ty_rr % 2
                                ty_rr += 1
                                tt("dve", ty.t[:, y_, :], py.t[:, :], gt2row.t[:, cls, hs_], ALU.mult, [py.b, gt2row.b],
                                   [ty.bs[y_]])
                                stt("dve", x_tm.t[:, t_, hs_], ty.t[:, y_, :], Wt.t[:, t_, e_:e_ + 1], x_tm.t[:, t_, hs_],
                                    ALU.mult, ALU.add, [ty.bs[y_], Wt.bs[t_], x_tm.bs[t_]], [x_tm.bs[t_]])
                P.barrier()
            if l == DL and not stopped:
                dump("xout", x_tm.bs, x_tm.t[:], lambda o: o[:, :, :])

        with ExitStack() as ph:
            gfin = sbuf(ph, "gfin", [128, D])
            ob = sbuf(ph, "ob", [128, 2, D], F32, 2)
            dma("sp", gfin.t[:], gfin_d.t[:, :], [], [gfin.b])
            compute_rstd(NT_OWN)
            for t_ in range(NT_OWN):
                s = t_ % 2
                stt("dve", ob.t[:, s, :], x_tm.t[:, t_, :], rstd.t[:, t_:t_ + 1], gfin.t[:], ALU.mult, ALU.mult,
                    [x_tm.bs[t_], rstd.b, gfin.b], [ob.bs[s]])
                dma("sp", out_d.t[t_ * 128:(t_ + 1) * 128, :], ob.t[:, s, :], [ob.bs[s]], [out_d.b])
        P.wait_all("sp", [out_d.b] + [v.b for v in dbg_out.values()])
        P.emit()
    return nc, names_in


def _kc(w):
    K, C = w.shape
    return np.ascontiguousarray(w.reshape(K // 128, 128, C).transpose(1, 0, 2))


def prep_inputs(inputs, nlayers=2, nei=NEXP):
    f = lambda a: np.ascontiguousarray(np.asarray(a, dtype=np.float32))
    I_ = {k: f(v) for k, v in inputs.items()}
    shared = {}
    e = np.arange(64)
    partner = np.where((e % 32) < 16, e + 16, e - 16)
    head_order = [h for c in range(4) for h in (c, c + 4)]
    qcols = np.concatenate([h * 64 + np.arange(64) for h in head_order])
    qrcols = np.concatenate([h * 64 + partner for h in head_order])
    kcols = 512 + np.arange(128)
    krcols = 512 + np.concatenate([hk * 64 + partner for hk in range(2)])
    vcols = 640 + np.arange(128)
    ucols = 768 + np.arange(512)
    w1cols = np.concatenate([qcols, qrcols, kcols, krcols, vcols, ucols])
    brarows = np.concatenate([h * 64 + np.arange(64) for h in head_order])
    shared["ident"] = np.eye(128, dtype=np.float32)
    shared["iota"] = np.ascontiguousarray(np.broadcast_to(np.arange(1, 513, dtype=np.float32), (128, 512)))
    shared["gfin"] = np.ascontiguousarray(np.broadcast_to(I_["g_final"], (128, D)))
    ee = np.arange(128) % 64
    ropef = np.stack([(ee % 16).astype(np.float32), np.where((ee % 32) < 16, -1.0, 1.0).astype(np.float32)], 1)
    shared["ropef"] = np.ascontiguousarray(ropef)
    for l in range(nlayers):
        win = I_["w_in"][l]
        shared[f"wmod{l}"] = _kc(I_["w_mod"][l])
        shared[f"bmodT{l}"] = np.ascontiguousarray(I_["b_mod"][l].reshape(48, 128).T)
        shared[f"g1T{l}"] = np.ascontiguousarray(I_["g_norm1"][l].reshape(8, 128).T)
        shared[f"g2T{l}"] = np.ascontiguousarray(I_["g_norm2"][l].reshape(8, 128).T)
        shared[f"w1_{l}"] = _kc(win[:, w1cols])
        ga = _kc(win[:, 1280:2304]).reshape(128, 8, 8, 128)
        gs = _kc(win[:, 2304:3328]).reshape(128, 8, 8, 128)
        shared[f"w2g{l}"] = np.ascontiguousarray(np.concatenate([ga, gs], axis=3).transpose(2, 0, 1, 3))
        shared[f"wglu{l}"] = _kc(I_["w_glu"][l])
        shared[f"wbra{l}"] = _kc(I_["w_br_attn"][l][brarows, :])
        shared[f"wbrs{l}"] = _kc(I_["w_br_ssm"][l])
        shared[f"wout{l}"] = _kc(I_["w_out"][l])
        shared[f"wr{l}"] = _kc(np.concatenate([I_["w_router_group"][l], I_["w_router_expert"][l]], axis=1))
        shared[f"weg{l}"] = np.ascontiguousarray(I_["w_exp_gate"][l][:nei].reshape(nei, 8, 128, 512).transpose(0, 2, 1, 3))
        shared[f"weu{l}"] = np.ascontiguousarray(I_["w_exp_up"][l][:nei].reshape(nei, 8, 128, 512).transpose(0, 2, 1, 3))
        shared[f"wed{l}"] = np.ascontiguousarray(I_["w_exp_down"][l][:nei].reshape(nei, 4, 128, D).transpose(0, 2, 1, 3))
        shared[f"sink{l}"] = np.ascontiguousarray(np.broadcast_to(I_["attn_sink"][l], (128, 8)))
    kk = np.arange(128)[:, None]
    qq = np.arange(128)[None, :]
    mprev = np.tile((kk >= qq).astype(np.float32), (1, 4))
    mnext = np.tile((kk <= qq).astype(np.float32), (1, 4))
    per_core = []
    for r in range(NCORES):
        b, j = r // 4, r % 4
        m = dict(shared)
        t0 = j * TOWN
        m["x_own"] = np.ascontiguousarray(I_["x"][b, t0:t0 + TOWN])
        m["ctx_b"] = np.ascontiguousarray(I_["ctx"][b])
        cT = np.stack([I_["c"][b].reshape(8, 128).T, I_["c_ctx"].reshape(8, 128).T], axis=2)
        m["cT"] = np.ascontiguousarray(cT)
        tpos = np.arange(t0, t0 + TOWN)
        rows = (tpos // 64).astype(np.float32)
        cols = (tpos % 64).astype(np.float32)
        pos = np.zeros((128, TALL), np.float32)
        is_row = (ee % 64) < 32
        pos[:, :TOWN] = np.where(is_row[:, None], rows[None, :], cols[None, :])
        m["pos"] = pos
        mk = np.zeros((128, 4, 512), np.float32)
        mk[:, 0] = mprev if j > 0 else 0.0
        mk[:, 1] = mprev
        mk[:, 2] = mnext if j < 3 else 0.0
        mk[:, 3] = mnext
        m["masks"] = mk
        sel = np.zeros((128, 12, 128), np.float32)
        eye = np.eye(128, dtype=np.float32)
        sel[:, j] = eye
        if j > 0:
            sel[:, 4 + j - 1] = eye
        if j < 3:
            sel[:, 8 + j + 1] = eye
        m["selmat"] = sel
        for l in range(nlayers):
            win = I_["w_in"][l]
            m[f"wus{l}"] = _kc(win[:, 768 + 128 * j:768 + 128 * (j + 1)])
            g0 = 8 * j

            def rowlay(a):
                a = a.reshape((2, 4, 2, 64) + a.shape[3:])
                perm = (2, 3, 0, 1) + tuple(range(4, a.ndim))
                a = a.transpose(perm)
                return np.ascontiguousarray(a.reshape((128, 8) + a.shape[4:]))

            m[f"lamre{l}"] = rowlay(I_["ssm_lam_re"][l][:, g0:g0 + 8])
            m[f"lamim{l}"] = rowlay(I_["ssm_lam_im"][l][:, g0:g0 + 8])
            ldt = np.broadcast_to(I_["ssm_log_dt"][l][:, g0:g0 + 8, None], (2, 8, 64))
            m[f"ldt{l}"] = rowlay(np.ascontiguousarray(ldt))
            m[f"bre{l}"] = rowlay(I_["ssm_b_re"][l][:, g0:g0 + 8])
            m[f"bim{l}"] = rowlay(I_["ssm_b_im"][l][:, g0:g0 + 8])
            m[f"cre{l}"] = rowlay(np.ascontiguousarray(I_["ssm_c_re"][l][:, g0:g0 + 8].transpose(0, 1, 3, 2)))
            m[f"cim{l}"] = rowlay(np.ascontiguousarray(I_["ssm_c_im"][l][:, g0:g0 + 8].transpose(0, 1, 3, 2)))
            m[f"dsk{l}"] = np.ascontiguousarray(I_["ssm_d"][l][128 * j:128 * (j + 1)].reshape(128, 1))
        for k in list(m.keys()):
            if False:
                a = m[k]
                a2 = a.reshape(-1, a.shape[-1])
                rr = a2.shape[0] // NCORES
                m[k] = np.ascontiguousarray(a2[r * rr:(r + 1) * rr])
        per_core.append(m)
    return per_core


_CACHE = {}


def kernel(**inputs):
    if "nc" not in _CACHE:
        _CACHE["nc"] = build_program(2)
    nc, names = _CACHE["nc"]
    per_core = prep_inputs(inputs, 2)
    in_maps = [{k: m[k] for k in names} for m in per_core]
    res = run_bass_kernel_spmd(nc, in_maps, core_ids=list(range(NCORES)))
    out = np.zeros((2, SEQ, D), np.float32)
    for r in range(NCORES):
        b, j = r // 4, r % 4
        out[b, j * TOWN:(j + 1) * TOWN] = res.results[r]["out"]
    return out
```

```python
from contextlib import ExitStack
import numpy as np
import concourse.bass as bass
import concourse.mybir as mybir
from concourse.bass_utils import run_bass_kernel_spmd

dt = mybir.dt
ALU = mybir.AluOpType
AF = mybir.ActivationFunctionType
AX = mybir.AxisListType
F32 = dt.float32
BF16 = dt.bfloat16
I32 = dt.int32

NCORES = 8
D = 1024
TOWN = 2048
NT_OWN = 16
NT = 18
TALL = 2304
SEQ = 8192
CTX = 256
STREAM = SEQ + CTX
NEXP = 32
PI = float(np.pi)
GROUPS = [[0, 1, 2, 3], [4, 5, 6, 7]]


class Buf:
    __slots__ = ("name", "last_w", "readers")

    def __init__(self, name=""):
        self.name = name
        self.last_w = None
        self.readers = {}


class Prog:
    ENG = ("pe", "act", "dve", "pool", "sp")

    def __init__(self, nc, stack, ndma=None):
        ndma = ndma or {"sp": 8, "act": 2, "pool": 6, "cc": 3}
        self.nc = nc
        self.q = {e: [] for e in self.ENG}
        self.sems = {}
        self.cnt = {e: 0 for e in self.ENG}
        self.waited = {e: {} for e in self.ENG}
        for e in self.ENG:
            self.sems[("c", e)] = stack.enter_context(nc.semaphore("c_" + e))
        self.dma_slots = {}
        self.dma_tot = {}
        self.dma_rr = {}
        for e, n in ndma.items():
            keys = []
            for i in range(n):
                k = ("d", e, i)
                self.sems[k] = stack.enter_context(nc.semaphore("d_%s%d" % (e, i)))
                self.dma_tot[k] = 0
                keys.append(k)
            self.dma_slots[e] = keys
            self.dma_rr[e] = 0

    def _wait(self, eng, k, v):
        if k == ("c", "pe") and eng == "pe":
            return
        if self.waited[eng].get(k, 0) >= v:
            return
        self.waited[eng][k] = v
        self.q[eng].append(("w", k, v))

    def _deps(self, eng, reads, writes):
        deps = {}

        def add(ev):
            if ev is None:
                return
            k, v = ev
            if deps.get(k, 0) < v:
                deps[k] = v

        for b in reads:
            add(b.last_w)
        for b in writes:
            add(b.last_w)
            for k, v in b.readers.items():
                add((k, v))
        for k, v in deps.items():
            self._wait(eng, k, v)

    def _commit(self, ev, reads, writes):
        k, v = ev
        for b in reads:
            if b.readers.get(k, 0) < v:
                b.readers[k] = v
        for b in writes:
            b.last_w = ev
            b.readers = {}

    def op(self, eng, fn, reads=(), writes=()):
        self._deps(eng, reads, writes)
        self.cnt[eng] += 1
        ev = (("c", eng), self.cnt[eng])
        self.q[eng].append(("o", fn, ("c", eng), 1))
        self._commit(ev, reads, writes)
        return ev

    def dma(self, qeng, fn, reads=(), writes=(), inc=16):
        skey = "cc" if inc == 1 else qeng
        slots = self.dma_slots[skey]
        k = slots[self.dma_rr[skey] % len(slots)]
        self.dma_rr[skey] += 1
        prev = self.dma_tot[k]
        if prev > 0:
            self._wait(qeng, k, prev)
        self._deps(qeng, reads, writes)
        self.dma_tot[k] = prev + inc
        ev = (k, prev + inc)
        self.q[qeng].append(("o", fn, k, inc))
        self._commit(ev, reads, writes)
        return ev

    def wait_all(self, eng, bufs):
        self._deps(eng, bufs, ())

    def barrier(self):
        tot = {("c", e): self.cnt[e] for e in self.ENG}
        tot.update(self.dma_tot)
        for e in self.ENG:
            for k, v in tot.items():
                if v > 0:
                    self._wait(e, k, v)

    def emit(self):
        nc = self.nc
        sems = self.sems

        def replay(e, name):
            for it in self.q[name]:
                if it[0] == "w":
                    e.wait_ge(sems[it[1]], it[2])
                else:
                    ins = it[1](e)
                    ins.then_inc(sems[it[2]], it[3])

        with nc.Block() as block:

            @block.tensor
            def _(e):
                replay(e, "pe")

            @block.vector
            def _(e):
                replay(e, "dve")

            @block.scalar
            def _(e):
                replay(e, "act")

            @block.gpsimd
            def _(e):
                replay(e, "pool")

            @block.sync
            def _(e):
                replay(e, "sp")


class TL:
    def __init__(self, t, nslots=1):
        self.t = t
        self.bs = [Buf() for _ in range(nslots)]

    @property
    def b(self):
        return self.bs[0]


def build_program(nlayers=2, dbg=None):
    dbg = dbg or {}
    NEI = dbg.get("_nexp", NEXP)
    nc = bass.Bass("TRN2", target_bir_lowering=False)
    names_in = []

    def inp(name, shape, d=F32):
        names_in.append(name)
        return TL(nc.dram_tensor(name, list(shape), d, kind="ExternalInput").ap())

    gathers = []

    def ginp(name, shape):
        return inp(name, shape)

    def ginp_unused(name, shape):
        R = int(np.prod(shape[:-1]))
        C = int(shape[-1])
        assert R % NCORES == 0
        names_in.append(name)
        shard = TL(nc.dram_tensor(name, [R // NCORES, C], F32, kind="ExternalInput").ap())
        bounce = TL(nc.dram_tensor(name + "_bnc", [R // NCORES, C], F32).ap())
        full = TL(nc.dram_tensor(name + "_full", list(shape), F32).ap())
        gathers.append((shard, bounce, full))
        return full

    x_own = inp("x_own", [TOWN, D])
    ctx_b = inp("ctx_b", [CTX, D])
    cT = inp("cT", [128, 8, 2])
    pos_d = inp("pos", [128, TALL])
    ropef_d = inp("ropef", [128, 2])
    masks_d = inp("masks", [128, 4, 512])
    selmat_d = inp("selmat", [128, 12, 128])
    ident_d = inp("ident", [128, 128])
    iota_d = inp("iota", [128, 512])
    gfin_d = inp("gfin", [128, D])
    L = []
    for l in range(nlayers):
        w = {}
        w["wmod"] = ginp(f"wmod{l}", [128, 8, 6 * D])
        w["bmodT"] = inp(f"bmodT{l}", [128, 48])
        w["g1T"] = inp(f"g1T{l}", [128, 8])
        w["g2T"] = inp(f"g2T{l}", [128, 8])
        w["w1"] = ginp(f"w1_{l}", [128, 8, 1920])
        w["wus"] = inp(f"wus{l}", [128, 8, 128])
        w["w2g"] = ginp(f"w2g{l}", [8, 128, 8, 256])
        w["wglu"] = inp(f"wglu{l}", [128, 4, 512])
        w["wbra"] = ginp(f"wbra{l}", [128, 4, D])
        w["wbrs"] = ginp(f"wbrs{l}", [128, 4, D])
        w["wout"] = ginp(f"wout{l}", [128, 8, D])
        w["wr"] = inp(f"wr{l}", [128, 8, 36])
        w["weg"] = ginp(f"weg{l}", [NEI, 128, 8, 512])
        w["weu"] = ginp(f"weu{l}", [NEI, 128, 8, 512])
        w["wed"] = ginp(f"wed{l}", [NEI, 128, 4, D])
        w["sink"] = inp(f"sink{l}", [128, 8])
        w["lamre"] = inp(f"lamre{l}", [128, 8])
        w["lamim"] = inp(f"lamim{l}", [128, 8])
        w["ldt"] = inp(f"ldt{l}", [128, 8])
        w["bre"] = inp(f"bre{l}", [128, 8, 16])
        w["bim"] = inp(f"bim{l}", [128, 8, 16])
        w["cre"] = inp(f"cre{l}", [128, 8, 16])
        w["cim"] = inp(f"cim{l}", [128, 8, 16])
        w["dsk"] = inp(f"dsk{l}", [128, 1])
        L.append(w)
    out_d = TL(nc.dram_tensor("out", [TOWN, D], F32, kind="ExternalOutput").ap())
    dbg_out = {}
    for k, shp in dbg.items():
        if k.startswith("_"):
            continue
        dbg_out[k] = TL(nc.dram_tensor("dbg_" + k, list(shp), F32, kind="ExternalOutput").ap())
    AGR = TOWN + 128
    ag1_in = [TL(nc.dram_tensor(f"ag1_in{i}", [r_, 512], BF16).ap()) for i, r_ in enumerate((1024, 1024, 128))]
    ag1_out = [TL(nc.dram_tensor(f"ag1_out{i}", [4 * r_, 512], BF16).ap()) for i, r_ in enumerate((1024, 1024, 128))]
    ag2_in = [TL(nc.dram_tensor(f"ag2_in{i}", [128, c_], BF16).ap()) for i, c_ in enumerate((4096, 4096, CTX))]
    ag2_out = [TL(nc.dram_tensor(f"ag2_out{i}", [512, c_], BF16).ap()) for i, c_ in enumerate((4096, 4096, CTX))]
    cos_d = TL(nc.dram_tensor("cos_d", [128, TALL], F32).ap())
    sin_d = TL(nc.dram_tensor("sin_d", [128, TALL], F32).ap())
    yTf_d = TL(nc.dram_tensor("yTf_d", [128, STREAM], F32).ap())
    attn_d = TL(nc.dram_tensor("attn_d", [128, 4, TALL], BF16).ap())

    with ExitStack() as st:
        P = Prog(nc, st)

        uid = [0]

        def sbuf(stack, name, shape, d=F32, nslots=1):
            uid[0] += 1
            return TL(stack.enter_context(nc.sbuf_tensor("sb%d_%s" % (uid[0], name), list(shape), d)), nslots)

        PS = [TL(st.enter_context(nc.psum_tensor(f"ps{i}", [128, 512], F32))) for i in range(8)]
        ps_rr = [0]

        def bank(exclude=()):
            while True:
                i = ps_rr[0] % 8
                ps_rr[0] += 1
                if i not in exclude:
                    return PS[i]

        def mm(out_tl, out_ap, lhsT_ap, rhs_ap, reads, start=True, stop=True):
            P.op("pe", lambda e: e.matmul(out_ap, lhsT_ap, rhs_ap, start=start, stop=stop), reads, [out_tl.b])

        def tr(out_tl, out_ap, in_ap, ident_ap, reads):
            P.op("pe", lambda e: e.transpose(out_ap, in_ap, ident_ap), reads, [out_tl.b])

        def act(out_ap, in_ap, func, reads, writes, bias=None, scale=None, accum=None):
            kw = {}
            if bias is not None:
                kw["bias"] = bias
            if scale is not None:
                kw["scale"] = scale
            if accum is not None:
                kw["accum_out"] = accum
            P.op("act", lambda e: e.activation(out_ap, in_ap, func, **kw), reads, writes)

        def tt(eng, out_ap, a_ap, b_ap, op, reads, writes):
            P.op(eng, lambda e: e.tensor_tensor(out_ap, a_ap, b_ap, op), reads, writes)

        def ts(eng, out_ap, a_ap, s1, s2, op0, op1, reads, writes):
            if op1 is None:
                P.op(eng, lambda e: e.tensor_scalar(out_ap, a_ap, s1, None, op0), reads, writes)
            else:
                P.op(eng, lambda e: e.tensor_scalar(out_ap, a_ap, s1, s2, op0, op1), reads, writes)

        def stt(eng, out_ap, a_ap, s, b_ap, op0, op1, reads, writes):
            P.op(eng, lambda e: e.scalar_tensor_tensor(out_ap, a_ap, s, b_ap, op0, op1), reads, writes)

        def cp(eng, out_ap, in_ap, reads, writes):
            P.op(eng, lambda e: e.tensor_copy(out_ap, in_ap), reads, writes)

        def dma(q, out_ap, in_ap, reads, writes, **kw):
            P.dma(q, lambda e: e.dma_start(out=out_ap, in_=in_ap, **kw), reads, writes)

        def dump(key, src_tl, src_ap, dst_ap_fn):
            if key in dbg_out:
                dma("sp", dst_ap_fn(dbg_out[key].t), src_ap, [src_tl.b] if isinstance(src_tl, TL) else src_tl,
                    [dbg_out[key].b])

        for shard, bounce, full in gathers:
            nr_ = shard.t.shape[0]
            for r0_ in range(0, nr_, 1024):
                r1_ = min(nr_, r0_ + 1024)
                dma("sp", bounce.t[r0_:r1_, :], shard.t[r0_:r1_, :], [], [bounce.b])
            P.dma("pool", lambda e, bounce=bounce, full=full: e.collective_compute(
                "AllGather", ALU.bypass, replica_groups=[list(range(NCORES))], ins=[bounce.t.opt()], outs=[full.t.opt()]),
                [bounce.b], [full.b], inc=1)

        WBF = []
        for l in range(nlayers):
            wb = {}
            for nm in ("w1", "w2g", "wout", "wbra", "wbrs", "wglu"):
                src = L[l][nm]
                dst = TL(nc.dram_tensor(f"{nm}bf{l}", list(src.t.shape), BF16).ap())
                for i0 in range(src.t.shape[0] if nm == "w2g" else src.t.shape[1]):
                    if nm == "w2g":
                        dma("pool", dst.t[i0, :, :, :], src.t[i0, :, :, :], [src.b], [dst.b])
                    else:
                        dma("pool", dst.t[:, i0, :], src.t[:, i0, :], [src.b], [dst.b])
                wb[nm] = dst
            WBF.append(wb)

        x_tm = sbuf(st, "x_tm", [128, NT, D], F32, NT)
        ident = sbuf(st, "ident", [128, 128])
        zeros = sbuf(st, "zeros", [128, 512])
        iota = sbuf(st, "iota", [128, 512])
        onesb = sbuf(st, "onesb", [128, 64], BF16)
        onesf = sbuf(st, "onesf", [128, 128])
        siluT = sbuf(st, "siluT", [128, 8, 2])
        modT = sbuf(st, "modT", [128, 48, 2])
        bmodT = sbuf(st, "bmodT", [128, 48])
        gT = sbuf(st, "gT", [128, 16])
        A1 = sbuf(st, "A1", [128, 2, 8])
        A2 = sbuf(st, "A2", [128, 2, 8])
        rstd = sbuf(st, "rstd", [128, NT])
        sstat = sbuf(st, "sstat", [128, NT])
        xs = sbuf(st, "xs", [128, D])
        sm = sbuf(st, "sm", [128, 8])
        ucT = sbuf(st, "ucT", [128, CTX], BF16)
        dg = sbuf(st, "dg", [128, 128])

        def AP_rev(tl, row_elems, col_last, n):
            return bass.AP(tl.t, col_last, [[row_elems, 128], [-1, n]])

        def range_reduce(kf_tl, kf_ap, ki_tl, ki_ap, out_ap, in_ap, shift, reads, writes):
            ts("dve", kf_ap, in_ap, shift, 1.0 / (2 * PI), ALU.add, ALU.mult, reads, [kf_tl.b])
            cp("dve", ki_ap, kf_ap, [kf_tl.b], [ki_tl.b])
            cp("dve", kf_ap, ki_ap, [ki_tl.b], [kf_tl.b])
            stt("dve", kf_ap, kf_ap, -2 * PI, in_ap, ALU.mult, ALU.add, [kf_tl.b] + list(reads), [kf_tl.b])
            ts("dve", out_ap, kf_ap, shift, None, ALU.add, None, [kf_tl.b], writes)
            ts("dve", out_ap, out_ap, 3.14159, -3.14159, ALU.min, ALU.max, writes, writes)

        with ExitStack() as ph:
            posT = sbuf(ph, "posT", [128, TALL])
            ang = sbuf(ph, "ang", [128, TALL])
            ki = sbuf(ph, "ki", [128, TALL], I32)
            kf = sbuf(ph, "kf", [128, TALL])
            tb = sbuf(ph, "tb", [128, TALL])
            ropef = sbuf(ph, "ropef", [128, 2])
            dma("sp", x_tm.t[:, 0:NT_OWN, :], x_own.t.rearrange("(t p) d -> p t d", p=128), [], x_tm.bs[0:NT_OWN])
            dma("sp", x_tm.t[:, NT_OWN:NT, :], ctx_b.t.rearrange("(t p) d -> p t d", p=128), [], x_tm.bs[NT_OWN:NT])
            dma("sp", ident.t[:], ident_d.t[:, :], [], [ident.b])
            dma("sp", posT.t[:], pos_d.t[:, :], [], [posT.b])
            dma("sp", ropef.t[:], ropef_d.t[:, :], [], [ropef.b])
            dma("sp", iota.t[:], iota_d.t[:, :], [], [iota.b])
            dma("sp", siluT.t[:], cT.t[:, :, :], [], [siluT.b])
            P.op("pool", lambda e: e.memset(zeros.t[:], 0.0), [], [zeros.b])
            P.op("pool", lambda e: e.memset(onesb.t[:], 1.0), [], [onesb.b])
            P.op("pool", lambda e: e.memset(onesf.t[:], 1.0), [], [onesf.b])
            act(siluT.t[:], siluT.t[:], AF.Silu, [siluT.b], [siluT.b])
            act(sm.t[:, 0:1], ropef.t[:, 0:1], AF.Exp, [ropef.b], [sm.b], scale=-float(np.log(10000.0)) / 16.0)
            ts("dve", ang.t[:], posT.t[:], sm.t[:, 0:1], None, ALU.mult, None, [posT.b, sm.b], [ang.b])
            range_reduce(kf, kf.t[:], ki, ki.t[:], tb.t[:], ang.t[:], 0.0, [ang.b], [tb.b])
            act(tb.t[:], tb.t[:], AF.Sin, [tb.b], [tb.b])
            ts("dve", tb.t[:], tb.t[:], ropef.t[:, 1:2], None, ALU.mult, None, [tb.b, ropef.b], [tb.b])
            dma("sp", sin_d.t[:, :], tb.t[:], [tb.b], [sin_d.b])
            range_reduce(kf, kf.t[:], ki, ki.t[:], tb.t[:], ang.t[:], PI / 2, [ang.b], [tb.b])
            act(tb.t[:], tb.t[:], AF.Sin, [tb.b], [tb.b])
            dma("sp", cos_d.t[:, :], tb.t[:], [tb.b], [cos_d.b])
            P.barrier()

        def tile_cls(t_):
            return 0 if t_ < NT_OWN else 1

        def compute_rstd(ntiles):
            for t_ in range(ntiles):
                act(xs.t[:], x_tm.t[:, t_, :], AF.Square, [x_tm.bs[t_]], [xs.b, sstat.b], accum=sstat.t[:, t_:t_ + 1])
            ts("dve", rstd.t[:, 0:ntiles], sstat.t[:, 0:ntiles], 1.0 / D, 1e-6, ALU.mult, ALU.add, [sstat.b], [rstd.b])
            act(rstd.t[:, 0:ntiles], rstd.t[:, 0:ntiles], AF.Sqrt, [rstd.b], [rstd.b])
            P.op("dve", lambda e: e.reciprocal(rstd.t[:, 0:ntiles], rstd.t[:, 0:ntiles]), [rstd.b], [rstd.b])

        def norm_tile(t_, Acls, shslot, dst_ap_fn, dst_bufs, f32_dst=None):
            cls = tile_cls(t_)
            act(xs.t[:], x_tm.t[:, t_, :], AF.Identity, [x_tm.bs[t_], rstd.b], [xs.b], scale=rstd.t[:, t_:t_ + 1])
            for half in range(2):
                pb_ = bank()
                for q4 in range(4):
                    fc = half * 4 + q4
                    tr(pb_, pb_.t[:, q4 * 128:(q4 + 1) * 128], xs.t[:, fc * 128:(fc + 1) * 128], ident.t[:], [xs.b, ident.b])
                for q4 in range(4):
                    fc = half * 4 + q4
                    act(dst_ap_fn(fc), pb_.t[:, q4 * 128:(q4 + 1) * 128], AF.Identity, [pb_.b, Acls.b, modT.b], dst_bufs,
                        scale=Acls.t[:, cls, fc:fc + 1], bias=modT.t[:, shslot * 8 + fc, cls:cls + 1])
                    if f32_dst is not None:
                        act(f32_dst.t[:, fc, :], pb_.t[:, q4 * 128:(q4 + 1) * 128], AF.Identity, [pb_.b, Acls.b, modT.b],
                            [f32_dst.b], scale=Acls.t[:, cls, fc:fc + 1], bias=modT.t[:, shslot * 8 + fc, cls:cls + 1])

        def make_gtrow(dst, slot):
            for cls in range(2):
                for fc in range(8):
                    ts("dve", dg.t[:], ident.t[:], modT.t[:, slot * 8 + fc, cls:cls + 1], None, ALU.mult, None,
                       [ident.b, modT.b], [dg.b])
                    pb_ = bank()
                    mm(pb_, pb_.t[:, 0:128], onesf.t[:], dg.t[:], [onesf.b, dg.b])
                    cp("dve", dst.t[:, cls, fc * 128:(fc + 1) * 128], pb_.t[:, 0:128], [pb_.b], [dst.b])

        def blk_of(t_):
            return t_ + 1 if t_ < NT_OWN else 18 + (t_ - NT_OWN)

        DL = dbg.get("_layer", 0)
        stopped = False
        for l in range(nlayers):
            if dbg.get("_stop") == "0":
                break
            W = L[l]
            WB = WBF[l]
            ctx_out = l < nlayers - 1
            with ExitStack() as ph:
                wm = sbuf(ph, "wm", [128, 2, 8, 512], F32, 2)
                dma("sp", bmodT.t[:], W["bmodT"].t[:, :], [], [bmodT.b])
                dma("sp", gT.t[:, 0:8], W["g1T"].t[:, :], [], [gT.b])
                dma("sp", gT.t[:, 8:16], W["g2T"].t[:, :], [], [gT.b])
                mps = bank()
                for piece in range(12):
                    s = piece % 2
                    dma("sp", wm.t[:, s, :, :], W["wmod"].t[:, :, piece * 512:(piece + 1) * 512], [W["wmod"].b], [wm.bs[s]])
                    for c4 in range(4):
                        cc = piece * 4 + c4
                        for kc in range(8):
                            mm(mps, mps.t[:, cc * 2:cc * 2 + 2], wm.t[:, s, kc, c4 * 128:(c4 + 1) * 128],
                               siluT.t[:, kc, :], [wm.bs[s], siluT.b], kc == 0, kc == 7)
                for cls in range(2):
                    tt("dve", modT.t[:, :, cls], mps.t[:, cls:96:2], bmodT.t[:], ALU.add, [mps.b, bmodT.b], [modT.b])
                for cls in range(2):
                    stt("dve", A1.t[:, cls, :], modT.t[:, 8:16, cls], 1.0, gT.t[:, 0:8], ALU.add, ALU.mult,
                        [modT.b, gT.b], [A1.b])
                    stt("dve", A2.t[:, cls, :], modT.t[:, 32:40, cls], 1.0, gT.t[:, 8:16], ALU.add, ALU.mult,
                        [modT.b, gT.b], [A2.b])
                if l == DL:
                    dump("modT", modT, modT.t[:], lambda o: o[:, :, :])
                P.barrier()

            kvst = ExitStack()
            kT = sbuf(kvst, "kT", [128, 20, 128], BF16, 20)
            vv = sbuf(kvst, "vv", [128, 20, 128], BF16, 20)
            with ExitStack() as ph:
                w1 = sbuf(ph, "w1", [128, 8, 1920], BF16)
                wus = sbuf(ph, "wus", [128, 8, 128], BF16)
                hT = sbuf(ph, "hT", [128, 8, 512], BF16)
                ust = sbuf(ph, "ust", [128, 2, 512], BF16, 2)
                tmpa = sbuf(ph, "tmpa", [128, 512])
                tmpb = sbuf(ph, "tmpb", [128, 512])
                cosT = sbuf(ph, "cosT", [128, TALL])
                sinT = sbuf(ph, "sinT", [128, TALL])
                dma("sp", cosT.t[:], cos_d.t[:, :], [cos_d.b], [cosT.b])
                dma("sp", sinT.t[:], sin_d.t[:, :], [sin_d.b], [sinT.b])
                dma("sp", w1.t[:, :, :], WB["w1"].t[:, :, :], [WB["w1"].b], [w1.b])
                dma("pool", wus.t[:], W["wus"].t[:, :, :], [], [wus.b])
                compute_rstd(NT)
                ust_rr = 0
                for ch in range(5):
                    tiles = list(range(ch * 4, min(ch * 4 + 4, NT)))
                    n = len(tiles) * 128
                    c0 = ch * 512
                    for ti, t_ in enumerate(tiles):
                        norm_tile(t_, A1, 0, lambda fc, ti=ti: hT.t[:, fc, ti * 128:(ti + 1) * 128], [hT.b])
                    pk = bank()
                    pkr = bank()
                    for kc in range(8):
                        mm(pk, pk.t[:, 0:n], w1.t[:, kc, 1024:1152], hT.t[:, kc, 0:n], [w1.b, hT.b], kc == 0, kc == 7)
                    for kc in range(8):
                        mm(pkr, pkr.t[:, 0:n], w1.t[:, kc, 1152:1280], hT.t[:, kc, 0:n], [w1.b, hT.b], kc == 0, kc == 7)
                    tt("dve", tmpa.t[:, 0:n], pk.t[:, 0:n], cosT.t[:, c0:c0 + n], ALU.mult, [pk.b, cosT.b], [tmpa.b])
                    tt("dve", tmpb.t[:, 0:n], pkr.t[:, 0:n], sinT.t[:, c0:c0 + n], ALU.mult, [pkr.b, sinT.b], [tmpb.b])
                    for ti, t_ in enumerate(tiles):
                        blk = blk_of(t_)
                        tt("dve", kT.t[:, blk, :], tmpa.t[:, ti * 128:(ti + 1) * 128], tmpb.t[:, ti * 128:(ti + 1) * 128],
                           ALU.add, [tmpa.b, tmpb.b], [kT.bs[blk]])
                    pv = bank()
                    for ti, t_ in enumerate(tiles):
                        for kc in range(8):
                            mm(pv, pv.t[:, ti * 128:(ti + 1) * 128], hT.t[:, kc, ti * 128:(ti + 1) * 128],
                               w1.t[:, kc, 1280:1408], [w1.b, hT.b], kc == 0, kc == 7)
                    for ti, t_ in enumerate(tiles):
                        blk = blk_of(t_)
                        act(vv.t[:, blk, :], pv.t[:, ti * 128:(ti + 1) * 128], AF.Identity, [pv.b], [vv.bs[blk]])
                    if ch < 4:
                        for ti, t_ in enumerate(tiles):
                            pu = bank()
                            for kc in range(8):
                                mm(pu, pu.t[:, :], hT.t[:, kc, ti * 128:(ti + 1) * 128], w1.t[:, kc, 1408:1920],
                                   [w1.b, hT.b], kc == 0, kc == 7)
                            s = ust_rr % 2
                            ust_rr += 1
                            act(ust.t[:, s, :], pu.t[:, :], AF.Identity, [pu.b], [ust.bs[s]])
                            pc_, lr_ = (t_ * 128) // 1024, (t_ * 128) % 1024
                            dma("sp", ag1_in[pc_].t[lr_:lr_ + 128, :], ust.t[:, s, :], [ust.bs[s]], [ag1_in[pc_].b])
                    else:
                        pu = bank()
                        for kc in range(8):
                            mm(pu, pu.t[:, 0:CTX], wus.t[:, kc, :], hT.t[:, kc, 0:CTX], [wus.b, hT.b], kc == 0, kc == 7)
                        act(ucT.t[:], pu.t[:, 0:CTX], AF.Identity, [pu.b], [ucT.b])
                for i4, (tl_, blk) in enumerate(((kT, 1), (kT, 16), (vv, 1), (vv, 16))):
                    dma("sp", ag1_in[2].t[:, i4 * 128:(i4 + 1) * 128], tl_.t[:, blk, :], [tl_.bs[blk]], [ag1_in[2].b])
                for pc_ in range(3):
                    P.dma("pool", lambda e, pc_=pc_: e.collective_compute(
                        "AllGather", ALU.bypass, replica_groups=GROUPS, ins=[ag1_in[pc_].t.opt()], outs=[ag1_out[pc_].t.opt()]),
                        [ag1_in[pc_].b], [ag1_out[pc_].b], inc=1)
                P.barrier()
            if dbg.get("_stop") == "B" and l == DL:
                stopped = True

            if not stopped:
              with ExitStack() as ph:
                uTf = sbuf(ph, "uTf", [128, STREAM], BF16, 17)
                hal = sbuf(ph, "hal", [128, 4, 512], BF16)
                utile = sbuf(ph, "utile", [128, 2, 512], BF16, 2)
                selm = sbuf(ph, "selm", [128, 12, 128], BF16)
                prm = sbuf(ph, "prm", [128, 16, 8])
                bb = sbuf(ph, "bb", [128, 4, 8, 16])
                cc_ = sbuf(ph, "cc", [128, 2, 8, 16])
                Wsc = sbuf(ph, "Wsc", [128, 128])
                lB = sbuf(ph, "lB", [128, 16, 128], BF16)
                lC = sbuf(ph, "lC", [128, 16, 128], BF16)
                dsk = sbuf(ph, "dsk", [128, 1])
                tabc = sbuf(ph, "tabc", [128, 4, 512], F32, 4)
                tabs = sbuf(ph, "tabs", [128, 4, 512], F32, 4)
                rhoT = sbuf(ph, "rhoT", [128, 4, 512], F32, 4)
                kis = sbuf(ph, "kis", [128, 512], I32)
                NW = 2
                wk = [[sbuf(ph, f"wk{s}_{i}", [128, 512]) for i in range(6)] for s in range(NW)]
                xrb = [[sbuf(ph, f"xb{s}_{i}", [128, 512], BF16) for i in range(2)] for s in range(NW)]
                kfs, angs = wk[0][0], wk[0][1]
                ub = sbuf(ph, "ub", [128, 2, 512], BF16, 2)
                init = sbuf(ph, "init", [128, 4, 2], F32, 4)
                ytmp = sbuf(ph, "ytmp", [128, 3, 512], F32, 3)
                yfs = sbuf(ph, "yfs", [128, 2, 512], F32, 2)
                zst = sbuf(ph, "zst", [128, 2, 512], BF16, 2)
                dma("pool", selm.t[:], selmat_d.t[:, :, :], [], [selm.b])
                for i in range(4):
                    dma("sp", hal.t[:, i, :], ag1_out[2].t[i * 128:(i + 1) * 128, :], [ag1_out[2].b], [hal.b])
                ph_ = bank()
                for i4, (selbase, col) in enumerate(((4, 128), (8, 0), (4, 384), (8, 256))):
                    for i in range(4):
                        mm(ph_, ph_.t[:, i4 * 128:(i4 + 1) * 128], selm.t[:, selbase + i, :], hal.t[:, i, col:col + 128],
                           [selm.b, hal.b], i == 0, i == 3)
                for i4, (tl_, blk) in enumerate(((kT, 0), (kT, 17), (vv, 0), (vv, 17))):
                    cp("dve", tl_.t[:, blk, :], ph_.t[:, i4 * 128:(i4 + 1) * 128], [ph_.b], [tl_.bs[blk]])
                cp("dve", uTf.t[:, 0:CTX], ucT.t[:], [ucT.b], [uTf.bs[0]])
                ut_rr = 0
                for i in range(4):
                    for tq in range(4):
                        pu = bank()
                        for t4 in range(4):
                            s = ut_rr % 2
                            ut_rr += 1
                            tr_ = (tq * 4 + t4) * 128
                            pc_, r0 = tr_ // 1024, i * 1024 + tr_ % 1024
                            dma("sp", utile.t[:, s, :], ag1_out[pc_].t[r0:r0 + 128, :], [ag1_out[pc_].b], [utile.bs[s]])
                            for sl in range(4):
                                mm(pu, pu.t[:, t4 * 128:(t4 + 1) * 128], utile.t[:, s, sl * 128:(sl + 1) * 128],
                                   selm.t[:, sl, :], [utile.bs[s], selm.b], sl == 0, sl == 3)
                        nk = i * 4 + tq
                        act(uTf.t[:, CTX + nk * 512:CTX + (nk + 1) * 512], pu.t[:, :], AF.Identity, [pu.b], [uTf.bs[1 + nk]])
                for nm, c_ in (("lamre", 0), ("lamim", 1), ("ldt", 2)):
                    dma("sp", prm.t[:, c_, :], W[nm].t[:, :], [], [prm.b])
                dma("sp", bb.t[:, 0, :, :], W["bre"].t[:, :, :], [], [bb.b])
                dma("sp", bb.t[:, 1, :, :], W["bim"].t[:, :, :], [], [bb.b])
                dma("sp", cc_.t[:, 0, :, :], W["cre"].t[:, :, :], [], [cc_.b])
                dma("sp", cc_.t[:, 1, :, :], W["cim"].t[:, :, :], [], [cc_.b])
                dma("sp", dsk.t[:], W["dsk"].t[:, :], [], [dsk.b])
                pr = lambda c_: prm.t[:, c_, :]
                PB = [prm.b]
                act(pr(3), pr(2), AF.Exp, PB, PB)
                tt("dve", pr(4), pr(0), pr(3), ALU.mult, PB, PB)
                tt("dve", pr(5), pr(1), pr(3), ALU.mult, PB, PB)
                act(pr(6), pr(4), AF.Exp, PB, PB)
                range_reduce(kfs, kfs.t[:, 0:8], kis, kis.t[:, 0:8], angs.t[:, 0:8], pr(5), 0.0, PB, [angs.b])
                act(pr(7), angs.t[:, 0:8], AF.Sin, [angs.b], PB)
                range_reduce(kfs, kfs.t[:, 0:8], kis, kis.t[:, 0:8], angs.t[:, 0:8], pr(5), PI / 2, PB, [angs.b])
                act(pr(8), angs.t[:, 0:8], AF.Sin, [angs.b], PB)
                tt("dve", pr(9), pr(6), pr(8), ALU.mult, PB, PB)
                tt("dve", pr(10), pr(6), pr(7), ALU.mult, PB, PB)
                tt("dve", pr(11), pr(0), pr(0), ALU.mult, PB, PB)
                tt("dve", pr(15), pr(1), pr(1), ALU.mult, PB, PB)
                tt("dve", pr(11), pr(11), pr(15), ALU.add, PB, PB)
                P.op("dve", lambda e: e.reciprocal(pr(11), pr(11)), PB, PB)
                ts("dve", pr(12), pr(9), -1.0, None, ALU.add, None, PB, PB)
                tt("dve", pr(13), pr(12), pr(0), ALU.mult, PB, PB)
                tt("dve", pr(15), pr(10), pr(1), ALU.mult, PB, PB)
                tt("dve", pr(13), pr(13), pr(15), ALU.add, PB, PB)
                tt("dve", pr(13), pr(13), pr(11), ALU.mult, PB, PB)
                tt("dve", pr(14), pr(10), pr(0), ALU.mult, PB, PB)
                tt("dve", pr(15), pr(12), pr(1), ALU.mult, PB, PB)
                tt("dve", pr(14), pr(14), pr(15), ALU.subtract, PB, PB)
                tt("dve", pr(14), pr(14), pr(11), ALU.mult, PB, PB)
                if l == DL:
                    dump("prm", prm, prm.t[:], lambda o: o[:, :, :])
                for r in range(8):
                    fre = prm.t[:, 13, r:r + 1]
                    fim = prm.t[:, 14, r:r + 1]
                    ts("dve", bb.t[:, 2, r, :], bb.t[:, 0, r, :], fre, None, ALU.mult, None, [bb.b, prm.b], [bb.b])
                    ts("dve", bb.t[:, 3, r, :], bb.t[:, 1, r, :], fim, None, ALU.mult, None, [bb.b, prm.b], [bb.b])
                    tt("dve", bb.t[:, 2, r, :], bb.t[:, 2, r, :], bb.t[:, 3, r, :], ALU.subtract, [bb.b], [bb.b])
                    ts("dve", bb.t[:, 3, r, :], bb.t[:, 1, r, :], fre, None, ALU.mult, None, [bb.b, prm.b], [bb.b])
                    stt("dve", bb.t[:, 3, r, :], bb.t[:, 0, r, :], fim, bb.t[:, 3, r, :], ALU.mult, ALU.add,
                        [bb.b, prm.b], [bb.b])
                    gp = r % 4
                    for ri in range(2):
                        P.op("pool", lambda e: e.memset(Wsc.t[:], 0.0), [], [Wsc.b])
                        for g2 in range(2):
                            c0_ = 16 * (2 * gp + g2)
                            cp("dve", Wsc.t[g2 * 64:(g2 + 1) * 64, c0_:c0_ + 16], bb.t[g2 * 64:(g2 + 1) * 64, 2 + ri, r, :],
                               [bb.b], [Wsc.b])
                        pb_ = bank()
                        tr(pb_, pb_.t[:, 0:128], Wsc.t[:], ident.t[:], [Wsc.b, ident.b])
                        act(lB.t[:, r * 2 + ri, :], pb_.t[:, 0:128], AF.Identity, [pb_.b], [lB.b])
                        P.op("pool", lambda e, r=r, ri=ri: e.memset(lC.t[:, r * 2 + ri, :], 0.0), [], [lC.b])
                        for g2 in range(2):
                            c0_ = 16 * (2 * gp + g2)
                            ts("dve", lC.t[g2 * 64:(g2 + 1) * 64, r * 2 + ri, c0_:c0_ + 16],
                               cc_.t[g2 * 64:(g2 + 1) * 64, ri, r, :], (1.0 if ri == 0 else -1.0), None, ALU.mult, None,
                               [cc_.b], [lC.b])
                chunks = [(0, CTX)] + [(CTX + 512 * k, 512) for k in range(16)]
                wk_rr = 0
                for d_ in range(2):
                    for gp in range(4):
                        r = d_ * 4 + gp
                        ts("dve", angs.t[:], iota.t[:], prm.t[:, 5, r:r + 1], None, ALU.mult, None, [iota.b, prm.b], [angs.b])
                        range_reduce(kfs, kfs.t[:], kis, kis.t[:], tabs.t[:, gp, :], angs.t[:], 0.0, [angs.b], [tabs.bs[gp]])
                        act(tabs.t[:, gp, :], tabs.t[:, gp, :], AF.Sin, [tabs.bs[gp]], [tabs.bs[gp]])
                        range_reduce(kfs, kfs.t[:], kis, kis.t[:], tabc.t[:, gp, :], angs.t[:], PI / 2, [angs.b], [tabc.bs[gp]])
                        act(tabc.t[:, gp, :], tabc.t[:, gp, :], AF.Sin, [tabc.bs[gp]], [tabc.bs[gp]])
                        ts("dve", rhoT.t[:, gp, :], zeros.t[:], prm.t[:, 6, r:r + 1], None, ALU.add, None, [zeros.b, prm.b],
                           [rhoT.bs[gp]])
                        P.op("pool", lambda e, gp=gp: e.memset(init.t[:, gp, :], 0.0), [], [init.bs[gp]])
                    for ci, (c0, n) in enumerate(chunks):
                        if d_ == 0:
                            nat_c0, slot = c0, ci
                            u_ap = uTf.t[:, c0:c0 + n]
                            u_reads = [uTf.bs[ci]]
                        else:
                            if ci == 0:
                                nat_c0, slot = 0, 0
                            else:
                                nkk = 16 - ci
                                nat_c0, slot = CTX + 512 * nkk, 1 + nkk
                            s_u = ci % 2
                            cp("dve", ub.t[:, s_u, 0:n], AP_rev(uTf, STREAM, nat_c0 + n - 1, n), [uTf.bs[slot]], [ub.bs[s_u]])
                            u_ap = ub.t[:, s_u, 0:n]
                            u_reads = [ub.bs[s_u]]
                        need = ctx_out or ci > 0
                        yps = bank()
                        excl = (PS.index(yps),)
                        for gp in range(4):
                            r = d_ * 4 + gp
                            ws = wk[wk_rr % NW]
                            xb_ = xrb[wk_rr % NW]
                            wk_rr += 1
                            pre = bank(exclude=excl)
                            pim = bank(exclude=excl)
                            mm(pre, pre.t[:, 0:n], lB.t[:, r * 2, :], u_ap, [lB.b] + u_reads)
                            mm(pim, pim.t[:, 0:n], lB.t[:, r * 2 + 1, :], u_ap, [lB.b] + u_reads)
                            c_ap = tabc.t[:, gp, 0:n]
                            s_ap = tabs.t[:, gp, 0:n]
                            TB = [tabc.bs[gp], tabs.bs[gp]]
                            t1, t2, t3, t4, zr, zi = [w_.t[:, 0:n] for w_ in ws]
                            b1, b2, b3, b4, bzr, bzi = [w_.b for w_ in ws]
                            tt("dve", t1, pre.t[:, 0:n], c_ap, ALU.mult, [pre.b] + TB, [b1])
                            tt("dve", t2, pim.t[:, 0:n], s_ap, ALU.mult, [pim.b] + TB, [b2])
                            tt("pool", t1, t1, t2, ALU.add, [b1, b2], [b1])
                            tt("dve", t3, pim.t[:, 0:n], c_ap, ALU.mult, [pim.b] + TB, [b3])
                            tt("dve", t4, pre.t[:, 0:n], s_ap, ALU.mult, [pre.b] + TB, [b4])
                            tt("pool", t3, t3, t4, ALU.subtract, [b3, b4], [b3])
                            P.op("dve", lambda e, zr=zr, t1=t1, gp=gp, n=n: e.tensor_tensor_scan(
                                zr, rhoT.t[:, gp, 0:n], t1, init.t[:, gp, 0:1], ALU.mult, ALU.add),
                                [rhoT.bs[gp], b1, init.bs[gp]], [bzr])
                            P.op("dve", lambda e, zi=zi, t3=t3, gp=gp, n=n: e.tensor_tensor_scan(
                                zi, rhoT.t[:, gp, 0:n], t3, init.t[:, gp, 1:2], ALU.mult, ALU.add),
                                [rhoT.bs[gp], b3, init.bs[gp]], [bzi])
                            tt("dve", t1, zr, c_ap, ALU.mult, [bzr] + TB, [b1])
                            tt("dve", t2, zi, s_ap, ALU.mult, [bzi] + TB, [b2])
                            tt("dve", t3, zr, s_ap, ALU.mult, [bzr] + TB, [b3])
                            tt("dve", t4, zi, c_ap, ALU.mult, [bzi] + TB, [b4])
                            tt("pool", xb_[0].t[:, 0:n], t1, t2, ALU.subtract, [b1, b2], [xb_[0].b])
                            tt("dve", xb_[1].t[:, 0:n], t3, t4, ALU.add, [b3, b4], [xb_[1].b])
                            tt("dve", init.t[:, gp, 0:1], ws[0].t[:, n - 1:n], ws[1].t[:, n - 1:n], ALU.subtract, [b1, b2],
                               [init.bs[gp]])
                            tt("dve", init.t[:, gp, 1:2], ws[2].t[:, n - 1:n], ws[3].t[:, n - 1:n], ALU.add, [b3, b4],
                               [init.bs[gp]])
                            if need:
                                mm(yps, yps.t[:, 0:n], lC.t[:, r * 2, :], xb_[0].t[:, 0:n], [lC.b, xb_[0].b], gp == 0, False)
                                mm(yps, yps.t[:, 0:n], lC.t[:, r * 2 + 1, :], xb_[1].t[:, 0:n], [lC.b, xb_[1].b], False, gp == 3)
                        if not need:
                            continue
                        if d_ == 0:
                            fs_ = ci % 2
                            act(yfs.t[:, fs_, 0:n], yps.t[:, 0:n], AF.Identity, [yps.b], [yfs.bs[fs_]])
                            dma("sp", yTf_d.t[:, c0:c0 + n], yfs.t[:, fs_, 0:n], [yfs.bs[fs_]], [yTf_d.b])
                        else:
                            ys = ci % 3
                            zs = ci % 2
                            ya = ytmp.t[:, ys, 0:n]
                            YB = [ytmp.bs[ys]]
                            yb2 = ytmp.t[:, (ys + 1) % 3, 0:n]
                            YB2 = [ytmp.bs[(ys + 1) % 3]]
                            fs_ = ci % 2
                            dma("sp", yfs.t[:, fs_, 0:n], yTf_d.t[:, nat_c0:nat_c0 + n], [yTf_d.b], [yfs.bs[fs_]])
                            act(ya, yps.t[:, 0:n], AF.Identity, [yps.b], YB)
                            tt("dve", yb2, AP_rev(ytmp, 3 * 512, ys * 512 + n - 1, n), yfs.t[:, fs_, 0:n], ALU.add,
                               YB + [yfs.bs[fs_]], YB2)
                            stt("dve", yb2, uTf.t[:, nat_c0:nat_c0 + n], dsk.t[:, 0:1], yb2, ALU.mult, ALU.add,
                                [uTf.bs[slot], dsk.b] + YB2, YB2)
                            if l == DL and ci in (0, 16):
                                cc0 = 0 if ci == 0 else 256
                                dump("ssmy", YB2, yb2, lambda o, cc0=cc0, n=n: o[:, cc0:cc0 + n])
                            act(ya, yb2, AF.Square, YB2, YB)
                            ts("dve", ya, ya, 0.044715, 1.0, ALU.mult, ALU.add, YB, YB)
                            tt("pool", ya, ya, yb2, ALU.mult, YB + YB2, YB)
                            act(ya, ya, AF.Sigmoid, YB, YB, scale=1.5957691216057308)
                            tt("pool", zst.t[:, zs, 0:n], ya, yb2, ALU.mult, YB + YB2, [zst.bs[zs]])
                            if ci == 0:
                                pc_, dcol = 2, 0
                            else:
                                pc_, dcol = (nat_c0 - CTX) // 4096, (nat_c0 - CTX) % 4096
                            dma("sp", ag2_in[pc_].t[:, dcol:dcol + n], zst.t[:, zs, 0:n], [zst.bs[zs]], [ag2_in[pc_].b])
                for pc_ in range(3 if ctx_out else 2):
                    P.dma("pool", lambda e, pc_=pc_: e.collective_compute(
                        "AllGather", ALU.bypass, replica_groups=GROUPS, ins=[ag2_in[pc_].t.opt()], outs=[ag2_out[pc_].t.opt()]),
                        [ag2_in[pc_].b], [ag2_out[pc_].b], inc=1)
                P.barrier()
            if dbg.get("_stop") == "C" and l == DL:
                stopped = True

            if not stopped:
              with ExitStack() as ph:
                wq = sbuf(ph, "wq", [128, 8, 1024], BF16)
                hT = sbuf(ph, "hTd", [128, 8, 512], BF16)
                qT = sbuf(ph, "qT", [128, 4, 512], BF16)
                pt = sbuf(ph, "pt", [128, 4, 512], BF16, 4)
                sg = sbuf(ph, "sg", [128, 2, 512], F32, 2)
                ta = sbuf(ph, "ta", [128, 2, 512], F32, 2)
                rec = sbuf(ph, "rec", [128, 512])
                attnT = sbuf(ph, "attnT", [128, 2, 4, 512], BF16, 2)
                cosT = sbuf(ph, "cosT", [128, TALL])
                sinT = sbuf(ph, "sinT", [128, TALL])
                masks = sbuf(ph, "masks", [128, 4, 512], BF16)
                sinkE = sbuf(ph, "sinkE", [128, 8])
                sinkrow = sbuf(ph, "sinkrow", [128, 512])
                dma("sp", cosT.t[:], cos_d.t[:, :], [cos_d.b], [cosT.b])
                dma("sp", sinT.t[:], sin_d.t[:, :], [sin_d.b], [sinT.b])
                for k4 in range(4):
                    dma("pool", masks.t[:, k4, :], masks_d.t[:, k4, :], [], [masks.b])
                dma("sp", wq.t[:, :, :], WB["w1"].t[:, :, 0:1024], [WB["w1"].b], [wq.b])
                dma("sp", sinkE.t[:], W["sink"].t[:, :], [], [sinkE.b])
                act(sinkE.t[:], sinkE.t[:], AF.Exp, [sinkE.b], [sinkE.b])
                for c in range(4):
                    ts("dve", sinkrow.t[0:64, c * 128:(c + 1) * 128], zeros.t[0:64, 0:128], sinkE.t[0:64, c:c + 1], None,
                       ALU.add, None, [zeros.b, sinkE.b], [sinkrow.b])
                    ts("dve", sinkrow.t[64:128, c * 128:(c + 1) * 128], zeros.t[64:128, 0:128], sinkE.t[64:128, 4 + c:5 + c],
                       None, ALU.add, None, [zeros.b, sinkE.b], [sinkrow.b])
                nchunks = 5 if ctx_out else 4
                pt_rr = 0
                sg_rr = 0
                for ch in range(nchunks):
                    tiles = list(range(ch * 4, min(ch * 4 + 4, NT)))
                    n = len(tiles) * 128
                    c0 = ch * 512
                    is_ctx = ch == 4
                    for ti, t_ in enumerate(tiles):
                        norm_tile(t_, A1, 0, lambda fc, ti=ti: hT.t[:, fc, ti * 128:(ti + 1) * 128], [hT.b])
                    for c in range(4):
                        pq = bank()
                        pqr = bank()
                        for kc in range(8):
                            mm(pq, pq.t[:, 0:n], wq.t[:, kc, c * 128:(c + 1) * 128], hT.t[:, kc, 0:n], [wq.b, hT.b], kc == 0, kc == 7)
                        for kc in range(8):
                            mm(pqr, pqr.t[:, 0:n], wq.t[:, kc, 512 + c * 128:512 + (c + 1) * 128], hT.t[:, kc, 0:n], [wq.b, hT.b],
                               kc == 0, kc == 7)
                        s_ = sg_rr % 2
                        sg_rr += 1
                        tt("dve", sg.t[:, s_, 0:n], pq.t[:, 0:n], cosT.t[:, c0:c0 + n], ALU.mult, [pq.b, cosT.b], [sg.bs[s_]])
                        tt("dve", ta.t[:, s_, 0:n], pqr.t[:, 0:n], sinT.t[:, c0:c0 + n], ALU.mult, [pqr.b, sinT.b], [ta.bs[s_]])
                        tt("dve", qT.t[:, c, 0:n], sg.t[:, s_, 0:n], ta.t[:, s_, 0:n], ALU.add, [sg.bs[s_], ta.bs[s_]], [qT.b])
                    as_ = ch % 2
                    for qi, t_ in enumerate(tiles):
                        pnum = bank()
                        pden = bank()
                        excl = (PS.index(pnum), PS.index(pden))
                        if is_ctx:
                            kbl = [(18, None), (19, None)]
                        else:
                            kbl = [(t_, 0 if t_ == 0 else 1), (t_ + 1, None), (t_ + 2, 2 if t_ == NT_OWN - 1 else 3),
                                   (18, None), (19, None)]
                        for gk in range(2):
                            pb = 64 * gk

                            def pv_(bi, kb, s_, pb=pb):
                                mm(pnum, pnum.t[pb:pb + 64, :], vv.t[:, kb, pb:pb + 64], pt.t[:, s_, :], [vv.bs[kb], pt.bs[s_]],
                                   bi == 0, bi == len(kbl) - 1)
                                mm(pden, pden.t[pb:pb + 64, :], onesb.t[:, 0:64], pt.t[:, s_, :], [onesb.b, pt.bs[s_]],
                                   bi == 0, bi == len(kbl) - 1)

                            pend_ = []
                            for bi, (kb, mk) in enumerate(kbl):
                                pst = bank(exclude=excl)
                                for c in range(4):
                                    mm(pst, pst.t[:, c * 128:(c + 1) * 128], kT.t[pb:pb + 64, kb, :],
                                       qT.t[pb:pb + 64, c, qi * 128:(qi + 1) * 128], [kT.bs[kb], qT.b])
                                s_ = pt_rr % 4
                                pt_rr += 1
                                act(pt.t[:, s_, :], pst.t[:, :], AF.Exp, [pst.b], [pt.bs[s_]], scale=0.125)
                                if mk is not None:
                                    tt("dve", pt.t[:, s_, :], pt.t[:, s_, :], masks.t[:, mk, :], ALU.mult, [pt.bs[s_], masks.b],
                                       [pt.bs[s_]])
                                pend_.append((bi, kb, s_))
                                if len(pend_) > 2:
                                    pv_(*pend_.pop(0))
                            for p_ in pend_:
                                pv_(*p_)
                        tt("dve", rec.t[:], pden.t[:, :], sinkrow.t[:], ALU.add, [pden.b, sinkrow.b], [rec.b])
                        P.op("dve", lambda e: e.reciprocal(rec.t[:], rec.t[:]), [rec.b], [rec.b])
                        for c in range(4):
                            tt("dve", attnT.t[:, as_, c, qi * 128:(qi + 1) * 128], pnum.t[:, c * 128:(c + 1) * 128],
                               rec.t[:, c * 128:(c + 1) * 128], ALU.mult, [pnum.b, rec.b], [attnT.bs[as_]])
                    dma("sp", attn_d.t[:, :, c0:c0 + n], attnT.t[:, as_, :, 0:n], [attnT.bs[as_]], [attn_d.b])
                P.barrier()
            kvst.close()
            if dbg.get("_stop") == "D1" and l == DL:
                stopped = True

            if not stopped:
              with ExitStack() as ph:
                wglu = sbuf(ph, "wglu", [128, 4, 512], BF16)
                wbra = sbuf(ph, "wbra", [128, 4, D], BF16)
                wbrs = sbuf(ph, "wbrs", [128, 4, D], BF16)
                wout = sbuf(ph, "wout", [128, 8, D], BF16)
                w2s = sbuf(ph, "w2s", [128, 2, 8, 256], BF16, 2)
                hT = sbuf(ph, "hTd2", [128, 8, 512], BF16)
                zstage = sbuf(ph, "zstage", [128, 4, 512], BF16)
                zsb = sbuf(ph, "zsb", [128, 4, 512], BF16)
                ssmT = sbuf(ph, "ssmT", [128, 4, 512], BF16)
                mT = sbuf(ph, "mT", [128, 8, 512], BF16)
                sg = sbuf(ph, "sg2", [128, 2, 512], F32, 2)
                ta = sbuf(ph, "ta2", [128, 2, 512], F32, 2)
                selm = sbuf(ph, "selm2", [128, 4, 128], BF16)
                gt1row = sbuf(ph, "gt1row", [128, 2, D])
                attc = sbuf(ph, "attc", [128, 4, 512], BF16)
                dma("pool", selm.t[:], selmat_d.t[:, 0:4, :], [], [selm.b])
                dma("sp", wout.t[:, :, :], WB["wout"].t[:, :, :], [WB["wout"].b], [wout.b])
                dma("sp", wglu.t[:, :, :], WB["wglu"].t[:, :, :], [WB["wglu"].b], [wglu.b])
                dma("sp", wbra.t[:, :, :], WB["wbra"].t[:, :, :], [WB["wbra"].b], [wbra.b])
                dma("sp", wbrs.t[:, :, :], WB["wbrs"].t[:, :, :], [WB["wbrs"].b], [wbrs.b])
                make_gtrow(gt1row, 2)
                nchunks = 5 if ctx_out else 4
                sg_rr = 0
                w2_rr = 0
                for ch in range(nchunks):
                    tiles = list(range(ch * 4, min(ch * 4 + 4, NT)))
                    n = len(tiles) * 128
                    c0 = ch * 512
                    is_ctx = ch == 4
                    cls = 1 if is_ctx else 0
                    for ti, t_ in enumerate(tiles):
                        norm_tile(t_, A1, 0, lambda fc, ti=ti: hT.t[:, fc, ti * 128:(ti + 1) * 128], [hT.b])
                    dma("sp", attc.t[:, :, 0:n], attn_d.t[:, :, c0:c0 + n], [attn_d.b], [attc.b])
                    for sl in range(4):
                        if is_ctx:
                            dma("sp", zsb.t[:, sl, 0:n], ag2_out[2].t[sl * 128:(sl + 1) * 128, :], [ag2_out[2].b], [zsb.b])
                        else:
                            for i in range(4):
                                gc_ = i * TOWN + c0
                                dma("sp", zstage.t[:, i, :],
                                    ag2_out[gc_ // 4096].t[sl * 128:(sl + 1) * 128, gc_ % 4096:gc_ % 4096 + 512],
                                    [ag2_out[gc_ // 4096].b], [zstage.b])
                            pz = bank()
                            for i in range(4):
                                mm(pz, pz.t[:, :], selm.t[:, i, :], zstage.t[:, i, :], [selm.b, zstage.b], i == 0, i == 3)
                            act(zsb.t[:, sl, :], pz.t[:, :], AF.Identity, [pz.b], [zsb.b])
                    for fc in range(4):
                        pg = bank()
                        for sl in range(4):
                            mm(pg, pg.t[:, 0:n], wglu.t[:, sl, fc * 128:(fc + 1) * 128], zsb.t[:, sl, 0:n], [wglu.b, zsb.b],
                               sl == 0, sl == 3)
                        s_ = sg_rr % 2
                        sg_rr += 1
                        act(sg.t[:, s_, 0:n], pg.t[:, 0:n], AF.Sigmoid, [pg.b], [sg.bs[s_]])
                        tt("dve", ssmT.t[:, fc, 0:n], zsb.t[:, fc, 0:n], sg.t[:, s_, 0:n], ALU.mult, [zsb.b, sg.bs[s_]], [ssmT.b])
                    if l == DL and ch == 0 and "ssmT" in dbg_out:
                        sdb = sbuf(ph, "sdb", [128, 4, 512])
                        cp("dve", sdb.t[:], ssmT.t[:], [ssmT.b], [sdb.b])
                        dump("ssmT", sdb, sdb.t[:], lambda o: o[:, :, :])
                    for fc in range(8):
                        ws_ = w2_rr % 2
                        w2_rr += 1
                        dma("sp", w2s.t[:, ws_, :, :], WB["w2g"].t[fc, :, :, :], [WB["w2g"].b], [w2s.bs[ws_]])
                        pA = bank()
                        pS = bank()
                        pga = bank()
                        pgs = bank()
                        fs = slice(fc * 128, (fc + 1) * 128)
                        for c in range(4):
                            mm(pA, pA.t[:, 0:n], wbra.t[:, c, fs], attc.t[:, c, 0:n], [wbra.b, attc.b], c == 0, c == 3)
                        for c in range(4):
                            mm(pS, pS.t[:, 0:n], wbrs.t[:, c, fs], ssmT.t[:, c, 0:n], [wbrs.b, ssmT.b], c == 0, c == 3)
                        for kc in range(8):
                            mm(pga, pga.t[:, 0:n], w2s.t[:, ws_, kc, 0:128], hT.t[:, kc, 0:n], [w2s.bs[ws_], hT.b], kc == 0, kc == 7)
                        for kc in range(8):
                            mm(pgs, pgs.t[:, 0:n], w2s.t[:, ws_, kc, 128:256], hT.t[:, kc, 0:n], [w2s.bs[ws_], hT.b], kc == 0, kc == 7)
                        act(sg.t[:, 0, 0:n], pga.t[:, 0:n], AF.Sigmoid, [pga.b], [sg.bs[0]])
                        act(sg.t[:, 1, 0:n], pgs.t[:, 0:n], AF.Sigmoid, [pgs.b], [sg.bs[1]])
                        tt("dve", ta.t[:, 0, 0:n], pA.t[:, 0:n], sg.t[:, 0, 0:n], ALU.mult, [pA.b, sg.bs[0]], [ta.bs[0]])
                        tt("dve", ta.t[:, 1, 0:n], pS.t[:, 0:n], sg.t[:, 1, 0:n], ALU.mult, [pS.b, sg.bs[1]], [ta.bs[1]])
                        tt("dve", mT.t[:, fc, 0:n], ta.t[:, 0, 0:n], ta.t[:, 1, 0:n], ALU.add, [ta.bs[0], ta.bs[1]], [mT.b])
                    for ti, t_ in enumerate(tiles):
                        for half in range(2):
                            py = bank()
                            hs_ = slice(half * 512, (half + 1) * 512)
                            for kc in range(8):
                                mm(py, py.t[:, :], mT.t[:, kc, ti * 128:(ti + 1) * 128], wout.t[:, kc, hs_], [mT.b, wout.b],
                                   kc == 0, kc == 7)
                            s_ = sg_rr % 2
                            sg_rr += 1
                            tt("dve", sg.t[:, s_, :], py.t[:, :], gt1row.t[:, cls, hs_], ALU.mult, [py.b, gt1row.b], [sg.bs[s_]])
                            tt("dve", x_tm.t[:, t_, hs_], x_tm.t[:, t_, hs_], sg.t[:, s_, :], ALU.add, [sg.bs[s_], x_tm.bs[t_]],
                               [x_tm.bs[t_]])
                P.barrier()
            if l == DL and not stopped:
                dump("xmix", x_tm.bs, x_tm.t[:], lambda o: o[:, :, :])
            if dbg.get("_stop") == "D2" and l == DL:
                stopped = True
            if stopped:
                break

            if not stopped:
              with ExitStack() as ph:
                ntl = NT if ctx_out else NT_OWN
                h2T = sbuf(ph, "h2T", [128, 8, TALL], BF16, NT)
                h2f = sbuf(ph, "h2f", [128, 8, 128])
                wr = sbuf(ph, "wr", [128, 8, 36])
                Wt = sbuf(ph, "Wt", [128, NT, 32], F32, NT)
                lg = sbuf(ph, "lg", [128, 36])
                rs = sbuf(ph, "rs", [128, 16])
                rt = sbuf(ph, "rt", [128, 4, 32])
                weg = sbuf(ph, "weg", [128, 2, 8, 512], BF16, 2)
                weu = sbuf(ph, "weu", [128, 2, 8, 512], BF16, 2)
                wed = sbuf(ph, "wed", [128, 2, 4, D], BF16, 2)
                hid = sbuf(ph, "hid", [128, 2, 4, 512], BF16, 2)
                sgm = sbuf(ph, "sgm", [128, 2, 512], F32, 2)
                ty = sbuf(ph, "ty", [128, 2, 512], F32, 2)
                gt2row = sbuf(ph, "gt2row", [128, 2, D])
                dma("sp", wr.t[:], W["wr"].t[:, :, :], [], [wr.b])
                make_gtrow(gt2row, 5)
                compute_rstd(ntl)
                RB = [rs.b]
                c_ = lambda i: rs.t[:, i:i + 1]
                def route_tile(t_):
                    norm_tile(t_, A2, 3, lambda fc, t_=t_: h2T.t[:, fc, t_ * 128:(t_ + 1) * 128], [h2T.bs[t_]], f32_dst=h2f)
                    pl = bank()
                    for kc in range(8):
                        mm(pl, pl.t[:, 0:36], h2f.t[:, kc, :], wr.t[:, kc, :], [h2f.b, wr.b], kc == 0, kc == 7)
                    cp("dve", lg.t[:], pl.t[:, 0:36], [pl.b], [lg.b])
                    P.op("dve", lambda e: e.tensor_reduce(rs.t[:, 0:1], lg.t[:, 0:4], AX.X, ALU.max), [lg.b], RB)
                    ts("dve", c_(1), c_(0), -1.0, None, ALU.mult, None, RB, RB)
                    act(rt.t[:, 0, 0:4], lg.t[:, 0:4], AF.Exp, [lg.b] + RB, [rt.b, rs.b], bias=rs.t[:, 1:2], accum=rs.t[:, 2:3])
                    P.op("dve", lambda e: e.reciprocal(rs.t[:, 3:4], rs.t[:, 2:3]), RB, RB)
                    ts("dve", rt.t[:, 0, 4:8], lg.t[:, 0:4], rs.t[:, 0:1], None, ALU.is_equal, None, [lg.b] + RB, [rt.b])
                    ts("dve", rt.t[:, 0, 4:8], rt.t[:, 0, 4:8], -1.0, 1e30, ALU.add, ALU.mult, [rt.b], [rt.b])
                    for g in range(4):
                        ts("dve", rt.t[:, 1, 8 * g:8 * g + 8], lg.t[:, 4 + 8 * g:12 + 8 * g], rt.t[:, 0, 4 + g:5 + g], None,
                           ALU.add, None, [lg.b, rt.b], [rt.b])
                    P.op("dve", lambda e: e.tensor_reduce(rs.t[:, 4:5], rt.t[:, 1, :], AX.X, ALU.max), [rt.b], RB)
                    ts("dve", rt.t[:, 2, :], rt.t[:, 1, :], rs.t[:, 4:5], None, ALU.is_equal, None, [rt.b] + RB, [rt.b])
                    stt("dve", rt.t[:, 2, :], rt.t[:, 2, :], -1e30, rt.t[:, 1, :], ALU.mult, ALU.add, [rt.b], [rt.b])
                    P.op("dve", lambda e: e.tensor_reduce(rs.t[:, 5:6], rt.t[:, 2, :], AX.X, ALU.max), [rt.b], RB)
                    ts("dve", rt.t[:, 2, :], rt.t[:, 1, :], rs.t[:, 5:6], None, ALU.is_ge, None, [rt.b] + RB, [rt.b])
                    ts("dve", c_(6), c_(4), -1.0, None, ALU.mult, None, RB, RB)
                    act(rt.t[:, 3, :], rt.t[:, 1, :], AF.Exp, [rt.b] + RB, [rt.b], bias=rs.t[:, 6:7])
                    tt("dve", rt.t[:, 3, :], rt.t[:, 3, :], rt.t[:, 2, :], ALU.mult, [rt.b], [rt.b])
                    P.op("dve", lambda e: e.tensor_reduce(rs.t[:, 7:8], rt.t[:, 3, :], AX.X, ALU.add), [rt.b], RB)
                    P.op("dve", lambda e: e.reciprocal(rs.t[:, 7:8], rs.t[:, 7:8]), RB, RB)
                    tt("dve", c_(7), c_(7), c_(3), ALU.mult, RB, RB)
                    ts("dve", Wt.t[:, t_, :], rt.t[:, 3, :], rs.t[:, 7:8], None, ALU.mult, None, [rt.b] + RB, [Wt.bs[t_]])

                nch = 5 if ctx_out else 4
                ty_rr = 0
                nexp = dbg.get("_nexp", NEXP)
                for e_ in range(nexp):
                    s = e_ % 2
                    for k2 in range(2):
                        dma("pool", weg.t[:, s, 4 * k2:4 * k2 + 4, :], W["weg"].t[e_, :, 4 * k2:4 * k2 + 4, :], [W["weg"].b], [weg.bs[s]])
                        dma("pool", weu.t[:, s, 4 * k2:4 * k2 + 4, :], W["weu"].t[e_, :, 4 * k2:4 * k2 + 4, :], [W["weu"].b], [weu.bs[s]])
                        dma("pool", wed.t[:, s, 2 * k2:2 * k2 + 2, :], W["wed"].t[e_, :, 2 * k2:2 * k2 + 2, :], [W["wed"].b], [wed.bs[s]])
                    for ch in range(nch):
                        tiles = list(range(ch * 4, min(ch * 4 + 4, NT)))
                        n = len(tiles) * 128
                        c0 = ch * 512
                        cls = 1 if ch == 4 else 0
                        hs = ch % 2
                        if e_ == 0:
                            for t_ in tiles:
                                route_tile(t_)
                        for hc in range(4):
                            pg = bank()
                            pu = bank()
                            hrd = [h2T.bs[t_] for t_ in tiles]
                            for kc in range(8):
                                mm(pg, pg.t[:, 0:n], weg.t[:, s, kc, hc * 128:(hc + 1) * 128], h2T.t[:, kc, c0:c0 + n],
                                   [weg.bs[s]] + hrd, kc == 0, kc == 7)
                            for kc in range(8):
                                mm(pu, pu.t[:, 0:n], weu.t[:, s, kc, hc * 128:(hc + 1) * 128], h2T.t[:, kc, c0:c0 + n],
                                   [weu.bs[s]] + hrd, kc == 0, kc == 7)
                            ss_ = hc % 2
                            act(sgm.t[:, ss_, 0:n], pg.t[:, 0:n], AF.Silu, [pg.b], [sgm.bs[ss_]])
                            tt("dve", hid.t[:, hs, hc, 0:n], pu.t[:, 0:n], sgm.t[:, ss_, 0:n], ALU.mult, [pu.b, sgm.bs[ss_]],
                               [hid.bs[hs]])
                        for ti, t_ in enumerate(tiles):
                            for half in range(2):
                                py = bank()
                                hs_ = slice(half * 512, (half + 1) * 512)
                                for hc in range(4):
                                    mm(py, py.t[:, :], hid.t[:, hs, hc, ti * 128:(ti + 1) * 128], wed.t[:, s, hc, hs_],
                                       [hid.bs[hs], wed.bs[s]], hc == 0, hc == 3)
                                y_ = ty_rr % 2
                                ty_rr += 1
                                tt("dve", ty.t[:, y_, :], py.t[:, :], gt2row.t[:, cls, hs_], ALU.mult, [py.b, gt2row.b],
                                   [ty.bs[y_]])
                                stt("dve", x_tm.t[:, t_, hs_], ty.t[:, y_, :], Wt.t[:, t_, e_:e_ + 1], x_tm.t[:, t_, hs_],
                                    ALU.mult, ALU.add, [ty.bs[y_], Wt.bs[t_], x_tm.bs[t_]], [x_tm.bs[t_]])
                P.barrier()
            if l == DL and not stopped:
                dump("xout", x_tm.bs, x_tm.t[:], lambda o: o[:, :, :])

        with ExitStack() as ph:
            gfin = sbuf(ph, "gfin", [128, D])
            ob = sbuf(ph, "ob", [128, 2, D], F32, 2)
            dma("sp", gfin.t[:], gfin_d.t[:, :], [], [gfin.b])
            compute_rstd(NT_OWN)
            for t_ in range(NT_OWN):
                s = t_ % 2
                stt("dve", ob.t[:, s, :], x_tm.t[:, t_, :], rstd.t[:, t_:t_ + 1], gfin.t[:], ALU.mult, ALU.mult,
                    [x_tm.bs[t_], rstd.b, gfin.b], [ob.bs[s]])
                dma("sp", out_d.t[t_ * 128:(t_ + 1) * 128, :], ob.t[:, s, :], [ob.bs[s]], [out_d.b])
        P.wait_all("sp", [out_d.b] + [v.b for v in dbg_out.values()])
        P.emit()
    return nc, names_in


def _kc(w):
    K, C = w.shape
    return np.ascontiguousarray(w.reshape(K // 128, 128, C).transpose(1, 0, 2))


def prep_inputs(inputs, nlayers=2, nei=NEXP):
    f = lambda a: np.ascontiguousarray(np.asarray(a, dtype=np.float32))
    I_ = {k: f(v) for k, v in inputs.items()}
    shared = {}
    e = np.arange(64)
    partner = np.where((e % 32) < 16, e + 16, e - 16)
    head_order = [h for c in range(4) for h in (c, c + 4)]
    qcols = np.concatenate([h * 64 + np.arange(64) for h in head_order])
    qrcols = np.concatenate([h * 64 + partner for h in head_order])
    kcols = 512 + np.arange(128)
    krcols = 512 + np.concatenate([hk * 64 + partner for hk in range(2)])
    vcols = 640 + np.arange(128)
    ucols = 768 + np.arange(512)
    w1cols = np.concatenate([qcols, qrcols, kcols, krcols, vcols, ucols])
    brarows = np.concatenate([h * 64 + np.arange(64) for h in head_order])
    shared["ident"] = np.eye(128, dtype=np.float32)
    shared["iota"] = np.ascontiguousarray(np.broadcast_to(np.arange(1, 513, dtype=np.float32), (128, 512)))
    shared["gfin"] = np.ascontiguousarray(np.broadcast_to(I_["g_final"], (128, D)))
    ee = np.arange(128) % 64
    ropef = np.stack([(ee % 16).astype(np.float32), np.where((ee % 32) < 16, -1.0, 1.0).astype(np.float32)], 1)
    shared["ropef"] = np.ascontiguousarray(ropef)
    for l in range(nlayers):
        win = I_["w_in"][l]
        shared[f"wmod{l}"] = _kc(I_["w_mod"][l])
        shared[f"bmodT{l}"] = np.ascontiguousarray(I_["b_mod"][l].reshape(48, 128).T)
        shared[f"g1T{l}"] = np.ascontiguousarray(I_["g_norm1"][l].reshape(8, 128).T)
        shared[f"g2T{l}"] = np.ascontiguousarray(I_["g_norm2"][l].reshape(8, 128).T)
        shared[f"w1_{l}"] = _kc(win[:, w1cols])
        ga = _kc(win[:, 1280:2304]).reshape(128, 8, 8, 128)
        gs = _kc(win[:, 2304:3328]).reshape(128, 8, 8, 128)
        shared[f"w2g{l}"] = np.ascontiguousarray(np.concatenate([ga, gs], axis=3).transpose(2, 0, 1, 3))
        shared[f"wglu{l}"] = _kc(I_["w_glu"][l])
        shared[f"wbra{l}"] = _kc(I_["w_br_attn"][l][brarows, :])
        shared[f"wbrs{l}"] = _kc(I_["w_br_ssm"][l])
        shared[f"wout{l}"] = _kc(I_["w_out"][l])
        shared[f"wr{l}"] = _kc(np.concatenate([I_["w_router_group"][l], I_["w_router_expert"][l]], axis=1))
        shared[f"weg{l}"] = np.ascontiguousarray(I_["w_exp_gate"][l][:nei].reshape(nei, 8, 128, 512).transpose(0, 2, 1, 3))
        shared[f"weu{l}"] = np.ascontiguousarray(I_["w_exp_up"][l][:nei].reshape(nei, 8, 128, 512).transpose(0, 2, 1, 3))
        shared[f"wed{l}"] = np.ascontiguousarray(I_["w_exp_down"][l][:nei].reshape(nei, 4, 128, D).transpose(0, 2, 1, 3))
        shared[f"sink{l}"] = np.ascontiguousarray(np.broadcast_to(I_["attn_sink"][l], (128, 8)))
    kk = np.arange(128)[:, None]
    qq = np.arange(128)[None, :]
    mprev = np.tile((kk >= qq).astype(np.float32), (1, 4))
    mnext = np.tile((kk <= qq).astype(np.float32), (1, 4))
    per_core = []
    for r in range(NCORES):
        b, j = r // 4, r % 4
        m = dict(shared)
        t0 = j * TOWN
        m["x_own"] = np.ascontiguousarray(I_["x"][b, t0:t0 + TOWN])
        m["ctx_b"] = np.ascontiguousarray(I_["ctx"][b])
        cT = np.stack([I_["c"][b].reshape(8, 128).T, I_["c_ctx"].reshape(8, 128).T], axis=2)
        m["cT"] = np.ascontiguousarray(cT)
        tpos = np.arange(t0, t0 + TOWN)
        rows = (tpos // 64).astype(np.float32)
        cols = (tpos % 64).astype(np.float32)
        pos = np.zeros((128, TALL), np.float32)
        is_row = (ee % 64) < 32
        pos[:, :TOWN] = np.where(is_row[:, None], rows[None, :], cols[None, :])
        m["pos"] = pos
        mk = np.zeros((128, 4, 512), np.float32)
        mk[:, 0] = mprev if j > 0 else 0.0
        mk[:, 1] = mprev
        mk[:, 2] = mnext if j < 3 else 0.0
        mk[:, 3] = mnext
        m["masks"] = mk
        sel = np.zeros((128, 12, 128), np.float32)
        eye = np.eye(128, dtype=np.float32)
        sel[:, j] = eye
        if j > 0:
            sel[:, 4 + j - 1] = eye
        if j < 3:
            sel[:, 8 + j + 1] = eye
        m["selmat"] = sel
        for l in range(nlayers):
            win = I_["w_in"][l]
            m[f"wus{l}"] = _kc(win[:, 768 + 128 * j:768 + 128 * (j + 1)])
            g0 = 8 * j

            def rowlay(a):
                a = a.reshape((2, 4, 2, 64) + a.shape[3:])
                perm = (2, 3, 0, 1) + tuple(range(4, a.ndim))
                a = a.transpose(perm)
                return np.ascontiguousarray(a.reshape((128, 8) + a.shape[4:]))

            m[f"lamre{l}"] = rowlay(I_["ssm_lam_re"][l][:, g0:g0 + 8])
            m[f"lamim{l}"] = rowlay(I_["ssm_lam_im"][l][:, g0:g0 + 8])
            ldt = np.broadcast_to(I_["ssm_log_dt"][l][:, g0:g0 + 8, None], (2, 8, 64))
            m[f"ldt{l}"] = rowlay(np.ascontiguousarray(ldt))
            m[f"bre{l}"] = rowlay(I_["ssm_b_re"][l][:, g0:g0 + 8])
            m[f"bim{l}"] = rowlay(I_["ssm_b_im"][l][:, g0:g0 + 8])
            m[f"cre{l}"] = rowlay(np.ascontiguousarray(I_["ssm_c_re"][l][:, g0:g0 + 8].transpose(0, 1, 3, 2)))
            m[f"cim{l}"] = rowlay(np.ascontiguousarray(I_["ssm_c_im"][l][:, g0:g0 + 8].transpose(0, 1, 3, 2)))
            m[f"dsk{l}"] = np.ascontiguousarray(I_["ssm_d"][l][128 * j:128 * (j + 1)].reshape(128, 1))
        for k in list(m.keys()):
            if False:
                a = m[k]
                a2 = a.reshape(-1, a.shape[-1])
                rr = a2.shape[0] // NCORES
                m[k] = np.ascontiguousarray(a2[r * rr:(r + 1) * rr])
        per_core.append(m)
    return per_core


_CACHE = {}


def kernel(**inputs):
    if "nc" not in _CACHE:
        _CACHE["nc"] = build_program(2)
    nc, names = _CACHE["nc"]
    per_core = prep_inputs(inputs, 2)
    in_maps = [{k: m[k] for k in names} for m in per_core]
    res = run_bass_kernel_spmd(nc, in_maps, core_ids=list(range(NCORES)))
    out = np.zeros((2, SEQ, D), np.float32)
    for r in range(NCORES):
        b, j = r // 4, r % 4
        out[b, j * TOWN:(j + 1) * TOWN] = res.results[r]["out"]
    return out
```

```python
from contextlib import ExitStack
import numpy as np
import concourse.bass as bass
import concourse.mybir as mybir
from concourse.bass_utils import run_bass_kernel_spmd

dt = mybir.dt
ALU = mybir.AluOpType
AF = mybir.ActivationFunctionType
AX = mybir.AxisListType
F32 = dt.float32
BF16 = dt.bfloat16
I32 = dt.int32

NCORES = 8
D = 1024
TOWN = 2048
NT_OWN = 16
NT = 18
TALL = 2304
SEQ = 8192
CTX = 256
STREAM = SEQ + CTX
NEXP = 32
PI = float(np.pi)
GROUPS = [[0, 1, 2, 3], [4, 5, 6, 7]]


class Buf:
    __slots__ = ("name", "last_w", "readers")

    def __init__(self, name=""):
        self.name = name
        self.last_w = None
        self.readers = {}


class Prog:
    ENG = ("pe", "act", "dve", "pool", "sp")

    def __init__(self, nc, stack, ndma=None):
        ndma = ndma or {"sp": 16, "act": 2, "pool": 12, "cc": 3}
        self.nc = nc
        self.q = {e: [] for e in self.ENG}
        self.sems = {}
        self.cnt = {e: 0 for e in self.ENG}
        self.waited = {e: {} for e in self.ENG}
        for e in self.ENG:
            self.sems[("c", e)] = stack.enter_context(nc.semaphore("c_" + e))
        self.dma_slots = {}
        self.dma_tot = {}
        self.dma_rr = {}
        for e, n in ndma.items():
            keys = []
            for i in range(n):
                k = ("d", e, i)
                self.sems[k] = stack.enter_context(nc.semaphore("d_%s%d" % (e, i)))
                self.dma_tot[k] = 0
                keys.append(k)
            self.dma_slots[e] = keys
            self.dma_rr[e] = 0

    def _wait(self, eng, k, v):
        if k == ("c", "pe") and eng == "pe":
            return
        if self.waited[eng].get(k, 0) >= v:
            return
        self.waited[eng][k] = v
        self.q[eng].append(("w", k, v))

    def _deps(self, eng, reads, writes):
        deps = {}

        def add(ev):
            if ev is None:
                return
            k, v = ev
            if deps.get(k, 0) < v:
                deps[k] = v

        for b in reads:
            add(b.last_w)
        for b in writes:
            add(b.last_w)
            for k, v in b.readers.items():
                add((k, v))
        for k, v in deps.items():
            self._wait(eng, k, v)

    def _commit(self, ev, reads, writes):
        k, v = ev
        for b in reads:
            if b.readers.get(k, 0) < v:
                b.readers[k] = v
        for b in writes:
            b.last_w = ev
            b.readers = {}

    def op(self, eng, fn, reads=(), writes=()):
        self._deps(eng, reads, writes)
        self.cnt[eng] += 1
        ev = (("c", eng), self.cnt[eng])
        self.q[eng].append(("o", fn, ("c", eng), 1))
        self._commit(ev, reads, writes)
        return ev

    def dma(self, qeng, fn, reads=(), writes=(), inc=16):
        skey = "cc" if inc == 1 else qeng
        slots = self.dma_slots[skey]
        k = slots[self.dma_rr[skey] % len(slots)]
        self.dma_rr[skey] += 1
        prev = self.dma_tot[k]
        if prev > 0:
            self._wait(qeng, k, prev)
        self._deps(qeng, reads, writes)
        self.dma_tot[k] = prev + inc
        ev = (k, prev + inc)
        self.q[qeng].append(("o", fn, k, inc))
        self._commit(ev, reads, writes)
        return ev

    def wait_all(self, eng, bufs):
        self._deps(eng, bufs, ())

    def barrier(self):
        tot = {("c", e): self.cnt[e] for e in self.ENG}
        tot.update(self.dma_tot)
        for e in self.ENG:
            for k, v in tot.items():
                if v > 0:
                    self._wait(e, k, v)

    def emit(self):
        nc = self.nc
        sems = self.sems

        def replay(e, name):
            for it in self.q[name]:
                if it[0] == "w":
                    e.wait_ge(sems[it[1]], it[2])
                else:
                    ins = it[1](e)
                    ins.then_inc(sems[it[2]], it[3])

        with nc.Block() as block:

            @block.tensor
            def _(e):
                replay(e, "pe")

            @block.vector
            def _(e):
                replay(e, "dve")

            @block.scalar
            def _(e):
                replay(e, "act")

            @block.gpsimd
            def _(e):
                replay(e, "pool")

            @block.sync
            def _(e):
                replay(e, "sp")


class TL:
    def __init__(self, t, nslots=1):
        self.t = t
        self.bs = [Buf() for _ in range(nslots)]

    @property
    def b(self):
        return self.bs[0]


def build_program(nlayers=2, dbg=None):
    dbg = dbg or {}
    NEI = dbg.get("_nexp", NEXP)
    nc = bass.Bass("TRN2", target_bir_lowering=False)
    names_in = []

    def inp(name, shape, d=F32):
        names_in.append(name)
        return TL(nc.dram_tensor(name, list(shape), d, kind="ExternalInput").ap())

    gathers = []

    def ginp(name, shape):
        return inp(name, shape)

    def ginp_unused(name, shape):
        R = int(np.prod(shape[:-1]))
        C = int(shape[-1])
        assert R % NCORES == 0
        names_in.append(name)
        shard = TL(nc.dram_tensor(name, [R // NCORES, C], F32, kind="ExternalInput").ap())
        bounce = TL(nc.dram_tensor(name + "_bnc", [R // NCORES, C], F32).ap())
        full = TL(nc.dram_tensor(name + "_full", list(shape), F32).ap())
        gathers.append((shard, bounce, full))
        return full

    x_own = inp("x_own", [TOWN, D])
    ctx_b = inp("ctx_b", [CTX, D])
    cT = inp("cT", [128, 8, 2])
    pos_d = inp("pos", [128, TALL])
    ropef_d = inp("ropef", [128, 2])
    masks_d = inp("masks", [128, 4, 512])
    selmat_d = inp("selmat", [128, 12, 128])
    ident_d = inp("ident", [128, 128])
    iota_d = inp("iota", [128, 512])
    gfin_d = inp("gfin", [128, D])
    L = []
    for l in range(nlayers):
        w = {}
        w["wmod"] = ginp(f"wmod{l}", [128, 8, 6 * D])
        w["bmodT"] = inp(f"bmodT{l}", [128, 48])
        w["g1T"] = inp(f"g1T{l}", [128, 8])
        w["g2T"] = inp(f"g2T{l}", [128, 8])
        w["w1"] = ginp(f"w1_{l}", [128, 8, 1920])
        w["wus"] = inp(f"wus{l}", [128, 8, 128])
        w["w2g"] = ginp(f"w2g{l}", [8, 128, 8, 256])
        w["wglu"] = inp(f"wglu{l}", [128, 4, 512])
        w["wbra"] = ginp(f"wbra{l}", [128, 4, D])
        w["wbrs"] = ginp(f"wbrs{l}", [128, 4, D])
        w["wout"] = ginp(f"wout{l}", [128, 8, D])
        w["wr"] = inp(f"wr{l}", [128, 8, 36])
        w["weg"] = ginp(f"weg{l}", [NEI, 128, 8, 512])
        w["weu"] = ginp(f"weu{l}", [NEI, 128, 8, 512])
        w["wed"] = ginp(f"wed{l}", [NEI, 128, 4, D])
        w["sink"] = inp(f"sink{l}", [128, 8])
        w["lamre"] = inp(f"lamre{l}", [128, 8])
        w["lamim"] = inp(f"lamim{l}", [128, 8])
        w["ldt"] = inp(f"ldt{l}", [128, 8])
        w["bre"] = inp(f"bre{l}", [128, 8, 16])
        w["bim"] = inp(f"bim{l}", [128, 8, 16])
        w["cre"] = inp(f"cre{l}", [128, 8, 16])
        w["cim"] = inp(f"cim{l}", [128, 8, 16])
        w["dsk"] = inp(f"dsk{l}", [128, 1])
        L.append(w)
    out_d = TL(nc.dram_tensor("out", [TOWN, D], F32, kind="ExternalOutput").ap())
    dbg_out = {}
    for k, shp in dbg.items():
        if k.startswith("_"):
            continue
        dbg_out[k] = TL(nc.dram_tensor("dbg_" + k, list(shp), F32, kind="ExternalOutput").ap())
    AGR = TOWN + 128
    ag1_in = [TL(nc.dram_tensor(f"ag1_in{i}", [r_, 512], BF16).ap()) for i, r_ in enumerate((1024, 1024, 128))]
    ag1_out = [TL(nc.dram_tensor(f"ag1_out{i}", [4 * r_, 512], BF16).ap()) for i, r_ in enumerate((1024, 1024, 128))]
    ag2_in = [TL(nc.dram_tensor(f"ag2_in{i}", [128, c_], BF16).ap()) for i, c_ in enumerate((4096, 4096, CTX))]
    ag2_out = [TL(nc.dram_tensor(f"ag2_out{i}", [512, c_], BF16).ap()) for i, c_ in enumerate((4096, 4096, CTX))]
    cos_d = TL(nc.dram_tensor("cos_d", [128, TALL], F32).ap())
    sin_d = TL(nc.dram_tensor("sin_d", [128, TALL], F32).ap())
    yTf_d = TL(nc.dram_tensor("yTf_d", [128, STREAM], F32).ap())
    attn_d = TL(nc.dram_tensor("attn_d", [128, 4, TALL], BF16).ap())

    with ExitStack() as st:
        P = Prog(nc, st)

        uid = [0]

        def sbuf(stack, name, shape, d=F32, nslots=1):
            uid[0] += 1
            return TL(stack.enter_context(nc.sbuf_tensor("sb%d_%s" % (uid[0], name), list(shape), d)), nslots)

        PS = [TL(st.enter_context(nc.psum_tensor(f"ps{i}", [128, 512], F32))) for i in range(8)]
        ps_rr = [0]

        def bank(exclude=()):
            while True:
                i = ps_rr[0] % 8
                ps_rr[0] += 1
                if i not in exclude:
                    return PS[i]

        def mm(out_tl, out_ap, lhsT_ap, rhs_ap, reads, start=True, stop=True):
            P.op("pe", lambda e: e.matmul(out_ap, lhsT_ap, rhs_ap, start=start, stop=stop), reads, [out_tl.b])

        def tr(out_tl, out_ap, in_ap, ident_ap, reads):
            P.op("pe", lambda e: e.transpose(out_ap, in_ap, ident_ap), reads, [out_tl.b])

        def act(out_ap, in_ap, func, reads, writes, bias=None, scale=None, accum=None):
            kw = {}
            if bias is not None:
                kw["bias"] = bias
            if scale is not None:
                kw["scale"] = scale
            if accum is not None:
                kw["accum_out"] = accum
            P.op("act", lambda e: e.activation(out_ap, in_ap, func, **kw), reads, writes)

        def tt(eng, out_ap, a_ap, b_ap, op, reads, writes):
            P.op(eng, lambda e: e.tensor_tensor(out_ap, a_ap, b_ap, op), reads, writes)

        def ts(eng, out_ap, a_ap, s1, s2, op0, op1, reads, writes):
            if op1 is None:
                P.op(eng, lambda e: e.tensor_scalar(out_ap, a_ap, s1, None, op0), reads, writes)
            else:
                P.op(eng, lambda e: e.tensor_scalar(out_ap, a_ap, s1, s2, op0, op1), reads, writes)

        def stt(eng, out_ap, a_ap, s, b_ap, op0, op1, reads, writes):
            P.op(eng, lambda e: e.scalar_tensor_tensor(out_ap, a_ap, s, b_ap, op0, op1), reads, writes)

        def cp(eng, out_ap, in_ap, reads, writes):
            P.op(eng, lambda e: e.tensor_copy(out_ap, in_ap), reads, writes)

        def dma(q, out_ap, in_ap, reads, writes, **kw):
            P.dma(q, lambda e: e.dma_start(out=out_ap, in_=in_ap, **kw), reads, writes)

        def dump(key, src_tl, src_ap, dst_ap_fn):
            if key in dbg_out:
                dma("sp", dst_ap_fn(dbg_out[key].t), src_ap, [src_tl.b] if isinstance(src_tl, TL) else src_tl,
                    [dbg_out[key].b])

        for shard, bounce, full in gathers:
            nr_ = shard.t.shape[0]
            for r0_ in range(0, nr_, 1024):
                r1_ = min(nr_, r0_ + 1024)
                dma("sp", bounce.t[r0_:r1_, :], shard.t[r0_:r1_, :], [], [bounce.b])
            P.dma("pool", lambda e, bounce=bounce, full=full: e.collective_compute(
                "AllGather", ALU.bypass, replica_groups=[list(range(NCORES))], ins=[bounce.t.opt()], outs=[full.t.opt()]),
                [bounce.b], [full.b], inc=1)

        WBF = []
        for l in range(nlayers):
            wb = {}
            for nm in ("w1", "w2g", "wout", "wbra", "wbrs", "wglu"):
                src = L[l][nm]
                dst = TL(nc.dram_tensor(f"{nm}bf{l}", list(src.t.shape), BF16).ap())
                for i0 in range(src.t.shape[0] if nm == "w2g" else src.t.shape[1]):
                    if nm == "w2g":
                        dma("pool", dst.t[i0, :, :, :], src.t[i0, :, :, :], [src.b], [dst.b])
                    else:
                        dma("pool", dst.t[:, i0, :], src.t[:, i0, :], [src.b], [dst.b])
                wb[nm] = dst
            WBF.append(wb)

        x_tm = sbuf(st, "x_tm", [128, NT, D], F32, NT)
        ident = sbuf(st, "ident", [128, 128])
        zeros = sbuf(st, "zeros", [128, 512])
        iota = sbuf(st, "iota", [128, 512])
        onesb = sbuf(st, "onesb", [128, 64], BF16)
        onesf = sbuf(st, "onesf", [128, 128])
        siluT = sbuf(st, "siluT", [128, 8, 2])
        modT = sbuf(st, "modT", [128, 48, 2])
        bmodT = sbuf(st, "bmodT", [128, 48])
        gT = sbuf(st, "gT", [128, 16])
        A1 = sbuf(st, "A1", [128, 2, 8])
        A2 = sbuf(st, "A2", [128, 2, 8])
        rstd = sbuf(st, "rstd", [128, NT])
        sstat = sbuf(st, "sstat", [128, NT])
        xs = sbuf(st, "xs", [128, D])
        sm = sbuf(st, "sm", [128, 8])
        ucT = sbuf(st, "ucT", [128, CTX], BF16)
        dg = sbuf(st, "dg", [128, 128])

        def AP_rev(tl, row_elems, col_last, n):
            return bass.AP(tl.t, col_last, [[row_elems, 128], [-1, n]])

        def range_reduce(kf_tl, kf_ap, ki_tl, ki_ap, out_ap, in_ap, shift, reads, writes):
            ts("dve", kf_ap, in_ap, shift, 1.0 / (2 * PI), ALU.add, ALU.mult, reads, [kf_tl.b])
            cp("dve", ki_ap, kf_ap, [kf_tl.b], [ki_tl.b])
            cp("dve", kf_ap, ki_ap, [ki_tl.b], [kf_tl.b])
            stt("dve", kf_ap, kf_ap, -2 * PI, in_ap, ALU.mult, ALU.add, [kf_tl.b] + list(reads), [kf_tl.b])
            ts("dve", out_ap, kf_ap, shift, None, ALU.add, None, [kf_tl.b], writes)
            ts("dve", out_ap, out_ap, 3.14159, -3.14159, ALU.min, ALU.max, writes, writes)

        with ExitStack() as ph:
            posT = sbuf(ph, "posT", [128, TALL])
            ang = sbuf(ph, "ang", [128, TALL])
            ki = sbuf(ph, "ki", [128, TALL], I32)
            kf = sbuf(ph, "kf", [128, TALL])
            tb = sbuf(ph, "tb", [128, TALL])
            ropef = sbuf(ph, "ropef", [128, 2])
            dma("sp", x_tm.t[:, 0:NT_OWN, :], x_own.t.rearrange("(t p) d -> p t d", p=128), [], x_tm.bs[0:NT_OWN])
            dma("sp", x_tm.t[:, NT_OWN:NT, :], ctx_b.t.rearrange("(t p) d -> p t d", p=128), [], x_tm.bs[NT_OWN:NT])
            dma("sp", ident.t[:], ident_d.t[:, :], [], [ident.b])
            dma("sp", posT.t[:], pos_d.t[:, :], [], [posT.b])
            dma("sp", ropef.t[:], ropef_d.t[:, :], [], [ropef.b])
            dma("sp", iota.t[:], iota_d.t[:, :], [], [iota.b])
            dma("sp", siluT.t[:], cT.t[:, :, :], [], [siluT.b])
            P.op("pool", lambda e: e.memset(zeros.t[:], 0.0), [], [zeros.b])
            P.op("pool", lambda e: e.memset(onesb.t[:], 1.0), [], [onesb.b])
            P.op("pool", lambda e: e.memset(onesf.t[:], 1.0), [], [onesf.b])
            act(siluT.t[:], siluT.t[:], AF.Silu, [siluT.b], [siluT.b])
            act(sm.t[:, 0:1], ropef.t[:, 0:1], AF.Exp, [ropef.b], [sm.b], scale=-float(np.log(10000.0)) / 16.0)
            ts("dve", ang.t[:], posT.t[:], sm.t[:, 0:1], None, ALU.mult, None, [posT.b, sm.b], [ang.b])
            range_reduce(kf, kf.t[:], ki, ki.t[:], tb.t[:], ang.t[:], 0.0, [ang.b], [tb.b])
            act(tb.t[:], tb.t[:], AF.Sin, [tb.b], [tb.b])
            ts("dve", tb.t[:], tb.t[:], ropef.t[:, 1:2], None, ALU.mult, None, [tb.b, ropef.b], [tb.b])
            dma("sp", sin_d.t[:, :], tb.t[:], [tb.b], [sin_d.b])
            range_reduce(kf, kf.t[:], ki, ki.t[:], tb.t[:], ang.t[:], PI / 2, [ang.b], [tb.b])
            act(tb.t[:], tb.t[:], AF.Sin, [tb.b], [tb.b])
            dma("sp", cos_d.t[:, :], tb.t[:], [tb.b], [cos_d.b])
            P.barrier()

        def tile_cls(t_):
            return 0 if t_ < NT_OWN else 1

        def compute_rstd(ntiles):
            for t_ in range(ntiles):
                act(xs.t[:], x_tm.t[:, t_, :], AF.Square, [x_tm.bs[t_]], [xs.b, sstat.b], accum=sstat.t[:, t_:t_ + 1])
            ts("dve", rstd.t[:, 0:ntiles], sstat.t[:, 0:ntiles], 1.0 / D, 1e-6, ALU.mult, ALU.add, [sstat.b], [rstd.b])
            act(rstd.t[:, 0:ntiles], rstd.t[:, 0:ntiles], AF.Sqrt, [rstd.b], [rstd.b])
            P.op("dve", lambda e: e.reciprocal(rstd.t[:, 0:ntiles], rstd.t[:, 0:ntiles]), [rstd.b], [rstd.b])

        def norm_tile(t_, Acls, shslot, dst_ap_fn, dst_bufs, f32_dst=None):
            cls = tile_cls(t_)
            act(xs.t[:], x_tm.t[:, t_, :], AF.Identity, [x_tm.bs[t_], rstd.b], [xs.b], scale=rstd.t[:, t_:t_ + 1])
            for half in range(2):
                pb_ = bank()
                for q4 in range(4):
                    fc = half * 4 + q4
                    tr(pb_, pb_.t[:, q4 * 128:(q4 + 1) * 128], xs.t[:, fc * 128:(fc + 1) * 128], ident.t[:], [xs.b, ident.b])
                for q4 in range(4):
                    fc = half * 4 + q4
                    act(dst_ap_fn(fc), pb_.t[:, q4 * 128:(q4 + 1) * 128], AF.Identity, [pb_.b, Acls.b, modT.b], dst_bufs,
                        scale=Acls.t[:, cls, fc:fc + 1], bias=modT.t[:, shslot * 8 + fc, cls:cls + 1])
                    if f32_dst is not None:
                        act(f32_dst.t[:, fc, :], pb_.t[:, q4 * 128:(q4 + 1) * 128], AF.Identity, [pb_.b, Acls.b, modT.b],
                            [f32_dst.b], scale=Acls.t[:, cls, fc:fc + 1], bias=modT.t[:, shslot * 8 + fc, cls:cls + 1])

        def make_gtrow(dst, slot):
            for cls in range(2):
                for fc in range(8):
                    ts("dve", dg.t[:], ident.t[:], modT.t[:, slot * 8 + fc, cls:cls + 1], None, ALU.mult, None,
                       [ident.b, modT.b], [dg.b])
                    pb_ = bank()
                    mm(pb_, pb_.t[:, 0:128], onesf.t[:], dg.t[:], [onesf.b, dg.b])
                    cp("dve", dst.t[:, cls, fc * 128:(fc + 1) * 128], pb_.t[:, 0:128], [pb_.b], [dst.b])

        def blk_of(t_):
            return t_ + 1 if t_ < NT_OWN else 18 + (t_ - NT_OWN)

        DL = dbg.get("_layer", 0)
        stopped = False
        for l in range(nlayers):
            if dbg.get("_stop") == "0":
                break
            W = L[l]
            WB = WBF[l]
            ctx_out = l < nlayers - 1
            with ExitStack() as ph:
                wm = sbuf(ph, "wm", [128, 2, 8, 512], F32, 2)
                dma("sp", bmodT.t[:], W["bmodT"].t[:, :], [], [bmodT.b])
                dma("sp", gT.t[:, 0:8], W["g1T"].t[:, :], [], [gT.b])
                dma("sp", gT.t[:, 8:16], W["g2T"].t[:, :], [], [gT.b])
                mps = bank()
                for piece in range(12):
                    s = piece % 2
                    dma("sp", wm.t[:, s, :, :], W["wmod"].t[:, :, piece * 512:(piece + 1) * 512], [W["wmod"].b], [wm.bs[s]])
                    for c4 in range(4):
                        cc = piece * 4 + c4
                        for kc in range(8):
                            mm(mps, mps.t[:, cc * 2:cc * 2 + 2], wm.t[:, s, kc, c4 * 128:(c4 + 1) * 128],
                               siluT.t[:, kc, :], [wm.bs[s], siluT.b], kc == 0, kc == 7)
                for cls in range(2):
                    tt("dve", modT.t[:, :, cls], mps.t[:, cls:96:2], bmodT.t[:], ALU.add, [mps.b, bmodT.b], [modT.b])
                for cls in range(2):
                    stt("dve", A1.t[:, cls, :], modT.t[:, 8:16, cls], 1.0, gT.t[:, 0:8], ALU.add, ALU.mult,
                        [modT.b, gT.b], [A1.b])
                    stt("dve", A2.t[:, cls, :], modT.t[:, 32:40, cls], 1.0, gT.t[:, 8:16], ALU.add, ALU.mult,
                        [modT.b, gT.b], [A2.b])
                if l == DL:
                    dump("modT", modT, modT.t[:], lambda o: o[:, :, :])
                P.barrier()

            kvst = ExitStack()
            kT = sbuf(kvst, "kT", [128, 20, 128], BF16, 20)
            vv = sbuf(kvst, "vv", [128, 20, 128], BF16, 20)
            with ExitStack() as ph:
                w1 = sbuf(ph, "w1", [128, 8, 1920], BF16)
                wus = sbuf(ph, "wus", [128, 8, 128], BF16)
                hT = sbuf(ph, "hT", [128, 8, 512], BF16)
                ust = sbuf(ph, "ust", [128, 2, 512], BF16, 2)
                tmpa = sbuf(ph, "tmpa", [128, 512])
                tmpb = sbuf(ph, "tmpb", [128, 512])
                cosT = sbuf(ph, "cosT", [128, TALL])
                sinT = sbuf(ph, "sinT", [128, TALL])
                dma("sp", cosT.t[:], cos_d.t[:, :], [cos_d.b], [cosT.b])
                dma("sp", sinT.t[:], sin_d.t[:, :], [sin_d.b], [sinT.b])
                dma("sp", w1.t[:, :, :], WB["w1"].t[:, :, :], [WB["w1"].b], [w1.b])
                dma("pool", wus.t[:], W["wus"].t[:, :, :], [], [wus.b])
                compute_rstd(NT)
                ust_rr = 0
                for ch in range(5):
                    tiles = list(range(ch * 4, min(ch * 4 + 4, NT)))
                    n = len(tiles) * 128
                    c0 = ch * 512
                    for ti, t_ in enumerate(tiles):
                        norm_tile(t_, A1, 0, lambda fc, ti=ti: hT.t[:, fc, ti * 128:(ti + 1) * 128], [hT.b])
                    pk = bank()
                    pkr = bank()
                    for kc in range(8):
                        mm(pk, pk.t[:, 0:n], w1.t[:, kc, 1024:1152], hT.t[:, kc, 0:n], [w1.b, hT.b], kc == 0, kc == 7)
                    for kc in range(8):
                        mm(pkr, pkr.t[:, 0:n], w1.t[:, kc, 1152:1280], hT.t[:, kc, 0:n], [w1.b, hT.b], kc == 0, kc == 7)
                    tt("dve", tmpa.t[:, 0:n], pk.t[:, 0:n], cosT.t[:, c0:c0 + n], ALU.mult, [pk.b, cosT.b], [tmpa.b])
                    tt("dve", tmpb.t[:, 0:n], pkr.t[:, 0:n], sinT.t[:, c0:c0 + n], ALU.mult, [pkr.b, sinT.b], [tmpb.b])
                    for ti, t_ in enumerate(tiles):
                        blk = blk_of(t_)
                        tt("dve", kT.t[:, blk, :], tmpa.t[:, ti * 128:(ti + 1) * 128], tmpb.t[:, ti * 128:(ti + 1) * 128],
                           ALU.add, [tmpa.b, tmpb.b], [kT.bs[blk]])
                    pv = bank()
                    for ti, t_ in enumerate(tiles):
                        for kc in range(8):
                            mm(pv, pv.t[:, ti * 128:(ti + 1) * 128], hT.t[:, kc, ti * 128:(ti + 1) * 128],
                               w1.t[:, kc, 1280:1408], [w1.b, hT.b], kc == 0, kc == 7)
                    for ti, t_ in enumerate(tiles):
                        blk = blk_of(t_)
                        act(vv.t[:, blk, :], pv.t[:, ti * 128:(ti + 1) * 128], AF.Identity, [pv.b], [vv.bs[blk]])
                    if ch < 4:
                        for ti, t_ in enumerate(tiles):
                            pu = bank()
                            for kc in range(8):
                                mm(pu, pu.t[:, :], hT.t[:, kc, ti * 128:(ti + 1) * 128], w1.t[:, kc, 1408:1920],
                                   [w1.b, hT.b], kc == 0, kc == 7)
                            s = ust_rr % 2
                            ust_rr += 1
                            act(ust.t[:, s, :], pu.t[:, :], AF.Identity, [pu.b], [ust.bs[s]])
                            pc_, lr_ = (t_ * 128) // 1024, (t_ * 128) % 1024
                            dma("sp", ag1_in[pc_].t[lr_:lr_ + 128, :], ust.t[:, s, :], [ust.bs[s]], [ag1_in[pc_].b])
                    else:
                        pu = bank()
                        for kc in range(8):
                            mm(pu, pu.t[:, 0:CTX], wus.t[:, kc, :], hT.t[:, kc, 0:CTX], [wus.b, hT.b], kc == 0, kc == 7)
                        act(ucT.t[:], pu.t[:, 0:CTX], AF.Identity, [pu.b], [ucT.b])
                for i4, (tl_, blk) in enumerate(((kT, 1), (kT, 16), (vv, 1), (vv, 16))):
                    dma("sp", ag1_in[2].t[:, i4 * 128:(i4 + 1) * 128], tl_.t[:, blk, :], [tl_.bs[blk]], [ag1_in[2].b])
                for pc_ in range(3):
                    P.dma("pool", lambda e, pc_=pc_: e.collective_compute(
                        "AllGather", ALU.bypass, replica_groups=GROUPS, ins=[ag1_in[pc_].t.opt()], outs=[ag1_out[pc_].t.opt()]),
                        [ag1_in[pc_].b], [ag1_out[pc_].b], inc=1)
                P.barrier()
            if dbg.get("_stop") == "B" and l == DL:
                stopped = True

            if not stopped:
              with ExitStack() as ph:
                uTf = sbuf(ph, "uTf", [128, STREAM], BF16, 17)
                hal = sbuf(ph, "hal", [128, 4, 512], BF16)
                utile = sbuf(ph, "utile", [128, 2, 512], BF16, 2)
                selm = sbuf(ph, "selm", [128, 12, 128], BF16)
                prm = sbuf(ph, "prm", [128, 16, 8])
                bb = sbuf(ph, "bb", [128, 4, 8, 16])
                cc_ = sbuf(ph, "cc", [128, 2, 8, 16])
                Wsc = sbuf(ph, "Wsc", [128, 128])
                lB = sbuf(ph, "lB", [128, 16, 128], BF16)
                lC = sbuf(ph, "lC", [128, 16, 128], BF16)
                dsk = sbuf(ph, "dsk", [128, 1])
                tabc = sbuf(ph, "tabc", [128, 4, 512], F32, 4)
                tabs = sbuf(ph, "tabs", [128, 4, 512], F32, 4)
                rhoT = sbuf(ph, "rhoT", [128, 4, 512], F32, 4)
                kis = sbuf(ph, "kis", [128, 512], I32)
                NW = 2
                wk = [[sbuf(ph, f"wk{s}_{i}", [128, 512]) for i in range(6)] for s in range(NW)]
                xrb = [[sbuf(ph, f"xb{s}_{i}", [128, 512], BF16) for i in range(2)] for s in range(NW)]
                kfs, angs = wk[0][0], wk[0][1]
                ub = sbuf(ph, "ub", [128, 2, 512], BF16, 2)
                init = sbuf(ph, "init", [128, 4, 2], F32, 4)
                ytmp = sbuf(ph, "ytmp", [128, 3, 512], F32, 3)
                yfs = sbuf(ph, "yfs", [128, 2, 512], F32, 2)
                zst = sbuf(ph, "zst", [128, 2, 512], BF16, 2)
                dma("pool", selm.t[:], selmat_d.t[:, :, :], [], [selm.b])
                for i in range(4):
                    dma("sp", hal.t[:, i, :], ag1_out[2].t[i * 128:(i + 1) * 128, :], [ag1_out[2].b], [hal.b])
                ph_ = bank()
                for i4, (selbase, col) in enumerate(((4, 128), (8, 0), (4, 384), (8, 256))):
                    for i in range(4):
                        mm(ph_, ph_.t[:, i4 * 128:(i4 + 1) * 128], selm.t[:, selbase + i, :], hal.t[:, i, col:col + 128],
                           [selm.b, hal.b], i == 0, i == 3)
                for i4, (tl_, blk) in enumerate(((kT, 0), (kT, 17), (vv, 0), (vv, 17))):
                    cp("dve", tl_.t[:, blk, :], ph_.t[:, i4 * 128:(i4 + 1) * 128], [ph_.b], [tl_.bs[blk]])
                cp("dve", uTf.t[:, 0:CTX], ucT.t[:], [ucT.b], [uTf.bs[0]])
                ut_rr = 0
                for i in range(4):
                    for tq in range(4):
                        pu = bank()
                        for t4 in range(4):
                            s = ut_rr % 2
                            ut_rr += 1
                            tr_ = (tq * 4 + t4) * 128
                            pc_, r0 = tr_ // 1024, i * 1024 + tr_ % 1024
                            dma("sp", utile.t[:, s, :], ag1_out[pc_].t[r0:r0 + 128, :], [ag1_out[pc_].b], [utile.bs[s]])
                            for sl in range(4):
                                mm(pu, pu.t[:, t4 * 128:(t4 + 1) * 128], utile.t[:, s, sl * 128:(sl + 1) * 128],
                                   selm.t[:, sl, :], [utile.bs[s], selm.b], sl == 0, sl == 3)
                        nk = i * 4 + tq
                        act(uTf.t[:, CTX + nk * 512:CTX + (nk + 1) * 512], pu.t[:, :], AF.Identity, [pu.b], [uTf.bs[1 + nk]])
                for nm, c_ in (("lamre", 0), ("lamim", 1), ("ldt", 2)):
                    dma("sp", prm.t[:, c_, :], W[nm].t[:, :], [], [prm.b])
                dma("sp", bb.t[:, 0, :, :], W["bre"].t[:, :, :], [], [bb.b])
                dma("sp", bb.t[:, 1, :, :], W["bim"].t[:, :, :], [], [bb.b])
                dma("sp", cc_.t[:, 0, :, :], W["cre"].t[:, :, :], [], [cc_.b])
                dma("sp", cc_.t[:, 1, :, :], W["cim"].t[:, :, :], [], [cc_.b])
                dma("sp", dsk.t[:], W["dsk"].t[:, :], [], [dsk.b])
                pr = lambda c_: prm.t[:, c_, :]
                PB = [prm.b]
                act(pr(3), pr(2), AF.Exp, PB, PB)
                tt("dve", pr(4), pr(0), pr(3), ALU.mult, PB, PB)
                tt("dve", pr(5), pr(1), pr(3), ALU.mult, PB, PB)
                act(pr(6), pr(4), AF.Exp, PB, PB)
                range_reduce(kfs, kfs.t[:, 0:8], kis, kis.t[:, 0:8], angs.t[:, 0:8], pr(5), 0.0, PB, [angs.b])
                act(pr(7), angs.t[:, 0:8], AF.Sin, [angs.b], PB)
                range_reduce(kfs, kfs.t[:, 0:8], kis, kis.t[:, 0:8], angs.t[:, 0:8], pr(5), PI / 2, PB, [angs.b])
                act(pr(8), angs.t[:, 0:8], AF.Sin, [angs.b], PB)
                tt("dve", pr(9), pr(6), pr(8), ALU.mult, PB, PB)
                tt("dve", pr(10), pr(6), pr(7), ALU.mult, PB, PB)
                tt("dve", pr(11), pr(0), pr(0), ALU.mult, PB, PB)
                tt("dve", pr(15), pr(1), pr(1), ALU.mult, PB, PB)
                tt("dve", pr(11), pr(11), pr(15), ALU.add, PB, PB)
                P.op("dve", lambda e: e.reciprocal(pr(11), pr(11)), PB, PB)
                ts("dve", pr(12), pr(9), -1.0, None, ALU.add, None, PB, PB)
                tt("dve", pr(13), pr(12), pr(0), ALU.mult, PB, PB)
                tt("dve", pr(15), pr(10), pr(1), ALU.mult, PB, PB)
                tt("dve", pr(13), pr(13), pr(15), ALU.add, PB, PB)
                tt("dve", pr(13), pr(13), pr(11), ALU.mult, PB, PB)
                tt("dve", pr(14), pr(10), pr(0), ALU.mult, PB, PB)
                tt("dve", pr(15), pr(12), pr(1), ALU.mult, PB, PB)
                tt("dve", pr(14), pr(14), pr(15), ALU.subtract, PB, PB)
                tt("dve", pr(14), pr(14), pr(11), ALU.mult, PB, PB)
                if l == DL:
                    dump("prm", prm, prm.t[:], lambda o: o[:, :, :])
                for r in range(8):
                    fre = prm.t[:, 13, r:r + 1]
                    fim = prm.t[:, 14, r:r + 1]
                    ts("dve", bb.t[:, 2, r, :], bb.t[:, 0, r, :], fre, None, ALU.mult, None, [bb.b, prm.b], [bb.b])
                    ts("dve", bb.t[:, 3, r, :], bb.t[:, 1, r, :], fim, None, ALU.mult, None, [bb.b, prm.b], [bb.b])
                    tt("dve", bb.t[:, 2, r, :], bb.t[:, 2, r, :], bb.t[:, 3, r, :], ALU.subtract, [bb.b], [bb.b])
                    ts("dve", bb.t[:, 3, r, :], bb.t[:, 1, r, :], fre, None, ALU.mult, None, [bb.b, prm.b], [bb.b])
                    stt("dve", bb.t[:, 3, r, :], bb.t[:, 0, r, :], fim, bb.t[:, 3, r, :], ALU.mult, ALU.add,
                        [bb.b, prm.b], [bb.b])
                    gp = r % 4
                    for ri in range(2):
                        P.op("pool", lambda e: e.memset(Wsc.t[:], 0.0), [], [Wsc.b])
                        for g2 in range(2):
                            c0_ = 16 * (2 * gp + g2)
                            cp("dve", Wsc.t[g2 * 64:(g2 + 1) * 64, c0_:c0_ + 16], bb.t[g2 * 64:(g2 + 1) * 64, 2 + ri, r, :],
                               [bb.b], [Wsc.b])
                        pb_ = bank()
                        tr(pb_, pb_.t[:, 0:128], Wsc.t[:], ident.t[:], [Wsc.b, ident.b])
                        act(lB.t[:, r * 2 + ri, :], pb_.t[:, 0:128], AF.Identity, [pb_.b], [lB.b])
                        P.op("pool", lambda e, r=r, ri=ri: e.memset(lC.t[:, r * 2 + ri, :], 0.0), [], [lC.b])
                        for g2 in range(2):
                            c0_ = 16 * (2 * gp + g2)
                            ts("dve", lC.t[g2 * 64:(g2 + 1) * 64, r * 2 + ri, c0_:c0_ + 16],
                               cc_.t[g2 * 64:(g2 + 1) * 64, ri, r, :], (1.0 if ri == 0 else -1.0), None, ALU.mult, None,
                               [cc_.b], [lC.b])
                chunks = [(0, CTX)] + [(CTX + 512 * k, 512) for k in range(16)]
                wk_rr = 0
                for d_ in range(2):
                    for gp in range(4):
                        r = d_ * 4 + gp
                        ts("dve", angs.t[:], iota.t[:], prm.t[:, 5, r:r + 1], None, ALU.mult, None, [iota.b, prm.b], [angs.b])
                        range_reduce(kfs, kfs.t[:], kis, kis.t[:], tabs.t[:, gp, :], angs.t[:], 0.0, [angs.b], [tabs.bs[gp]])
                        act(tabs.t[:, gp, :], tabs.t[:, gp, :], AF.Sin, [tabs.bs[gp]], [tabs.bs[gp]])
                        range_reduce(kfs, kfs.t[:], kis, kis.t[:], tabc.t[:, gp, :], angs.t[:], PI / 2, [angs.b], [tabc.bs[gp]])
                        act(tabc.t[:, gp, :], tabc.t[:, gp, :], AF.Sin, [tabc.bs[gp]], [tabc.bs[gp]])
                        ts("dve", rhoT.t[:, gp, :], zeros.t[:], prm.t[:, 6, r:r + 1], None, ALU.add, None, [zeros.b, prm.b],
                           [rhoT.bs[gp]])
                        P.op("pool", lambda e, gp=gp: e.memset(init.t[:, gp, :], 0.0), [], [init.bs[gp]])
                    for ci, (c0, n) in enumerate(chunks):
                        if d_ == 0:
                            nat_c0, slot = c0, ci
                            u_ap = uTf.t[:, c0:c0 + n]
                            u_reads = [uTf.bs[ci]]
                        else:
                            if ci == 0:
                                nat_c0, slot = 0, 0
                            else:
                                nkk = 16 - ci
                                nat_c0, slot = CTX + 512 * nkk, 1 + nkk
                            s_u = ci % 2
                            cp("dve", ub.t[:, s_u, 0:n], AP_rev(uTf, STREAM, nat_c0 + n - 1, n), [uTf.bs[slot]], [ub.bs[s_u]])
                            u_ap = ub.t[:, s_u, 0:n]
                            u_reads = [ub.bs[s_u]]
                        need = ctx_out or ci > 0
                        yps = bank()
                        excl = (PS.index(yps),)
                        for gp in range(4):
                            r = d_ * 4 + gp
                            ws = wk[wk_rr % NW]
                            xb_ = xrb[wk_rr % NW]
                            wk_rr += 1
                            pre = bank(exclude=excl)
                            pim = bank(exclude=excl)
                            mm(pre, pre.t[:, 0:n], lB.t[:, r * 2, :], u_ap, [lB.b] + u_reads)
                            mm(pim, pim.t[:, 0:n], lB.t[:, r * 2 + 1, :], u_ap, [lB.b] + u_reads)
                            c_ap = tabc.t[:, gp, 0:n]
                            s_ap = tabs.t[:, gp, 0:n]
                            TB = [tabc.bs[gp], tabs.bs[gp]]
                            t1, t2, t3, t4, zr, zi = [w_.t[:, 0:n] for w_ in ws]
                            b1, b2, b3, b4, bzr, bzi = [w_.b for w_ in ws]
                            tt("dve", t1, pre.t[:, 0:n], c_ap, ALU.mult, [pre.b] + TB, [b1])
                            tt("dve", t2, pim.t[:, 0:n], s_ap, ALU.mult, [pim.b] + TB, [b2])
                            tt("pool", t1, t1, t2, ALU.add, [b1, b2], [b1])
                            tt("dve", t3, pim.t[:, 0:n], c_ap, ALU.mult, [pim.b] + TB, [b3])
                            tt("dve", t4, pre.t[:, 0:n], s_ap, ALU.mult, [pre.b] + TB, [b4])
                            tt("pool", t3, t3, t4, ALU.subtract, [b3, b4], [b3])
                            P.op("dve", lambda e, zr=zr, t1=t1, gp=gp, n=n: e.tensor_tensor_scan(
                                zr, rhoT.t[:, gp, 0:n], t1, init.t[:, gp, 0:1], ALU.mult, ALU.add),
                                [rhoT.bs[gp], b1, init.bs[gp]], [bzr])
                            P.op("dve", lambda e, zi=zi, t3=t3, gp=gp, n=n: e.tensor_tensor_scan(
                                zi, rhoT.t[:, gp, 0:n], t3, init.t[:, gp, 1:2], ALU.mult, ALU.add),
                                [rhoT.bs[gp], b3, init.bs[gp]], [bzi])
                            tt("dve", t1, zr, c_ap, ALU.mult, [bzr] + TB, [b1])
                            tt("dve", t2, zi, s_ap, ALU.mult, [bzi] + TB, [b2])
                            tt("dve", t3, zr, s_ap, ALU.mult, [bzr] + TB, [b3])
                            tt("dve", t4, zi, c_ap, ALU.mult, [bzi] + TB, [b4])
                            tt("pool", xb_[0].t[:, 0:n], t1, t2, ALU.subtract, [b1, b2], [xb_[0].b])
                            tt("dve", xb_[1].t[:, 0:n], t3, t4, ALU.add, [b3, b4], [xb_[1].b])
                            tt("dve", init.t[:, gp, 0:1], ws[0].t[:, n - 1:n], ws[1].t[:, n - 1:n], ALU.subtract, [b1, b2],
                               [init.bs[gp]])
                            tt("dve", init.t[:, gp, 1:2], ws[2].t[:, n - 1:n], ws[3].t[:, n - 1:n], ALU.add, [b3, b4],
                               [init.bs[gp]])
                            if need:
                                mm(yps, yps.t[:, 0:n], lC.t[:, r * 2, :], xb_[0].t[:, 0:n], [lC.b, xb_[0].b], gp == 0, False)
                                mm(yps, yps.t[:, 0:n], lC.t[:, r * 2 + 1, :], xb_[1].t[:, 0:n], [lC.b, xb_[1].b], False, gp == 3)
                        if not need:
                            continue
                        if d_ == 0:
                            fs_ = ci % 2
                            act(yfs.t[:, fs_, 0:n], yps.t[:, 0:n], AF.Identity, [yps.b], [yfs.bs[fs_]])
                            dma("sp", yTf_d.t[:, c0:c0 + n], yfs.t[:, fs_, 0:n], [yfs.bs[fs_]], [yTf_d.b])
                        else:
                            ys = ci % 3
                            zs = ci % 2
                            ya = ytmp.t[:, ys, 0:n]
                            YB = [ytmp.bs[ys]]
                            yb2 = ytmp.t[:, (ys + 1) % 3, 0:n]
                            YB2 = [ytmp.bs[(ys + 1) % 3]]
                            fs_ = ci % 2
                            dma("sp", yfs.t[:, fs_, 0:n], yTf_d.t[:, nat_c0:nat_c0 + n], [yTf_d.b], [yfs.bs[fs_]])
                            act(ya, yps.t[:, 0:n], AF.Identity, [yps.b], YB)
                            tt("dve", yb2, AP_rev(ytmp, 3 * 512, ys * 512 + n - 1, n), yfs.t[:, fs_, 0:n], ALU.add,
                               YB + [yfs.bs[fs_]], YB2)
                            stt("dve", yb2, uTf.t[:, nat_c0:nat_c0 + n], dsk.t[:, 0:1], yb2, ALU.mult, ALU.add,
                                [uTf.bs[slot], dsk.b] + YB2, YB2)
                            if l == DL and ci in (0, 16):
                                cc0 = 0 if ci == 0 else 256
                                dump("ssmy", YB2, yb2, lambda o, cc0=cc0, n=n: o[:, cc0:cc0 + n])
                            act(ya, yb2, AF.Square, YB2, YB)
                            ts("dve", ya, ya, 0.044715, 1.0, ALU.mult, ALU.add, YB, YB)
                            tt("pool", ya, ya, yb2, ALU.mult, YB + YB2, YB)
                            act(ya, ya, AF.Sigmoid, YB, YB, scale=1.5957691216057308)
                            tt("pool", zst.t[:, zs, 0:n], ya, yb2, ALU.mult, YB + YB2, [zst.bs[zs]])
                            if ci == 0:
                                pc_, dcol = 2, 0
                            else:
                                pc_, dcol = (nat_c0 - CTX) // 4096, (nat_c0 - CTX) % 4096
                            dma("sp", ag2_in[pc_].t[:, dcol:dcol + n], zst.t[:, zs, 0:n], [zst.bs[zs]], [ag2_in[pc_].b])
                for pc_ in range(3 if ctx_out else 2):
                    P.dma("pool", lambda e, pc_=pc_: e.collective_compute(
                        "AllGather", ALU.bypass, replica_groups=GROUPS, ins=[ag2_in[pc_].t.opt()], outs=[ag2_out[pc_].t.opt()]),
                        [ag2_in[pc_].b], [ag2_out[pc_].b], inc=1)
                P.barrier()
            if dbg.get("_stop") == "C" and l == DL:
                stopped = True

            if not stopped:
              with ExitStack() as ph:
                wq = sbuf(ph, "wq", [128, 8, 1024], BF16)
                hT = sbuf(ph, "hTd", [128, 8, 512], BF16)
                qT = sbuf(ph, "qT", [128, 4, 512], BF16)
                pt = sbuf(ph, "pt", [128, 4, 512], BF16, 4)
                sg = sbuf(ph, "sg", [128, 2, 512], F32, 2)
                ta = sbuf(ph, "ta", [128, 2, 512], F32, 2)
                rec = sbuf(ph, "rec", [128, 512])
                attnT = sbuf(ph, "attnT", [128, 2, 4, 512], BF16, 2)
                cosT = sbuf(ph, "cosT", [128, TALL])
                sinT = sbuf(ph, "sinT", [128, TALL])
                masks = sbuf(ph, "masks", [128, 4, 512], BF16)
                sinkE = sbuf(ph, "sinkE", [128, 8])
                sinkrow = sbuf(ph, "sinkrow", [128, 512])
                dma("sp", cosT.t[:], cos_d.t[:, :], [cos_d.b], [cosT.b])
                dma("sp", sinT.t[:], sin_d.t[:, :], [sin_d.b], [sinT.b])
                for k4 in range(4):
                    dma("pool", masks.t[:, k4, :], masks_d.t[:, k4, :], [], [masks.b])
                dma("sp", wq.t[:, :, :], WB["w1"].t[:, :, 0:1024], [WB["w1"].b], [wq.b])
                dma("sp", sinkE.t[:], W["sink"].t[:, :], [], [sinkE.b])
                act(sinkE.t[:], sinkE.t[:], AF.Exp, [sinkE.b], [sinkE.b])
                for c in range(4):
                    ts("dve", sinkrow.t[0:64, c * 128:(c + 1) * 128], zeros.t[0:64, 0:128], sinkE.t[0:64, c:c + 1], None,
                       ALU.add, None, [zeros.b, sinkE.b], [sinkrow.b])
                    ts("dve", sinkrow.t[64:128, c * 128:(c + 1) * 128], zeros.t[64:128, 0:128], sinkE.t[64:128, 4 + c:5 + c],
                       None, ALU.add, None, [zeros.b, sinkE.b], [sinkrow.b])
                nchunks = 5 if ctx_out else 4
                pt_rr = 0
                sg_rr = 0
                for ch in range(nchunks):
                    tiles = list(range(ch * 4, min(ch * 4 + 4, NT)))
                    n = len(tiles) * 128
                    c0 = ch * 512
                    is_ctx = ch == 4
                    for ti, t_ in enumerate(tiles):
                        norm_tile(t_, A1, 0, lambda fc, ti=ti: hT.t[:, fc, ti * 128:(ti + 1) * 128], [hT.b])
                    for c in range(4):
                        pq = bank()
                        pqr = bank()
                        for kc in range(8):
                            mm(pq, pq.t[:, 0:n], wq.t[:, kc, c * 128:(c + 1) * 128], hT.t[:, kc, 0:n], [wq.b, hT.b], kc == 0, kc == 7)
                        for kc in range(8):
                            mm(pqr, pqr.t[:, 0:n], wq.t[:, kc, 512 + c * 128:512 + (c + 1) * 128], hT.t[:, kc, 0:n], [wq.b, hT.b],
                               kc == 0, kc == 7)
                        s_ = sg_rr % 2
                        sg_rr += 1
                        tt("dve", sg.t[:, s_, 0:n], pq.t[:, 0:n], cosT.t[:, c0:c0 + n], ALU.mult, [pq.b, cosT.b], [sg.bs[s_]])
                        tt("dve", ta.t[:, s_, 0:n], pqr.t[:, 0:n], sinT.t[:, c0:c0 + n], ALU.mult, [pqr.b, sinT.b], [ta.bs[s_]])
                        tt("dve", qT.t[:, c, 0:n], sg.t[:, s_, 0:n], ta.t[:, s_, 0:n], ALU.add, [sg.bs[s_], ta.bs[s_]], [qT.b])
                    as_ = ch % 2
                    for qi, t_ in enumerate(tiles):
                        pnum = bank()
                        pden = bank()
                        excl = (PS.index(pnum), PS.index(pden))
                        if is_ctx:
                            kbl = [(18, None), (19, None)]
                        else:
                            kbl = [(t_, 0 if t_ == 0 else 1), (t_ + 1, None), (t_ + 2, 2 if t_ == NT_OWN - 1 else 3),
                                   (18, None), (19, None)]
                        for gk in range(2):
                            pb = 64 * gk

                            def pv_(bi, kb, s_, pb=pb):
                                mm(pnum, pnum.t[pb:pb + 64, :], vv.t[:, kb, pb:pb + 64], pt.t[:, s_, :], [vv.bs[kb], pt.bs[s_]],
                                   bi == 0, bi == len(kbl) - 1)
                                mm(pden, pden.t[pb:pb + 64, :], onesb.t[:, 0:64], pt.t[:, s_, :], [onesb.b, pt.bs[s_]],
                                   bi == 0, bi == len(kbl) - 1)

                            pend_ = []
                            for bi, (kb, mk) in enumerate(kbl):
                                pst = bank(exclude=excl)
                                for c in range(4):
                                    mm(pst, pst.t[:, c * 128:(c + 1) * 128], kT.t[pb:pb + 64, kb, :],
                                       qT.t[pb:pb + 64, c, qi * 128:(qi + 1) * 128], [kT.bs[kb], qT.b])
                                s_ = pt_rr % 4
                                pt_rr += 1
                                act(pt.t[:, s_, :], pst.t[:, :], AF.Exp, [pst.b], [pt.bs[s_]], scale=0.125)
                                if mk is not None:
                                    tt("dve", pt.t[:, s_, :], pt.t[:, s_, :], masks.t[:, mk, :], ALU.mult, [pt.bs[s_], masks.b],
                                       [pt.bs[s_]])
                                pend_.append((bi, kb, s_))
                                if len(pend_) > 2:
                                    pv_(*pend_.pop(0))
                            for p_ in pend_:
                                pv_(*p_)
                        tt("dve", rec.t[:], pden.t[:, :], sinkrow.t[:], ALU.add, [pden.b, sinkrow.b], [rec.b])
                        P.op("dve", lambda e: e.reciprocal(rec.t[:], rec.t[:]), [rec.b], [rec.b])
                        for c in range(4):
                            tt("dve", attnT.t[:, as_, c, qi * 128:(qi + 1) * 128], pnum.t[:, c * 128:(c + 1) * 128],
                               rec.t[:, c * 128:(c + 1) * 128], ALU.mult, [pnum.b, rec.b], [attnT.bs[as_]])
                    dma("sp", attn_d.t[:, :, c0:c0 + n], attnT.t[:, as_, :, 0:n], [attnT.bs[as_]], [attn_d.b])
                P.barrier()
            kvst.close()
            if dbg.get("_stop") == "D1" and l == DL:
                stopped = True

            if not stopped:
              with ExitStack() as ph:
                wglu = sbuf(ph, "wglu", [128, 4, 512], BF16)
                wbra = sbuf(ph, "wbra", [128, 4, D], BF16)
                wbrs = sbuf(ph, "wbrs", [128, 4, D], BF16)
                wout = sbuf(ph, "wout", [128, 8, D], BF16)
                w2s = sbuf(ph, "w2s", [128, 2, 8, 256], BF16, 2)
                hT = sbuf(ph, "hTd2", [128, 8, 512], BF16)
                zstage = sbuf(ph, "zstage", [128, 4, 512], BF16)
                zsb = sbuf(ph, "zsb", [128, 4, 512], BF16)
                ssmT = sbuf(ph, "ssmT", [128, 4, 512], BF16)
                mT = sbuf(ph, "mT", [128, 8, 512], BF16)
                sg = sbuf(ph, "sg2", [128, 2, 512], F32, 2)
                ta = sbuf(ph, "ta2", [128, 2, 512], F32, 2)
                selm = sbuf(ph, "selm2", [128, 4, 128], BF16)
                gt1row = sbuf(ph, "gt1row", [128, 2, D])
                attc = sbuf(ph, "attc", [128, 4, 512], BF16)
                dma("pool", selm.t[:], selmat_d.t[:, 0:4, :], [], [selm.b])
                dma("sp", wout.t[:, :, :], WB["wout"].t[:, :, :], [WB["wout"].b], [wout.b])
                dma("sp", wglu.t[:, :, :], WB["wglu"].t[:, :, :], [WB["wglu"].b], [wglu.b])
                dma("sp", wbra.t[:, :, :], WB["wbra"].t[:, :, :], [WB["wbra"].b], [wbra.b])
                dma("sp", wbrs.t[:, :, :], WB["wbrs"].t[:, :, :], [WB["wbrs"].b], [wbrs.b])
                make_gtrow(gt1row, 2)
                nchunks = 5 if ctx_out else 4
                sg_rr = 0
                w2_rr = 0
                for ch in range(nchunks):
                    tiles = list(range(ch * 4, min(ch * 4 + 4, NT)))
                    n = len(tiles) * 128
                    c0 = ch * 512
                    is_ctx = ch == 4
                    cls = 1 if is_ctx else 0
                    for ti, t_ in enumerate(tiles):
                        norm_tile(t_, A1, 0, lambda fc, ti=ti: hT.t[:, fc, ti * 128:(ti + 1) * 128], [hT.b])
                    dma("sp", attc.t[:, :, 0:n], attn_d.t[:, :, c0:c0 + n], [attn_d.b], [attc.b])
                    for sl in range(4):
                        if is_ctx:
                            dma("sp", zsb.t[:, sl, 0:n], ag2_out[2].t[sl * 128:(sl + 1) * 128, :], [ag2_out[2].b], [zsb.b])
                        else:
                            for i in range(4):
                                gc_ = i * TOWN + c0
                                dma("sp", zstage.t[:, i, :],
                                    ag2_out[gc_ // 4096].t[sl * 128:(sl + 1) * 128, gc_ % 4096:gc_ % 4096 + 512],
                                    [ag2_out[gc_ // 4096].b], [zstage.b])
                            pz = bank()
                            for i in range(4):
                                mm(pz, pz.t[:, :], selm.t[:, i, :], zstage.t[:, i, :], [selm.b, zstage.b], i == 0, i == 3)
                            act(zsb.t[:, sl, :], pz.t[:, :], AF.Identity, [pz.b], [zsb.b])
                    for fc in range(4):
                        pg = bank()
                        for sl in range(4):
                            mm(pg, pg.t[:, 0:n], wglu.t[:, sl, fc * 128:(fc + 1) * 128], zsb.t[:, sl, 0:n], [wglu.b, zsb.b],
                               sl == 0, sl == 3)
                        s_ = sg_rr % 2
                        sg_rr += 1
                        act(sg.t[:, s_, 0:n], pg.t[:, 0:n], AF.Sigmoid, [pg.b], [sg.bs[s_]])
                        tt("dve", ssmT.t[:, fc, 0:n], zsb.t[:, fc, 0:n], sg.t[:, s_, 0:n], ALU.mult, [zsb.b, sg.bs[s_]], [ssmT.b])
                    if l == DL and ch == 0 and "ssmT" in dbg_out:
                        sdb = sbuf(ph, "sdb", [128, 4, 512])
                        cp("dve", sdb.t[:], ssmT.t[:], [ssmT.b], [sdb.b])
                        dump("ssmT", sdb, sdb.t[:], lambda o: o[:, :, :])
                    for fc in range(8):
                        ws_ = w2_rr % 2
                        w2_rr += 1
                        dma("sp", w2s.t[:, ws_, :, :], WB["w2g"].t[fc, :, :, :], [WB["w2g"].b], [w2s.bs[ws_]])
                        pA = bank()
                        pS = bank()
                        pga = bank()
                        pgs = bank()
                        fs = slice(fc * 128, (fc + 1) * 128)
                        for c in range(4):
                            mm(pA, pA.t[:, 0:n], wbra.t[:, c, fs], attc.t[:, c, 0:n], [wbra.b, attc.b], c == 0, c == 3)
                        for c in range(4):
                            mm(pS, pS.t[:, 0:n], wbrs.t[:, c, fs], ssmT.t[:, c, 0:n], [wbrs.b, ssmT.b], c == 0, c == 3)
                        for kc in range(8):
                            mm(pga, pga.t[:, 0:n], w2s.t[:, ws_, kc, 0:128], hT.t[:, kc, 0:n], [w2s.bs[ws_], hT.b], kc == 0, kc == 7)
                        for kc in range(8):
                            mm(pgs, pgs.t[:, 0:n], w2s.t[:, ws_, kc, 128:256], hT.t[:, kc, 0:n], [w2s.bs[ws_], hT.b], kc == 0, kc == 7)
                        act(sg.t[:, 0, 0:n], pga.t[:, 0:n], AF.Sigmoid, [pga.b], [sg.bs[0]])
                        act(sg.t[:, 1, 0:n], pgs.t[:, 0:n], AF.Sigmoid, [pgs.b], [sg.bs[1]])
                        tt("dve", ta.t[:, 0, 0:n], pA.t[:, 0:n], sg.t[:, 0, 0:n], ALU.mult, [pA.b, sg.bs[0]], [ta.bs[0]])
                        tt("dve", ta.t[:, 1, 0:n], pS.t[:, 0:n], sg.t[:, 1, 0:n], ALU.mult, [pS.b, sg.bs[1]], [ta.bs[1]])
                        tt("dve", mT.t[:, fc, 0:n], ta.t[:, 0, 0:n], ta.t[:, 1, 0:n], ALU.add, [ta.bs[0], ta.bs[1]], [mT.b])
                    for ti, t_ in enumerate(tiles):
                        for half in range(2):
                            py = bank()
                            hs_ = slice(half * 512, (half + 1) * 512)
                            for kc in range(8):
                                mm(py, py.t[:, :], mT.t[:, kc, ti * 128:(ti + 1) * 128], wout.t[:, kc, hs_], [mT.b, wout.b],
                                   kc == 0, kc == 7)
                            s_ = sg_rr % 2
                            sg_rr += 1
                            tt("dve", sg.t[:, s_, :], py.t[:, :], gt1row.t[:, cls, hs_], ALU.mult, [py.b, gt1row.b], [sg.bs[s_]])
                            tt("dve", x_tm.t[:, t_, hs_], x_tm.t[:, t_, hs_], sg.t[:, s_, :], ALU.add, [sg.bs[s_], x_tm.bs[t_]],
                               [x_tm.bs[t_]])
                P.barrier()
            if l == DL and not stopped:
                dump("xmix", x_tm.bs, x_tm.t[:], lambda o: o[:, :, :])
            if dbg.get("_stop") == "D2" and l == DL:
                stopped = True
            if stopped:
                break

            if not stopped:
              with ExitStack() as ph:
                ntl = NT if ctx_out else NT_OWN
                h2T = sbuf(ph, "h2T", [128, 8, TALL], BF16, NT)
                h2f = sbuf(ph, "h2f", [128, 8, 128])
                wr = sbuf(ph, "wr", [128, 8, 36])
                Wt = sbuf(ph, "Wt", [128, NT, 32], F32, NT)
                lg = sbuf(ph, "lg", [128, 36])
                rs = sbuf(ph, "rs", [128, 16])
                rt = sbuf(ph, "rt", [128, 4, 32])
                weg = sbuf(ph, "weg", [128, 2, 8, 512], BF16, 2)
                weu = sbuf(ph, "weu", [128, 2, 8, 512], BF16, 2)
                wed = sbuf(ph, "wed", [128, 2, 4, D], BF16, 2)
                hid = sbuf(ph, "hid", [128, 2, 4, 512], BF16, 2)
                sgm = sbuf(ph, "sgm", [128, 2, 512], F32, 2)
                ty = sbuf(ph, "ty", [128, 2, 512], F32, 2)
                gt2row = sbuf(ph, "gt2row", [128, 2, D])
                dma("sp", wr.t[:], W["wr"].t[:, :, :], [], [wr.b])
                make_gtrow(gt2row, 5)
                compute_rstd(ntl)
                RB = [rs.b]
                c_ = lambda i: rs.t[:, i:i + 1]
                def route_tile(t_):
                    norm_tile(t_, A2, 3, lambda fc, t_=t_: h2T.t[:, fc, t_ * 128:(t_ + 1) * 128], [h2T.bs[t_]], f32_dst=h2f)
                    pl = bank()
                    for kc in range(8):
                        mm(pl, pl.t[:, 0:36], h2f.t[:, kc, :], wr.t[:, kc, :], [h2f.b, wr.b], kc == 0, kc == 7)
                    cp("dve", lg.t[:], pl.t[:, 0:36], [pl.b], [lg.b])
                    P.op("dve", lambda e: e.tensor_reduce(rs.t[:, 0:1], lg.t[:, 0:4], AX.X, ALU.max), [lg.b], RB)
                    ts("dve", c_(1), c_(0), -1.0, None, ALU.mult, None, RB, RB)
                    act(rt.t[:, 0, 0:4], lg.t[:, 0:4], AF.Exp, [lg.b] + RB, [rt.b, rs.b], bias=rs.t[:, 1:2], accum=rs.t[:, 2:3])
                    P.op("dve", lambda e: e.reciprocal(rs.t[:, 3:4], rs.t[:, 2:3]), RB, RB)
                    ts("dve", rt.t[:, 0, 4:8], lg.t[:, 0:4], rs.t[:, 0:1], None, ALU.is_equal, None, [lg.b] + RB, [rt.b])
                    ts("dve", rt.t[:, 0, 4:8], rt.t[:, 0, 4:8], -1.0, 1e30, ALU.add, ALU.mult, [rt.b], [rt.b])
                    for g in range(4):
                        ts("dve", rt.t[:, 1, 8 * g:8 * g + 8], lg.t[:, 4 + 8 * g:12 + 8 * g], rt.t[:, 0, 4 + g:5 + g], None,
                           ALU.add, None, [lg.b, rt.b], [rt.b])
                    P.op("dve", lambda e: e.tensor_reduce(rs.t[:, 4:5], rt.t[:, 1, :], AX.X, ALU.max), [rt.b], RB)
                    ts("dve", rt.t[:, 2, :], rt.t[:, 1, :], rs.t[:, 4:5], None, ALU.is_equal, None, [rt.b] + RB, [rt.b])
                    stt("dve", rt.t[:, 2, :], rt.t[:, 2, :], -1e30, rt.t[:, 1, :], ALU.mult, ALU.add, [rt.b], [rt.b])
                    P.op("dve", lambda e: e.tensor_reduce(rs.t[:, 5:6], rt.t[:, 2, :], AX.X, ALU.max), [rt.b], RB)
                    ts("dve", rt.t[:, 2, :], rt.t[:, 1, :], rs.t[:, 5:6], None, ALU.is_ge, None, [rt.b] + RB, [rt.b])
                    ts("dve", c_(6), c_(4), -1.0, None, ALU.mult, None, RB, RB)
                    act(rt.t[:, 3, :], rt.t[:, 1, :], AF.Exp, [rt.b] + RB, [rt.b], bias=rs.t[:, 6:7])
                    tt("dve", rt.t[:, 3, :], rt.t[:, 3, :], rt.t[:, 2, :], ALU.mult, [rt.b], [rt.b])
                    P.op("dve", lambda e: e.tensor_reduce(rs.t[:, 7:8], rt.t[:, 3, :], AX.X, ALU.add), [rt.b], RB)
                    P.op("dve", lambda e: e.reciprocal(rs.t[:, 7:8], rs.t[:, 7:8]), RB, RB)
                    tt("dve", c_(7), c_(7), c_(3), ALU.mult, RB, RB)
                    ts("dve", Wt.t[:, t_, :], rt.t[:, 3, :], rs.t[:, 7:8], None, ALU.mult, None, [rt.b] + RB, [Wt.bs[t_]])

                nch = 5 if ctx_out else 4
                ty_rr = 0
                nexp = dbg.get("_nexp", NEXP)
                for e_ in range(nexp):
                    s = e_ % 2
                    for k2 in range(2):
                        dma("pool", weg.t[:, s, 4 * k2:4 * k2 + 4, :], W["weg"].t[e_, :, 4 * k2:4 * k2 + 4, :], [W["weg"].b], [weg.bs[s]])
                        dma("pool", weu.t[:, s, 4 * k2:4 * k2 + 4, :], W["weu"].t[e_, :, 4 * k2:4 * k2 + 4, :], [W["weu"].b], [weu.bs[s]])
                        dma("pool", wed.t[:, s, 2 * k2:2 * k2 + 2, :], W["wed"].t[e_, :, 2 * k2:2 * k2 + 2, :], [W["wed"].b], [wed.bs[s]])
                    for ch in range(nch):
                        tiles = list(range(ch * 4, min(ch * 4 + 4, NT)))
                        n = len(tiles) * 128
                        c0 = ch * 512
                        cls = 1 if ch == 4 else 0
                        hs = ch % 2
                        if e_ == 0:
                            for t_ in tiles:
                                route_tile(t_)
                        for hc in range(4):
                            pg = bank()
                            pu = bank()
                            hrd = [h2T.bs[t_] for t_ in tiles]
                            for kc in range(8):
                                mm(pg, pg.t[:, 0:n], weg.t[:, s, kc, hc * 128:(hc + 1) * 128], h2T.t[:, kc, c0:c0 + n],
                                   [weg.bs[s]] + hrd, kc == 0, kc == 7)
                            for kc in range(8):
                                mm(pu, pu.t[:, 0:n], weu.t[:, s, kc, hc * 128:(hc + 1) * 128], h2T.t[:, kc, c0:c0 + n],
                                   [weu.bs[s]] + hrd, kc == 0, kc == 7)
                            ss_ = hc % 2
                            act(sgm.t[:, ss_, 0:n], pg.t[:, 0:n], AF.Silu, [pg.b], [sgm.bs[ss_]])
                            tt("dve", hid.t[:, hs, hc, 0:n], pu.t[:, 0:n], sgm.t[:, ss_, 0:n], ALU.mult, [pu.b, sgm.bs[ss_]],
                               [hid.bs[hs]])
                        for ti, t_ in enumerate(tiles):
                            for half in range(2):
                                py = bank()
                                hs_ = slice(half * 512, (half + 1) * 512)
                                for hc in range(4):
                                    mm(py, py.t[:, :], hid.t[:, hs, hc, ti * 128:(ti + 1) * 128], wed.t[:, s, hc, hs_],
                                       [hid.bs[hs], wed.bs[s]], hc == 0, hc == 3)
                                y_ = ty_rr % 2
                                ty_rr += 1
                                tt("dve", ty.t[:, y_, :], py.t[:, :], gt2row.t[:, cls, hs_], ALU.mult, [py.b, gt2row.b],
                                   [ty.bs[y_]])
                                stt("dve", x_tm.t[:, t_, hs_], ty.t[:, y_, :], Wt.t[:, t_, e_:e_ + 1], x_tm.t[:, t_, hs_],
                                    ALU.mult, ALU.add, [ty.bs[y_], Wt.bs[t_], x_tm.bs[t_]], [x_tm.bs[t_]])
                P.barrier()
            if l == DL and not stopped:
                dump("xout", x_tm.bs, x_tm.t[:], lambda o: o[:, :, :])

        with ExitStack() as ph:
            gfin = sbuf(ph, "gfin", [128, D])
            ob = sbuf(ph, "ob", [128, 2, D], F32, 2)
            dma("sp", gfin.t[:], gfin_d.t[:, :], [], [gfin.b])
            compute_rstd(NT_OWN)
            for t_ in range(NT_OWN):
                s = t_ % 2
                stt("dve", ob.t[:, s, :], x_tm.t[:, t_, :], rstd.t[:, t_:t_ + 1], gfin.t[:], ALU.mult, ALU.mult,
                    [x_tm.bs[t_], rstd.b, gfin.b], [ob.bs[s]])
                dma("sp", out_d.t[t_ * 128:(t_ + 1) * 128, :], ob.t[:, s, :], [ob.bs[s]], [out_d.b])
        P.wait_all("sp", [out_d.b] + [v.b for v in dbg_out.values()])
        P.emit()
    return nc, names_in


def _kc(w):
    K, C = w.shape
    return np.ascontiguousarray(w.reshape(K // 128, 128, C).transpose(1, 0, 2))


def prep_inputs(inputs, nlayers=2, nei=NEXP):
    f = lambda a: np.ascontiguousarray(np.asarray(a, dtype=np.float32))
    I_ = {k: f(v) for k, v in inputs.items()}
    shared = {}
    e = np.arange(64)
    partner = np.where((e % 32) < 16, e + 16, e - 16)
    head_order = [h for c in range(4) for h in (c, c + 4)]
    qcols = np.concatenate([h * 64 + np.arange(64) for h in head_order])
    qrcols = np.concatenate([h * 64 + partner for h in head_order])
    kcols = 512 + np.arange(128)
    krcols = 512 + np.concatenate([hk * 64 + partner for hk in range(2)])
    vcols = 640 + np.arange(128)
    ucols = 768 + np.arange(512)
    w1cols = np.concatenate([qcols, qrcols, kcols, krcols, vcols, ucols])
    brarows = np.concatenate([h * 64 + np.arange(64) for h in head_order])
    shared["ident"] = np.eye(128, dtype=np.float32)
    shared["iota"] = np.ascontiguousarray(np.broadcast_to(np.arange(1, 513, dtype=np.float32), (128, 512)))
    shared["gfin"] = np.ascontiguousarray(np.broadcast_to(I_["g_final"], (128, D)))
    ee = np.arange(128) % 64
    ropef = np.stack([(ee % 16).astype(np.float32), np.where((ee % 32) < 16, -1.0, 1.0).astype(np.float32)], 1)
    shared["ropef"] = np.ascontiguousarray(ropef)
    for l in range(nlayers):
        win = I_["w_in"][l]
        shared[f"wmod{l}"] = _kc(I_["w_mod"][l])
        shared[f"bmodT{l}"] = np.ascontiguousarray(I_["b_mod"][l].reshape(48, 128).T)
        shared[f"g1T{l}"] = np.ascontiguousarray(I_["g_norm1"][l].reshape(8, 128).T)
        shared[f"g2T{l}"] = np.ascontiguousarray(I_["g_norm2"][l].reshape(8, 128).T)
        shared[f"w1_{l}"] = _kc(win[:, w1cols])
        ga = _kc(win[:, 1280:2304]).reshape(128, 8, 8, 128)
        gs = _kc(win[:, 2304:3328]).reshape(128, 8, 8, 128)
        shared[f"w2g{l}"] = np.ascontiguousarray(np.concatenate([ga, gs], axis=3).transpose(2, 0, 1, 3))
        shared[f"wglu{l}"] = _kc(I_["w_glu"][l])
        shared[f"wbra{l}"] = _kc(I_["w_br_attn"][l][brarows, :])
        shared[f"wbrs{l}"] = _kc(I_["w_br_ssm"][l])
        shared[f"wout{l}"] = _kc(I_["w_out"][l])
        shared[f"wr{l}"] = _kc(np.concatenate([I_["w_router_group"][l], I_["w_router_expert"][l]], axis=1))
        shared[f"weg{l}"] = np.ascontiguousarray(I_["w_exp_gate"][l][:nei].reshape(nei, 8, 128, 512).transpose(0, 2, 1, 3))
        shared[f"weu{l}"] = np.ascontiguousarray(I_["w_exp_up"][l][:nei].reshape(nei, 8, 128, 512).transpose(0, 2, 1, 3))
        shared[f"wed{l}"] = np.ascontiguousarray(I_["w_exp_down"][l][:nei].reshape(nei, 4, 128, D).transpose(0, 2, 1, 3))
        shared[f"sink{l}"] = np.ascontiguousarray(np.broadcast_to(I_["attn_sink"][l], (128, 8)))
    kk = np.arange(128)[:, None]
    qq = np.arange(128)[None, :]
    mprev = np.tile((kk >= qq).astype(np.float32), (1, 4))
    mnext = np.tile((kk <= qq).astype(np.float32), (1, 4))
    per_core = []
    for r in range(NCORES):
        b, j = r // 4, r % 4
        m = dict(shared)
        t0 = j * TOWN
        m["x_own"] = np.ascontiguousarray(I_["x"][b, t0:t0 + TOWN])
        m["ctx_b"] = np.ascontiguousarray(I_["ctx"][b])
        cT = np.stack([I_["c"][b].reshape(8, 128).T, I_["c_ctx"].reshape(8, 128).T], axis=2)
        m["cT"] = np.ascontiguousarray(cT)
        tpos = np.arange(t0, t0 + TOWN)
        rows = (tpos // 64).astype(np.float32)
        cols = (tpos % 64).astype(np.float32)
        pos = np.zeros((128, TALL), np.float32)
        is_row = (ee % 64) < 32
        pos[:, :TOWN] = np.where(is_row[:, None], rows[None, :], cols[None, :])
        m["pos"] = pos
        mk = np.zeros((128, 4, 512), np.float32)
        mk[:, 0] = mprev if j > 0 else 0.0
        mk[:, 1] = mprev
        mk[:, 2] = mnext if j < 3 else 0.0
        mk[:, 3] = mnext
        m["masks"] = mk
        sel = np.zeros((128, 12, 128), np.float32)
        eye = np.eye(128, dtype=np.float32)
        sel[:, j] = eye
        if j > 0:
            sel[:, 4 + j - 1] = eye
        if j < 3:
            sel[:, 8 + j + 1] = eye
        m["selmat"] = sel
        for l in range(nlayers):
            win = I_["w_in"][l]
            m[f"wus{l}"] = _kc(win[:, 768 + 128 * j:768 + 128 * (j + 1)])
            g0 = 8 * j

            def rowlay(a):
                a = a.reshape((2, 4, 2, 64) + a.shape[3:])
                perm = (2, 3, 0, 1) + tuple(range(4, a.ndim))
                a = a.transpose(perm)
                return np.ascontiguousarray(a.reshape((128, 8) + a.shape[4:]))

            m[f"lamre{l}"] = rowlay(I_["ssm_lam_re"][l][:, g0:g0 + 8])
            m[f"lamim{l}"] = rowlay(I_["ssm_lam_im"][l][:, g0:g0 + 8])
            ldt = np.broadcast_to(I_["ssm_log_dt"][l][:, g0:g0 + 8, None], (2, 8, 64))
            m[f"ldt{l}"] = rowlay(np.ascontiguousarray(ldt))
            m[f"bre{l}"] = rowlay(I_["ssm_b_re"][l][:, g0:g0 + 8])
            m[f"bim{l}"] = rowlay(I_["ssm_b_im"][l][:, g0:g0 + 8])
            m[f"cre{l}"] = rowlay(np.ascontiguousarray(I_["ssm_c_re"][l][:, g0:g0 + 8].transpose(0, 1, 3, 2)))
            m[f"cim{l}"] = rowlay(np.ascontiguousarray(I_["ssm_c_im"][l][:, g0:g0 + 8].transpose(0, 1, 3, 2)))
            m[f"dsk{l}"] = np.ascontiguousarray(I_["ssm_d"][l][128 * j:128 * (j + 1)].reshape(128, 1))
        for k in list(m.keys()):
            if False:
                a = m[k]
                a2 = a.reshape(-1, a.shape[-1])
                rr = a2.shape[0] // NCORES
                m[k] = np.ascontiguousarray(a2[r * rr:(r + 1) * rr])
        per_core.append(m)
    return per_core


_CACHE = {}


def kernel(**inputs):
    if "nc" not in _CACHE:
        _CACHE["nc"] = build_program(2)
    nc, names = _CACHE["nc"]
    per_core = prep_inputs(inputs, 2)
    in_maps = [{k: m[k] for k in names} for m in per_core]
    res = run_bass_kernel_spmd(nc, in_maps, core_ids=list(range(NCORES)))
    out = np.zeros((2, SEQ, D), np.float32)
    for r in range(NCORES):
        b, j = r // 4, r % 4
        out[b, j * TOWN:(j + 1) * TOWN] = res.results[r]["out"]
    return out
```
